# Optimizing a Trainium2 kernel written in Bass

```python
import math
import jax, jax.numpy as jnp
from jax import lax
import numpy as np

D_MODEL = 1024
BATCH = 2
SEQ = 8192
DEPTH = 2

N_MIXERS = 2
HEAD_DIM = 64
N_MIX_HEADS = 12
N_MEM_HEADS = 4
MEM_LEN = 256
D_MIX = N_MIX_HEADS * HEAD_DIM
D_MEM = N_MEM_HEADS * HEAD_DIM
D_CAT = D_MIX + D_MEM
D_FF = -(-8 * D_MODEL // (3 * 256)) * 256
N_REL_BUCKETS = 32
REL_MAX_EXACT = 16
REL_MAX_DIST = 2048
Q_BLOCK = 128
NSA_KV_HEADS = 2
NSA_GROUP = N_MIX_HEADS // NSA_KV_HEADS
CMP_LEN = 32
CMP_STRIDE = 16
CMP_HIDDEN = 256
SEL_BLOCK = 64
N_SEL = 16
WINDOW = 512
N_BRANCH = 3
FORCE_BONUS = 1e4
NSA_IN = D_MIX + 6 * NSA_KV_HEADS * HEAD_DIM + N_MIX_HEADS * N_BRANCH + D_MEM
Q_LORA = 256
KV_LORA = 128
IDX_HEADS = 8
IDX_DIM = 64
DSA_TOPK = 256
DSA_IN = Q_LORA + KV_LORA + IDX_DIM + IDX_HEADS + D_MEM
N_NSA_LAYERS = (DEPTH + 1) // 2
N_DSA_LAYERS = DEPTH // 2
NEG = -1e30
EPS = 1e-6

kernel_name = 'hybrid_nsa_dsa_memory_trunk'


def rmsnorm(x, g):
    xf = x.astype(jnp.float32)
    y = xf * lax.rsqrt(jnp.mean(xf * xf, axis=-1, keepdims=True) + EPS)
    return (y * g.astype(jnp.float32)).astype(x.dtype)


def rel_bucket(dist):
    n = jnp.maximum(dist, 0)
    nf = jnp.maximum(n, REL_MAX_EXACT).astype(jnp.float32)
    large = REL_MAX_EXACT + (jnp.log(nf / REL_MAX_EXACT) / math.log(REL_MAX_DIST / REL_MAX_EXACT)
                             * (N_REL_BUCKETS - REL_MAX_EXACT)).astype(jnp.int32)
    large = jnp.minimum(large, N_REL_BUCKETS - 1)
    return jnp.where(n < REL_MAX_EXACT, n, large)


def masked_softmax(s, mask):
    s = jnp.where(mask, s, NEG)
    p = jax.nn.softmax(s, axis=-1)
    return jnp.where(mask, p, 0.0)


def split_cols(a, sizes):
    return jnp.split(a, [int(v) for v in np.cumsum(sizes)[:-1]], axis=-1)


def compress(raw, pos, w1, b1, w2, b2):
    B, T = raw.shape[0], raw.shape[1]
    n_cmp = (T - CMP_LEN) // CMP_STRIDE + 1
    idx = jnp.arange(n_cmp)[:, None] * CMP_STRIDE + jnp.arange(CMP_LEN)[None, :]
    blk = raw[:, idx] + pos[None, None, :, None, :]
    blk = jnp.transpose(blk, (0, 1, 3, 2, 4)).reshape(B, n_cmp, NSA_KV_HEADS, CMP_LEN * HEAD_DIM)
    h = jax.nn.gelu(blk @ w1 + b1)
    return h @ w2 + b2


def nsa_mixer(xn, w_in, gate_b, pos_k, pos_v, k_w1, k_b1, k_w2, k_b2, v_w1, v_b1, v_w2, v_b2, rel_bias):
    B, T, _ = xn.shape
    G, R, dh, H = NSA_KV_HEADS, NSA_GROUP, HEAD_DIM, N_MIX_HEADS
    kvw = G * dh
    q, kc_raw, vc_raw, ks, vs, kw, vw, gl, q_mem = split_cols(
        xn @ w_in, [D_MIX, kvw, kvw, kvw, kvw, kvw, kvw, H * N_BRANCH, D_MEM])
    q = q.reshape(B, T, G, R, dh) * dh ** -0.5
    kc = compress(kc_raw.reshape(B, T, G, dh), pos_k, k_w1, k_b1, k_w2, k_b2)
    vc = compress(vc_raw.reshape(B, T, G, dh), pos_v, v_w1, v_b1, v_w2, v_b2)
    gates = jax.nn.sigmoid(gl + gate_b).reshape(B, T, G, R, N_BRANCH)
    n_sel_blk = T // SEL_BLOCK
    kb = jnp.transpose(ks.reshape(B, n_sel_blk, SEL_BLOCK, G, dh), (0, 3, 1, 2, 4))
    vb = jnp.transpose(vs.reshape(B, n_sel_blk, SEL_BLOCK, G, dh), (0, 3, 1, 2, 4))
    pad = ((0, 0), (WINDOW, 0), (0, 0), (0, 0))
    kwp = jnp.pad(kw.reshape(B, T, G, dh), pad)
    vwp = jnp.pad(vw.reshape(B, T, G, dh), pad)

    n_cmp = kc.shape[1]
    cmp_start = jnp.arange(n_cmp) * CMP_STRIDE
    cmp_end = cmp_start + CMP_LEN - 1
    sel_start = jnp.arange(n_sel_blk) * SEL_BLOCK
    overlap = ((cmp_start[:, None] <= sel_start[None, :] + SEL_BLOCK - 1)
               & (cmp_end[:, None] >= sel_start[None, :])).astype(jnp.float32)
    n_top = min(N_SEL, n_sel_blk)
    nk = n_top * SEL_BLOCK
    bias_gr = jnp.transpose(rel_bias.reshape(N_REL_BUCKETS, G, R), (1, 0, 2))
    bi = jnp.arange(B)[:, None, None, None]
    gi = jnp.arange(G)[None, :, None, None]
    in_blk = jnp.arange(SEL_BLOCK)
    blk_ids = jnp.arange(n_sel_blk)

    def block(qb_idx):
        qs = qb_idx * Q_BLOCK
        t = qs + jnp.arange(Q_BLOCK)
        qb = lax.dynamic_slice_in_dim(q, qs, Q_BLOCK, axis=1)
        s = jnp.einsum('bqgrd,bngd->bgrqn', qb, kc).astype(jnp.float32)
        cb = rel_bias[rel_bucket(t[:, None] - cmp_end[None, :])]
        s = s + jnp.transpose(cb, (2, 0, 1)).reshape(G, R, Q_BLOCK, n_cmp)
        p_cmp = masked_softmax(s, cmp_end[None, :] <= t[:, None])
        o_cmp = jnp.einsum('bgrqn,bngd->bqgrd', p_cmp.astype(vc.dtype), vc)
        p_slc = jnp.einsum('bgqn,nj->bgqj', p_cmp.sum(axis=2), overlap)
        cur = t // SEL_BLOCK
        forced = ((blk_ids[None, :] == 0) | (blk_ids[None, :] == cur[:, None])
                  | (blk_ids[None, :] == cur[:, None] - 1))
        admissible = sel_start[None, :] <= t[:, None]
        score = jnp.where(admissible, p_slc + FORCE_BONUS * forced, NEG)
        top_val, top_idx = lax.top_k(score, n_top)
        k_sel = kb[bi, gi, top_idx].reshape(B, G, Q_BLOCK, nk, dh)
        v_sel = vb[bi, gi, top_idx].reshape(B, G, Q_BLOCK, nk, dh)
        pos5 = top_idx[..., None] * SEL_BLOCK + in_blk
        mask5 = (top_val > 0.5 * NEG)[..., None] & (pos5 <= t[None, None, :, None, None])
        pos = pos5.reshape(B, G, Q_BLOCK, nk)
        smask = mask5.reshape(B, G, Q_BLOCK, nk)
        sb = bias_gr[gi, rel_bucket(t[None, None, :, None] - pos)]
        s = jnp.einsum('bqgrd,bgqkd->bgrqk', qb, k_sel).astype(jnp.float32) + jnp.moveaxis(sb, -1, 2)
        p = masked_softmax(s, smask[:, :, None])
        o_slc = jnp.einsum('bgrqk,bgqkd->bqgrd', p.astype(v_sel.dtype), v_sel)
        kwb = lax.dynamic_slice_in_dim(kwp, qs, Q_BLOCK + WINDOW, axis=1)
        vwb = lax.dynamic_slice_in_dim(vwp, qs, Q_BLOCK + WINDOW, axis=1)
        spos = qs - WINDOW + jnp.arange(Q_BLOCK + WINDOW)
        d = t[:, None] - spos[None, :]
        wmask = (spos[None, :] >= 0) & (d >= 0) & (d < WINDOW)
        wb = jnp.transpose(rel_bias[rel_bucket(d)], (2, 0, 1)).reshape(G, R, Q_BLOCK, Q_BLOCK + WINDOW)
        s = jnp.einsum('bqgrd,bkgd->bgrqk', qb, kwb).astype(jnp.float32) + wb
        p = masked_softmax(s, wmask)
        o_win = jnp.einsum('bgrqk,bkgd->bqgrd', p.astype(vwb.dtype), vwb)
        gb = lax.dynamic_slice_in_dim(gates, qs, Q_BLOCK, axis=1)
        return gb[..., 0:1] * o_cmp + gb[..., 1:2] * o_slc + gb[..., 2:3] * o_win

    out = lax.map(block, jnp.arange(T // Q_BLOCK))
    out = jnp.moveaxis(out, 0, 1).reshape(B, T, D_MIX)
    return out, q_mem


def dsa_mixer(xn, w_in, q_norm, kv_norm, w_q_up, w_uk, w_uv, w_q_idx, kidx_norm, rel_bias):
    B, T, _ = xn.shape
    H, dh = N_MIX_HEADS, HEAD_DIM
    c_q, c_kv, k_idx, w_idx, q_mem = split_cols(xn @ w_in, [Q_LORA, KV_LORA, IDX_DIM, IDX_HEADS, D_MEM])
    c_q = rmsnorm(c_q, q_norm)
    c_kv = rmsnorm(c_kv, kv_norm)
    q = (c_q @ w_q_up).reshape(B, T, H, dh) * dh ** -0.5
    q_abs = jnp.einsum('bthd,rhd->bthr', q, w_uk)
    q_idx = (c_q @ w_q_idx).reshape(B, T, IDX_HEADS, IDX_DIM)
    k_idx = rmsnorm(k_idx, kidx_norm)
    w_idx = w_idx * (IDX_HEADS ** -0.5 * IDX_DIM ** -0.5)
    k_top = min(DSA_TOPK, T // 4)
    s_all = jnp.arange(T)
    bi = jnp.arange(B)[:, None, None]

    def block(qb_idx):
        qs = qb_idx * Q_BLOCK
        t = qs + jnp.arange(Q_BLOCK)
        qi = lax.dynamic_slice_in_dim(q_idx, qs, Q_BLOCK, axis=1)
        wi = lax.dynamic_slice_in_dim(w_idx, qs, Q_BLOCK, axis=1)
        qa = lax.dynamic_slice_in_dim(q_abs, qs, Q_BLOCK, axis=1)
        logits = jax.nn.relu(jnp.einsum('bqhd,bsd->bqhs', qi, k_idx).astype(jnp.float32))
        score = jnp.einsum('bqhs,bqh->bqs', logits, wi.astype(jnp.float32))
        score = jnp.where(s_all[None, :] <= t[:, None], score, NEG)
        _, idx = lax.top_k(score, k_top)
        valid = idx <= t[None, :, None]
        c_sel = c_kv[bi, idx]
        s = jnp.einsum('bqhr,bqkr->bhqk', qa, c_sel).astype(jnp.float32)
        bias = rel_bias[rel_bucket(t[None, :, None] - idx)]
        s = s + jnp.transpose(bias, (0, 3, 1, 2))
        p = masked_softmax(s, valid[:, None])
        return jnp.einsum('bhqk,bqkr->bqhr', p.astype(c_sel.dtype), c_sel)

    o_lat = lax.map(block, jnp.arange(T // Q_BLOCK))
    o_lat = jnp.moveaxis(o_lat, 0, 1).reshape(B, T, H, KV_LORA)
    out = jnp.einsum('bthr,rhd->bthd', o_lat, w_uv).reshape(B, T, D_MIX)
    return out, q_mem


def memory_attention(q_mem, mem_n, w_mem_kv):
    B, M, _ = mem_n.shape
    kv = (mem_n @ w_mem_kv).reshape(B, M, 2, N_MEM_HEADS, HEAD_DIM)
    k, v = kv[:, :, 0], kv[:, :, 1]
    s = jnp.einsum('bthd,bmhd->bhtm', q_mem, k).astype(jnp.float32)
    p = jax.nn.softmax(s, axis=-1).astype(v.dtype)
    o = jnp.einsum('bhtm,bmhd->bthd', p, v)
    return o.reshape(o.shape[0], o.shape[1], D_MEM)


def setup_inputs(seed: int = 0) -> dict:
    key = jax.random.key(seed)
    keys = iter(jax.random.split(key, 48))
    D = D_MODEL

    def nrm(shape, scale):
        return jax.random.normal(next(keys), shape, jnp.float32) * scale

    def gain(shape):
        return 1.0 + nrm(shape, 0.01)

    cin = CMP_LEN * HEAD_DIM
    return {
        'x': nrm((BATCH, SEQ, D), 1.0),
        'mem': nrm((BATCH, MEM_LEN, D), 1.0),
        'rel_bias': nrm((N_REL_BUCKETS, N_MIX_HEADS), 0.1),
        'norm_mix': gain((DEPTH, D)),
        'norm_ffn': gain((DEPTH, D)),
        'norm_mem': gain((DEPTH, D)),
        'w_mem_kv': nrm((DEPTH, D, 2 * D_MEM), D ** -0.5),
        'w_out': nrm((DEPTH, D_CAT, D), D_CAT ** -0.5),
        'ffn_gate': nrm((DEPTH, D, D_FF), D ** -0.5),
        'ffn_up': nrm((DEPTH, D, D_FF), D ** -0.5),
        'ffn_down': nrm((DEPTH, D_FF, D), D_FF ** -0.5),
        'nsa_w_in': nrm((N_NSA_LAYERS, D, NSA_IN), D ** -0.5),
        'nsa_gate_b': nrm((N_NSA_LAYERS, N_MIX_HEADS * N_BRANCH), 0.01),
        'nsa_cmp_pos_k': nrm((N_NSA_LAYERS, CMP_LEN, HEAD_DIM), 0.1),
        'nsa_cmp_pos_v': nrm((N_NSA_LAYERS, CMP_LEN, HEAD_DIM), 0.1),
        'nsa_cmp_k_w1': nrm((N_NSA_LAYERS, cin, CMP_HIDDEN), cin ** -0.5),
        'nsa_cmp_k_b1': nrm((N_NSA_LAYERS, CMP_HIDDEN), 0.01),
        'nsa_cmp_k_w2': nrm((N_NSA_LAYERS, CMP_HIDDEN, HEAD_DIM), CMP_HIDDEN ** -0.5),
        'nsa_cmp_k_b2': nrm((N_NSA_LAYERS, HEAD_DIM), 0.01),
        'nsa_cmp_v_w1': nrm((N_NSA_LAYERS, cin, CMP_HIDDEN), cin ** -0.5),
        'nsa_cmp_v_b1': nrm((N_NSA_LAYERS, CMP_HIDDEN), 0.01),
        'nsa_cmp_v_w2': nrm((N_NSA_LAYERS, CMP_HIDDEN, HEAD_DIM), CMP_HIDDEN ** -0.5),
        'nsa_cmp_v_b2': nrm((N_NSA_LAYERS, HEAD_DIM), 0.01),
        'dsa_w_in': nrm((N_DSA_LAYERS, D, DSA_IN), D ** -0.5),
        'dsa_q_norm': gain((N_DSA_LAYERS, Q_LORA)),
        'dsa_kv_norm': gain((N_DSA_LAYERS, KV_LORA)),
        'dsa_w_q_up': nrm((N_DSA_LAYERS, Q_LORA, D_MIX), Q_LORA ** -0.5),
        'dsa_w_uk': nrm((N_DSA_LAYERS, KV_LORA, N_MIX_HEADS, HEAD_DIM), KV_LORA ** -0.5),
        'dsa_w_uv': nrm((N_DSA_LAYERS, KV_LORA, N_MIX_HEADS, HEAD_DIM), KV_LORA ** -0.5),
        'dsa_w_q_idx': nrm((N_DSA_LAYERS, Q_LORA, IDX_HEADS * IDX_DIM), Q_LORA ** -0.5),
        'dsa_kidx_norm': gain((N_DSA_LAYERS, IDX_DIM)),
        'norm_final': gain((D,)),
    }


def reference(x, mem, rel_bias, norm_mix, norm_ffn, norm_mem, w_mem_kv, w_out, ffn_gate, ffn_up, ffn_down,
              nsa_w_in, nsa_gate_b, nsa_cmp_pos_k, nsa_cmp_pos_v,
              nsa_cmp_k_w1, nsa_cmp_k_b1, nsa_cmp_k_w2, nsa_cmp_k_b2,
              nsa_cmp_v_w1, nsa_cmp_v_b1, nsa_cmp_v_w2, nsa_cmp_v_b2,
              dsa_w_in, dsa_q_norm, dsa_kv_norm, dsa_w_q_up, dsa_w_uk, dsa_w_uv, dsa_w_q_idx, dsa_kidx_norm,
              norm_final):
    B, T, _ = x.shape
    h = x
    for i in range(DEPTH):
        xn = rmsnorm(h, norm_mix[i])
        j = i // N_MIXERS
        if i % N_MIXERS == 0:
            mix, q_mem = nsa_mixer(xn, nsa_w_in[j], nsa_gate_b[j], nsa_cmp_pos_k[j], nsa_cmp_pos_v[j],
                                   nsa_cmp_k_w1[j], nsa_cmp_k_b1[j], nsa_cmp_k_w2[j], nsa_cmp_k_b2[j],
                                   nsa_cmp_v_w1[j], nsa_cmp_v_b1[j], nsa_cmp_v_w2[j], nsa_cmp_v_b2[j],
                                   rel_bias)
        else:
            mix, q_mem = dsa_mixer(xn, dsa_w_in[j], dsa_q_norm[j], dsa_kv_norm[j], dsa_w_q_up[j],
                                   dsa_w_uk[j], dsa_w_uv[j], dsa_w_q_idx[j], dsa_kidx_norm[j], rel_bias)
        q_mem = q_mem.reshape(B, T, N_MEM_HEADS, HEAD_DIM) * HEAD_DIM ** -0.5
        mem_o = memory_attention(q_mem, rmsnorm(mem, norm_mem[i]), w_mem_kv[i])
        h = h + jnp.concatenate([mix, mem_o], axis=-1) @ w_out[i]
        hn = rmsnorm(h, norm_ffn[i])
        h = h + (jax.nn.silu(hn @ ffn_gate[i]) * (hn @ ffn_up[i])) @ ffn_down[i]
    return rmsnorm(h, norm_final)
```

```python
import math
from contextlib import ExitStack
import numpy as np
import ml_dtypes
import concourse.bass as bass
import concourse.mybir as mybir
from concourse.bass import AP
from concourse.bass_utils import run_bass_kernel_spmd

F32 = mybir.dt.float32
BF16 = mybir.dt.bfloat16
AF = mybir.ActivationFunctionType
ALU = mybir.AluOpType
AX = mybir.AxisListType
NPBF = ml_dtypes.bfloat16

D = 1024
T = 8192
NT = 16
TOK = 2048
DFF = 2816
EPS = 1e-6
NEGB = -30000.0


class Buf:
    __slots__ = ("name", "w", "r")

    def __init__(self, name=""):
        self.name = name
        self.w = None
        self.r = {}


class Prog:
    NSLOT = 12

    def __init__(self, nc):
        self.nc = nc
        self.eng = {"pe": nc.tensor, "act": nc.scalar, "dve": nc.vector,
                    "pool": nc.gpsimd, "sp": nc.sync}
        self.es = ExitStack()
        self.sem = {}
        self.cnt = {}
        for e in ("pe", "act", "dve", "pool"):
            self.sem[e] = self.es.enter_context(nc.semaphore("c_" + e))
            self.cnt[e] = 0
        self.slots = {}
        self.slot_i = {}
        for q in ("sp", "pool"):
            lst = []
            for i in range(self.NSLOT):
                key = "d_%s%d" % (q, i)
                self.sem[key] = self.es.enter_context(nc.semaphore(key))
                self.cnt[key] = 0
                lst.append(key)
            self.slots[q] = lst
            self.slot_i[q] = 0
        self.seen = {e: {} for e in self.eng}
        self.outtoks = []

    def _need(self, e, tok):
        if tok is None:
            return
        key, val = tok
        if self.seen[e].get(key, 0) >= val:
            return
        if key in ("pe", "act", "dve", "pool"):
            assert self.cnt[key] >= val, "missing signal on %s" % key
        self.eng[e].wait_ge(self.sem[key], val)
        self.seen[e][key] = val

    def _deps(self, e, reads, writes):
        for b in reads:
            if b.w is not None and not (e == "pe" and b.w[0] == "pe"):
                self._need(e, b.w)
        for b in writes:
            if b.w is not None and b.w[0] != e:
                self._need(e, b.w)
            for k, v in b.r.items():
                if k != e:
                    self._need(e, (k, v))

    def _mark(self, tok, reads, writes):
        for b in reads:
            if b.r.get(tok[0], 0) < tok[1]:
                b.r[tok[0]] = tok[1]
        for b in writes:
            b.w = tok
            b.r = {}

    def op(self, e, fn, reads=(), writes=(), sig=True):
        self._deps(e, reads, writes)
        ins = fn()
        if sig:
            self.cnt[e] += 1
            ins.then_inc(self.sem[e], 1)
            tok = (e, self.cnt[e])
        else:
            tok = (e, self.cnt[e] + 1)
        self._mark(tok, reads, writes)
        return ins

    def dma(self, q, out, in_, reads=(), writes=(), final=False):
        lst = self.slots[q]
        key = lst[self.slot_i[q] % self.NSLOT]
        self.slot_i[q] += 1
        if self.cnt[key] > 0:
            self._need(q, (key, self.cnt[key]))
        self._deps(q, reads, writes)
        ins = self.eng[q].dma_start(out=out, in_=in_)
        self.cnt[key] += 16
        ins.then_inc(self.sem[key], 16)
        tok = (key, self.cnt[key])
        self._mark(tok, reads, writes)
        if final:
            self.outtoks.append(tok)
        return tok

    def barrier(self):
        toks = [(e, self.cnt[e]) for e in ("pe", "act", "dve", "pool") if self.cnt[e] > 0]
        for q in ("sp", "pool"):
            toks += [(k, self.cnt[k]) for k in self.slots[q] if self.cnt[k] > 0]
        for e in self.eng:
            for t in toks:
                if t[0] != e:
                    self._need(e, t)

    def finish(self):
        for tok in self.outtoks:
            self._need("sp", tok)
        for q in ("sp", "pool"):
            for key in self.slots[q]:
                if self.cnt[key] > 0:
                    self._need("sp", (key, self.cnt[key]))
        self.es.close()


class K:
    def __init__(self, name):
        self.nc = bass.Bass("TRN2", target_bir_lowering=False)
        self.P = Prog(self.nc)
        self.es = ExitStack()
        self.n = 0
        self.rr = 0
        nc = self.nc
        self.es.enter_context(nc.allow_non_contiguous_dma(reason="small strided parameter loads"))
        self.ps = [self.es.enter_context(nc.psum_tensor("ps%d" % i, [128, 512], F32)) for i in range(6)]
        self.psb = [Buf("ps%d" % i) for i in range(6)]
        self.pt = [self.es.enter_context(nc.psum_tensor("pt%d" % i, [128, 1024], BF16)) for i in range(2)]
        self.ptb = [Buf("pt%d" % i) for i in range(2)]
        self.pti = 0
        self.ident, self.identb = self.sb("ident", [128, 128], BF16)
        self.anti, self.antib = self.sb("anti", [128, 128], BF16)

    def sb(self, name, shape, dt):
        self.n += 1
        t = self.es.enter_context(self.nc.sbuf_tensor("%s_%d" % (name, self.n), shape, dt))
        return t, Buf(name)

    def scope(self):
        kb = self

        class _S:
            def __enter__(self_):
                self_.old = kb.es
                kb.es = ExitStack()
                return kb.es

            def __exit__(self_, *a):
                kb.P.barrier()
                kb.es.close()
                kb.es = self_.old
        return _S()

    def din(self, name, shape, dt=F32):
        return self.nc.dram_tensor(name, list(shape), dt, kind="ExternalInput").ap()

    def dout(self, name, shape, dt=F32):
        return self.nc.dram_tensor(name, list(shape), dt, kind="ExternalOutput").ap()

    def dscr(self, name, shape, dt=F32):
        return self.nc.dram_tensor(name, list(shape), dt, kind="Internal")

    def dq(self):
        self.rr += 1
        return "sp" if self.rr % 2 else "pool"

    def load_consts(self, ident_d, anti_d):
        st, stb = self.sb("cst", [128, 256], F32)
        self.P.dma("sp", st[:, 0:128], ident_d, writes=[stb])
        self.P.dma("sp", st[:, 128:256], anti_d, writes=[stb])
        nc = self.nc
        self.P.op("dve", lambda: nc.vector.tensor_copy(out=self.ident[:], in_=st[:, 0:128]), reads=[stb], writes=[self.identb])
        self.P.op("dve", lambda: nc.vector.tensor_copy(out=self.anti[:], in_=st[:, 128:256]), reads=[stb], writes=[self.antib])

    def load_w(self, dram, kdim, ncol, name, eng="pool", stage=None):
        nc, P = self.nc, self.P
        kp = min(128, kdim)
        nk = max(1, kdim // 128)
        w, wb = self.sb(name, [kp, nk, ncol], BF16)
        CH = 2048
        if stage is None:
            stage = [self.sb("wst", [128, CH], F32) for _ in range(2)]
        self._wst = stage
        i = 0
        for k in range(nk):
            for c0 in range(0, ncol, CH):
                cw = min(CH, ncol - c0)
                st, stb = stage[i % 2]
                i += 1
                P.dma(self.dq(), st[0:kp, 0:cw], dram[k * 128:k * 128 + kp, c0:c0 + cw], writes=[stb])
                self.cast(eng, w[:, k, c0:c0 + cw], st[0:kp, 0:cw], [stb], [wb])
        return w, wb

    def cast(self, eng, out, in_, reads, writes, sig=True):
        nc = self.nc
        if eng == "pool":
            self.P.op("pool", lambda: nc.gpsimd.tensor_copy(out=out, in_=in_), reads=reads, writes=writes, sig=sig)
        elif eng == "dve":
            self.P.op("dve", lambda: nc.vector.tensor_copy(out=out, in_=in_), reads=reads, writes=writes, sig=sig)
        else:
            self.P.op("act", lambda: nc.scalar.copy(out=out, in_=in_), reads=reads, writes=writes, sig=sig)

    def bcast_row(self, dram_row, ncol, name):
        t, tb = self.sb(name, [128, ncol], F32)
        src = AP(tensor=dram_row.tensor, offset=dram_row.offset, ap=[[0, 128], [1, ncol]])
        self.P.dma("sp", t[:], src, writes=[tb])
        return t, tb

    def rmsnorm(self, x, xb, g, gb, out, outb, dim, scr, scrb, np_=128):
        nc, P = self.nc, self.P
        P.op("act", lambda: nc.scalar.activation(out=out, in_=x, func=AF.Square, accum_out=scr[0:np_, 0:1]),
             reads=[xb], writes=[outb, scrb])
        P.op("act", lambda: nc.scalar.activation(out=scr[0:np_, 1:2], in_=scr[0:np_, 0:1], func=AF.Ln, scale=1.0 / dim, bias=self.eps_t[0:np_, 0:1]),
             reads=[scrb, self.eps_b], writes=[scrb])
        P.op("act", lambda: nc.scalar.activation(out=scr[0:np_, 2:3], in_=scr[0:np_, 1:2], func=AF.Exp, scale=-0.5),
             reads=[scrb], writes=[scrb])
        P.op("dve", lambda: nc.vector.scalar_tensor_tensor(out=out, in0=x, scalar=scr[0:np_, 2:3], in1=g, op0=ALU.mult, op1=ALU.mult),
             reads=[xb, scrb, gb], writes=[outb])

    def mk_eps(self):
        self.eps_t, self.eps_b = self.sb("eps", [128, 1], F32)
        nc = self.nc
        self.P.op("dve", lambda: nc.vector.memset(self.eps_t[:], EPS), writes=[self.eps_b])

    def transpose_to(self, src_list, dst, dstb, reads, evac="dve", np_in=128):
        nc, P = self.nc, self.P
        n = len(src_list)
        i0 = 0
        while i0 < n:
            nb = min(8, n - i0)
            pt = self.pt[self.pti % 2]
            ptb = self.ptb[self.pti % 2]
            self.pti += 1
            w = src_list[i0].shape[-1]
            for j in range(nb):
                s = src_list[i0 + j]
                P.op("pe", lambda s=s, j=j: nc.tensor.transpose(pt[0:w, j * 128:j * 128 + np_in], s, self.ident[0:np_in, 0:np_in]),
                     reads=list(reads) + [self.identb], writes=[ptb], sig=(j == nb - 1))
            src = pt[0:w, 0:nb * 128].rearrange("p (a b) -> p a b", b=128)[:, :, 0:np_in]
            o = dst[0:w, i0:i0 + nb, :]
            if evac == "dve":
                P.op("dve", lambda: nc.vector.tensor_copy(out=o, in_=src), reads=[ptb], writes=[dstb])
            else:
                P.op("act", lambda: nc.scalar.copy(out=o, in_=src), reads=[ptb], writes=[dstb])
            i0 += nb

    def run(self, in_maps):
        self.P.finish()
        self.es.close()
        res = run_bass_kernel_spmd(self.nc, in_maps, core_ids=list(range(8)))
        return res.results


def rel_bucket_np(dist):
    n = np.maximum(dist, 0)
    nf = np.maximum(n, 16).astype(np.float32)
    large = 16 + (np.log(nf / np.float32(16)) / np.float32(math.log(2048 / 16)) * np.float32(16)).astype(np.int32)
    large = np.minimum(large, 31)
    return np.where(n < 16, n, large)


def onehot_table(dist, masked):
    Y = dist.shape[0]
    oh = np.zeros((33, Y), np.float32)
    b = rel_bucket_np(dist)
    ok = ~masked
    oh[b[ok], np.nonzero(ok)[0]] = 1.0
    oh[32, masked] = 1.0
    return oh


Y_SLC = 2304
Y_WIN = 1280
Y_CMP = 5376


def host_tables(c):
    y = np.arange(Y_SLC)
    d = y - 511 + 128 * c
    oh_s = onehot_table(d, d < 0)
    y = np.arange(Y_WIN)
    d = y - 511 + 128 * c
    oh_w = onehot_table(d, (d < 0) | (d >= 512))
    y = np.arange(Y_CMP)
    d = y + 128 * c - 2063
    oh_c = onehot_table(d, d < 0)
    return oh_s, oh_w, oh_c


def build_tables(kb, rel_bias_d, ohs, names_Y):
    nc, P = kb.nc, kb.P
    bt, btb = kb.sb("biasT", [33, 12], F32)
    P.dma("sp", bt[0:32, :], rel_bias_d, writes=[btb])
    b31, b31b = kb.sb("b31", [33, 12], F32)
    src = AP(tensor=rel_bias_d.tensor, offset=rel_bias_d.offset + 31 * 12, ap=[[0, 32], [1, 12]])
    P.dma("sp", b31[0:32, :], src, writes=[b31b])
    bb, bbb = kb.sb("biasb", [33, 12], BF16)
    P.op("dve", lambda: nc.vector.memset(bb[:], NEGB), writes=[bbb])
    P.op("dve", lambda: nc.vector.tensor_tensor(out=bb[0:32, :], in0=bt[0:32, :], in1=b31[0:32, :], op=ALU.subtract),
         reads=[btb, b31b], writes=[bbb])
    outs = []
    for (oh_d, Y, nm) in [(o, y, n) for o, (n, y) in zip(ohs, names_Y)]:
        oh, ohb = kb.sb("oh" + nm, [33, Y], BF16)
        st, stb = kb.sb("ohst" + nm, [33, Y], F32)
        P.dma("sp", st[:], oh_d, writes=[stb])
        P.op("dve", lambda: nc.vector.tensor_copy(out=oh[:], in_=st[:]), reads=[stb], writes=[ohb])
        tt, ttb = kb.sb("tt" + nm, [12, Y], BF16)
        for c0 in range(0, Y, 512):
            cw = min(512, Y - c0)
            ps, psb = kb.ps[0], kb.psb[0]
            P.op("pe", lambda: nc.tensor.matmul(ps[0:12, 0:cw], lhsT=bb[:], rhs=oh[:, c0:c0 + cw], start=True, stop=True),
                 reads=[bbb, ohb], writes=[psb])
            P.op("dve", lambda: nc.vector.tensor_copy(out=tt[:, c0:c0 + cw], in_=ps[0:12, 0:cw]), reads=[psb], writes=[ttb])
        scr = kb.dscr("ttd" + nm, [12, Y], BF16)
        scrb = Buf("ttd" + nm)
        P.dma("sp", scr.ap(), tt[:], reads=[ttb], writes=[scrb])
        outs.append((scr, scrb, Y))
    return outs


def load_band(kb, scr, scrb, Y, Z, pstep, name, zoff=0, dst=None):
    if dst is None:
        dst = kb.sb(name, [128, 12, Z], BF16)
    t, tb = dst
    src = AP(tensor=scr, offset=zoff, ap=[[pstep, 128], [Y, 12], [1, Z]])
    kb.P.dma(kb.dq(), t[:, :, 0:Z], src, reads=[scrb], writes=[tb])
    return t, tb


FM_A = [(0 + 64 * i, 0.125) for i in range(12)] + [(768 + 64 * i, 1.0) for i in range(4)] + \
       [(1024, 1.0), (1088, 1.0), (1280, 1.0), (1344, 1.0)] + [(1572 + 64 * i, 0.125) for i in range(4)]


def proj_phase(kb, x_d, g_d, w, wb, ncols, fm_groups, tm_ranges, fmT_d, tm_d, post_tm=None):
    nc, P = kb.nc, kb.P
    g, gb = kb.bcast_row(g_d, D, "gain")
    xt = [kb.sb("xt", [128, D], F32) for _ in range(2)]
    xn = [kb.sb("xn", [128, D], BF16) for _ in range(2)]
    scr, scrb = kb.sb("scr", [128, 4], F32)
    xnT, xnTb = kb.sb("xnT", [128, 8, 512], BF16)
    ng = len(fm_groups)
    fst, fstb = kb.sb("fst", [64, ng, 512], BF16)
    ntm = sum(n for _, n in tm_ranges)
    tst = [kb.sb("tst", [128, max(ntm, 1)], F32) for _ in range(2)]
    ei = 0
    for blk in range(4):
        for tt in range(4):
            ti = blk * 4 + tt
            x_, xb_ = xt[ti % 2]
            n_, nb_ = xn[ti % 2]
            P.dma(kb.dq(), x_[:], x_d[ti * 128:(ti + 1) * 128, :], writes=[xb_])
            kb.rmsnorm(x_[:], xb_, g[:], gb, n_[:], nb_, D, scr, scrb)
            for half in range(1):
                pt = kb.pt[kb.pti % 2]
                ptb = kb.ptb[kb.pti % 2]
                kb.pti += 1
                for j in range(8):
                    P.op("pe", lambda j=j: nc.tensor.transpose(pt[:, j * 128:(j + 1) * 128], n_[:, j * 128:(j + 1) * 128], kb.ident[:]),
                         reads=[nb_, kb.identb], writes=[ptb], sig=(j == 7))
                P.op("dve", lambda: nc.vector.tensor_copy(out=xnT[:, :, tt * 128:(tt + 1) * 128],
                                                          in_=pt[:, :].rearrange("p (a b) -> p a b", b=128)),
                     reads=[ptb], writes=[xnTb])
            if ntm:
                ts_, tsb_ = tst[ti % 2]
                ps, psb = kb.ps[4], kb.psb[4]
                o = 0
                for (c0, n) in tm_ranges:
                    for k in range(8):
                        P.op("pe", lambda k=k, o=o, c0=c0, n=n: nc.tensor.matmul(ps[:, o:o + n], lhsT=xnT[:, k, tt * 128:(tt + 1) * 128], rhs=w[:, k, c0:c0 + n],
                                                                       start=(k == 0 and o == 0), stop=(k == 7)),
                             reads=[xnTb, wb], writes=[psb], sig=(k == 7))
                    o += n
                P.op("act", lambda: nc.scalar.copy(out=ts_[:, 0:ntm], in_=ps[:, 0:ntm]), reads=[psb], writes=[tsb_])
                if post_tm is not None:
                    post_tm(ts_, tsb_)
                P.dma(kb.dq(), tm_d[ti * 128:(ti + 1) * 128, :], ts_[:, 0:ntm], reads=[tsb_], final=True)
        for gi, (c0, sc) in enumerate(fm_groups):
            ps, psb = kb.ps[gi % 4], kb.psb[gi % 4]
            for k in range(8):
                P.op("pe", lambda k=k, c0=c0: nc.tensor.matmul(ps[0:64, :], lhsT=w[:, k, c0:c0 + 64], rhs=xnT[:, k, :], start=(k == 0), stop=(k == 7)),
                     reads=[xnTb, wb], writes=[psb], sig=(k == 7))
            if ei % 2 == 0:
                P.op("act", lambda: nc.scalar.activation(out=fst[:, gi, :], in_=ps[0:64, :], func=AF.Copy, scale=sc), reads=[psb], writes=[fstb])
            else:
                P.op("dve", lambda: nc.vector.tensor_scalar(out=fst[:, gi, :], in0=ps[0:64, :], scalar1=sc, scalar2=None, op0=ALU.mult), reads=[psb], writes=[fstb])
            ei += 1
        P.dma(kb.dq(), fmT_d[:, :, blk * 512:(blk + 1) * 512], fst[:], reads=[fstb], final=True)


def build_A():
    kb = K("A")
    nc, P = kb.nc, kb.P
    x_d = kb.din("x", [TOK, D])
    g_d = kb.din("g", [1, D])
    w_d = kb.din("w_in", [D, 1828])
    gb_d = kb.din("gate_b", [1, 36])
    id_d = kb.din("ident", [128, 128])
    an_d = kb.din("anti", [128, 128])
    fm_d = kb.dout("fmT", [64, 24, TOK], BF16)
    tm_d = kb.dout("tm", [TOK, 256 + 36])
    kb.load_consts(id_d, an_d)
    kb.mk_eps()
    w, wb = kb.load_w(w_d, D, 1828, "w_in")
    gbt, gbb = kb.bcast_row(gb_d, 36, "gateb")

    def post(ts_, tsb_):
        P.op("dve", lambda: nc.vector.tensor_tensor(out=ts_[:, 256:292], in0=ts_[:, 256:292], in1=gbt[:], op=ALU.add), reads=[tsb_, gbb], writes=[tsb_])
        P.op("act", lambda: nc.scalar.activation(out=ts_[:, 256:292], in_=ts_[:, 256:292], func=AF.Exp, scale=-1.0), reads=[tsb_], writes=[tsb_])
        P.op("dve", lambda: nc.vector.tensor_scalar(out=ts_[:, 256:292], in0=ts_[:, 256:292], scalar1=1.0, scalar2=None, op0=ALU.add), reads=[tsb_], writes=[tsb_])
        P.op("dve", lambda: nc.vector.reciprocal(out=ts_[:, 256:292], in_=ts_[:, 256:292]), reads=[tsb_], writes=[tsb_])

    proj_phase(kb, x_d, g_d, w, wb, 1828, FM_A, [(1152, 128), (1408, 128), (1536, 36)], fm_d, tm_d, post_tm=post)
    return kb


def core_tokens(c):
    return np.concatenate([np.arange(128 * (4 * m + c), 128 * (4 * m + c) + 128) for m in range(NT)])


def run_A(inputs):
    kb = build_A()
    ident = np.eye(128, dtype=np.float32)
    anti = np.ascontiguousarray(ident[::-1])
    maps = []
    for core in range(8):
        b, c = core // 4, core % 4
        maps.append({"x": np.ascontiguousarray(inputs["x"][b][core_tokens(c)]),
                     "g": inputs["norm_mix"][0:1], "w_in": inputs["nsa_w_in"][0],
                     "gate_b": inputs["nsa_gate_b"][0:1], "ident": ident, "anti": anti})
    return kb.run(maps)


def mm(kb, out, lhsT, rhs, start, reads, writes, sig=False, stop=True):
    nc = kb.nc
    kb.P.op("pe", lambda: nc.tensor.matmul(out, lhsT=lhsT, rhs=rhs, start=start, stop=stop), reads=reads, writes=writes, sig=sig)


def bc3(ap2d):
    return AP(tensor=ap2d.tensor, offset=ap2d.offset, ap=[list(ap2d.ap[0]), [0, 3], list(ap2d.ap[1])])


class Attn:
    def __init__(self, kb):
        self.kb = kb
        self.pT = [kb.sb("pT", [128, 384], BF16) for _ in range(3)]
        self.i = 0
        self.sbank = 0

    def unit(self, s_terms, near_terms, accs, accb, v_ap, vbufs, first):
        kb = self.kb
        nc, P = kb.nc, kb.P
        ps, psb = kb.ps[self.sbank], kb.psb[self.sbank]
        self.sbank ^= 1
        n = len(s_terms)
        for t, (l, r, bufs) in enumerate(s_terms):
            mm(kb, ps[:, 0:384], l, r, t == 0, bufs, [psb], sig=(t == n - 1 and not near_terms))
        if near_terms:
            for hh, (l, r, bufs) in enumerate(near_terms):
                mm(kb, ps[:, hh * 128:(hh + 1) * 128], l, r, False, bufs, [psb], sig=(hh == 2))
        pT, pTb = self.pT[self.i % 3]
        self.i += 1
        P.op("act", lambda: nc.scalar.activation(out=pT[:], in_=ps[:, 0:384], func=AF.Exp), reads=[psb], writes=[pTb])
        for hh in range(3):
            mm(kb, accs[hh], pT[:, hh * 128:(hh + 1) * 128], v_ap, first[hh], [pTb] + vbufs, [accb[hh]], sig=True)


def build_B(layer):
    kb = K("B")
    nc, P = kb.nc, kb.P
    x_d = kb.din("x", [TOK, D])
    id_d = kb.din("identb", [128, 128], BF16)
    an_d = kb.din("antib", [128, 128], BF16)
    relb_d = kb.din("rel_bias", [32, 12])
    oh_s_d = kb.din("oh_s", [33, Y_SLC])
    oh_w_d = kb.din("oh_w", [33, Y_WIN])
    oh_c_d = kb.din("oh_c", [33, Y_CMP])
    qT_d = kb.din("qT2", [128, 6, TOK], BF16)
    qm_d = kb.din("qmT", [64, 4, TOK], BF16)
    gates_d = kb.din("gates", [TOK, 36])
    ks_d = kb.din("ksT2", [128, T], BF16)
    kw_d = kb.din("kwT2", [128, T], BF16)
    kcr_d = kb.din("kcrT2", [128, T], BF16)
    vcr_d = kb.din("vcrT2", [128, T], BF16)
    vs_d = kb.din("vs", [T, 128])
    vw_d = kb.din("vw", [T, 128])
    ew_d = kb.din("ew", [128, T], BF16)
    ov_d = kb.din("ovl", [128, 4, 128], BF16)
    fb_d = kb.din("fb", [NT, 128, 128])
    w1k_d = kb.din("w1k", [2048, 256]); b1k_d = kb.din("b1k", [256, 1]); w2k_d = kb.din("w2k", [256, 64]); b2k_d = kb.din("b2k", [64, 1])
    w1v_d = kb.din("w1v", [2048, 256]); b1v_d = kb.din("b1v", [256, 1]); w2v_d = kb.din("w2v", [256, 64]); b2v_d = kb.din("b2v", [1, 64])
    posk_d = kb.din("posk", [32, 64]); posv_d = kb.din("posv", [32, 64])
    mem_d = kb.din("mem", [256, D]); gmem_d = kb.din("g_mem", [1, D]); wkv_d = kb.din("w_mem_kv", [D, 512])
    wout_d = kb.din("w_out", [D, D])
    hmid_d = kb.dout("hmid", [TOK, D])
    dcat_d = kb.dout("dbg_cat", [TOK, D])
    dband_d = kb.dout("dbg_band", [128, 2048], BF16)
    dpslc_d = kb.dout("dbg_pslc", [TOK, 128])

    P.dma("sp", kb.ident[:], id_d, writes=[kb.identb])
    P.dma("sp", kb.anti[:], an_d, writes=[kb.antib])
    kb.mk_eps()
    with kb.scope():
        tabs = build_tables(kb, relb_d, [oh_s_d, oh_w_d, oh_c_d], [("s", Y_SLC), ("w", Y_WIN), ("c", Y_CMP)])
    band_s, band_sb = load_band(kb, tabs[0][0], tabs[0][1], Y_SLC, 2048, 1, "band_s")
    band_w, band_wb = load_band(kb, tabs[1][0], tabs[1][1], Y_WIN, 1024, 1, "band_w")
    bandc = [kb.sb("bandc", [128, 12, 128], BF16) for _ in range(4)]
    P.dma("sp", dband_d, band_s[:, 0, :], reads=[band_sb], final=True)
    bandc_i = 0

    ksT, ksTb = kb.sb("ksT", [128, T], BF16)
    P.dma("sp", ksT[:, 0:4096], ks_d[:, 0:4096], writes=[ksTb])
    P.dma("pool", ksT[:, 4096:T], ks_d[:, 4096:T], writes=[ksTb])
    ew, ewb = kb.sb("ew", [128, T], BF16)
    P.dma("sp", ew[:, 0:4096], ew_d[:, 0:4096], writes=[ewb])
    P.dma("pool", ew[:, 4096:T], ew_d[:, 4096:T], writes=[ewb])
    vs1, vs1b = kb.sb("vs1", [128, 64, 2, 65], BF16)
    P.op("pool", lambda: nc.gpsimd.memset(vs1[:], 1.0), writes=[vs1b])
    with kb.scope():
        vst = [kb.sb("vst", [128, 8, 128], F32) for _ in range(2)]
        for i in range(8):
            st, stb = vst[i % 2]
            P.dma(kb.dq(), st[:], vs_d[i * 1024:(i + 1) * 1024, :].rearrange("(a p) c -> p a c", p=128), writes=[stb])
            P.op("pool", lambda: nc.gpsimd.tensor_copy(out=vs1[:, i * 8:(i + 1) * 8, :, 0:64], in_=st[:].rearrange("p a (g d) -> p a g d", g=2)),
                 reads=[stb], writes=[vs1b])
    RC, RCb = kb.sb("RC", [128, 4, 2, 193], BF16)
    P.op("pool", lambda: nc.gpsimd.memset(RC[:], 1.0), writes=[RCb])
    ovt, ovtb = kb.sb("ovt", [128, 4, 128], BF16)
    P.dma("sp", ovt[:], ov_d, writes=[ovtb])
    for g in range(2):
        P.op("pool", lambda: nc.gpsimd.tensor_copy(out=RC[:, :, g, 0:128], in_=ovt[:]), reads=[ovtb], writes=[RCb])
    kcT, kcTb = kb.sb("kcT", [128, 512], BF16)

    with ExitStack() as es2:
        def tmp(name, shape, dt):
            kb.n += 1
            return es2.enter_context(nc.sbuf_tensor("%s_%d" % (name, kb.n), shape, dt)), Buf(name)
        rawT, rawTb = tmp("rawT", [128, T + 32], BF16)
        w1, w1b = tmp("w1", [128, 32, 256], BF16)
        w1st, w1stb = tmp("w1st", [128, 8, 256], F32)
        w2, w2b = tmp("w2", [128, 2, 128], BF16)
        w2st, w2stb = tmp("w2st", [128, 2, 64], F32)
        h1T, h1Tb = tmp("h1T", [128, 2, 512], BF16)
        u_, ub = tmp("u", [128, 512], F32)
        t1, t1b = tmp("t1", [128, 512], F32)
        t2, t2b = tmp("t2", [128, 512], F32)
        cb, cbb = tmp("cb", [128, 2], F32)
        b1t, b1tb = tmp("b1t", [128, 2], F32)
        b2d, b2db = tmp("b2d", [128, 1], F32)
        b2r, b2rb = tmp("b2r", [128, 64], F32)
        pst, pstb = tmp("pst", [32, 64], F32)
        psb16, psb16b = tmp("psb16", [32, 64], BF16)
        posT, posTb = tmp("posT", [64, 1, 32], BF16)
        for kv in range(2):
            raw_d = kcr_d if kv == 0 else vcr_d
            w1_d = w1k_d if kv == 0 else w1v_d
            b1_d = b1k_d if kv == 0 else b1v_d
            w2_d = w2k_d if kv == 0 else w2v_d
            pos_d = posk_d if kv == 0 else posv_d
            P.op("dve", lambda: nc.vector.memset(rawT[:, T:T + 32], 0.0), writes=[rawTb])
            P.dma("sp", rawT[:, 0:4096], raw_d[:, 0:4096], writes=[rawTb])
            P.dma("pool", rawT[:, 4096:T], raw_d[:, 4096:T], writes=[rawTb])
            for q4 in range(4):
                for half in range(2):
                    P.dma(kb.dq(), w1st[half * 64:(half + 1) * 64, :, :], w1_d[q4 * 512:(q4 + 1) * 512, :].rearrange("(l d) h -> d l h", d=64), writes=[w1stb])
                P.op("dve", lambda: nc.vector.tensor_copy(out=w1[:, q4 * 8:(q4 + 1) * 8, :], in_=w1st[:]), reads=[w1stb], writes=[w1b])
            P.dma("sp", w2st[:], w2_d.rearrange("(c p) d -> p c d", p=128), writes=[w2stb])
            for half in range(2):
                P.op("dve", lambda: nc.vector.tensor_copy(out=w2[:, :, half * 64:(half + 1) * 64], in_=w2st[:]), reads=[w2stb], writes=[w2b])
            P.dma("sp", b1t[:], b1_d.rearrange("(c p) o -> p (c o)", p=128), writes=[b1tb])
            P.dma("sp", pst[:], pos_d, writes=[pstb])
            P.op("dve", lambda: nc.vector.tensor_copy(out=psb16[:], in_=pst[:]), reads=[pstb], writes=[psb16b])
            kb.transpose_to([psb16[:, :]], posT, posTb, [psb16b], np_in=32)
            for hc in range(2):
                ps, psb = kb.ps[2], kb.psb[2]
                for l in range(32):
                    mm(kb, ps[:, 0:1], w1[0:64, l, hc * 128:(hc + 1) * 128], posT[:, 0, l:l + 1], l == 0, [w1b, posTb], [psb], sig=(l == 31))
                P.op("dve", lambda: nc.vector.tensor_tensor(out=cb[:, hc:hc + 1], in0=ps[:, 0:1], in1=b1t[:, hc:hc + 1], op=ALU.add), reads=[psb, b1tb], writes=[cbb])
            if kv == 0:
                for half in range(2):
                    P.dma("sp", b2d[half * 64:(half + 1) * 64, :], b2k_d, writes=[b2db])
            else:
                src = AP(tensor=b2v_d.tensor, offset=b2v_d.offset, ap=[[0, 128], [1, 64]])
                P.dma("sp", b2r[:], src, writes=[b2rb])
            for g in range(2):
                gs = slice(g * 64, (g + 1) * 64)
                for hc in range(2):
                    ps, psb = kb.ps[2 + hc], kb.psb[2 + hc]
                    for l in range(32):
                        rhs = AP(tensor=rawT.tensor if hasattr(rawT, "tensor") else rawT[:].tensor, offset=rawT[gs, l:l + 1].offset,
                                 ap=[list(rawT[gs, :].ap[0]), [16, 512]])
                        mm(kb, ps[:, :], w1[gs, l, hc * 128:(hc + 1) * 128], rhs, l == 0, [w1b, rawTb], [psb], sig=(l == 31))
                    P.op("act", lambda: nc.scalar.activation(out=u_[:], in_=ps[:, :], func=AF.Identity, bias=cb[:, hc:hc + 1]), reads=[psb, cbb], writes=[ub])
                    P.op("dve", lambda: nc.vector.tensor_tensor(out=t1[:], in0=u_[:], in1=u_[:], op=ALU.mult), reads=[ub], writes=[t1b])
                    P.op("dve", lambda: nc.vector.tensor_scalar(out=t1[:], in0=t1[:], scalar1=0.044715, scalar2=1.0, op0=ALU.mult, op1=ALU.add), reads=[t1b], writes=[t1b])
                    P.op("dve", lambda: nc.vector.tensor_tensor(out=t1[:], in0=t1[:], in1=u_[:], op=ALU.mult), reads=[t1b, ub], writes=[t1b])
                    P.op("act", lambda: nc.scalar.activation(out=t2[:], in_=t1[:], func=AF.Tanh, scale=0.7978845608028654), reads=[t1b], writes=[t2b])
                    P.op("dve", lambda: nc.vector.tensor_scalar(out=t2[:], in0=t2[:], scalar1=0.5, scalar2=0.5, op0=ALU.mult, op1=ALU.add), reads=[t2b], writes=[t2b])
                    P.op("dve", lambda: nc.vector.tensor_tensor(out=h1T[:, hc, :], in0=t2[:], in1=u_[:], op=ALU.mult), reads=[t2b, ub], writes=[h1Tb])
                if kv == 0:
                    ps, psb = kb.ps[4], kb.psb[4]
                    for hc in range(2):
                        mm(kb, ps[:, :], w2[:, hc, :], h1T[:, hc, :], hc == 0, [w2b, h1Tb], [psb], sig=(hc == 1))
                    P.op("act", lambda: nc.scalar.activation(out=kcT[gs, :], in_=ps[gs, :], func=AF.Identity, bias=b2d[gs, 0:1]), reads=[psb, b2db], writes=[kcTb])
                else:
                    for j in range(4):
                        ps, psb = kb.ps[4], kb.psb[4]
                        for hc in range(2):
                            mm(kb, ps[:, 0:64], h1T[:, hc, j * 128:(j + 1) * 128], w2[:, hc, 0:64], hc == 0, [w2b, h1Tb], [psb], sig=(hc == 1))
                        P.op("dve", lambda: nc.vector.tensor_tensor(out=RC[:, j, g, 129:193], in0=ps[:, 0:64], in1=b2r[:], op=ALU.add), reads=[psb, b2rb], writes=[RCb])

    P.barrier()
    kmT, kmTb = kb.sb("kmT", [64, 4, 256], BF16)
    vm1, vm1b = kb.sb("vm1", [128, 2, 4, 65], BF16)
    P.op("pool", lambda: nc.gpsimd.memset(vm1[:], 1.0), writes=[vm1b])
    gm, gmb = kb.bcast_row(gmem_d, D, "gmem")
    wkv, wkvb = kb.sb("wkv", [128, 8, 512], BF16)
    with ExitStack() as es2:
        def tmp(name, shape, dt):
            kb.n += 1
            return es2.enter_context(nc.sbuf_tensor("%s_%d" % (name, kb.n), shape, dt)), Buf(name)
        mt, mtb = tmp("mt", [128, D], F32)
        mn, mnb = tmp("mn", [128, D], BF16)
        mnT, mnTb = tmp("mnT", [128, 8, 256], BF16)
        scr, scrb = tmp("mscr", [128, 4], F32)
        wst = [tmp("wst2", [128, 512], F32) for _ in range(2)]
        for k in range(8):
            st, stb = wst[k % 2]
            P.dma(kb.dq(), st[:], wkv_d[k * 128:(k + 1) * 128, :], writes=[stb])
            P.op("dve", lambda: nc.vector.tensor_copy(out=wkv[:, k, :], in_=st[:]), reads=[stb], writes=[wkvb])
        for tl in range(2):
            P.dma("sp", mt[:], mem_d[tl * 128:(tl + 1) * 128, :], writes=[mtb])
            kb.rmsnorm(mt[:], mtb, gm[:], gmb, mn[:], mnb, D, scr, scrb)
            pt = kb.pt[kb.pti % 2]; ptb = kb.ptb[kb.pti % 2]; kb.pti += 1
            for j in range(8):
                P.op("pe", lambda j=j: nc.tensor.transpose(pt[:, j * 128:(j + 1) * 128], mn[:, j * 128:(j + 1) * 128], kb.ident[:]),
                     reads=[mnb, kb.identb], writes=[ptb], sig=(j == 7))
            P.op("dve", lambda: nc.vector.tensor_copy(out=mnT[:, :, tl * 128:(tl + 1) * 128], in_=pt[:, :].rearrange("p (a b) -> p a b", b=128)), reads=[ptb], writes=[mnTb])
        for h in range(4):
            ps, psb = kb.ps[2], kb.psb[2]
            for k in range(8):
                mm(kb, ps[0:64, 0:256], wkv[:, k, h * 64:(h + 1) * 64], mnT[:, k, :], k == 0, [wkvb, mnTb], [psb], sig=(k == 7))
            P.op("dve", lambda: nc.vector.tensor_copy(out=kmT[:, h, :], in_=ps[0:64, 0:256]), reads=[psb], writes=[kmTb])
        for tl in range(2):
            ps, psb = kb.ps[3], kb.psb[3]
            for k in range(8):
                mm(kb, ps[:, 0:256], mnT[:, k, tl * 128:(tl + 1) * 128], wkv[:, k, 256:512], k == 0, [wkvb, mnTb], [psb], sig=(k == 7))
            P.op("dve", lambda: nc.vector.tensor_copy(out=vm1[:, tl, :, 0:64], in_=ps[:, 0:256].rearrange("p (h d) -> p h d", d=64)), reads=[psb], writes=[vm1b])
    P.barrier()
    wout, woutb = kb.sb("wout", [128, 8, D], BF16)
    with kb.scope():
        wst = [kb.sb("wst3", [128, 1024], F32) for _ in range(2)]
        for k in range(8):
            st, stb = wst[k % 2]
            P.dma(kb.dq(), st[:], wout_d[k * 128:(k + 1) * 128, :], writes=[stb])
            P.op("dve", lambda: nc.vector.tensor_copy(out=wout[:, k, :], in_=st[:]), reads=[stb], writes=[woutb])

    at = Attn(kb)
    qs = [kb.sb("qs", [128, 6, 128], BF16) for _ in range(2)]
    qms = [kb.sb("qms", [64, 4, 128], BF16) for _ in range(2)]
    gts = [kb.sb("gts", [128, 12, 3], F32) for _ in range(2)]
    kws = [kb.sb("kws", [128, 1024], BF16) for _ in range(2)]
    vws = [kb.sb("vws", [128, 8, 2, 65], BF16) for _ in range(2)]
    vwst = [kb.sb("vwst", [128, 8, 128], F32)] * 2
    for (t_, b_) in vws:
        P.op("pool", lambda: nc.gpsimd.memset(t_[:], 1.0), writes=[b_])
    fbs = [kb.sb("fbs", [128, 128], F32) for _ in range(2)]
    xts = [kb.sb("xts", [128, D], F32)] * 2
    cat, catb = kb.sb("cat", [128, D], F32)
    catbf, catbfb = kb.sb("catbf", [128, D], BF16)
    catT, catTb = kb.sb("catT", [128, 8, 128], BF16)
    pslc, pslcb = kb.sb("pslc", [128, 2, 128], F32)
    wk, wkb = kb.sb("wk", [128, 128], F32)
    m8, m8b = kb.sb("m8", [128, 16], F32)
    sbf, sbfb = kb.sb("sbf", [128, 128], BF16)
    sbT, sbTb = kb.sb("sbT", [128, 2, 128], BF16)
    sm, smb = kb.sb("sm", [128, 16], F32)
    pm, pmb = kb.sb("pm", [128, 256], BF16)
    accbank = 2

    def next_acc():
        nonlocal accbank
        b = accbank
        accbank = 2 + (accbank - 2 + 1) % 4
        return b

    for m in range(NT):
        q_, qb_ = qs[m % 2]
        qm_, qmb_ = qms[m % 2]
        gt_, gtb_ = gts[m % 2]
        kw_, kwb_ = kws[m % 2]
        vw_, vwb_ = vws[m % 2]
        vwst_, vwstb_ = vwst[m % 2]
        fb_, fbb_ = fbs[m % 2]
        xt_, xtb_ = xts[m % 2]
        tsl = slice(m * 128, (m + 1) * 128)
        P.dma("sp", q_[:], qT_d[:, :, tsl], writes=[qb_])
        P.dma("sp", qm_[:], qm_d[:, :, tsl], writes=[qmb_])
        P.dma("sp", gt_[:], gates_d[tsl, :].rearrange("p (h b) -> p h b", b=3), writes=[gtb_])
        P.dma("sp", fb_[:], fb_d[m], writes=[fbb_])
        P.dma("pool", xt_[:], x_d[tsl, :], writes=[xtb_])
        kt0 = max(0, 4 * m - 4)
        nkw = 4 * m + 4 - kt0
        P.dma("pool", kw_[:, 0:nkw * 128], kw_d[:, kt0 * 128:(4 * m + 4) * 128], writes=[kwb_])
        P.dma("sp", vwst_[:, 0:nkw, :], vw_d[kt0 * 128:(4 * m + 4) * 128, :].rearrange("(a p) c -> p a c", p=128), writes=[vwstb_])
        P.op("pool", lambda: nc.gpsimd.tensor_copy(out=vw_[:, 0:nkw, :, 0:64], in_=vwst_[:, 0:nkw, :].rearrange("p a (g d) -> p a g d", g=2)),
             reads=[vwstb_], writes=[vwb_])
        cband = {}
        for j in range(4):
            v = m - 4 * j
            if 0 <= v <= 6:
                dst = bandc[bandc_i % 4]
                bandc_i += 1
                load_band(kb, tabs[2][0], tabs[2][1], Y_CMP, 128, 16, "bc", zoff=512 * v, dst=dst)
                cband[j] = dst
        for g in range(2):
            gs = slice(g * 64, (g + 1) * 64)
            for tr in range(2):
                bA, bB = next_acc(), next_acc()
                accs = [kb.ps[bA][:, 0:193], kb.ps[bA][:, 193:386], kb.ps[bB][:, 0:193]]
                accb = [kb.psb[bA], kb.psb[bA], kb.psb[bB]]
                js = [j for j in range(4) if m - 4 * j >= 0]
                for ji, j in enumerate(js):
                    s_terms = [(kcT[gs, j * 128:(j + 1) * 128], q_[gs, 3 * tr:3 * tr + 3, :], [kcTb, qb_])]
                    near = None
                    if j in cband:
                        bt, btb = cband[j]
                        near = [(kb.anti[:], bt[:, 6 * g + 3 * tr + hh, :], [kb.antib, btb]) for hh in range(3)]
                    at.unit(s_terms, near, accs, accb, RC[:, j, g, :], [RCb], [ji == 0, False, ji == 0])
                for hh in range(3):
                    h = 6 * g + 3 * tr + hh
                    U = accs[hh]
                    P.op("dve", lambda: nc.vector.tensor_scalar(out=sm[:, 0:1], in0=U[:, 128:129], scalar1=1e-30, scalar2=None, op0=ALU.max), reads=[accb[hh]], writes=[smb])
                    P.op("dve", lambda: nc.vector.reciprocal(out=sm[:, 1:2], in_=sm[:, 0:1]), reads=[smb], writes=[smb])
                    if tr == 0 and hh == 0:
                        P.op("dve", lambda: nc.vector.tensor_scalar(out=pslc[:, g, :], in0=U[:, 0:128], scalar1=sm[:, 1:2], scalar2=None, op0=ALU.mult), reads=[accb[hh], smb], writes=[pslcb])
                    else:
                        P.op("dve", lambda: nc.vector.scalar_tensor_tensor(out=pslc[:, g, :], in0=U[:, 0:128], scalar=sm[:, 1:2], in1=pslc[:, g, :], op0=ALU.mult, op1=ALU.add), reads=[accb[hh], smb, pslcb], writes=[pslcb])
                    P.op("dve", lambda: nc.vector.tensor_tensor(out=sm[:, 2:3], in0=sm[:, 1:2], in1=gt_[:, h, 0:1], op=ALU.mult), reads=[smb, gtb_], writes=[smb])
                    P.op("dve", lambda: nc.vector.tensor_scalar(out=cat[:, h * 64:(h + 1) * 64], in0=U[:, 129:193], scalar1=sm[:, 2:3], scalar2=None, op0=ALU.mult), reads=[accb[hh], smb], writes=[catb])
            P.op("dve", lambda: nc.vector.tensor_tensor(out=wk[:], in0=pslc[:, g, :], in1=fb_[:], op=ALU.add), reads=[pslcb, fbb_], writes=[wkb])
            P.op("dve", lambda: nc.vector.max(out=m8[:, 0:8], in_=wk[:]), reads=[wkb], writes=[m8b])
            P.op("dve", lambda: nc.vector.match_replace(out=pslc[:, g, :], in_to_replace=m8[:, 0:8], in_values=wk[:], imm_value=-3e30), reads=[wkb, m8b], writes=[pslcb])
            P.op("dve", lambda: nc.vector.max(out=m8[:, 8:16], in_=pslc[:, g, :]), reads=[pslcb], writes=[m8b])
            P.op("dve", lambda: nc.vector.tensor_scalar(out=wk[:], in0=wk[:], scalar1=m8[:, 15:16], scalar2=-NEGB, op0=ALU.is_ge, op1=ALU.mult), reads=[wkb, m8b], writes=[wkb])
            P.op("dve", lambda: nc.vector.tensor_scalar(out=sbf[:], in0=wk[:], scalar1=NEGB, scalar2=None, op0=ALU.add), reads=[wkb], writes=[sbfb])
            pt = kb.pt[kb.pti % 2]; ptb = kb.ptb[kb.pti % 2]; kb.pti += 1
            P.op("pe", lambda: nc.tensor.transpose(pt[:, 0:128], sbf[:], kb.ident[:]), reads=[sbfb, kb.identb], writes=[ptb])
            P.op("dve", lambda: nc.vector.tensor_copy(out=sbT[:, g, :], in_=pt[:, 0:128]), reads=[ptb], writes=[sbTb])
        for br in range(2):
            for g in range(2):
                gs = slice(g * 64, (g + 1) * 64)
                for tr in range(2):
                    bA = next_acc()
                    accs = [kb.ps[bA][:, 65 * hh:65 * hh + 65] for hh in range(3)]
                    accb = [kb.psb[bA]] * 3
                    kts = list(range(0, 4 * m + 4)) if br == 0 else list(range(kt0, 4 * m + 4))
                    for ki, kt in enumerate(kts):
                        u = kt - 4 * m
                        if br == 0:
                            s_terms = [(ksT[gs, kt * 128:(kt + 1) * 128], q_[gs, 3 * tr:3 * tr + 3, :], [ksTb, qb_]),
                                       (ew[:, kt * 128:(kt + 1) * 128], bc3(sbT[:, g, :]), [ewb, sbTb])]
                            near = None
                            if u >= -12:
                                z0 = 128 * (3 - u)
                                near = [(kb.anti[:], band_s[:, 6 * g + 3 * tr + hh, z0:z0 + 128], [kb.antib, band_sb]) for hh in range(3)]
                            v_ap, vb_ = vs1[:, kt, g, :], [vs1b]
                        else:
                            kk = kt - kt0
                            s_terms = [(kw_[gs, kk * 128:(kk + 1) * 128], q_[gs, 3 * tr:3 * tr + 3, :], [kwb_, qb_])]
                            z0 = 128 * (3 - u)
                            near = [(kb.anti[:], band_w[:, 6 * g + 3 * tr + hh, z0:z0 + 128], [kb.antib, band_wb]) for hh in range(3)]
                            v_ap, vb_ = vw_[:, kk, g, :], [vwb_]
                        at.unit(s_terms, near, accs, accb, v_ap, vb_, [ki == 0, False, False])
                    for hh in range(3):
                        h = 6 * g + 3 * tr + hh
                        O = accs[hh]
                        P.op("dve", lambda: nc.vector.tensor_scalar(out=sm[:, 0:1], in0=O[:, 64:65], scalar1=1e-30, scalar2=None, op0=ALU.max), reads=[accb[hh]], writes=[smb])
                        P.op("dve", lambda: nc.vector.reciprocal(out=sm[:, 1:2], in_=sm[:, 0:1]), reads=[smb], writes=[smb])
                        P.op("dve", lambda: nc.vector.tensor_tensor(out=sm[:, 2:3], in0=sm[:, 1:2], in1=gt_[:, h, 1 + br:2 + br], op=ALU.mult), reads=[smb, gtb_], writes=[smb])
                        P.op("dve", lambda: nc.vector.scalar_tensor_tensor(out=cat[:, h * 64:(h + 1) * 64], in0=O[:, 0:64], scalar=sm[:, 2:3], in1=cat[:, h * 64:(h + 1) * 64], op0=ALU.mult, op1=ALU.add),
                             reads=[accb[hh], smb, catb], writes=[catb])
        for h in range(4):
            ps, psb = kb.ps[at.sbank], kb.psb[at.sbank]
            at.sbank ^= 1
            for tl in range(2):
                mm(kb, ps[:, tl * 128:(tl + 1) * 128], kmT[:, h, tl * 128:(tl + 1) * 128], qm_[:, h, :], tl == 0, [kmTb, qmb_], [psb], sig=(tl == 1))
            P.op("act", lambda: nc.scalar.activation(out=pm[:], in_=ps[:, 0:256], func=AF.Exp), reads=[psb], writes=[pmb])
            bA = next_acc()
            O = kb.ps[bA][:, 0:65]
            for tl in range(2):
                mm(kb, O, pm[:, tl * 128:(tl + 1) * 128], vm1[:, tl, h, :], tl == 0, [pmb, vm1b], [kb.psb[bA]], sig=(tl == 1))
            P.op("dve", lambda: nc.vector.reciprocal(out=sm[:, 4:5], in_=O[:, 64:65]), reads=[kb.psb[bA]], writes=[smb])
            P.op("dve", lambda: nc.vector.tensor_scalar(out=cat[:, 768 + h * 64:768 + (h + 1) * 64], in0=O[:, 0:64], scalar1=sm[:, 4:5], scalar2=None, op0=ALU.mult), reads=[kb.psb[bA], smb], writes=[catb])
        P.dma("sp", dcat_d[tsl, :], cat[:], reads=[catb], final=True)
        P.dma("sp", dpslc_d[tsl, :], wk[:], reads=[wkb], final=True)
        P.op("act", lambda: nc.scalar.copy(out=catbf[:], in_=cat[:]), reads=[catb], writes=[catbfb])
        pt = kb.pt[kb.pti % 2]; ptb = kb.ptb[kb.pti % 2]; kb.pti += 1
        for j in range(8):
            P.op("pe", lambda j=j: nc.tensor.transpose(pt[:, j * 128:(j + 1) * 128], catbf[:, j * 128:(j + 1) * 128], kb.ident[:]),
                 reads=[catbfb, kb.identb], writes=[ptb], sig=(j == 7))
        P.op("dve", lambda: nc.vector.tensor_copy(out=catT[:], in_=pt[:, :].rearrange("p (a b) -> p a b", b=128)), reads=[ptb], writes=[catTb])
        for half in range(2):
            bA = next_acc()
            ps, psb = kb.ps[bA], kb.psb[bA]
            for k in range(8):
                mm(kb, ps[:, :], catT[:, k, :], wout[:, k, half * 512:(half + 1) * 512], k == 0, [catTb, woutb], [psb], sig=(k == 7))
            P.op("dve", lambda: nc.vector.tensor_tensor(out=xt_[:, half * 512:(half + 1) * 512], in0=ps[:, :], in1=xt_[:, half * 512:(half + 1) * 512], op=ALU.add), reads=[psb, xtb_], writes=[xtb_])
        P.dma("sp", hmid_d[tsl, :], xt_[:], reads=[xtb_], final=True)
    return kb


def gather_seq(arrs, axis):
    shp = list(arrs[0].shape)
    shp[axis] = T
    out = np.zeros(shp, arrs[0].dtype)
    for c in range(4):
        idx = [slice(None)] * len(shp)
        idx[axis] = core_tokens(c)
        out[tuple(idx)] = arrs[c]
    return out


def consts_B(c):
    ident = np.eye(128, dtype=np.float32)
    oh_s, oh_w, oh_c = host_tables(c)
    ew = (np.arange(T)[None, :] // 64 == np.arange(128)[:, None]).astype(NPBF)
    n = np.arange(512)
    cs, ce = n * 16, n * 16 + 31
    ss = np.arange(128) * 64
    ov = ((cs[:, None] <= ss[None, :] + 63) & (ce[:, None] >= ss[None, :])).astype(np.float32)
    ov[511] = 0
    ovl = np.ascontiguousarray(ov.reshape(4, 128, 128).transpose(1, 0, 2)).astype(NPBF)
    fb = np.zeros((NT, 128, 128), np.float32)
    blk = np.arange(128)[None, :]
    for m in range(NT):
        t = 128 * (4 * m + c) + np.arange(128)[:, None]
        cur = t // 64
        forced = (blk == 0) | (blk == cur) | (blk == cur - 1)
        adm = (blk * 64) <= t
        fb[m] = np.where(adm, 1e4 * forced, -1e30)
    return {"identb": ident.astype(NPBF), "antib": np.ascontiguousarray(ident[::-1]).astype(NPBF),
            "oh_s": oh_s, "oh_w": oh_w, "oh_c": oh_c, "ew": ew, "ovl": ovl, "fb": fb}


def to2(fm, lo):
    return np.ascontiguousarray(np.concatenate([fm[:, lo, :], fm[:, lo + 1, :]], axis=0))


def run_B(inputs, resA):
    kb = build_B(0)
    maps = []
    for core in range(8):
        b, c = core // 4, core % 4
        grp = [resA[4 * b + cc] for cc in range(4)]
        fms = [np.asarray(r["fmT"]) for r in grp]
        tms = [np.asarray(r["tm"]) for r in grp]
        fm = fms[c]
        mp = consts_B(c)
        mp["x"] = np.ascontiguousarray(inputs["x"][b][core_tokens(c)])
        mp["rel_bias"] = inputs["rel_bias"]
        q = fm[:, 0:12, :]
        mp["qT2"] = np.ascontiguousarray(np.concatenate([q[:, 0:6, :], q[:, 6:12, :]], axis=0))
        mp["qmT"] = np.ascontiguousarray(fm[:, 20:24, :])
        mp["gates"] = np.ascontiguousarray(tms[c][:, 256:292])
        mp["kcrT2"] = gather_seq([to2(f, 12) for f in fms], 1)
        mp["vcrT2"] = gather_seq([to2(f, 14) for f in fms], 1)
        mp["ksT2"] = gather_seq([to2(f, 16) for f in fms], 1)
        mp["kwT2"] = gather_seq([to2(f, 18) for f in fms], 1)
        mp["vs"] = gather_seq([np.ascontiguousarray(t_[:, 0:128]) for t_ in tms], 0)
        mp["vw"] = gather_seq([np.ascontiguousarray(t_[:, 128:256]) for t_ in tms], 0)
        mp["w1k"] = inputs["nsa_cmp_k_w1"][0]; mp["b1k"] = inputs["nsa_cmp_k_b1"][0].reshape(256, 1)
        mp["w2k"] = inputs["nsa_cmp_k_w2"][0]; mp["b2k"] = inputs["nsa_cmp_k_b2"][0].reshape(64, 1)
        mp["w1v"] = inputs["nsa_cmp_v_w1"][0]; mp["b1v"] = inputs["nsa_cmp_v_b1"][0].reshape(256, 1)
        mp["w2v"] = inputs["nsa_cmp_v_w2"][0]; mp["b2v"] = inputs["nsa_cmp_v_b2"][0].reshape(1, 64)
        mp["posk"] = inputs["nsa_cmp_pos_k"][0]; mp["posv"] = inputs["nsa_cmp_pos_v"][0]
        mp["mem"] = inputs["mem"][b]; mp["g_mem"] = inputs["norm_mem"][0:1]; mp["w_mem_kv"] = inputs["w_mem_kv"][0]
        mp["w_out"] = inputs["w_out"][0]
        maps.append(mp)
    return kb.run(maps)


def scatter_tokens(per_core, key):
    out = np.zeros((2, T, D), np.float32)
    for core in range(8):
        b, c = core // 4, core % 4
        out[b][core_tokens(c)] = np.asarray(per_core[core][key])
    return out


def kernel(**inputs):
    inputs = {k: np.asarray(v) for k, v in inputs.items()}
    resA = run_A(inputs)
    resB = run_B(inputs, resA)
    resF = run_F(inputs, [r["hmid"] for r in resB], 0, False)
    resC = run_C(inputs, resF)
    resG = run_F(inputs, [r["hmid"] for r in resC], 1, True)
    return scatter_tokens(resG, "h")


def build_F(final, nproj):
    kb = K("F")
    nc, P = kb.nc, kb.P
    hm_d = kb.din("hmid", [TOK, D])
    id_d = kb.din("identb", [128, 128], BF16)
    g_d = kb.din("g_ffn", [1, D])
    wg_d = kb.din("wg", [D, DFF]); wu_d = kb.din("wu", [D, DFF]); wd_d = kb.din("wd", [DFF, D])
    g2_d = kb.din("g2", [1, D])
    h_d = kb.dout("h", [TOK, D])
    if not final:
        win_d = kb.din("w_in2", [D, nproj])
        pr_d = kb.dout("pr", [TOK, nproj])
    P.dma("sp", kb.ident[:], id_d, writes=[kb.identb])
    kb.mk_eps()
    g, gb = kb.bcast_row(g_d, D, "gffn")
    g2, g2b = kb.bcast_row(g2_d, D, "g2")
    H, Hb = kb.sb("H", [128, NT, D], F32)
    Hbs = [Buf("H%d" % i) for i in range(NT)]
    hnT, hnTb = kb.sb("hnT", [128, 8, TOK], BF16)
    hn, hnb = kb.sb("hn", [128, D], BF16)
    scr, scrb = kb.sb("scr", [128, 4], F32)

    def norm_T(gt, gtb):
        for ti in range(NT):
            kb.rmsnorm(H[:, ti, :], Hbs[ti], gt[:], gtb, hn[:], hnb, D, scr, scrb)
            pt = kb.pt[kb.pti % 2]; ptb = kb.ptb[kb.pti % 2]; kb.pti += 1
            for j in range(8):
                P.op("pe", lambda j=j: nc.tensor.transpose(pt[:, j * 128:(j + 1) * 128], hn[:, j * 128:(j + 1) * 128], kb.ident[:]),
                     reads=[hnb, kb.identb], writes=[ptb], sig=(j == 7))
            P.op("dve", lambda: nc.vector.tensor_copy(out=hnT[:, :, ti * 128:(ti + 1) * 128], in_=pt[:, :].rearrange("p (a b) -> p a b", b=128)), reads=[ptb], writes=[hnTb])

    for ti in range(NT):
        P.dma(kb.dq(), H[:, ti, :], hm_d[ti * 128:(ti + 1) * 128, :], writes=[Hbs[ti]])
    norm_T(g, gb)
    wgs, wgsb = kb.sb("wgs", [128, 8, 512], BF16)
    wus, wusb = kb.sb("wus", [128, 8, 512], BF16)
    wds, wdsb = kb.sb("wds", [128, 4, D], BF16)
    stg = [kb.sb("stg", [128, 1024], F32) for _ in range(2)]
    sg, sgb = kb.sb("sg", [128, 512], F32)
    ab, abb = kb.sb("ab", [128, 512], BF16)
    aT, aTb = kb.sb("aT", [128, 4, 128], BF16)
    si = 0
    for fg in range(6):
        c0 = fg * 512
        cw = min(512, DFF - c0)
        nch = cw // 128
        for (wsrc, wdst, wdstb) in ((wg_d, wgs, wgsb), (wu_d, wus, wusb)):
            for k in range(8):
                st, stb = stg[si % 2]; si += 1
                P.dma(kb.dq(), st[:, 0:cw], wsrc[k * 128:(k + 1) * 128, c0:c0 + cw], writes=[stb])
                kb.cast("pool" if k % 2 else "act", wdst[:, k, 0:cw], st[:, 0:cw], [stb], [wdstb])
        for ch in range(nch):
            st, stb = stg[si % 2]; si += 1
            P.dma(kb.dq(), st[:], wd_d[c0 + ch * 128:c0 + (ch + 1) * 128, :], writes=[stb])
            kb.cast("pool" if ch % 2 else "act", wds[:, ch, :], st[:], [stb], [wdsb])
        for ti in range(NT):
            tsl = slice(ti * 128, (ti + 1) * 128)
            pg, pgb = kb.ps[0], kb.psb[0]
            pu, pub = kb.ps[1], kb.psb[1]
            for k in range(8):
                mm(kb, pg[:, 0:cw], hnT[:, k, tsl], wgs[:, k, 0:cw], k == 0, [hnTb, wgsb], [pgb], sig=(k == 7))
            for k in range(8):
                mm(kb, pu[:, 0:cw], hnT[:, k, tsl], wus[:, k, 0:cw], k == 0, [hnTb, wusb], [pub], sig=(k == 7))
            P.op("act", lambda: nc.scalar.activation(out=sg[:, 0:cw], in_=pg[:, 0:cw], func=AF.Exp, scale=-1.0), reads=[pgb], writes=[sgb])
            P.op("dve", lambda: nc.vector.tensor_scalar(out=sg[:, 0:cw], in0=sg[:, 0:cw], scalar1=1.0, scalar2=None, op0=ALU.add), reads=[sgb], writes=[sgb])
            P.op("dve", lambda: nc.vector.reciprocal(out=sg[:, 0:cw], in_=sg[:, 0:cw]), reads=[sgb], writes=[sgb])
            P.op("dve", lambda: nc.vector.tensor_tensor(out=sg[:, 0:cw], in0=sg[:, 0:cw], in1=pg[:, 0:cw], op=ALU.mult), reads=[sgb, pgb], writes=[sgb])
            P.op("dve", lambda: nc.vector.tensor_tensor(out=ab[:, 0:cw], in0=sg[:, 0:cw], in1=pu[:, 0:cw], op=ALU.mult), reads=[sgb, pub], writes=[abb])
            pt = kb.pt[kb.pti % 2]; ptb = kb.ptb[kb.pti % 2]; kb.pti += 1
            for j in range(nch):
                P.op("pe", lambda j=j: nc.tensor.transpose(pt[:, j * 128:(j + 1) * 128], ab[:, j * 128:(j + 1) * 128], kb.ident[:]),
                     reads=[abb, kb.identb], writes=[ptb], sig=(j == nch - 1))
            P.op("act", lambda: nc.scalar.copy(out=aT[:, 0:nch, :], in_=pt[:, 0:nch * 128].rearrange("p (a b) -> p a b", b=128)), reads=[ptb], writes=[aTb])
            for half in range(2):
                py, pyb = kb.ps[2 + half], kb.psb[2 + half]
                for ch in range(nch):
                    mm(kb, py[:, :], aT[:, ch, :], wds[:, ch, half * 512:(half + 1) * 512], ch == 0, [aTb, wdsb], [pyb], sig=(ch == nch - 1))
                P.op("dve", lambda: nc.vector.tensor_tensor(out=H[:, ti, half * 512:(half + 1) * 512], in0=py[:, :], in1=H[:, ti, half * 512:(half + 1) * 512], op=ALU.add),
                     reads=[pyb, Hbs[ti]], writes=[Hbs[ti]])
    if final:
        o, ob = kb.sb("o", [128, D], F32)
        for ti in range(NT):
            kb.rmsnorm(H[:, ti, :], Hbs[ti], g2[:], g2b, o[:], ob, D, scr, scrb)
            P.dma(kb.dq(), h_d[ti * 128:(ti + 1) * 128, :], o[:], reads=[ob], final=True)
    else:
        for ti in range(NT):
            P.dma(kb.dq(), h_d[ti * 128:(ti + 1) * 128, :], H[:, ti, :], reads=[Hbs[ti]], final=True)
        norm_T(g2, g2b)
        win, winb = kb.sb("win", [128, 8, nproj], BF16)
        for k in range(8):
            st, stb = stg[si % 2]; si += 1
            P.dma(kb.dq(), st[:, 0:nproj], win_d[k * 128:(k + 1) * 128, :], writes=[stb])
            kb.cast("pool" if k % 2 else "act", win[:, k, :], st[:, 0:nproj], [stb], [winb])
        pro, prob = kb.sb("pro", [128, nproj], F32)
        for ti in range(NT):
            tsl = slice(ti * 128, (ti + 1) * 128)
            o0 = 0
            bi = 0
            while o0 < nproj:
                n = min(512, nproj - o0)
                ps, psb = kb.ps[bi % 2], kb.psb[bi % 2]
                for k in range(8):
                    mm(kb, ps[:, 0:n], hnT[:, k, tsl], win[:, k, o0:o0 + n], k == 0, [hnTb, winb], [psb], sig=(k == 7))
                P.op("act", lambda: nc.scalar.copy(out=pro[:, o0:o0 + n], in_=ps[:, 0:n]), reads=[psb], writes=[prob])
                o0 += n
                bi += 1
            P.dma(kb.dq(), pr_d[tsl, :], pro[:], reads=[prob], final=True)
    return kb


def run_F(inputs, hmids, layer, final):
    kb = build_F(final, 712)
    ident = np.eye(128, dtype=np.float32).astype(NPBF)
    maps = []
    for core in range(8):
        mp = {"hmid": np.asarray(hmids[core]), "identb": ident, "g_ffn": inputs["norm_ffn"][layer:layer + 1],
              "wg": inputs["ffn_gate"][layer], "wu": inputs["ffn_up"][layer], "wd": inputs["ffn_down"][layer]}
        if final:
            mp["g2"] = inputs["norm_final"].reshape(1, D)
        else:
            mp["g2"] = inputs["norm_mix"][layer + 1:layer + 2]
            mp["w_in2"] = inputs["dsa_w_in"][0]
        maps.append(mp)
    return kb.run(maps)


NIT = 20


def mem_setup(kb, mem_d, gmem_d, wkv_d):
    nc, P = kb.nc, kb.P
    kmT, kmTb = kb.sb("kmT", [64, 4, 256], BF16)
    vm1, vm1b = kb.sb("vm1", [128, 2, 4, 65], BF16)
    P.op("pool", lambda: nc.gpsimd.memset(vm1[:], 1.0), writes=[vm1b])
    with kb.scope():
        gm, gmb = kb.bcast_row(gmem_d, D, "gmem")
        wkv, wkvb = kb.sb("wkv", [128, 8, 512], BF16)
        mt, mtb = kb.sb("mt", [128, D], F32)
        mn, mnb = kb.sb("mn", [128, D], BF16)
        mnT, mnTb = kb.sb("mnT", [128, 8, 256], BF16)
        scr, scrb = kb.sb("mscr", [128, 4], F32)
        wst = [kb.sb("wst2", [128, 512], F32) for _ in range(2)]
        for k in range(8):
            st, stb = wst[k % 2]
            P.dma(kb.dq(), st[:], wkv_d[k * 128:(k + 1) * 128, :], writes=[stb])
            P.op("dve", lambda: nc.vector.tensor_copy(out=wkv[:, k, :], in_=st[:]), reads=[stb], writes=[wkvb])
        for tl in range(2):
            P.dma("sp", mt[:], mem_d[tl * 128:(tl + 1) * 128, :], writes=[mtb])
            kb.rmsnorm(mt[:], mtb, gm[:], gmb, mn[:], mnb, D, scr, scrb)
            pt = kb.pt[kb.pti % 2]; ptb = kb.ptb[kb.pti % 2]; kb.pti += 1
            for j in range(8):
                P.op("pe", lambda j=j: nc.tensor.transpose(pt[:, j * 128:(j + 1) * 128], mn[:, j * 128:(j + 1) * 128], kb.ident[:]),
                     reads=[mnb, kb.identb], writes=[ptb], sig=(j == 7))
            P.op("dve", lambda: nc.vector.tensor_copy(out=mnT[:, :, tl * 128:(tl + 1) * 128], in_=pt[:, :].rearrange("p (a b) -> p a b", b=128)), reads=[ptb], writes=[mnTb])
        for h in range(4):
            ps, psb = kb.ps[2], kb.psb[2]
            for k in range(8):
                mm(kb, ps[0:64, 0:256], wkv[:, k, h * 64:(h + 1) * 64], mnT[:, k, :], k == 0, [wkvb, mnTb], [psb], sig=(k == 7))
            P.op("dve", lambda: nc.vector.tensor_copy(out=kmT[:, h, :], in_=ps[0:64, 0:256]), reads=[psb], writes=[kmTb])
        for tl in range(2):
            ps, psb = kb.ps[3], kb.psb[3]
            for k in range(8):
                mm(kb, ps[:, 0:256], mnT[:, k, tl * 128:(tl + 1) * 128], wkv[:, k, 256:512], k == 0, [wkvb, mnTb], [psb], sig=(k == 7))
            P.op("dve", lambda: nc.vector.tensor_copy(out=vm1[:, tl, :, 0:64], in_=ps[:, 0:256].rearrange("p (h d) -> p h d", d=64)), reads=[psb], writes=[vm1b])
    return kmT, kmTb, vm1, vm1b


def load_wout(kb, wout_d):
    nc, P = kb.nc, kb.P
    wout, woutb = kb.sb("wout", [128, 8, D], BF16)
    with kb.scope():
        wst = [kb.sb("wst3", [128, 1024], F32) for _ in range(2)]
        for k in range(8):
            st, stb = wst[k % 2]
            P.dma(kb.dq(), st[:], wout_d[k * 128:(k + 1) * 128, :], writes=[stb])
            P.op("dve", lambda: nc.vector.tensor_copy(out=wout[:, k, :], in_=st[:]), reads=[stb], writes=[woutb])
    return wout, woutb


def rms_small(kb, x, xb, A, d, g, gb, out, outb, tmp, tmpb, ss, ssb):
    nc, P = kb.nc, kb.P
    P.op("dve", lambda: nc.vector.tensor_tensor(out=tmp, in0=x, in1=x, op=ALU.mult), reads=[xb], writes=[tmpb])
    P.op("dve", lambda: nc.vector.tensor_reduce(out=ss[:, 0:A], in_=tmp, axis=AX.X, op=ALU.add), reads=[tmpb], writes=[ssb])
    P.op("act", lambda: nc.scalar.activation(out=ss[:, A:2 * A], in_=ss[:, 0:A], func=AF.Ln, scale=1.0 / d, bias=kb.eps_t[:, 0:1]), reads=[ssb, kb.eps_b], writes=[ssb])
    P.op("act", lambda: nc.scalar.activation(out=ss[:, 0:A], in_=ss[:, A:2 * A], func=AF.Exp, scale=-0.5), reads=[ssb], writes=[ssb])
    r = ss[:, 0:A]
    rb_ = AP(tensor=r.tensor, offset=r.offset, ap=[list(r.ap[0]), list(r.ap[1]), [0, d]])
    g2 = g[:, 0:d]
    gb_ = AP(tensor=g2.tensor, offset=g2.offset, ap=[list(g2.ap[0]), [0, A], list(g2.ap[1])])
    P.op("dve", lambda: nc.vector.tensor_tensor(out=tmp, in0=x, in1=rb_, op=ALU.mult), reads=[xb, ssb], writes=[tmpb])
    P.op("dve", lambda: nc.vector.tensor_tensor(out=out, in0=tmp, in1=gb_, op=ALU.mult), reads=[tmpb, gb], writes=[outb])


def build_C(stage=99, nslots=NT):
    kb = K("C")
    nc, P = kb.nc, kb.P
    x_d = kb.din("x", [TOK, D])
    pr_d = kb.din("pr", [TOK, 712])
    ckv_d = kb.din("ckv_seq", [T, 128])
    kidx_d = kb.din("kidx_seq", [T, 64])
    id_d = kb.din("identb", [128, 128], BF16)
    an_d = kb.din("antib", [128, 128], BF16)
    relb_d = kb.din("rel_bias", [32, 12])
    oh_s_d = kb.din("oh_s", [33, Y_SLC])
    cm_d = kb.din("cm", [128, 512])
    pw_d = kb.din("pw", [128, NIT])
    qn_d = kb.din("q_norm", [1, 256]); kvn_d = kb.din("kv_norm", [1, 128]); kin_d = kb.din("kidx_norm", [1, 64])
    wqup_d = kb.din("w_q_up", [256, 768]); wuk_d = kb.din("w_uk", [128, 768]); wuv_d = kb.din("w_uv", [128, 768])
    wqi_d = kb.din("w_q_idx", [256, 512])
    mem_d = kb.din("mem", [256, D]); gmem_d = kb.din("g_mem", [1, D]); wkv_d = kb.din("w_mem_kv", [D, 512])
    wout_d = kb.din("w_out", [D, D])
    hmid_d = kb.dout("hmid", [TOK, D])

    P.dma("sp", kb.ident[:], id_d, writes=[kb.identb])
    P.dma("sp", kb.anti[:], an_d, writes=[kb.antib])
    kb.mk_eps()
    with kb.scope():
        tabs = build_tables(kb, relb_d, [oh_s_d], [("s", Y_SLC)])
    band_s, band_sb = load_band(kb, tabs[0][0], tabs[0][1], Y_SLC, 2048, 1, "band_s")
    ckvT, ckvTb = kb.sb("ckvT", [128, T], BF16)
    ckv1, ckv1b = kb.sb("ckv1", [128, 64, 129], BF16)
    kidxT, kidxTb = kb.sb("kidxT", [64, T], BF16)
    P.op("pool", lambda: nc.gpsimd.memset(ckv1[:], 1.0), writes=[ckv1b])
    gq, gqb = kb.bcast_row(qn_d, 256, "gq")
    with kb.scope():
        gkv, gkvb = kb.bcast_row(kvn_d, 128, "gkv")
        gki, gkib = kb.bcast_row(kin_d, 64, "gki")
        st, stb = kb.sb("kst", [128, 8, 128], F32)
        tmp, tmpb = kb.sb("ktmp", [128, 8, 128], F32)
        ss, ssb = kb.sb("kss", [128, 16], F32)
        kin, kinb = kb.sb("kin", [128, 8, 64], BF16)
        for i in range(8):
            P.dma(kb.dq(), st[:], ckv_d[i * 1024:(i + 1) * 1024, :].rearrange("(a p) c -> p a c", p=128), writes=[stb])
            rms_small(kb, st[:], stb, 8, 128, gkv, gkvb, ckv1[:, i * 8:(i + 1) * 8, 0:128], ckv1b, tmp[:], tmpb, ss, ssb)
            pt = kb.pt[kb.pti % 2]; ptb = kb.ptb[kb.pti % 2]; kb.pti += 1
            for j in range(8):
                P.op("pe", lambda j=j: nc.tensor.transpose(pt[:, j * 128:(j + 1) * 128], ckv1[:, i * 8 + j, 0:128], kb.ident[:]),
                     reads=[ckv1b, kb.identb], writes=[ptb], sig=(j == 7))
            P.op("act", lambda: nc.scalar.copy(out=ckvT[:, i * 1024:(i + 1) * 1024], in_=pt[:, :]), reads=[ptb], writes=[ckvTb])
        for i in range(8):
            P.dma(kb.dq(), st[:, :, 0:64], kidx_d[i * 1024:(i + 1) * 1024, :].rearrange("(a p) c -> p a c", p=128), writes=[stb])
            rms_small(kb, st[:, :, 0:64], stb, 8, 64, gki, gkib, kin[:], kinb, tmp[:, :, 0:64], tmpb, ss, ssb)
            pt = kb.pt[kb.pti % 2]; ptb = kb.ptb[kb.pti % 2]; kb.pti += 1
            for j in range(8):
                P.op("pe", lambda j=j: nc.tensor.transpose(pt[0:64, j * 128:(j + 1) * 128], kin[:, j, :], kb.ident[:]),
                     reads=[kinb, kb.identb], writes=[ptb], sig=(j == 7))
            P.op("act", lambda: nc.scalar.copy(out=kidxT[:, i * 1024:(i + 1) * 1024], in_=pt[0:64, :]), reads=[ptb], writes=[kidxTb])
    wqup, wqupb = kb.sb("wqup", [128, 2, 768], BF16)
    wqi, wqib = kb.sb("wqi", [128, 2, 512], BF16)
    wuv, wuvb = kb.sb("wuv", [128, 768], BF16)
    wukT, wukTb = kb.sb("wukT", [64, 12, 128], BF16)
    with kb.scope():
        wst = [kb.sb("wst4", [128, 768], F32) for _ in range(2)]
        wukb, wukbb = kb.sb("wukb", [128, 768], BF16)
        i = 0
        for (src, rows, ncol, dstf) in [(wqup_d, 0, 768, lambda: wqup[:, 0, :]), (wqup_d, 128, 768, lambda: wqup[:, 1, :]),
                                        (wqi_d, 0, 512, lambda: wqi[:, 0, :]), (wqi_d, 128, 512, lambda: wqi[:, 1, :]),
                                        (wuv_d, 0, 768, lambda: wuv[:]), (wuk_d, 0, 768, lambda: wukb[:])]:
            st, stb = wst[i % 2]; i += 1
            P.dma(kb.dq(), st[:, 0:ncol], src[rows:rows + 128, :], writes=[stb])
            dst = dstf()
            P.op("dve", lambda: nc.vector.tensor_copy(out=dst, in_=st[:, 0:ncol]), reads=[stb], writes=[wqupb, wqib, wuvb, wukbb])
        for h0 in (0, 8):
            nb = min(8, 12 - h0)
            pt = kb.pt[kb.pti % 2]; ptb = kb.ptb[kb.pti % 2]; kb.pti += 1
            for j in range(nb):
                P.op("pe", lambda j=j: nc.tensor.transpose(pt[0:64, j * 128:(j + 1) * 128], wukb[:, (h0 + j) * 64:(h0 + j + 1) * 64], kb.ident[:]),
                     reads=[wukbb, kb.identb], writes=[ptb], sig=(j == nb - 1))
            P.op("dve", lambda: nc.vector.tensor_copy(out=wukT[:, h0:h0 + nb, :], in_=pt[0:64, 0:nb * 128].rearrange("p (a b) -> p a b", b=128)), reads=[ptb], writes=[wukTb])
    kmT, kmTb, vm1, vm1b = mem_setup(kb, mem_d, gmem_d, wkv_d)
    wout, woutb = load_wout(kb, wout_d)
    cm, cmb = kb.sb("cm", [128, 512], F32)
    P.dma("sp", cm[:], cm_d, writes=[cmb])
    pw, pwb = kb.sb("pw", [128, NIT], F32)
    P.dma("sp", pw[:], pw_d, writes=[pwb])

    at = Attn(kb)
    score, scoreb = kb.sb("score", [128, T], F32)
    mb, mbb = kb.sb("mb", [128, T], BF16)
    mTs = [kb.sb("mT", [128, 8, 128], BF16) for _ in range(2)]
    big, bigb = kb.sb("big", [128, D], F32)
    cqn, cqnb = kb.sb("cqn", [128, 256], BF16)
    cqT, cqTb = kb.sb("cqT", [128, 2, 128], BF16)
    qhT, qhTb = kb.sb("qhT", [128, 12, 128], BF16)
    qabs, qabsb = kb.sb("qabs", [128, 12, 128], BF16)
    qiT, qiTb = kb.sb("qiT", [64, 8, 128], BF16)
    qmb16, qmb16b = kb.sb("qmb16", [128, 256], BF16)
    qmT, qmTb = kb.sb("qmT", [64, 4, 128], BF16)
    rt, rtb = kb.sb("rt", [128, 512], F32)
    catbf, catbfb = kb.sb("catbf", [128, D], BF16)
    catT, catTb = kb.sb("catT", [128, 8, 128], BF16)
    pm, pmb = kb.sb("pm", [128, 256], BF16)
    sm, smb = kb.sb("sm", [128, 16], F32)
    wv, wvb = kb.sb("wv", [128, 32], F32)
    bs, bsb = kb.sb("bs", [128, 8 + 2 * NIT], F32)
    accbank = 2

    def next_acc():
        nonlocal accbank
        b = accbank
        accbank = 2 + (accbank - 2 + 1) % 4
        return b

    for m in range(nslots):
        tsl = slice(m * 128, (m + 1) * 128)
        L = 128 * (4 * m + 4)
        nkt = 4 * m + 4
        P.dma("sp", big[:, 0:712], pr_d[tsl, :], writes=[bigb])
        if stage == 0:
            P.dma("sp", hmid_d[tsl, :], big[:], reads=[bigb], final=True)
            continue
        ssq = wv[:, 16:18]
        P.op("act", lambda: nc.scalar.activation(out=cqn[:], in_=big[:, 0:256], func=AF.Square, accum_out=wv[:, 16:17]), reads=[bigb], writes=[cqnb, wvb])
        P.op("act", lambda: nc.scalar.activation(out=wv[:, 17:18], in_=wv[:, 16:17], func=AF.Ln, scale=1.0 / 256, bias=kb.eps_t[:, 0:1]), reads=[wvb, kb.eps_b], writes=[wvb])
        P.op("act", lambda: nc.scalar.activation(out=wv[:, 18:19], in_=wv[:, 17:18], func=AF.Exp, scale=-0.5), reads=[wvb], writes=[wvb])
        P.op("dve", lambda: nc.vector.scalar_tensor_tensor(out=cqn[:], in0=big[:, 0:256], scalar=wv[:, 18:19], in1=gq[:], op0=ALU.mult, op1=ALU.mult), reads=[bigb, wvb, gqb], writes=[cqnb])
        pt = kb.pt[kb.pti % 2]; ptb = kb.ptb[kb.pti % 2]; kb.pti += 1
        for j in range(2):
            P.op("pe", lambda j=j: nc.tensor.transpose(pt[:, j * 128:(j + 1) * 128], cqn[:, j * 128:(j + 1) * 128], kb.ident[:]), reads=[cqnb, kb.identb], writes=[ptb], sig=(j == 1))
        P.op("dve", lambda: nc.vector.tensor_copy(out=cqT[:], in_=pt[:, 0:256].rearrange("p (a b) -> p a b", b=128)), reads=[ptb], writes=[cqTb])
        for b4 in range(3):
            ps, psb = kb.ps[b4 % 2], kb.psb[b4 % 2]
            for hh in range(4):
                h = 4 * b4 + hh
                for c in range(2):
                    mm(kb, ps[0:64, hh * 128:(hh + 1) * 128], wqup[:, c, h * 64:(h + 1) * 64], cqT[:, c, :], hh == 0 and c == 0, [wqupb, cqTb], [psb], sig=(hh == 3 and c == 1))
            P.op("act", lambda: nc.scalar.activation(out=qhT[0:64, 4 * b4:4 * b4 + 4, :], in_=ps[0:64, :].rearrange("p (a b) -> p a b", b=128), func=AF.Copy, scale=0.125), reads=[psb], writes=[qhTb])
        for b4 in range(2):
            ps, psb = kb.ps[b4 % 2], kb.psb[b4 % 2]
            for hh in range(4):
                h = 4 * b4 + hh
                for c in range(2):
                    mm(kb, ps[0:64, hh * 128:(hh + 1) * 128], wqi[:, c, h * 64:(h + 1) * 64], cqT[:, c, :], hh == 0 and c == 0, [wqib, cqTb], [psb], sig=(hh == 3 and c == 1))
            P.op("dve", lambda: nc.vector.tensor_copy(out=qiT[:, 4 * b4:4 * b4 + 4, :], in_=ps[0:64, :].rearrange("p (a b) -> p a b", b=128)), reads=[psb], writes=[qiTb])
        for b4 in range(3):
            ps, psb = kb.ps[b4 % 2], kb.psb[b4 % 2]
            for hh in range(4):
                h = 4 * b4 + hh
                mm(kb, ps[:, hh * 128:(hh + 1) * 128], wukT[:, h, :], qhT[0:64, h, :], hh == 0, [wukTb, qhTb], [psb], sig=(hh == 3))
            P.op("dve", lambda: nc.vector.tensor_copy(out=qabs[:, 4 * b4:4 * b4 + 4, :], in_=ps[:, :].rearrange("p (a b) -> p a b", b=128)), reads=[psb], writes=[qabsb])
        P.op("dve", lambda: nc.vector.tensor_scalar(out=wv[:, 0:8], in0=big[:, 448:456], scalar1=0.04419417382415922, scalar2=None, op0=ALU.mult), reads=[bigb], writes=[wvb])
        P.op("dve", lambda: nc.vector.tensor_scalar(out=wv[:, 8:16], in0=wv[:, 0:8], scalar1=0.0, scalar2=2.0, op0=ALU.is_ge, op1=ALU.mult), reads=[wvb], writes=[wvb])
        P.op("dve", lambda: nc.vector.tensor_scalar(out=wv[:, 8:16], in0=wv[:, 8:16], scalar1=-1.0, scalar2=None, op0=ALU.add), reads=[wvb], writes=[wvb])
        P.op("dve", lambda: nc.vector.tensor_tensor(out=wv[:, 0:8], in0=wv[:, 0:8], in1=wv[:, 8:16], op=ALU.mult), reads=[wvb], writes=[wvb])
        P.op("act", lambda: nc.scalar.activation(out=qmb16[:], in_=big[:, 456:712], func=AF.Copy, scale=0.125), reads=[bigb], writes=[qmb16b])
        pt = kb.pt[kb.pti % 2]; ptb = kb.ptb[kb.pti % 2]; kb.pti += 1
        for j in range(4):
            P.op("pe", lambda j=j: nc.tensor.transpose(pt[0:64, j * 128:(j + 1) * 128], qmb16[:, j * 64:(j + 1) * 64], kb.ident[:]), reads=[qmb16b, kb.identb], writes=[ptb], sig=(j == 3))
        P.op("dve", lambda: nc.vector.tensor_copy(out=qmT[:], in_=pt[0:64, 0:512].rearrange("p (a b) -> p a b", b=128)), reads=[ptb], writes=[qmTb])
        P.dma("pool", big[:], x_d[tsl, :], reads=[], writes=[bigb])
        if stage == 1:
            P.dma("sp", hmid_d[tsl, :], big[:], reads=[bigb], final=True)
            continue
        for kc in range(m + 1):
            csl = slice(kc * 512, (kc + 1) * 512)
            for h in range(8):
                ps, psb = kb.ps[h % 2], kb.psb[h % 2]
                mm(kb, ps[:, :], qiT[:, h, :], kidxT[:, csl], True, [qiTb, kidxTb], [psb], sig=True)
                P.op("act", lambda: nc.scalar.activation(out=rt[:], in_=ps[:, :], func=AF.Relu, scale=wv[:, h:h + 1]), reads=[psb, wvb], writes=[rtb])
                if h == 0:
                    P.op("dve", lambda: nc.vector.tensor_scalar(out=score[:, csl], in0=rt[:], scalar1=wv[:, 8:9], scalar2=None, op0=ALU.mult), reads=[rtb, wvb], writes=[scoreb])
                else:
                    P.op("dve", lambda: nc.vector.scalar_tensor_tensor(out=score[:, csl], in0=rt[:], scalar=wv[:, 8 + h:9 + h], in1=score[:, csl], op0=ALU.mult, op1=ALU.add), reads=[rtb, wvb, scoreb], writes=[scoreb])
        if stage == 2:
            P.dma("sp", hmid_d[tsl, 0:512], score[:, 0:512], reads=[scoreb], final=True)
            continue
        P.op("dve", lambda: nc.vector.tensor_reduce(out=bs[:, 0:1], in_=score[:, 0:L], axis=AX.X, op=ALU.min), reads=[scoreb], writes=[bsb])
        P.op("dve", lambda: nc.vector.tensor_reduce(out=bs[:, 1:2], in_=score[:, 0:L], axis=AX.X, op=ALU.max), reads=[scoreb], writes=[bsb])
        P.op("dve", lambda: nc.vector.tensor_tensor(out=score[:, L - 512:L], in0=score[:, L - 512:L], in1=cm[:], op=ALU.add), reads=[scoreb, cmb], writes=[scoreb])
        P.op("dve", lambda: nc.vector.tensor_tensor(out=bs[:, 2:3], in0=bs[:, 1:2], in1=bs[:, 0:1], op=ALU.subtract), reads=[bsb], writes=[bsb])
        P.op("dve", lambda: nc.vector.tensor_scalar(out=bs[:, 8:8 + NIT], in0=pw[:], scalar1=bs[:, 2:3], scalar2=None, op0=ALU.mult), reads=[bsb, pwb], writes=[bsb])
        for it in range(NIT):
            hw = bs[:, 8 + it:9 + it]
            P.op("dve", lambda: nc.vector.tensor_tensor(out=bs[:, 3:4], in0=bs[:, 0:1], in1=hw, op=ALU.add), reads=[bsb], writes=[bsb])
            P.op("dve", lambda: nc.vector.tensor_scalar(out=mb[:, 0:L], in0=score[:, 0:L], scalar1=bs[:, 3:4], scalar2=None, op0=ALU.is_ge, op1=ALU.add, accum_out=bs[:, 4:5]),
                 reads=[scoreb, bsb], writes=[mbb, bsb])
            P.op("dve", lambda: nc.vector.tensor_scalar(out=bs[:, 5:6], in0=bs[:, 4:5], scalar1=255.5, scalar2=hw, op0=ALU.is_ge, op1=ALU.mult), reads=[bsb], writes=[bsb])
            P.op("dve", lambda: nc.vector.tensor_tensor(out=bs[:, 0:1], in0=bs[:, 0:1], in1=bs[:, 5:6], op=ALU.add), reads=[bsb], writes=[bsb])
        P.op("dve", lambda: nc.vector.tensor_scalar(out=mb[:, 0:L], in0=score[:, 0:L], scalar1=bs[:, 0:1], scalar2=NEGB, op0=ALU.is_lt, op1=ALU.mult), reads=[scoreb, bsb], writes=[mbb])
        if stage == 3:
            P.dma("sp", hmid_d[tsl, 0:512], score[:, 0:512], reads=[scoreb], final=True)
            P.dma("sp", hmid_d[tsl, 512:512 + 8 + 2 * NIT], bs[:], reads=[bsb], final=True)
            continue
        banks = [next_acc() for _ in range(4)]
        for g8 in range(0, nkt, 8):
            nb = min(8, nkt - g8)
            mT, mTb = mTs[(g8 // 8) % 2]
            pt = kb.pt[kb.pti % 2]; ptb = kb.ptb[kb.pti % 2]; kb.pti += 1
            for j in range(nb):
                P.op("pe", lambda j=j: nc.tensor.transpose(pt[:, j * 128:(j + 1) * 128], mb[:, (g8 + j) * 128:(g8 + j + 1) * 128], kb.ident[:]), reads=[mbb, kb.identb], writes=[ptb], sig=(j == nb - 1))
            P.op("dve", lambda: nc.vector.tensor_copy(out=mT[:, 0:nb, :], in_=pt[:, 0:nb * 128].rearrange("p (a b) -> p a b", b=128)), reads=[ptb], writes=[mTb])
            for j in range(nb):
                kt = g8 + j
                u = kt - 4 * m
                for tr in range(4):
                    bA = banks[tr]
                    accs = [kb.ps[bA][:, 129 * hh:129 * hh + 129] for hh in range(3)]
                    accb = [kb.psb[bA]] * 3
                    s_terms = [(ckvT[:, kt * 128:(kt + 1) * 128], qabs[:, 3 * tr:3 * tr + 3, :], [ckvTb, qabsb]),
                               (kb.ident[:], bc3(mT[:, j, :]), [kb.identb, mTb])]
                    near = None
                    if u >= -12:
                        z0 = 128 * (3 - u)
                        near = [(kb.anti[:], band_s[:, 3 * tr + hh, z0:z0 + 128], [kb.antib, band_sb]) for hh in range(3)]
                    at.unit(s_terms, near, accs, accb, ckv1[:, kt, :], [ckv1b], [kt == 0, False, False])
        for tr in range(4):
            bA = banks[tr]
            for hh in range(3):
                h = 3 * tr + hh
                O = kb.ps[bA][:, 129 * hh:129 * hh + 129]
                P.op("dve", lambda: nc.vector.tensor_scalar(out=sm[:, 0:1], in0=O[:, 128:129], scalar1=1e-30, scalar2=None, op0=ALU.max), reads=[kb.psb[bA]], writes=[smb])
                P.op("dve", lambda: nc.vector.reciprocal(out=sm[:, 1:2], in_=sm[:, 0:1]), reads=[smb], writes=[smb])
                P.op("dve", lambda: nc.vector.tensor_scalar(out=qabs[:, h, :], in0=O[:, 0:128], scalar1=sm[:, 1:2], scalar2=None, op0=ALU.mult), reads=[kb.psb[bA], smb], writes=[qabsb])
        for h0 in (0, 8):
            nb = min(8, 12 - h0)
            pt = kb.pt[kb.pti % 2]; ptb = kb.ptb[kb.pti % 2]; kb.pti += 1
            for j in range(nb):
                P.op("pe", lambda j=j: nc.tensor.transpose(pt[:, j * 128:(j + 1) * 128], qabs[:, h0 + j, :], kb.ident[:]), reads=[qabsb, kb.identb], writes=[ptb], sig=(j == nb - 1))
            P.op("dve", lambda: nc.vector.tensor_copy(out=qhT[:, h0:h0 + nb, :], in_=pt[:, 0:nb * 128].rearrange("p (a b) -> p a b", b=128)), reads=[ptb], writes=[qhTb])
        for h0 in (0, 8):
            nb = min(8, 12 - h0)
            bA = next_acc()
            ps, psb = kb.ps[bA], kb.psb[bA]
            for j in range(nb):
                h = h0 + j
                mm(kb, ps[:, j * 64:(j + 1) * 64], qhT[:, h, :], wuv[:, h * 64:(h + 1) * 64], j == 0, [qhTb, wuvb], [psb], sig=(j == nb - 1))
            P.op("act", lambda: nc.scalar.copy(out=catbf[:, h0 * 64:(h0 + nb) * 64], in_=ps[:, 0:nb * 64]), reads=[psb], writes=[catbfb])
        for h in range(4):
            ps, psb = kb.ps[at.sbank], kb.psb[at.sbank]
            at.sbank ^= 1
            for tl in range(2):
                mm(kb, ps[:, tl * 128:(tl + 1) * 128], kmT[:, h, tl * 128:(tl + 1) * 128], qmT[:, h, :], tl == 0, [kmTb, qmTb], [psb], sig=(tl == 1))
            P.op("act", lambda: nc.scalar.activation(out=pm[:], in_=ps[:, 0:256], func=AF.Exp), reads=[psb], writes=[pmb])
            bA = next_acc()
            O = kb.ps[bA][:, 0:65]
            for tl in range(2):
                mm(kb, O, pm[:, tl * 128:(tl + 1) * 128], vm1[:, tl, h, :], tl == 0, [pmb, vm1b], [kb.psb[bA]], sig=(tl == 1))
            P.op("dve", lambda: nc.vector.reciprocal(out=sm[:, 4:5], in_=O[:, 64:65]), reads=[kb.psb[bA]], writes=[smb])
            P.op("dve", lambda: nc.vector.tensor_scalar(out=catbf[:, 768 + h * 64:768 + (h + 1) * 64], in0=O[:, 0:64], scalar1=sm[:, 4:5], scalar2=None, op0=ALU.mult), reads=[kb.psb[bA], smb], writes=[catbfb])
        pt = kb.pt[kb.pti % 2]; ptb = kb.ptb[kb.pti % 2]; kb.pti += 1
        for j in range(8):
            P.op("pe", lambda j=j: nc.tensor.transpose(pt[:, j * 128:(j + 1) * 128], catbf[:, j * 128:(j + 1) * 128], kb.ident[:]),
                 reads=[catbfb, kb.identb], writes=[ptb], sig=(j == 7))
        P.op("dve", lambda: nc.vector.tensor_copy(out=catT[:], in_=pt[:, :].rearrange("p (a b) -> p a b", b=128)), reads=[ptb], writes=[catTb])
        for half in range(2):
            bA = next_acc()
            ps, psb = kb.ps[bA], kb.psb[bA]
            for k in range(8):
                mm(kb, ps[:, :], catT[:, k, :], wout[:, k, half * 512:(half + 1) * 512], k == 0, [catTb, woutb], [psb], sig=(k == 7))
            P.op("dve", lambda: nc.vector.tensor_tensor(out=big[:, half * 512:(half + 1) * 512], in0=ps[:, :], in1=big[:, half * 512:(half + 1) * 512], op=ALU.add), reads=[psb, bigb], writes=[bigb])
        P.dma("sp", hmid_d[tsl, :], big[:], reads=[bigb], final=True)
    return kb


def run_C(inputs, resF, stage=99, nslots=NT):
    kb = build_C(stage, nslots)
    ident = np.eye(128, dtype=np.float32)
    pw = np.tile((0.5 ** np.arange(1, NIT + 1)).astype(np.float32)[None, :], (128, 1))
    maps = []
    for core in range(8):
        b, c = core // 4, core % 4
        prs = [np.asarray(resF[4 * b + cc]["pr"]) for cc in range(4)]
        oh_s, _, _ = host_tables(c)
        z = np.arange(512)[None, :]
        q = np.arange(128)[:, None]
        cm = np.where(z <= 128 * c + q, 0.0, -1e30).astype(np.float32)
        mp = {"x": np.asarray(resF[core]["h"]), "pr": prs[c],
              "ckv_seq": gather_seq([np.ascontiguousarray(p[:, 256:384]) for p in prs], 0),
              "kidx_seq": gather_seq([np.ascontiguousarray(p[:, 384:448]) for p in prs], 0),
              "identb": ident.astype(NPBF), "antib": np.ascontiguousarray(ident[::-1]).astype(NPBF),
              "rel_bias": inputs["rel_bias"], "oh_s": oh_s, "cm": cm, "pw": pw,
              "q_norm": inputs["dsa_q_norm"][0:1], "kv_norm": inputs["dsa_kv_norm"][0:1], "kidx_norm": inputs["dsa_kidx_norm"][0:1],
              "w_q_up": inputs["dsa_w_q_up"][0], "w_uk": inputs["dsa_w_uk"][0].reshape(128, 768), "w_uv": inputs["dsa_w_uv"][0].reshape(128, 768),
              "w_q_idx": inputs["dsa_w_q_idx"][0],
              "mem": inputs["mem"][b], "g_mem": inputs["norm_mem"][1:2], "w_mem_kv": inputs["w_mem_kv"][1], "w_out": inputs["w_out"][1]}
        maps.append(mp)
    return kb.run(maps)
```

```python
import math
from contextlib import ExitStack
import numpy as np
import ml_dtypes
import concourse.bass as bass
import concourse.mybir as mybir
from concourse.bass import AP
from concourse.bass_utils import run_bass_kernel_spmd

F32 = mybir.dt.float32
BF16 = mybir.dt.bfloat16
AF = mybir.ActivationFunctionType
ALU = mybir.AluOpType
AX = mybir.AxisListType
NPBF = ml_dtypes.bfloat16

D = 1024
T = 8192
NT = 16
TOK = 2048
DFF = 2816
EPS = 1e-6
NEGB = -30000.0


class Buf:
    __slots__ = ("name", "w", "r")

    def __init__(self, name=""):
        self.name = name
        self.w = None
        self.r = {}


class Prog:
    NSLOT = 12

    def __init__(self, nc):
        self.nc = nc
        self.eng = {"pe": nc.tensor, "act": nc.scalar, "dve": nc.vector,
                    "pool": nc.gpsimd, "sp": nc.sync}
        self.es = ExitStack()
        self.sem = {}
        self.cnt = {}
        for e in ("pe", "act", "dve", "pool"):
            self.sem[e] = self.es.enter_context(nc.semaphore("c_" + e))
            self.cnt[e] = 0
        self.slots = {}
        self.slot_i = {}
        for q in ("sp", "pool"):
            lst = []
            for i in range(self.NSLOT):
                key = "d_%s%d" % (q, i)
                self.sem[key] = self.es.enter_context(nc.semaphore(key))
                self.cnt[key] = 0
                lst.append(key)
            self.slots[q] = lst
            self.slot_i[q] = 0
        self.seen = {e: {} for e in self.eng}
        self.outtoks = []

    def _need(self, e, tok):
        if tok is None:
            return
        key, val = tok
        if self.seen[e].get(key, 0) >= val:
            return
        if key in ("pe", "act", "dve", "pool"):
            assert self.cnt[key] >= val, "missing signal on %s" % key
        self.eng[e].wait_ge(self.sem[key], val)
        self.seen[e][key] = val

    def _deps(self, e, reads, writes):
        for b in reads:
            if b.w is not None and not (e == "pe" and b.w[0] == "pe"):
                self._need(e, b.w)
        for b in writes:
            if b.w is not None and b.w[0] != e:
                self._need(e, b.w)
            for k, v in b.r.items():
                if k != e:
                    self._need(e, (k, v))

    def _mark(self, tok, reads, writes):
        for b in reads:
            if b.r.get(tok[0], 0) < tok[1]:
                b.r[tok[0]] = tok[1]
        for b in writes:
            b.w = tok
            b.r = {}

    def op(self, e, fn, reads=(), writes=(), sig=True):
        self._deps(e, reads, writes)
        ins = fn()
        if sig:
            self.cnt[e] += 1
            ins.then_inc(self.sem[e], 1)
            tok = (e, self.cnt[e])
        else:
            tok = (e, self.cnt[e] + 1)
        self._mark(tok, reads, writes)
        return ins

    def dma(self, q, out, in_, reads=(), writes=(), final=False):
        lst = self.slots[q]
        key = lst[self.slot_i[q] % self.NSLOT]
        self.slot_i[q] += 1
        if self.cnt[key] > 0:
            self._need(q, (key, self.cnt[key]))
        self._deps(q, reads, writes)
        ins = self.eng[q].dma_start(out=out, in_=in_)
        self.cnt[key] += 16
        ins.then_inc(self.sem[key], 16)
        tok = (key, self.cnt[key])
        self._mark(tok, reads, writes)
        if final:
            self.outtoks.append(tok)
        return tok

    def barrier(self):
        toks = [(e, self.cnt[e]) for e in ("pe", "act", "dve", "pool") if self.cnt[e] > 0]
        for q in ("sp", "pool"):
            toks += [(k, self.cnt[k]) for k in self.slots[q] if self.cnt[k] > 0]
        for e in self.eng:
            for t in toks:
                if t[0] != e:
                    self._need(e, t)

    def finish(self):
        for tok in self.outtoks:
            self._need("sp", tok)
        for q in ("sp", "pool"):
            for key in self.slots[q]:
                if self.cnt[key] > 0:
                    self._need("sp", (key, self.cnt[key]))
        self.es.close()


class K:
    def __init__(self, name):
        self.nc = bass.Bass("TRN2", target_bir_lowering=False)
        self.P = Prog(self.nc)
        self.es = ExitStack()
        self.n = 0
        self.rr = 0
        nc = self.nc
        self.es.enter_context(nc.allow_non_contiguous_dma(reason="small strided parameter loads"))
        self.ps = [self.es.enter_context(nc.psum_tensor("ps%d" % i, [128, 512], F32)) for i in range(6)]
        self.psb = [Buf("ps%d" % i) for i in range(6)]
        self.pt = [self.es.enter_context(nc.psum_tensor("pt%d" % i, [128, 1024], BF16)) for i in range(2)]
        self.ptb = [Buf("pt%d" % i) for i in range(2)]
        self.pti = 0
        self.ident, self.identb = self.sb("ident", [128, 128], BF16)
        self.anti, self.antib = self.sb("anti", [128, 128], BF16)

    def sb(self, name, shape, dt):
        self.n += 1
        t = self.es.enter_context(self.nc.sbuf_tensor("%s_%d" % (name, self.n), shape, dt))
        return t, Buf(name)

    def scope(self):
        kb = self

        class _S:
            def __enter__(self_):
                self_.old = kb.es
                kb.es = ExitStack()
                return kb.es

            def __exit__(self_, *a):
                kb.P.barrier()
                kb.es.close()
                kb.es = self_.old
        return _S()

    alias = None

    def din(self, name, shape, dt=F32):
        if self.alias is not None:
            ap = self.alias[name]
            assert list(ap.shape) == list(shape), (name, ap.shape, shape)
            return ap
        return self.nc.dram_tensor(name, list(shape), dt, kind="ExternalInput").ap()

    def dout(self, name, shape, dt=F32):
        if self.alias is not None:
            ap = self.alias[name]
            assert list(ap.shape) == list(shape), (name, ap.shape, shape)
            return ap
        return self.nc.dram_tensor(name, list(shape), dt, kind="ExternalOutput").ap()

    def dscr(self, name, shape, dt=F32):
        self.n += 1
        return self.nc.dram_tensor("%s_%d" % (name, self.n), list(shape), dt, kind="Internal")

    def dq(self):
        self.rr += 1
        return "sp" if self.rr % 2 else "pool"

    def load_consts(self, ident_d, anti_d):
        st, stb = self.sb("cst", [128, 256], F32)
        self.P.dma("sp", st[:, 0:128], ident_d, writes=[stb])
        self.P.dma("sp", st[:, 128:256], anti_d, writes=[stb])
        nc = self.nc
        self.P.op("dve", lambda: nc.vector.tensor_copy(out=self.ident[:], in_=st[:, 0:128]), reads=[stb], writes=[self.identb])
        self.P.op("dve", lambda: nc.vector.tensor_copy(out=self.anti[:], in_=st[:, 128:256]), reads=[stb], writes=[self.antib])

    def load_w(self, dram, kdim, ncol, name, eng="pool", stage=None):
        nc, P = self.nc, self.P
        kp = min(128, kdim)
        nk = max(1, kdim // 128)
        w, wb = self.sb(name, [kp, nk, ncol], BF16)
        CH = 2048
        if stage is None:
            stage = [self.sb("wst", [128, CH], F32) for _ in range(2)]
        self._wst = stage
        i = 0
        for k in range(nk):
            for c0 in range(0, ncol, CH):
                cw = min(CH, ncol - c0)
                st, stb = stage[i % 2]
                i += 1
                P.dma(self.dq(), st[0:kp, 0:cw], dram[k * 128:k * 128 + kp, c0:c0 + cw], writes=[stb])
                self.cast(eng, w[:, k, c0:c0 + cw], st[0:kp, 0:cw], [stb], [wb])
        return w, wb

    def cast(self, eng, out, in_, reads, writes, sig=True):
        nc = self.nc
        if eng == "pool":
            self.P.op("pool", lambda: nc.gpsimd.tensor_copy(out=out, in_=in_), reads=reads, writes=writes, sig=sig)
        elif eng == "dve":
            self.P.op("dve", lambda: nc.vector.tensor_copy(out=out, in_=in_), reads=reads, writes=writes, sig=sig)
        else:
            self.P.op("act", lambda: nc.scalar.copy(out=out, in_=in_), reads=reads, writes=writes, sig=sig)

    def bcast_row(self, dram_row, ncol, name):
        t, tb = self.sb(name, [128, ncol], F32)
        src = AP(tensor=dram_row.tensor, offset=dram_row.offset, ap=[[0, 128], [1, ncol]])
        self.P.dma("sp", t[:], src, writes=[tb])
        return t, tb

    def rmsnorm(self, x, xb, g, gb, out, outb, dim, scr, scrb, np_=128):
        nc, P = self.nc, self.P
        P.op("act", lambda: nc.scalar.activation(out=out, in_=x, func=AF.Square, accum_out=scr[0:np_, 0:1]),
             reads=[xb], writes=[outb, scrb])
        P.op("act", lambda: nc.scalar.activation(out=scr[0:np_, 1:2], in_=scr[0:np_, 0:1], func=AF.Ln, scale=1.0 / dim, bias=self.eps_t[0:np_, 0:1]),
             reads=[scrb, self.eps_b], writes=[scrb])
        P.op("act", lambda: nc.scalar.activation(out=scr[0:np_, 2:3], in_=scr[0:np_, 1:2], func=AF.Exp, scale=-0.5),
             reads=[scrb], writes=[scrb])
        P.op("dve", lambda: nc.vector.scalar_tensor_tensor(out=out, in0=x, scalar=scr[0:np_, 2:3], in1=g, op0=ALU.mult, op1=ALU.mult),
             reads=[xb, scrb, gb], writes=[outb])

    def mk_eps(self):
        self.eps_t, self.eps_b = self.sb("eps", [128, 1], F32)
        nc = self.nc
        self.P.op("dve", lambda: nc.vector.memset(self.eps_t[:], EPS), writes=[self.eps_b])

    def transpose_to(self, src_list, dst, dstb, reads, evac="dve", np_in=128):
        nc, P = self.nc, self.P
        n = len(src_list)
        i0 = 0
        while i0 < n:
            nb = min(8, n - i0)
            pt = self.pt[self.pti % 2]
            ptb = self.ptb[self.pti % 2]
            self.pti += 1
            w = src_list[i0].shape[-1]
            for j in range(nb):
                s = src_list[i0 + j]
                P.op("pe", lambda s=s, j=j: nc.tensor.transpose(pt[0:w, j * 128:j * 128 + np_in], s, self.ident[0:np_in, 0:np_in]),
                     reads=list(reads) + [self.identb], writes=[ptb], sig=(j == nb - 1))
            src = pt[0:w, 0:nb * 128].rearrange("p (a b) -> p a b", b=128)[:, :, 0:np_in]
            o = dst[0:w, i0:i0 + nb, :]
            if evac == "dve":
                P.op("dve", lambda: nc.vector.tensor_copy(out=o, in_=src), reads=[ptb], writes=[dstb])
            else:
                P.op("act", lambda: nc.scalar.copy(out=o, in_=src), reads=[ptb], writes=[dstb])
            i0 += nb

    def run(self, in_maps):
        self.P.finish()
        self.es.close()
        res = run_bass_kernel_spmd(self.nc, in_maps, core_ids=list(range(8)))
        return res.results


def rel_bucket_np(dist):
    n = np.maximum(dist, 0)
    nf = np.maximum(n, 16).astype(np.float32)
    large = 16 + (np.log(nf / np.float32(16)) / np.float32(math.log(2048 / 16)) * np.float32(16)).astype(np.int32)
    large = np.minimum(large, 31)
    return np.where(n < 16, n, large)


def onehot_table(dist, masked):
    Y = dist.shape[0]
    oh = np.zeros((33, Y), np.float32)
    b = rel_bucket_np(dist)
    ok = ~masked
    oh[b[ok], np.nonzero(ok)[0]] = 1.0
    oh[32, masked] = 1.0
    return oh


Y_SLC = 2304
Y_WIN = 1280
Y_CMP = 5376


def host_tables(c):
    y = np.arange(Y_SLC)
    d = y - 511 + 128 * c
    oh_s = onehot_table(d, d < 0)
    y = np.arange(Y_WIN)
    d = y - 511 + 128 * c
    oh_w = onehot_table(d, (d < 0) | (d >= 512))
    y = np.arange(Y_CMP)
    d = y + 128 * c - 2063
    oh_c = onehot_table(d, d < 0)
    return oh_s, oh_w, oh_c


def build_tables(kb, rel_bias_d, ohs, names_Y):
    nc, P = kb.nc, kb.P
    bt, btb = kb.sb("biasT", [33, 12], F32)
    P.dma("sp", bt[0:32, :], rel_bias_d, writes=[btb])
    b31, b31b = kb.sb("b31", [33, 12], F32)
    src = AP(tensor=rel_bias_d.tensor, offset=rel_bias_d.offset + 31 * 12, ap=[[0, 32], [1, 12]])
    P.dma("sp", b31[0:32, :], src, writes=[b31b])
    bb, bbb = kb.sb("biasb", [33, 12], BF16)
    P.op("dve", lambda: nc.vector.memset(bb[:], NEGB), writes=[bbb])
    P.op("dve", lambda: nc.vector.tensor_tensor(out=bb[0:32, :], in0=bt[0:32, :], in1=b31[0:32, :], op=ALU.subtract),
         reads=[btb, b31b], writes=[bbb])
    outs = []
    for (oh_d, Y, nm) in [(o, y, n) for o, (n, y) in zip(ohs, names_Y)]:
        oh, ohb = kb.sb("oh" + nm, [33, Y], BF16)
        st, stb = kb.sb("ohst" + nm, [33, Y], F32)
        P.dma("sp", st[:], oh_d, writes=[stb])
        P.op("dve", lambda: nc.vector.tensor_copy(out=oh[:], in_=st[:]), reads=[stb], writes=[ohb])
        tt, ttb = kb.sb("tt" + nm, [12, Y], BF16)
        for c0 in range(0, Y, 512):
            cw = min(512, Y - c0)
            ps, psb = kb.ps[0], kb.psb[0]
            P.op("pe", lambda: nc.tensor.matmul(ps[0:12, 0:cw], lhsT=bb[:], rhs=oh[:, c0:c0 + cw], start=True, stop=True),
                 reads=[bbb, ohb], writes=[psb])
            P.op("dve", lambda: nc.vector.tensor_copy(out=tt[:, c0:c0 + cw], in_=ps[0:12, 0:cw]), reads=[psb], writes=[ttb])
        scr = kb.dscr("ttd" + nm, [12, Y], BF16)
        scrb = Buf("ttd" + nm)
        P.dma("sp", scr.ap(), tt[:], reads=[ttb], writes=[scrb])
        outs.append((scr, scrb, Y))
    return outs


def load_band(kb, scr, scrb, Y, Z, pstep, name, zoff=0, dst=None):
    if dst is None:
        dst = kb.sb(name, [128, 12, Z], BF16)
    t, tb = dst
    src = AP(tensor=scr, offset=zoff, ap=[[pstep, 128], [Y, 12], [1, Z]])
    kb.P.dma(kb.dq(), t[:, :, 0:Z], src, reads=[scrb], writes=[tb])
    return t, tb


FM_A = [(0 + 64 * i, 0.125) for i in range(12)] + [(768 + 64 * i, 1.0) for i in range(4)] + \
       [(1024, 1.0), (1088, 1.0), (1280, 1.0), (1344, 1.0)] + [(1572 + 64 * i, 0.125) for i in range(4)]


def proj_phase(kb, x_d, g_d, w, wb, ncols, fm_groups, tm_ranges, fmT_d, tm_d, post_tm=None):
    nc, P = kb.nc, kb.P
    g, gb = kb.bcast_row(g_d, D, "gain")
    xt = [kb.sb("xt", [128, D], F32) for _ in range(2)]
    xn = [kb.sb("xn", [128, D], BF16) for _ in range(2)]
    scr, scrb = kb.sb("scr", [128, 4], F32)
    xnT, xnTb = kb.sb("xnT", [128, 8, 512], BF16)
    ng = len(fm_groups)
    fst, fstb = kb.sb("fst", [64, ng, 512], BF16)
    ntm = sum(n for _, n in tm_ranges)
    tst = [kb.sb("tst", [128, max(ntm, 1)], F32) for _ in range(2)]
    ei = 0
    for blk in range(4):
        for tt in range(4):
            ti = blk * 4 + tt
            x_, xb_ = xt[ti % 2]
            n_, nb_ = xn[ti % 2]
            P.dma(kb.dq(), x_[:], x_d[ti * 128:(ti + 1) * 128, :], writes=[xb_])
            kb.rmsnorm(x_[:], xb_, g[:], gb, n_[:], nb_, D, scr, scrb)
            for half in range(1):
                pt = kb.pt[kb.pti % 2]
                ptb = kb.ptb[kb.pti % 2]
                kb.pti += 1
                for j in range(8):
                    P.op("pe", lambda j=j: nc.tensor.transpose(pt[:, j * 128:(j + 1) * 128], n_[:, j * 128:(j + 1) * 128], kb.ident[:]),
                         reads=[nb_, kb.identb], writes=[ptb], sig=(j == 7))
                P.op("dve", lambda: nc.vector.tensor_copy(out=xnT[:, :, tt * 128:(tt + 1) * 128],
                                                          in_=pt[:, :].rearrange("p (a b) -> p a b", b=128)),
                     reads=[ptb], writes=[xnTb])
            if ntm:
                ts_, tsb_ = tst[ti % 2]
                ps, psb = kb.ps[4], kb.psb[4]
                o = 0
                for (c0, n) in tm_ranges:
                    for k in range(8):
                        P.op("pe", lambda k=k, o=o, c0=c0, n=n: nc.tensor.matmul(ps[:, o:o + n], lhsT=xnT[:, k, tt * 128:(tt + 1) * 128], rhs=w[:, k, c0:c0 + n],
                                                                       start=(k == 0 and o == 0), stop=(k == 7)),
                             reads=[xnTb, wb], writes=[psb], sig=(k == 7))
                    o += n
                P.op("act", lambda: nc.scalar.copy(out=ts_[:, 0:ntm], in_=ps[:, 0:ntm]), reads=[psb], writes=[tsb_])
                if post_tm is not None:
                    post_tm(ts_, tsb_)
                P.dma(kb.dq(), tm_d[ti * 128:(ti + 1) * 128, :], ts_[:, 0:ntm], reads=[tsb_], final=True)
        for gi, (c0, sc) in enumerate(fm_groups):
            ps, psb = kb.ps[gi % 4], kb.psb[gi % 4]
            for k in range(8):
                P.op("pe", lambda k=k, c0=c0: nc.tensor.matmul(ps[0:64, :], lhsT=w[:, k, c0:c0 + 64], rhs=xnT[:, k, :], start=(k == 0), stop=(k == 7)),
                     reads=[xnTb, wb], writes=[psb], sig=(k == 7))
            if ei % 2 == 0:
                P.op("act", lambda: nc.scalar.activation(out=fst[:, gi, :], in_=ps[0:64, :], func=AF.Copy, scale=sc), reads=[psb], writes=[fstb])
            else:
                P.op("dve", lambda: nc.vector.tensor_scalar(out=fst[:, gi, :], in0=ps[0:64, :], scalar1=sc, scalar2=None, op0=ALU.mult), reads=[psb], writes=[fstb])
            ei += 1
        P.dma(kb.dq(), fmT_d[:, :, blk * 512:(blk + 1) * 512], fst[:], reads=[fstb], final=True)


def build_A(kb=None):
    kb = kb or K("A")
    nc, P = kb.nc, kb.P
    x_d = kb.din("x", [TOK, D])
    g_d = kb.din("g", [1, D])
    w_d = kb.din("w_in", [D, 1828])
    gb_d = kb.din("gate_b", [1, 36])
    id_d = kb.din("ident", [128, 128])
    an_d = kb.din("anti", [128, 128])
    fm_d = kb.dout("fmT", [64, 24, TOK], BF16)
    tm_d = kb.dout("tm", [TOK, 256 + 36])
    kb.load_consts(id_d, an_d)
    kb.mk_eps()
    w, wb = kb.load_w(w_d, D, 1828, "w_in")
    gbt, gbb = kb.bcast_row(gb_d, 36, "gateb")

    def post(ts_, tsb_):
        P.op("dve", lambda: nc.vector.tensor_tensor(out=ts_[:, 256:292], in0=ts_[:, 256:292], in1=gbt[:], op=ALU.add), reads=[tsb_, gbb], writes=[tsb_])
        P.op("act", lambda: nc.scalar.activation(out=ts_[:, 256:292], in_=ts_[:, 256:292], func=AF.Exp, scale=-1.0), reads=[tsb_], writes=[tsb_])
        P.op("dve", lambda: nc.vector.tensor_scalar(out=ts_[:, 256:292], in0=ts_[:, 256:292], scalar1=1.0, scalar2=None, op0=ALU.add), reads=[tsb_], writes=[tsb_])
        P.op("dve", lambda: nc.vector.reciprocal(out=ts_[:, 256:292], in_=ts_[:, 256:292]), reads=[tsb_], writes=[tsb_])

    proj_phase(kb, x_d, g_d, w, wb, 1828, FM_A, [(1152, 128), (1408, 128), (1536, 36)], fm_d, tm_d, post_tm=post)
    return kb


def core_tokens(c):
    return np.concatenate([np.arange(128 * (4 * m + c), 128 * (4 * m + c) + 128) for m in range(NT)])


def run_A(inputs):
    kb = build_A()
    ident = np.eye(128, dtype=np.float32)
    anti = np.ascontiguousarray(ident[::-1])
    maps = []
    for core in range(8):
        b, c = core // 4, core % 4
        maps.append({"x": np.ascontiguousarray(inputs["x"][b][core_tokens(c)]),
                     "g": inputs["norm_mix"][0:1], "w_in": inputs["nsa_w_in"][0],
                     "gate_b": inputs["nsa_gate_b"][0:1], "ident": ident, "anti": anti})
    return kb.run(maps)


def mm(kb, out, lhsT, rhs, start, reads, writes, sig=False, stop=True):
    nc = kb.nc
    kb.P.op("pe", lambda: nc.tensor.matmul(out, lhsT=lhsT, rhs=rhs, start=start, stop=stop), reads=reads, writes=writes, sig=sig)


def bc3(ap2d):
    return AP(tensor=ap2d.tensor, offset=ap2d.offset, ap=[list(ap2d.ap[0]), [0, 3], list(ap2d.ap[1])])


class Attn:
    def __init__(self, kb):
        self.kb = kb
        self.pT = [kb.sb("pT", [128, 384], BF16) for _ in range(3)]
        self.i = 0
        self.sbank = 0
        self.pending = None

    def unit(self, s_terms, near_terms, accs, accb, v_ap, vbufs, first):
        kb = self.kb
        nc, P = kb.nc, kb.P
        ps, psb = kb.ps[self.sbank], kb.psb[self.sbank]
        self.sbank ^= 1
        n = len(s_terms)
        for t, (l, r, bufs) in enumerate(s_terms):
            mm(kb, ps[:, 0:384], l, r, t == 0, bufs, [psb], sig=(t == n - 1 and not near_terms))
        if near_terms:
            l, r, bufs = near_terms
            mm(kb, ps[:, 0:384].rearrange("p (a b) -> p a b", b=128), l, r, False, bufs, [psb], sig=True)
        pT, pTb = self.pT[self.i % 3]
        self.i += 1
        P.op("act", lambda: nc.scalar.activation(out=pT[:], in_=ps[:, 0:384], func=AF.Exp), reads=[psb], writes=[pTb])
        prev = self.pending
        self.pending = (pT, pTb, accs, accb, v_ap, vbufs, first)
        if prev is not None:
            self._pv(*prev)

    def _pv(self, pT, pTb, accs, accb, v_ap, vbufs, first):
        kb = self.kb
        for hh in range(3):
            mm(kb, accs[hh], pT[:, hh * 128:(hh + 1) * 128], v_ap, first[hh], [pTb] + vbufs, [accb[hh]], sig=(hh == 2))

    def flush(self):
        if self.pending is not None:
            self._pv(*self.pending)
            self.pending = None


def build_B(layer, kb=None):
    kb = kb or K("B")
    nc, P = kb.nc, kb.P
    x_d = kb.din("x", [TOK, D])
    id_d = kb.din("identb", [128, 128], BF16)
    an_d = kb.din("antib", [128, 128], BF16)
    relb_d = kb.din("rel_bias", [32, 12])
    oh_s_d = kb.din("oh_s", [33, Y_SLC])
    oh_w_d = kb.din("oh_w", [33, Y_WIN])
    oh_c_d = kb.din("oh_c", [33, Y_CMP])
    qT_d = kb.din("qT2", [128, 6, TOK], BF16)
    qm_d = kb.din("qmT", [64, 4, TOK], BF16)
    gates_d = kb.din("gates", [TOK, 36])
    ks_d = kb.din("ksT2", [128, T], BF16)
    kw_d = kb.din("kwT2", [128, T], BF16)
    kcr_d = kb.din("kcrT2", [128, T], BF16)
    vcr_d = kb.din("vcrT2", [128, T], BF16)
    vs_d = kb.din("vs", [T, 128])
    vw_d = kb.din("vw", [T, 128])
    ew_d = kb.din("ew", [128, T], BF16)
    ov_d = kb.din("ovl", [128, 4, 128], BF16)
    fb_d = kb.din("fb", [NT, 128, 128])
    w1k_d = kb.din("w1k", [2048, 256]); b1k_d = kb.din("b1k", [256, 1]); w2k_d = kb.din("w2k", [256, 64]); b2k_d = kb.din("b2k", [64, 1])
    w1v_d = kb.din("w1v", [2048, 256]); b1v_d = kb.din("b1v", [256, 1]); w2v_d = kb.din("w2v", [256, 64]); b2v_d = kb.din("b2v", [1, 64])
    posk_d = kb.din("posk", [32, 64]); posv_d = kb.din("posv", [32, 64])
    mem_d = kb.din("mem", [256, D]); gmem_d = kb.din("g_mem", [1, D]); wkv_d = kb.din("w_mem_kv", [D, 512])
    wout_d = kb.din("w_out", [D, D])
    hmid_d = kb.dout("hmid", [TOK, D])

    P.dma("sp", kb.ident[:], id_d, writes=[kb.identb])
    P.dma("sp", kb.anti[:], an_d, writes=[kb.antib])
    kb.mk_eps()
    with kb.scope():
        tabs = build_tables(kb, relb_d, [oh_s_d, oh_w_d, oh_c_d], [("s", Y_SLC), ("w", Y_WIN), ("c", Y_CMP)])
    band_s, band_sb = load_band(kb, tabs[0][0], tabs[0][1], Y_SLC, 2048, 1, "band_s")
    band_w, band_wb = load_band(kb, tabs[1][0], tabs[1][1], Y_WIN, 1024, 1, "band_w")
    bandc = [kb.sb("bandc", [128, 12, 128], BF16) for _ in range(4)]
    bandc_i = 0

    ksT, ksTb = kb.sb("ksT", [128, T], BF16)
    P.dma("sp", ksT[:, 0:4096], ks_d[:, 0:4096], writes=[ksTb])
    P.dma("pool", ksT[:, 4096:T], ks_d[:, 4096:T], writes=[ksTb])
    ew, ewb = kb.sb("ew", [128, T], BF16)
    P.dma("sp", ew[:, 0:4096], ew_d[:, 0:4096], writes=[ewb])
    P.dma("pool", ew[:, 4096:T], ew_d[:, 4096:T], writes=[ewb])
    vs1, vs1b = kb.sb("vs1", [128, 64, 2, 65], BF16)
    P.op("pool", lambda: nc.gpsimd.memset(vs1[:], 1.0), writes=[vs1b])
    with kb.scope():
        vst = [kb.sb("vst", [128, 8, 128], F32) for _ in range(2)]
        for i in range(8):
            st, stb = vst[i % 2]
            P.dma(kb.dq(), st[:], vs_d[i * 1024:(i + 1) * 1024, :].rearrange("(a p) c -> p a c", p=128), writes=[stb])
            P.op("pool", lambda: nc.gpsimd.tensor_copy(out=vs1[:, i * 8:(i + 1) * 8, :, 0:64], in_=st[:].rearrange("p a (g d) -> p a g d", g=2)),
                 reads=[stb], writes=[vs1b])
    RC, RCb = kb.sb("RC", [128, 4, 2, 193], BF16)
    P.op("pool", lambda: nc.gpsimd.memset(RC[:], 1.0), writes=[RCb])
    ovt, ovtb = kb.sb("ovt", [128, 4, 128], BF16)
    P.dma("sp", ovt[:], ov_d, writes=[ovtb])
    for g in range(2):
        P.op("pool", lambda: nc.gpsimd.tensor_copy(out=RC[:, :, g, 0:128], in_=ovt[:]), reads=[ovtb], writes=[RCb])
    kcT, kcTb = kb.sb("kcT", [128, 512], BF16)

    with ExitStack() as es2:
        def tmp(name, shape, dt):
            kb.n += 1
            return es2.enter_context(nc.sbuf_tensor("%s_%d" % (name, kb.n), shape, dt)), Buf(name)
        rawT, rawTb = tmp("rawT", [128, T + 32], BF16)
        w1, w1b = tmp("w1", [128, 32, 256], BF16)
        w1st, w1stb = tmp("w1st", [128, 8, 256], F32)
        w2, w2b = tmp("w2", [128, 2, 128], BF16)
        w2st, w2stb = tmp("w2st", [128, 2, 64], F32)
        h1T, h1Tb = tmp("h1T", [128, 2, 512], BF16)
        u_, ub = tmp("u", [128, 512], F32)
        t1, t1b = tmp("t1", [128, 512], F32)
        t2, t2b = tmp("t2", [128, 512], F32)
        cb, cbb = tmp("cb", [128, 2], F32)
        b1t, b1tb = tmp("b1t", [128, 2], F32)
        b2d, b2db = tmp("b2d", [128, 1], F32)
        b2r, b2rb = tmp("b2r", [128, 64], F32)
        pst, pstb = tmp("pst", [32, 64], F32)
        psb16, psb16b = tmp("psb16", [32, 64], BF16)
        posT, posTb = tmp("posT", [64, 1, 32], BF16)
        for kv in range(2):
            raw_d = kcr_d if kv == 0 else vcr_d
            w1_d = w1k_d if kv == 0 else w1v_d
            b1_d = b1k_d if kv == 0 else b1v_d
            w2_d = w2k_d if kv == 0 else w2v_d
            pos_d = posk_d if kv == 0 else posv_d
            P.op("dve", lambda: nc.vector.memset(rawT[:, T:T + 32], 0.0), writes=[rawTb])
            P.dma("sp", rawT[:, 0:4096], raw_d[:, 0:4096], writes=[rawTb])
            P.dma("pool", rawT[:, 4096:T], raw_d[:, 4096:T], writes=[rawTb])
            for q4 in range(4):
                for half in range(2):
                    P.dma(kb.dq(), w1st[half * 64:(half + 1) * 64, :, :], w1_d[q4 * 512:(q4 + 1) * 512, :].rearrange("(l d) h -> d l h", d=64), writes=[w1stb])
                P.op("dve", lambda: nc.vector.tensor_copy(out=w1[:, q4 * 8:(q4 + 1) * 8, :], in_=w1st[:]), reads=[w1stb], writes=[w1b])
            P.dma("sp", w2st[:], w2_d.rearrange("(c p) d -> p c d", p=128), writes=[w2stb])
            for half in range(2):
                P.op("dve", lambda: nc.vector.tensor_copy(out=w2[:, :, half * 64:(half + 1) * 64], in_=w2st[:]), reads=[w2stb], writes=[w2b])
            P.dma("sp", b1t[:], b1_d.rearrange("(c p) o -> p (c o)", p=128), writes=[b1tb])
            P.dma("sp", pst[:], pos_d, writes=[pstb])
            P.op("dve", lambda: nc.vector.tensor_copy(out=psb16[:], in_=pst[:]), reads=[pstb], writes=[psb16b])
            kb.transpose_to([psb16[:, :]], posT, posTb, [psb16b], np_in=32)
            for hc in range(2):
                ps, psb = kb.ps[2], kb.psb[2]
                for l in range(32):
                    mm(kb, ps[:, 0:1], w1[0:64, l, hc * 128:(hc + 1) * 128], posT[:, 0, l:l + 1], l == 0, [w1b, posTb], [psb], sig=(l == 31))
                P.op("dve", lambda: nc.vector.tensor_tensor(out=cb[:, hc:hc + 1], in0=ps[:, 0:1], in1=b1t[:, hc:hc + 1], op=ALU.add), reads=[psb, b1tb], writes=[cbb])
            if kv == 0:
                for half in range(2):
                    P.dma("sp", b2d[half * 64:(half + 1) * 64, :], b2k_d, writes=[b2db])
            else:
                src = AP(tensor=b2v_d.tensor, offset=b2v_d.offset, ap=[[0, 128], [1, 64]])
                P.dma("sp", b2r[:], src, writes=[b2rb])
            for g in range(2):
                gs = slice(g * 64, (g + 1) * 64)
                for hc in range(2):
                    ps, psb = kb.ps[2 + hc], kb.psb[2 + hc]
                    for l in range(32):
                        rhs = AP(tensor=rawT.tensor if hasattr(rawT, "tensor") else rawT[:].tensor, offset=rawT[gs, l:l + 1].offset,
                                 ap=[list(rawT[gs, :].ap[0]), [16, 512]])
                        mm(kb, ps[:, :], w1[gs, l, hc * 128:(hc + 1) * 128], rhs, l == 0, [w1b, rawTb], [psb], sig=(l == 31))
                    P.op("act", lambda: nc.scalar.activation(out=u_[:], in_=ps[:, :], func=AF.Identity, bias=cb[:, hc:hc + 1]), reads=[psb, cbb], writes=[ub])
                    P.op("dve", lambda: nc.vector.tensor_tensor(out=t1[:], in0=u_[:], in1=u_[:], op=ALU.mult), reads=[ub], writes=[t1b])
                    P.op("dve", lambda: nc.vector.tensor_scalar(out=t1[:], in0=t1[:], scalar1=0.044715, scalar2=1.0, op0=ALU.mult, op1=ALU.add), reads=[t1b], writes=[t1b])
                    P.op("dve", lambda: nc.vector.tensor_tensor(out=t1[:], in0=t1[:], in1=u_[:], op=ALU.mult), reads=[t1b, ub], writes=[t1b])
                    P.op("act", lambda: nc.scalar.activation(out=t2[:], in_=t1[:], func=AF.Tanh, scale=0.7978845608028654), reads=[t1b], writes=[t2b])
                    P.op("dve", lambda: nc.vector.tensor_scalar(out=t2[:], in0=t2[:], scalar1=0.5, scalar2=0.5, op0=ALU.mult, op1=ALU.add), reads=[t2b], writes=[t2b])
                    P.op("dve", lambda: nc.vector.tensor_tensor(out=h1T[:, hc, :], in0=t2[:], in1=u_[:], op=ALU.mult), reads=[t2b, ub], writes=[h1Tb])
                if kv == 0:
                    ps, psb = kb.ps[4], kb.psb[4]
                    for hc in range(2):
                        mm(kb, ps[:, :], w2[:, hc, :], h1T[:, hc, :], hc == 0, [w2b, h1Tb], [psb], sig=(hc == 1))
                    P.op("act", lambda: nc.scalar.activation(out=kcT[gs, :], in_=ps[gs, :], func=AF.Identity, bias=b2d[gs, 0:1]), reads=[psb, b2db], writes=[kcTb])
                else:
                    for j in range(4):
                        ps, psb = kb.ps[4], kb.psb[4]
                        for hc in range(2):
                            mm(kb, ps[:, 0:64], h1T[:, hc, j * 128:(j + 1) * 128], w2[:, hc, 0:64], hc == 0, [w2b, h1Tb], [psb], sig=(hc == 1))
                        P.op("dve", lambda: nc.vector.tensor_tensor(out=RC[:, j, g, 129:193], in0=ps[:, 0:64], in1=b2r[:], op=ALU.add), reads=[psb, b2rb], writes=[RCb])

    P.barrier()
    kmT, kmTb = kb.sb("kmT", [64, 4, 256], BF16)
    vm1, vm1b = kb.sb("vm1", [128, 2, 4, 65], BF16)
    P.op("pool", lambda: nc.gpsimd.memset(vm1[:], 1.0), writes=[vm1b])
    gm, gmb = kb.bcast_row(gmem_d, D, "gmem")
    wkv, wkvb = kb.sb("wkv", [128, 8, 512], BF16)
    with ExitStack() as es2:
        def tmp(name, shape, dt):
            kb.n += 1
            return es2.enter_context(nc.sbuf_tensor("%s_%d" % (name, kb.n), shape, dt)), Buf(name)
        mt, mtb = tmp("mt", [128, D], F32)
        mn, mnb = tmp("mn", [128, D], BF16)
        mnT, mnTb = tmp("mnT", [128, 8, 256], BF16)
        scr, scrb = tmp("mscr", [128, 4], F32)
        wst = [tmp("wst2", [128, 512], F32) for _ in range(2)]
        for k in range(8):
            st, stb = wst[k % 2]
            P.dma(kb.dq(), st[:], wkv_d[k * 128:(k + 1) * 128, :], writes=[stb])
            P.op("dve", lambda: nc.vector.tensor_copy(out=wkv[:, k, :], in_=st[:]), reads=[stb], writes=[wkvb])
        for tl in range(2):
            P.dma("sp", mt[:], mem_d[tl * 128:(tl + 1) * 128, :], writes=[mtb])
            kb.rmsnorm(mt[:], mtb, gm[:], gmb, mn[:], mnb, D, scr, scrb)
            pt = kb.pt[kb.pti % 2]; ptb = kb.ptb[kb.pti % 2]; kb.pti += 1
            for j in range(8):
                P.op("pe", lambda j=j: nc.tensor.transpose(pt[:, j * 128:(j + 1) * 128], mn[:, j * 128:(j + 1) * 128], kb.ident[:]),
                     reads=[mnb, kb.identb], writes=[ptb], sig=(j == 7))
            P.op("dve", lambda: nc.vector.tensor_copy(out=mnT[:, :, tl * 128:(tl + 1) * 128], in_=pt[:, :].rearrange("p (a b) -> p a b", b=128)), reads=[ptb], writes=[mnTb])
        for h in range(4):
            ps, psb = kb.ps[2], kb.psb[2]
            for k in range(8):
                mm(kb, ps[0:64, 0:256], wkv[:, k, h * 64:(h + 1) * 64], mnT[:, k, :], k == 0, [wkvb, mnTb], [psb], sig=(k == 7))
            P.op("dve", lambda: nc.vector.tensor_copy(out=kmT[:, h, :], in_=ps[0:64, 0:256]), reads=[psb], writes=[kmTb])
        for tl in range(2):
            ps, psb = kb.ps[3], kb.psb[3]
            for k in range(8):
                mm(kb, ps[:, 0:256], mnT[:, k, tl * 128:(tl + 1) * 128], wkv[:, k, 256:512], k == 0, [wkvb, mnTb], [psb], sig=(k == 7))
            P.op("dve", lambda: nc.vector.tensor_copy(out=vm1[:, tl, :, 0:64], in_=ps[:, 0:256].rearrange("p (h d) -> p h d", d=64)), reads=[psb], writes=[vm1b])
    P.barrier()
    wout, woutb = kb.sb("wout", [128, 8, D], BF16)
    with kb.scope():
        wst = [kb.sb("wst3", [128, 1024], F32) for _ in range(2)]
        for k in range(8):
            st, stb = wst[k % 2]
            P.dma(kb.dq(), st[:], wout_d[k * 128:(k + 1) * 128, :], writes=[stb])
            P.op("dve", lambda: nc.vector.tensor_copy(out=wout[:, k, :], in_=st[:]), reads=[stb], writes=[woutb])

    at = Attn(kb)
    qs = [kb.sb("qs", [128, 6, 128], BF16) for _ in range(2)]
    qms = [kb.sb("qms", [64, 4, 128], BF16) for _ in range(2)]
    gts = [kb.sb("gts", [128, 12, 3], F32) for _ in range(2)]
    kws = [kb.sb("kws", [128, 1024], BF16) for _ in range(2)]
    vws = [kb.sb("vws", [128, 8, 2, 65], BF16) for _ in range(2)]
    vwst = [kb.sb("vwst", [128, 8, 128], F32)] * 2
    for (t_, b_) in vws:
        P.op("pool", lambda: nc.gpsimd.memset(t_[:], 1.0), writes=[b_])
    fbs = [kb.sb("fbs", [128, 128], F32) for _ in range(2)]
    xts = [kb.sb("xts", [128, D], F32)] * 2
    cat, catb = kb.sb("cat", [128, D], F32)
    catbf, catbfb = kb.sb("catbf", [128, D], BF16)
    catT, catTb = kb.sb("catT", [128, 8, 128], BF16)
    pslc, pslcb = kb.sb("pslc", [128, 2, 128], F32)
    wk, wkb = kb.sb("wk", [128, 128], F32)
    m8, m8b = kb.sb("m8", [128, 16], F32)
    sbf, sbfb = kb.sb("sbf", [128, 128], BF16)
    sbT, sbTb = kb.sb("sbT", [128, 2, 128], BF16)
    sm, smb = kb.sb("sm", [128, 16], F32)
    pm, pmb = kb.sb("pm", [128, 256], BF16)
    accbank = 2

    def next_acc():
        nonlocal accbank
        b = accbank
        accbank = 2 + (accbank - 2 + 1) % 4
        return b

    for m in range(NT):
        q_, qb_ = qs[m % 2]
        qm_, qmb_ = qms[m % 2]
        gt_, gtb_ = gts[m % 2]
        kw_, kwb_ = kws[m % 2]
        vw_, vwb_ = vws[m % 2]
        vwst_, vwstb_ = vwst[m % 2]
        fb_, fbb_ = fbs[m % 2]
        xt_, xtb_ = xts[m % 2]
        tsl = slice(m * 128, (m + 1) * 128)
        P.dma("sp", q_[:], qT_d[:, :, tsl], writes=[qb_])
        P.dma("sp", qm_[:], qm_d[:, :, tsl], writes=[qmb_])
        P.dma("sp", gt_[:], gates_d[tsl, :].rearrange("p (h b) -> p h b", b=3), writes=[gtb_])
        P.dma("sp", fb_[:], fb_d[m], writes=[fbb_])
        P.dma("pool", xt_[:], x_d[tsl, :], writes=[xtb_])
        kt0 = max(0, 4 * m - 4)
        nkw = 4 * m + 4 - kt0
        P.dma("pool", kw_[:, 0:nkw * 128], kw_d[:, kt0 * 128:(4 * m + 4) * 128], writes=[kwb_])
        P.dma("sp", vwst_[:, 0:nkw, :], vw_d[kt0 * 128:(4 * m + 4) * 128, :].rearrange("(a p) c -> p a c", p=128), writes=[vwstb_])
        P.op("pool", lambda: nc.gpsimd.tensor_copy(out=vw_[:, 0:nkw, :, 0:64], in_=vwst_[:, 0:nkw, :].rearrange("p a (g d) -> p a g d", g=2)),
             reads=[vwstb_], writes=[vwb_])
        cband = {}
        for j in range(4):
            v = m - 4 * j
            if 0 <= v <= 6:
                dst = bandc[bandc_i % 4]
                bandc_i += 1
                load_band(kb, tabs[2][0], tabs[2][1], Y_CMP, 128, 16, "bc", zoff=512 * v, dst=dst)
                cband[j] = dst
        for g in range(2):
            gs = slice(g * 64, (g + 1) * 64)
            for tr in range(2):
                bA, bB = next_acc(), next_acc()
                accs = [kb.ps[bA][:, 0:193], kb.ps[bA][:, 193:386], kb.ps[bB][:, 0:193]]
                accb = [kb.psb[bA], kb.psb[bA], kb.psb[bB]]
                js = [j for j in range(4) if m - 4 * j >= 0]
                for ji, j in enumerate(js):
                    s_terms = [(kcT[gs, j * 128:(j + 1) * 128], q_[gs, 3 * tr:3 * tr + 3, :], [kcTb, qb_])]
                    near = None
                    if j in cband:
                        bt, btb = cband[j]
                        near = (kb.anti[:], bt[:, 6 * g + 3 * tr:6 * g + 3 * tr + 3, :], [kb.antib, btb])
                    at.unit(s_terms, near, accs, accb, RC[:, j, g, :], [RCb], [ji == 0, False, ji == 0])
                at.flush()
                for hh in range(3):
                    h = 6 * g + 3 * tr + hh
                    U = accs[hh]
                    P.op("dve", lambda: nc.vector.tensor_scalar(out=sm[:, 0:1], in0=U[:, 128:129], scalar1=1e-30, scalar2=None, op0=ALU.max), reads=[accb[hh]], writes=[smb])
                    P.op("dve", lambda: nc.vector.reciprocal(out=sm[:, 1:2], in_=sm[:, 0:1]), reads=[smb], writes=[smb])
                    if tr == 0 and hh == 0:
                        P.op("dve", lambda: nc.vector.tensor_scalar(out=pslc[:, g, :], in0=U[:, 0:128], scalar1=sm[:, 1:2], scalar2=None, op0=ALU.mult), reads=[accb[hh], smb], writes=[pslcb])
                    else:
                        P.op("dve", lambda: nc.vector.scalar_tensor_tensor(out=pslc[:, g, :], in0=U[:, 0:128], scalar=sm[:, 1:2], in1=pslc[:, g, :], op0=ALU.mult, op1=ALU.add), reads=[accb[hh], smb, pslcb], writes=[pslcb])
                    P.op("dve", lambda: nc.vector.tensor_tensor(out=sm[:, 2:3], in0=sm[:, 1:2], in1=gt_[:, h, 0:1], op=ALU.mult), reads=[smb, gtb_], writes=[smb])
                    P.op("dve", lambda: nc.vector.tensor_scalar(out=cat[:, h * 64:(h + 1) * 64], in0=U[:, 129:193], scalar1=sm[:, 2:3], scalar2=None, op0=ALU.mult), reads=[accb[hh], smb], writes=[catb])
            P.op("dve", lambda: nc.vector.tensor_tensor(out=wk[:], in0=pslc[:, g, :], in1=fb_[:], op=ALU.add), reads=[pslcb, fbb_], writes=[wkb])
            P.op("dve", lambda: nc.vector.max(out=m8[:, 0:8], in_=wk[:]), reads=[wkb], writes=[m8b])
            P.op("dve", lambda: nc.vector.match_replace(out=pslc[:, g, :], in_to_replace=m8[:, 0:8], in_values=wk[:], imm_value=-3e30), reads=[wkb, m8b], writes=[pslcb])
            P.op("dve", lambda: nc.vector.max(out=m8[:, 8:16], in_=pslc[:, g, :]), reads=[pslcb], writes=[m8b])
            P.op("dve", lambda: nc.vector.tensor_scalar(out=wk[:], in0=wk[:], scalar1=m8[:, 15:16], scalar2=-NEGB, op0=ALU.is_ge, op1=ALU.mult), reads=[wkb, m8b], writes=[wkb])
            P.op("dve", lambda: nc.vector.tensor_scalar(out=sbf[:], in0=wk[:], scalar1=NEGB, scalar2=None, op0=ALU.add), reads=[wkb], writes=[sbfb])
            pt = kb.pt[kb.pti % 2]; ptb = kb.ptb[kb.pti % 2]; kb.pti += 1
            P.op("pe", lambda: nc.tensor.transpose(pt[:, 0:128], sbf[:], kb.ident[:]), reads=[sbfb, kb.identb], writes=[ptb])
            P.op("dve", lambda: nc.vector.tensor_copy(out=sbT[:, g, :], in_=pt[:, 0:128]), reads=[ptb], writes=[sbTb])
        for br in range(2):
            for g in range(2):
                gs = slice(g * 64, (g + 1) * 64)
                for tr in range(2):
                    bA = next_acc()
                    accs = [kb.ps[bA][:, 65 * hh:65 * hh + 65] for hh in range(3)]
                    accb = [kb.psb[bA]] * 3
                    kts = list(range(0, 4 * m + 4)) if br == 0 else list(range(kt0, 4 * m + 4))
                    for ki, kt in enumerate(kts):
                        u = kt - 4 * m
                        if br == 0:
                            s_terms = [(ksT[gs, kt * 128:(kt + 1) * 128], q_[gs, 3 * tr:3 * tr + 3, :], [ksTb, qb_]),
                                       (ew[:, kt * 128:(kt + 1) * 128], bc3(sbT[:, g, :]), [ewb, sbTb])]
                            near = None
                            if u >= -12:
                                z0 = 128 * (3 - u)
                                near = (kb.anti[:], band_s[:, 6 * g + 3 * tr:6 * g + 3 * tr + 3, z0:z0 + 128], [kb.antib, band_sb])
                            v_ap, vb_ = vs1[:, kt, g, :], [vs1b]
                        else:
                            kk = kt - kt0
                            s_terms = [(kw_[gs, kk * 128:(kk + 1) * 128], q_[gs, 3 * tr:3 * tr + 3, :], [kwb_, qb_])]
                            z0 = 128 * (3 - u)
                            near = (kb.anti[:], band_w[:, 6 * g + 3 * tr:6 * g + 3 * tr + 3, z0:z0 + 128], [kb.antib, band_wb])
                            v_ap, vb_ = vw_[:, kk, g, :], [vwb_]
                        at.unit(s_terms, near, accs, accb, v_ap, vb_, [ki == 0, False, False])
                    at.flush()
                    for hh in range(3):
                        h = 6 * g + 3 * tr + hh
                        O = accs[hh]
                        P.op("dve", lambda: nc.vector.tensor_scalar(out=sm[:, 0:1], in0=O[:, 64:65], scalar1=1e-30, scalar2=None, op0=ALU.max), reads=[accb[hh]], writes=[smb])
                        P.op("dve", lambda: nc.vector.reciprocal(out=sm[:, 1:2], in_=sm[:, 0:1]), reads=[smb], writes=[smb])
                        P.op("dve", lambda: nc.vector.tensor_tensor(out=sm[:, 2:3], in0=sm[:, 1:2], in1=gt_[:, h, 1 + br:2 + br], op=ALU.mult), reads=[smb, gtb_], writes=[smb])
                        P.op("dve", lambda: nc.vector.scalar_tensor_tensor(out=cat[:, h * 64:(h + 1) * 64], in0=O[:, 0:64], scalar=sm[:, 2:3], in1=cat[:, h * 64:(h + 1) * 64], op0=ALU.mult, op1=ALU.add),
                             reads=[accb[hh], smb, catb], writes=[catb])
        for h in range(4):
            ps, psb = kb.ps[at.sbank], kb.psb[at.sbank]
            at.sbank ^= 1
            for tl in range(2):
                mm(kb, ps[:, tl * 128:(tl + 1) * 128], kmT[:, h, tl * 128:(tl + 1) * 128], qm_[:, h, :], tl == 0, [kmTb, qmb_], [psb], sig=(tl == 1))
            P.op("act", lambda: nc.scalar.activation(out=pm[:], in_=ps[:, 0:256], func=AF.Exp), reads=[psb], writes=[pmb])
            bA = next_acc()
            O = kb.ps[bA][:, 0:65]
            for tl in range(2):
                mm(kb, O, pm[:, tl * 128:(tl + 1) * 128], vm1[:, tl, h, :], tl == 0, [pmb, vm1b], [kb.psb[bA]], sig=(tl == 1))
            P.op("dve", lambda: nc.vector.reciprocal(out=sm[:, 4:5], in_=O[:, 64:65]), reads=[kb.psb[bA]], writes=[smb])
            P.op("dve", lambda: nc.vector.tensor_scalar(out=cat[:, 768 + h * 64:768 + (h + 1) * 64], in0=O[:, 0:64], scalar1=sm[:, 4:5], scalar2=None, op0=ALU.mult), reads=[kb.psb[bA], smb], writes=[catb])
        P.op("act", lambda: nc.scalar.copy(out=catbf[:], in_=cat[:]), reads=[catb], writes=[catbfb])
        pt = kb.pt[kb.pti % 2]; ptb = kb.ptb[kb.pti % 2]; kb.pti += 1
        for j in range(8):
            P.op("pe", lambda j=j: nc.tensor.transpose(pt[:, j * 128:(j + 1) * 128], catbf[:, j * 128:(j + 1) * 128], kb.ident[:]),
                 reads=[catbfb, kb.identb], writes=[ptb], sig=(j == 7))
        P.op("dve", lambda: nc.vector.tensor_copy(out=catT[:], in_=pt[:, :].rearrange("p (a b) -> p a b", b=128)), reads=[ptb], writes=[catTb])
        for half in range(2):
            bA = next_acc()
            ps, psb = kb.ps[bA], kb.psb[bA]
            for k in range(8):
                mm(kb, ps[:, :], catT[:, k, :], wout[:, k, half * 512:(half + 1) * 512], k == 0, [catTb, woutb], [psb], sig=(k == 7))
            P.op("dve", lambda: nc.vector.tensor_tensor(out=xt_[:, half * 512:(half + 1) * 512], in0=ps[:, :], in1=xt_[:, half * 512:(half + 1) * 512], op=ALU.add), reads=[psb, xtb_], writes=[xtb_])
        P.dma("sp", hmid_d[tsl, :], xt_[:], reads=[xtb_], final=True)
    return kb


def gather_seq(arrs, axis):
    shp = list(arrs[0].shape)
    shp[axis] = T
    out = np.zeros(shp, arrs[0].dtype)
    for c in range(4):
        idx = [slice(None)] * len(shp)
        idx[axis] = core_tokens(c)
        out[tuple(idx)] = arrs[c]
    return out


def consts_B(c):
    ident = np.eye(128, dtype=np.float32)
    oh_s, oh_w, oh_c = host_tables(c)
    ew = (np.arange(T)[None, :] // 64 == np.arange(128)[:, None]).astype(NPBF)
    n = np.arange(512)
    cs, ce = n * 16, n * 16 + 31
    ss = np.arange(128) * 64
    ov = ((cs[:, None] <= ss[None, :] + 63) & (ce[:, None] >= ss[None, :])).astype(np.float32)
    ov[511] = 0
    ovl = np.ascontiguousarray(ov.reshape(4, 128, 128).transpose(1, 0, 2)).astype(NPBF)
    fb = np.zeros((NT, 128, 128), np.float32)
    blk = np.arange(128)[None, :]
    for m in range(NT):
        t = 128 * (4 * m + c) + np.arange(128)[:, None]
        cur = t // 64
        forced = (blk == 0) | (blk == cur) | (blk == cur - 1)
        adm = (blk * 64) <= t
        fb[m] = np.where(adm, 1e4 * forced, -1e30)
    return {"identb": ident.astype(NPBF), "antib": np.ascontiguousarray(ident[::-1]).astype(NPBF),
            "oh_s": oh_s, "oh_w": oh_w, "oh_c": oh_c, "ew": ew, "ovl": ovl, "fb": fb}


def to2(fm, lo):
    return np.ascontiguousarray(np.concatenate([fm[:, lo, :], fm[:, lo + 1, :]], axis=0))


def run_B(inputs, resA):
    kb = build_B(0)
    maps = []
    for core in range(8):
        b, c = core // 4, core % 4
        grp = [resA[4 * b + cc] for cc in range(4)]
        fms = [np.asarray(r["fmT"]) for r in grp]
        tms = [np.asarray(r["tm"]) for r in grp]
        fm = fms[c]
        mp = consts_B(c)
        mp["x"] = np.ascontiguousarray(inputs["x"][b][core_tokens(c)])
        mp["rel_bias"] = inputs["rel_bias"]
        q = fm[:, 0:12, :]
        mp["qT2"] = np.ascontiguousarray(np.concatenate([q[:, 0:6, :], q[:, 6:12, :]], axis=0))
        mp["qmT"] = np.ascontiguousarray(fm[:, 20:24, :])
        mp["gates"] = np.ascontiguousarray(tms[c][:, 256:292])
        mp["kcrT2"] = gather_seq([to2(f, 12) for f in fms], 1)
        mp["vcrT2"] = gather_seq([to2(f, 14) for f in fms], 1)
        mp["ksT2"] = gather_seq([to2(f, 16) for f in fms], 1)
        mp["kwT2"] = gather_seq([to2(f, 18) for f in fms], 1)
        mp["vs"] = gather_seq([np.ascontiguousarray(t_[:, 0:128]) for t_ in tms], 0)
        mp["vw"] = gather_seq([np.ascontiguousarray(t_[:, 128:256]) for t_ in tms], 0)
        mp["w1k"] = inputs["nsa_cmp_k_w1"][0]; mp["b1k"] = inputs["nsa_cmp_k_b1"][0].reshape(256, 1)
        mp["w2k"] = inputs["nsa_cmp_k_w2"][0]; mp["b2k"] = inputs["nsa_cmp_k_b2"][0].reshape(64, 1)
        mp["w1v"] = inputs["nsa_cmp_v_w1"][0]; mp["b1v"] = inputs["nsa_cmp_v_b1"][0].reshape(256, 1)
        mp["w2v"] = inputs["nsa_cmp_v_w2"][0]; mp["b2v"] = inputs["nsa_cmp_v_b2"][0].reshape(1, 64)
        mp["posk"] = inputs["nsa_cmp_pos_k"][0]; mp["posv"] = inputs["nsa_cmp_pos_v"][0]
        mp["mem"] = inputs["mem"][b]; mp["g_mem"] = inputs["norm_mem"][0:1]; mp["w_mem_kv"] = inputs["w_mem_kv"][0]
        mp["w_out"] = inputs["w_out"][0]
        maps.append(mp)
    return kb.run(maps)


def scatter_tokens(per_core, key):
    out = np.zeros((2, T, D), np.float32)
    for core in range(8):
        b, c = core // 4, core % 4
        out[b][core_tokens(c)] = np.asarray(per_core[core][key])
    return out


def kernel_unfused(**inputs):
    inputs = {k: np.asarray(v) for k, v in inputs.items()}
    resA = run_A(inputs)
    resB = run_B(inputs, resA)
    resF = run_F(inputs, [r["hmid"] for r in resB], 0, False)
    resC = run_C(inputs, resF)
    resG = run_F(inputs, [r["hmid"] for r in resC], 1, True)
    return scatter_tokens(resG, "h")


def build_F(final, nproj, kb=None):
    kb = kb or K("F")
    nc, P = kb.nc, kb.P
    hm_d = kb.din("hmid", [TOK, D])
    id_d = kb.din("identb", [128, 128], BF16)
    g_d = kb.din("g_ffn", [1, D])
    wg_d = kb.din("wg", [D, DFF]); wu_d = kb.din("wu", [D, DFF]); wd_d = kb.din("wd", [DFF, D])
    g2_d = kb.din("g2", [1, D])
    h_d = kb.dout("h", [TOK, D])
    if not final:
        win_d = kb.din("w_in2", [D, nproj])
        pr_d = kb.dout("pr", [TOK, nproj])
    P.dma("sp", kb.ident[:], id_d, writes=[kb.identb])
    kb.mk_eps()
    g, gb = kb.bcast_row(g_d, D, "gffn")
    g2, g2b = kb.bcast_row(g2_d, D, "g2")
    H, Hb = kb.sb("H", [128, NT, D], F32)
    Hbs = [Buf("H%d" % i) for i in range(NT)]
    hnT, hnTb = kb.sb("hnT", [128, 8, TOK], BF16)
    hn, hnb = kb.sb("hn", [128, D], BF16)
    scr, scrb = kb.sb("scr", [128, 4], F32)

    def norm_T(gt, gtb):
        for ti in range(NT):
            kb.rmsnorm(H[:, ti, :], Hbs[ti], gt[:], gtb, hn[:], hnb, D, scr, scrb)
            pt = kb.pt[kb.pti % 2]; ptb = kb.ptb[kb.pti % 2]; kb.pti += 1
            for j in range(8):
                P.op("pe", lambda j=j: nc.tensor.transpose(pt[:, j * 128:(j + 1) * 128], hn[:, j * 128:(j + 1) * 128], kb.ident[:]),
                     reads=[hnb, kb.identb], writes=[ptb], sig=(j == 7))
            P.op("dve", lambda: nc.vector.tensor_copy(out=hnT[:, :, ti * 128:(ti + 1) * 128], in_=pt[:, :].rearrange("p (a b) -> p a b", b=128)), reads=[ptb], writes=[hnTb])

    for ti in range(NT):
        P.dma(kb.dq(), H[:, ti, :], hm_d[ti * 128:(ti + 1) * 128, :], writes=[Hbs[ti]])
    norm_T(g, gb)
    wgs, wgsb = kb.sb("wgs", [128, 8, 512], BF16)
    wus, wusb = kb.sb("wus", [128, 8, 512], BF16)
    wds, wdsb = kb.sb("wds", [128, 4, D], BF16)
    stg = [kb.sb("stg", [128, 1024], F32) for _ in range(2)]
    sg, sgb = kb.sb("sg", [128, 512], F32)
    ab, abb = kb.sb("ab", [128, 512], BF16)
    aT, aTb = kb.sb("aT", [128, 4, 128], BF16)
    si = 0
    for fg in range(6):
        c0 = fg * 512
        cw = min(512, DFF - c0)
        nch = cw // 128
        for (wsrc, wdst, wdstb) in ((wg_d, wgs, wgsb), (wu_d, wus, wusb)):
            for k in range(8):
                st, stb = stg[si % 2]; si += 1
                P.dma(kb.dq(), st[:, 0:cw], wsrc[k * 128:(k + 1) * 128, c0:c0 + cw], writes=[stb])
                kb.cast("pool" if k % 2 else "act", wdst[:, k, 0:cw], st[:, 0:cw], [stb], [wdstb])
        for ch in range(nch):
            st, stb = stg[si % 2]; si += 1
            P.dma(kb.dq(), st[:], wd_d[c0 + ch * 128:c0 + (ch + 1) * 128, :], writes=[stb])
            kb.cast("pool" if ch % 2 else "act", wds[:, ch, :], st[:], [stb], [wdsb])
        for ti in range(NT):
            tsl = slice(ti * 128, (ti + 1) * 128)
            pg, pgb = kb.ps[0], kb.psb[0]
            pu, pub = kb.ps[1], kb.psb[1]
            for k in range(8):
                mm(kb, pg[:, 0:cw], hnT[:, k, tsl], wgs[:, k, 0:cw], k == 0, [hnTb, wgsb], [pgb], sig=(k == 7))
            for k in range(8):
                mm(kb, pu[:, 0:cw], hnT[:, k, tsl], wus[:, k, 0:cw], k == 0, [hnTb, wusb], [pub], sig=(k == 7))
            P.op("act", lambda: nc.scalar.activation(out=sg[:, 0:cw], in_=pg[:, 0:cw], func=AF.Silu), reads=[pgb], writes=[sgb])
            P.op("dve", lambda: nc.vector.tensor_tensor(out=ab[:, 0:cw], in0=sg[:, 0:cw], in1=pu[:, 0:cw], op=ALU.mult), reads=[sgb, pub], writes=[abb])
            pt = kb.pt[kb.pti % 2]; ptb = kb.ptb[kb.pti % 2]; kb.pti += 1
            for j in range(nch):
                P.op("pe", lambda j=j: nc.tensor.transpose(pt[:, j * 128:(j + 1) * 128], ab[:, j * 128:(j + 1) * 128], kb.ident[:]),
                     reads=[abb, kb.identb], writes=[ptb], sig=(j == nch - 1))
            P.op("act", lambda: nc.scalar.copy(out=aT[:, 0:nch, :], in_=pt[:, 0:nch * 128].rearrange("p (a b) -> p a b", b=128)), reads=[ptb], writes=[aTb])
            for half in range(2):
                py, pyb = kb.ps[2 + half], kb.psb[2 + half]
                for ch in range(nch):
                    mm(kb, py[:, :], aT[:, ch, :], wds[:, ch, half * 512:(half + 1) * 512], ch == 0, [aTb, wdsb], [pyb], sig=(ch == nch - 1))
                P.op("dve", lambda: nc.vector.tensor_tensor(out=H[:, ti, half * 512:(half + 1) * 512], in0=py[:, :], in1=H[:, ti, half * 512:(half + 1) * 512], op=ALU.add),
                     reads=[pyb, Hbs[ti]], writes=[Hbs[ti]])
    if final:
        o, ob = kb.sb("o", [128, D], F32)
        for ti in range(NT):
            kb.rmsnorm(H[:, ti, :], Hbs[ti], g2[:], g2b, o[:], ob, D, scr, scrb)
            P.dma(kb.dq(), h_d[ti * 128:(ti + 1) * 128, :], o[:], reads=[ob], final=True)
    else:
        for ti in range(NT):
            P.dma(kb.dq(), h_d[ti * 128:(ti + 1) * 128, :], H[:, ti, :], reads=[Hbs[ti]], final=True)
        norm_T(g2, g2b)
        win, winb = kb.sb("win", [128, 8, nproj], BF16)
        for k in range(8):
            st, stb = stg[si % 2]; si += 1
            P.dma(kb.dq(), st[:, 0:nproj], win_d[k * 128:(k + 1) * 128, :], writes=[stb])
            kb.cast("pool" if k % 2 else "act", win[:, k, :], st[:, 0:nproj], [stb], [winb])
        pro, prob = kb.sb("pro", [128, nproj], F32)
        for ti in range(NT):
            tsl = slice(ti * 128, (ti + 1) * 128)
            o0 = 0
            bi = 0
            while o0 < nproj:
                n = min(512, nproj - o0)
                ps, psb = kb.ps[bi % 2], kb.psb[bi % 2]
                for k in range(8):
                    mm(kb, ps[:, 0:n], hnT[:, k, tsl], win[:, k, o0:o0 + n], k == 0, [hnTb, winb], [psb], sig=(k == 7))
                P.op("act", lambda: nc.scalar.copy(out=pro[:, o0:o0 + n], in_=ps[:, 0:n]), reads=[psb], writes=[prob])
                o0 += n
                bi += 1
            P.dma(kb.dq(), pr_d[tsl, :], pro[:], reads=[prob], final=True)
    return kb


def run_F(inputs, hmids, layer, final):
    kb = build_F(final, 712)
    ident = np.eye(128, dtype=np.float32).astype(NPBF)
    maps = []
    for core in range(8):
        mp = {"hmid": np.asarray(hmids[core]), "identb": ident, "g_ffn": inputs["norm_ffn"][layer:layer + 1],
              "wg": inputs["ffn_gate"][layer], "wu": inputs["ffn_up"][layer], "wd": inputs["ffn_down"][layer]}
        if final:
            mp["g2"] = inputs["norm_final"].reshape(1, D)
        else:
            mp["g2"] = inputs["norm_mix"][layer + 1:layer + 2]
            mp["w_in2"] = inputs["dsa_w_in"][0]
        maps.append(mp)
    return kb.run(maps)


NIT = 16


def mem_setup(kb, mem_d, gmem_d, wkv_d):
    nc, P = kb.nc, kb.P
    kmT, kmTb = kb.sb("kmT", [64, 4, 256], BF16)
    vm1, vm1b = kb.sb("vm1", [128, 2, 4, 65], BF16)
    P.op("pool", lambda: nc.gpsimd.memset(vm1[:], 1.0), writes=[vm1b])
    with kb.scope():
        gm, gmb = kb.bcast_row(gmem_d, D, "gmem")
        wkv, wkvb = kb.sb("wkv", [128, 8, 512], BF16)
        mt, mtb = kb.sb("mt", [128, D], F32)
        mn, mnb = kb.sb("mn", [128, D], BF16)
        mnT, mnTb = kb.sb("mnT", [128, 8, 256], BF16)
        scr, scrb = kb.sb("mscr", [128, 4], F32)
        wst = [kb.sb("wst2", [128, 512], F32) for _ in range(2)]
        for k in range(8):
            st, stb = wst[k % 2]
            P.dma(kb.dq(), st[:], wkv_d[k * 128:(k + 1) * 128, :], writes=[stb])
            P.op("dve", lambda: nc.vector.tensor_copy(out=wkv[:, k, :], in_=st[:]), reads=[stb], writes=[wkvb])
        for tl in range(2):
            P.dma("sp", mt[:], mem_d[tl * 128:(tl + 1) * 128, :], writes=[mtb])
            kb.rmsnorm(mt[:], mtb, gm[:], gmb, mn[:], mnb, D, scr, scrb)
            pt = kb.pt[kb.pti % 2]; ptb = kb.ptb[kb.pti % 2]; kb.pti += 1
            for j in range(8):
                P.op("pe", lambda j=j: nc.tensor.transpose(pt[:, j * 128:(j + 1) * 128], mn[:, j * 128:(j + 1) * 128], kb.ident[:]),
                     reads=[mnb, kb.identb], writes=[ptb], sig=(j == 7))
            P.op("dve", lambda: nc.vector.tensor_copy(out=mnT[:, :, tl * 128:(tl + 1) * 128], in_=pt[:, :].rearrange("p (a b) -> p a b", b=128)), reads=[ptb], writes=[mnTb])
        for h in range(4):
            ps, psb = kb.ps[2], kb.psb[2]
            for k in range(8):
                mm(kb, ps[0:64, 0:256], wkv[:, k, h * 64:(h + 1) * 64], mnT[:, k, :], k == 0, [wkvb, mnTb], [psb], sig=(k == 7))
            P.op("dve", lambda: nc.vector.tensor_copy(out=kmT[:, h, :], in_=ps[0:64, 0:256]), reads=[psb], writes=[kmTb])
        for tl in range(2):
            ps, psb = kb.ps[3], kb.psb[3]
            for k in range(8):
                mm(kb, ps[:, 0:256], mnT[:, k, tl * 128:(tl + 1) * 128], wkv[:, k, 256:512], k == 0, [wkvb, mnTb], [psb], sig=(k == 7))
            P.op("dve", lambda: nc.vector.tensor_copy(out=vm1[:, tl, :, 0:64], in_=ps[:, 0:256].rearrange("p (h d) -> p h d", d=64)), reads=[psb], writes=[vm1b])
    return kmT, kmTb, vm1, vm1b


def load_wout(kb, wout_d):
    nc, P = kb.nc, kb.P
    wout, woutb = kb.sb("wout", [128, 8, D], BF16)
    with kb.scope():
        wst = [kb.sb("wst3", [128, 1024], F32) for _ in range(2)]
        for k in range(8):
            st, stb = wst[k % 2]
            P.dma(kb.dq(), st[:], wout_d[k * 128:(k + 1) * 128, :], writes=[stb])
            P.op("dve", lambda: nc.vector.tensor_copy(out=wout[:, k, :], in_=st[:]), reads=[stb], writes=[woutb])
    return wout, woutb


def rms_small(kb, x, xb, A, d, g, gb, out, outb, tmp, tmpb, ss, ssb):
    nc, P = kb.nc, kb.P
    P.op("dve", lambda: nc.vector.tensor_tensor(out=tmp, in0=x, in1=x, op=ALU.mult), reads=[xb], writes=[tmpb])
    P.op("dve", lambda: nc.vector.tensor_reduce(out=ss[:, 0:A], in_=tmp, axis=AX.X, op=ALU.add), reads=[tmpb], writes=[ssb])
    P.op("act", lambda: nc.scalar.activation(out=ss[:, A:2 * A], in_=ss[:, 0:A], func=AF.Ln, scale=1.0 / d, bias=kb.eps_t[:, 0:1]), reads=[ssb, kb.eps_b], writes=[ssb])
    P.op("act", lambda: nc.scalar.activation(out=ss[:, 0:A], in_=ss[:, A:2 * A], func=AF.Exp, scale=-0.5), reads=[ssb], writes=[ssb])
    r = ss[:, 0:A]
    rb_ = AP(tensor=r.tensor, offset=r.offset, ap=[list(r.ap[0]), list(r.ap[1]), [0, d]])
    g2 = g[:, 0:d]
    gb_ = AP(tensor=g2.tensor, offset=g2.offset, ap=[list(g2.ap[0]), [0, A], list(g2.ap[1])])
    P.op("dve", lambda: nc.vector.tensor_tensor(out=tmp, in0=x, in1=rb_, op=ALU.mult), reads=[xb, ssb], writes=[tmpb])
    P.op("dve", lambda: nc.vector.tensor_tensor(out=out, in0=tmp, in1=gb_, op=ALU.mult), reads=[tmpb, gb], writes=[outb])


def build_C(stage=99, nslots=NT, kb=None):
    kb = kb or K("C")
    nc, P = kb.nc, kb.P
    x_d = kb.din("x", [TOK, D])
    pr_d = kb.din("pr", [TOK, 712])
    ckv_d = kb.din("ckv_seq", [T, 128])
    kidx_d = kb.din("kidx_seq", [T, 64])
    id_d = kb.din("identb", [128, 128], BF16)
    an_d = kb.din("antib", [128, 128], BF16)
    relb_d = kb.din("rel_bias", [32, 12])
    oh_s_d = kb.din("oh_s", [33, Y_SLC])
    cm_d = kb.din("cm", [128, 512])
    pw_d = kb.din("pw", [128, NIT])
    qn_d = kb.din("q_norm", [1, 256]); kvn_d = kb.din("kv_norm", [1, 128]); kin_d = kb.din("kidx_norm", [1, 64])
    wqup_d = kb.din("w_q_up", [256, 768]); wuk_d = kb.din("w_uk", [128, 768]); wuv_d = kb.din("w_uv", [128, 768])
    wqi_d = kb.din("w_q_idx", [256, 512])
    mem_d = kb.din("mem", [256, D]); gmem_d = kb.din("g_mem", [1, D]); wkv_d = kb.din("w_mem_kv", [D, 512])
    wout_d = kb.din("w_out", [D, D])
    hmid_d = kb.dout("hmid", [TOK, D])

    P.dma("sp", kb.ident[:], id_d, writes=[kb.identb])
    P.dma("sp", kb.anti[:], an_d, writes=[kb.antib])
    kb.mk_eps()
    with kb.scope():
        tabs = build_tables(kb, relb_d, [oh_s_d], [("s", Y_SLC)])
    band_s, band_sb = load_band(kb, tabs[0][0], tabs[0][1], Y_SLC, 2048, 1, "band_s")
    ckvT, ckvTb = kb.sb("ckvT", [128, T], BF16)
    ckv1, ckv1b = kb.sb("ckv1", [128, 64, 129], BF16)
    kidxT, kidxTb = kb.sb("kidxT", [64, T], BF16)
    P.op("pool", lambda: nc.gpsimd.memset(ckv1[:], 1.0), writes=[ckv1b])
    gq, gqb = kb.bcast_row(qn_d, 256, "gq")
    with kb.scope():
        gkv, gkvb = kb.bcast_row(kvn_d, 128, "gkv")
        gki, gkib = kb.bcast_row(kin_d, 64, "gki")
        st, stb = kb.sb("kst", [128, 8, 128], F32)
        tmp, tmpb = kb.sb("ktmp", [128, 8, 128], F32)
        ss, ssb = kb.sb("kss", [128, 16], F32)
        kin, kinb = kb.sb("kin", [128, 8, 64], BF16)
        for i in range(8):
            P.dma(kb.dq(), st[:], ckv_d[i * 1024:(i + 1) * 1024, :].rearrange("(a p) c -> p a c", p=128), writes=[stb])
            rms_small(kb, st[:], stb, 8, 128, gkv, gkvb, ckv1[:, i * 8:(i + 1) * 8, 0:128], ckv1b, tmp[:], tmpb, ss, ssb)
            pt = kb.pt[kb.pti % 2]; ptb = kb.ptb[kb.pti % 2]; kb.pti += 1
            for j in range(8):
                P.op("pe", lambda j=j: nc.tensor.transpose(pt[:, j * 128:(j + 1) * 128], ckv1[:, i * 8 + j, 0:128], kb.ident[:]),
                     reads=[ckv1b, kb.identb], writes=[ptb], sig=(j == 7))
            P.op("act", lambda: nc.scalar.copy(out=ckvT[:, i * 1024:(i + 1) * 1024], in_=pt[:, :]), reads=[ptb], writes=[ckvTb])
        for i in range(8):
            P.dma(kb.dq(), st[:, :, 0:64], kidx_d[i * 1024:(i + 1) * 1024, :].rearrange("(a p) c -> p a c", p=128), writes=[stb])
            rms_small(kb, st[:, :, 0:64], stb, 8, 64, gki, gkib, kin[:], kinb, tmp[:, :, 0:64], tmpb, ss, ssb)
            pt = kb.pt[kb.pti % 2]; ptb = kb.ptb[kb.pti % 2]; kb.pti += 1
            for j in range(8):
                P.op("pe", lambda j=j: nc.tensor.transpose(pt[0:64, j * 128:(j + 1) * 128], kin[:, j, :], kb.ident[:]),
                     reads=[kinb, kb.identb], writes=[ptb], sig=(j == 7))
            P.op("act", lambda: nc.scalar.copy(out=kidxT[:, i * 1024:(i + 1) * 1024], in_=pt[0:64, :]), reads=[ptb], writes=[kidxTb])
    wqup, wqupb = kb.sb("wqup", [128, 2, 768], BF16)
    wqi, wqib = kb.sb("wqi", [128, 2, 512], BF16)
    wuv, wuvb = kb.sb("wuv", [128, 768], BF16)
    wukT, wukTb = kb.sb("wukT", [64, 12, 128], BF16)
    with kb.scope():
        wst = [kb.sb("wst4", [128, 768], F32) for _ in range(2)]
        wukb, wukbb = kb.sb("wukb", [128, 768], BF16)
        i = 0
        for (src, rows, ncol, dstf) in [(wqup_d, 0, 768, lambda: wqup[:, 0, :]), (wqup_d, 128, 768, lambda: wqup[:, 1, :]),
                                        (wqi_d, 0, 512, lambda: wqi[:, 0, :]), (wqi_d, 128, 512, lambda: wqi[:, 1, :]),
                                        (wuv_d, 0, 768, lambda: wuv[:]), (wuk_d, 0, 768, lambda: wukb[:])]:
            st, stb = wst[i % 2]; i += 1
            P.dma(kb.dq(), st[:, 0:ncol], src[rows:rows + 128, :], writes=[stb])
            dst = dstf()
            P.op("dve", lambda: nc.vector.tensor_copy(out=dst, in_=st[:, 0:ncol]), reads=[stb], writes=[wqupb, wqib, wuvb, wukbb])
        for h0 in (0, 8):
            nb = min(8, 12 - h0)
            pt = kb.pt[kb.pti % 2]; ptb = kb.ptb[kb.pti % 2]; kb.pti += 1
            for j in range(nb):
                P.op("pe", lambda j=j: nc.tensor.transpose(pt[0:64, j * 128:(j + 1) * 128], wukb[:, (h0 + j) * 64:(h0 + j + 1) * 64], kb.ident[:]),
                     reads=[wukbb, kb.identb], writes=[ptb], sig=(j == nb - 1))
            P.op("dve", lambda: nc.vector.tensor_copy(out=wukT[:, h0:h0 + nb, :], in_=pt[0:64, 0:nb * 128].rearrange("p (a b) -> p a b", b=128)), reads=[ptb], writes=[wukTb])
    kmT, kmTb, vm1, vm1b = mem_setup(kb, mem_d, gmem_d, wkv_d)
    wout, woutb = load_wout(kb, wout_d)
    cm, cmb = kb.sb("cm", [128, 512], F32)
    P.dma("sp", cm[:], cm_d, writes=[cmb])
    pw, pwb = kb.sb("pw", [128, NIT], F32)
    P.dma("sp", pw[:], pw_d, writes=[pwb])

    at = Attn(kb)
    score, scoreb = kb.sb("score", [128, T], F32)
    mb, mbb = kb.sb("mb", [128, T], BF16)
    mTs = [kb.sb("mT", [128, 8, 128], BF16) for _ in range(2)]
    big, bigb = kb.sb("big", [128, D], F32)
    cqn, cqnb = kb.sb("cqn", [128, 256], BF16)
    cqT, cqTb = kb.sb("cqT", [128, 2, 128], BF16)
    qhT, qhTb = kb.sb("qhT", [128, 12, 128], BF16)
    qabs, qabsb = kb.sb("qabs", [128, 12, 128], BF16)
    qiT, qiTb = kb.sb("qiT", [64, 8, 128], BF16)
    qmb16, qmb16b = kb.sb("qmb16", [128, 256], BF16)
    qmT, qmTb = kb.sb("qmT", [64, 4, 128], BF16)
    rts = [kb.sb("rt", [128, 512], F32) for _ in range(2)]
    catbf, catbfb = kb.sb("catbf", [128, D], BF16)
    catT, catTb = kb.sb("catT", [128, 8, 128], BF16)
    pm, pmb = kb.sb("pm", [128, 256], BF16)
    sm, smb = kb.sb("sm", [128, 16], F32)
    wv, wvb = kb.sb("wv", [128, 32], F32)
    bs, bsb = kb.sb("bs", [128, 8 + 2 * NIT], F32)
    accbank = 2

    def next_acc():
        nonlocal accbank
        b = accbank
        accbank = 2 + (accbank - 2 + 1) % 4
        return b

    for m in range(nslots):
        tsl = slice(m * 128, (m + 1) * 128)
        L = 128 * (4 * m + 4)
        nkt = 4 * m + 4
        P.dma("sp", big[:, 0:712], pr_d[tsl, :], writes=[bigb])
        if stage == 0:
            P.dma("sp", hmid_d[tsl, :], big[:], reads=[bigb], final=True)
            continue
        ssq = wv[:, 16:18]
        P.op("act", lambda: nc.scalar.activation(out=cqn[:], in_=big[:, 0:256], func=AF.Square, accum_out=wv[:, 16:17]), reads=[bigb], writes=[cqnb, wvb])
        P.op("act", lambda: nc.scalar.activation(out=wv[:, 17:18], in_=wv[:, 16:17], func=AF.Ln, scale=1.0 / 256, bias=kb.eps_t[:, 0:1]), reads=[wvb, kb.eps_b], writes=[wvb])
        P.op("act", lambda: nc.scalar.activation(out=wv[:, 18:19], in_=wv[:, 17:18], func=AF.Exp, scale=-0.5), reads=[wvb], writes=[wvb])
        P.op("dve", lambda: nc.vector.scalar_tensor_tensor(out=cqn[:], in0=big[:, 0:256], scalar=wv[:, 18:19], in1=gq[:], op0=ALU.mult, op1=ALU.mult), reads=[bigb, wvb, gqb], writes=[cqnb])
        pt = kb.pt[kb.pti % 2]; ptb = kb.ptb[kb.pti % 2]; kb.pti += 1
        for j in range(2):
            P.op("pe", lambda j=j: nc.tensor.transpose(pt[:, j * 128:(j + 1) * 128], cqn[:, j * 128:(j + 1) * 128], kb.ident[:]), reads=[cqnb, kb.identb], writes=[ptb], sig=(j == 1))
        P.op("dve", lambda: nc.vector.tensor_copy(out=cqT[:], in_=pt[:, 0:256].rearrange("p (a b) -> p a b", b=128)), reads=[ptb], writes=[cqTb])
        for b4 in range(3):
            ps, psb = kb.ps[b4 % 2], kb.psb[b4 % 2]
            for hh in range(4):
                h = 4 * b4 + hh
                for c in range(2):
                    mm(kb, ps[0:64, hh * 128:(hh + 1) * 128], wqup[:, c, h * 64:(h + 1) * 64], cqT[:, c, :], hh == 0 and c == 0, [wqupb, cqTb], [psb], sig=(hh == 3 and c == 1))
            P.op("act", lambda: nc.scalar.activation(out=qhT[0:64, 4 * b4:4 * b4 + 4, :], in_=ps[0:64, :].rearrange("p (a b) -> p a b", b=128), func=AF.Copy, scale=0.125), reads=[psb], writes=[qhTb])
        for b4 in range(2):
            ps, psb = kb.ps[b4 % 2], kb.psb[b4 % 2]
            for hh in range(4):
                h = 4 * b4 + hh
                for c in range(2):
                    mm(kb, ps[0:64, hh * 128:(hh + 1) * 128], wqi[:, c, h * 64:(h + 1) * 64], cqT[:, c, :], hh == 0 and c == 0, [wqib, cqTb], [psb], sig=(hh == 3 and c == 1))
            P.op("dve", lambda: nc.vector.tensor_copy(out=qiT[:, 4 * b4:4 * b4 + 4, :], in_=ps[0:64, :].rearrange("p (a b) -> p a b", b=128)), reads=[psb], writes=[qiTb])
        for b4 in range(3):
            ps, psb = kb.ps[b4 % 2], kb.psb[b4 % 2]
            for hh in range(4):
                h = 4 * b4 + hh
                mm(kb, ps[:, hh * 128:(hh + 1) * 128], wukT[:, h, :], qhT[0:64, h, :], hh == 0, [wukTb, qhTb], [psb], sig=(hh == 3))
            P.op("dve", lambda: nc.vector.tensor_copy(out=qabs[:, 4 * b4:4 * b4 + 4, :], in_=ps[:, :].rearrange("p (a b) -> p a b", b=128)), reads=[psb], writes=[qabsb])
        P.op("dve", lambda: nc.vector.tensor_scalar(out=wv[:, 0:8], in0=big[:, 448:456], scalar1=0.04419417382415922, scalar2=None, op0=ALU.mult), reads=[bigb], writes=[wvb])
        P.op("dve", lambda: nc.vector.tensor_scalar(out=wv[:, 8:16], in0=wv[:, 0:8], scalar1=0.0, scalar2=2.0, op0=ALU.is_ge, op1=ALU.mult), reads=[wvb], writes=[wvb])
        P.op("dve", lambda: nc.vector.tensor_scalar(out=wv[:, 8:16], in0=wv[:, 8:16], scalar1=-1.0, scalar2=None, op0=ALU.add), reads=[wvb], writes=[wvb])
        P.op("dve", lambda: nc.vector.tensor_tensor(out=wv[:, 0:8], in0=wv[:, 0:8], in1=wv[:, 8:16], op=ALU.mult), reads=[wvb], writes=[wvb])
        P.op("act", lambda: nc.scalar.activation(out=qmb16[:], in_=big[:, 456:712], func=AF.Copy, scale=0.125), reads=[bigb], writes=[qmb16b])
        pt = kb.pt[kb.pti % 2]; ptb = kb.ptb[kb.pti % 2]; kb.pti += 1
        for j in range(4):
            P.op("pe", lambda j=j: nc.tensor.transpose(pt[0:64, j * 128:(j + 1) * 128], qmb16[:, j * 64:(j + 1) * 64], kb.ident[:]), reads=[qmb16b, kb.identb], writes=[ptb], sig=(j == 3))
        P.op("dve", lambda: nc.vector.tensor_copy(out=qmT[:], in_=pt[0:64, 0:512].rearrange("p (a b) -> p a b", b=128)), reads=[ptb], writes=[qmTb])
        P.dma("pool", big[:], x_d[tsl, :], reads=[], writes=[bigb])
        if stage == 1:
            P.dma("sp", hmid_d[tsl, :], big[:], reads=[bigb], final=True)
            continue
        for kc in range(m + 1):
            csl = slice(kc * 512, (kc + 1) * 512)
            for h in range(8):
                ps, psb = kb.ps[h % 2], kb.psb[h % 2]
                rt, rtb = rts[h % 2]
                mm(kb, ps[:, :], qiT[:, h, :], kidxT[:, csl], True, [qiTb, kidxTb], [psb], sig=True)
                P.op("act", lambda: nc.scalar.activation(out=rt[:], in_=ps[:, :], func=AF.Relu, scale=wv[:, h:h + 1]), reads=[psb, wvb], writes=[rtb])
                if h == 0:
                    P.op("dve", lambda: nc.vector.tensor_scalar(out=score[:, csl], in0=rt[:], scalar1=wv[:, 8:9], scalar2=None, op0=ALU.mult), reads=[rtb, wvb], writes=[scoreb])
                else:
                    P.op("dve", lambda: nc.vector.scalar_tensor_tensor(out=score[:, csl], in0=rt[:], scalar=wv[:, 8 + h:9 + h], in1=score[:, csl], op0=ALU.mult, op1=ALU.add), reads=[rtb, wvb, scoreb], writes=[scoreb])
        if stage == 2:
            P.dma("sp", hmid_d[tsl, 0:512], score[:, 0:512], reads=[scoreb], final=True)
            continue
        P.op("dve", lambda: nc.vector.tensor_reduce(out=bs[:, 0:1], in_=score[:, 0:L], axis=AX.X, op=ALU.min), reads=[scoreb], writes=[bsb])
        P.op("dve", lambda: nc.vector.tensor_reduce(out=bs[:, 1:2], in_=score[:, 0:L], axis=AX.X, op=ALU.max), reads=[scoreb], writes=[bsb])
        P.op("dve", lambda: nc.vector.tensor_tensor(out=score[:, L - 512:L], in0=score[:, L - 512:L], in1=cm[:], op=ALU.add), reads=[scoreb, cmb], writes=[scoreb])
        P.op("dve", lambda: nc.vector.tensor_tensor(out=bs[:, 2:3], in0=bs[:, 1:2], in1=bs[:, 0:1], op=ALU.subtract), reads=[bsb], writes=[bsb])
        P.op("dve", lambda: nc.vector.tensor_scalar(out=bs[:, 8:8 + NIT], in0=pw[:], scalar1=bs[:, 2:3], scalar2=None, op0=ALU.mult), reads=[bsb, pwb], writes=[bsb])
        for it in range(NIT):
            hw = bs[:, 8 + it:9 + it]
            P.op("dve", lambda: nc.vector.tensor_tensor(out=bs[:, 3:4], in0=bs[:, 0:1], in1=hw, op=ALU.add), reads=[bsb], writes=[bsb])
            P.op("dve", lambda: nc.vector.tensor_scalar(out=mb[:, 0:L], in0=score[:, 0:L], scalar1=bs[:, 3:4], scalar2=None, op0=ALU.is_ge, op1=ALU.add, accum_out=bs[:, 4:5]),
                 reads=[scoreb, bsb], writes=[mbb, bsb])
            P.op("dve", lambda: nc.vector.tensor_scalar(out=bs[:, 5:6], in0=bs[:, 4:5], scalar1=255.5, scalar2=hw, op0=ALU.is_ge, op1=ALU.mult), reads=[bsb], writes=[bsb])
            P.op("dve", lambda: nc.vector.tensor_tensor(out=bs[:, 0:1], in0=bs[:, 0:1], in1=bs[:, 5:6], op=ALU.add), reads=[bsb], writes=[bsb])
        P.op("dve", lambda: nc.vector.tensor_scalar(out=mb[:, 0:L], in0=score[:, 0:L], scalar1=bs[:, 0:1], scalar2=NEGB, op0=ALU.is_lt, op1=ALU.mult), reads=[scoreb, bsb], writes=[mbb])
        if stage == 3:
            P.dma("sp", hmid_d[tsl, 0:512], score[:, 0:512], reads=[scoreb], final=True)
            P.dma("sp", hmid_d[tsl, 512:512 + 8 + 2 * NIT], bs[:], reads=[bsb], final=True)
            continue
        banks = [next_acc() for _ in range(4)]
        for g8 in range(0, nkt, 8):
            nb = min(8, nkt - g8)
            mT, mTb = mTs[(g8 // 8) % 2]
            pt = kb.pt[kb.pti % 2]; ptb = kb.ptb[kb.pti % 2]; kb.pti += 1
            for j in range(nb):
                P.op("pe", lambda j=j: nc.tensor.transpose(pt[:, j * 128:(j + 1) * 128], mb[:, (g8 + j) * 128:(g8 + j + 1) * 128], kb.ident[:]), reads=[mbb, kb.identb], writes=[ptb], sig=(j == nb - 1))
            P.op("dve", lambda: nc.vector.tensor_copy(out=mT[:, 0:nb, :], in_=pt[:, 0:nb * 128].rearrange("p (a b) -> p a b", b=128)), reads=[ptb], writes=[mTb])
            for j in range(nb):
                kt = g8 + j
                u = kt - 4 * m
                for tr in range(4):
                    bA = banks[tr]
                    accs = [kb.ps[bA][:, 129 * hh:129 * hh + 129] for hh in range(3)]
                    accb = [kb.psb[bA]] * 3
                    s_terms = [(ckvT[:, kt * 128:(kt + 1) * 128], qabs[:, 3 * tr:3 * tr + 3, :], [ckvTb, qabsb]),
                               (kb.ident[:], bc3(mT[:, j, :]), [kb.identb, mTb])]
                    near = None
                    if u >= -12:
                        z0 = 128 * (3 - u)
                        near = (kb.anti[:], band_s[:, 3 * tr:3 * tr + 3, z0:z0 + 128], [kb.antib, band_sb])
                    at.unit(s_terms, near, accs, accb, ckv1[:, kt, :], [ckv1b], [kt == 0, False, False])
        at.flush()
        for tr in range(4):
            bA = banks[tr]
            for hh in range(3):
                h = 3 * tr + hh
                O = kb.ps[bA][:, 129 * hh:129 * hh + 129]
                P.op("dve", lambda: nc.vector.tensor_scalar(out=sm[:, 0:1], in0=O[:, 128:129], scalar1=1e-30, scalar2=None, op0=ALU.max), reads=[kb.psb[bA]], writes=[smb])
                P.op("dve", lambda: nc.vector.reciprocal(out=sm[:, 1:2], in_=sm[:, 0:1]), reads=[smb], writes=[smb])
                P.op("dve", lambda: nc.vector.tensor_scalar(out=qabs[:, h, :], in0=O[:, 0:128], scalar1=sm[:, 1:2], scalar2=None, op0=ALU.mult), reads=[kb.psb[bA], smb], writes=[qabsb])
        for h0 in (0, 8):
            nb = min(8, 12 - h0)
            pt = kb.pt[kb.pti % 2]; ptb = kb.ptb[kb.pti % 2]; kb.pti += 1
            for j in range(nb):
                P.op("pe", lambda j=j: nc.tensor.transpose(pt[:, j * 128:(j + 1) * 128], qabs[:, h0 + j, :], kb.ident[:]), reads=[qabsb, kb.identb], writes=[ptb], sig=(j == nb - 1))
            P.op("dve", lambda: nc.vector.tensor_copy(out=qhT[:, h0:h0 + nb, :], in_=pt[:, 0:nb * 128].rearrange("p (a b) -> p a b", b=128)), reads=[ptb], writes=[qhTb])
        for h0 in (0, 8):
            nb = min(8, 12 - h0)
            bA = next_acc()
            ps, psb = kb.ps[bA], kb.psb[bA]
            for j in range(nb):
                h = h0 + j
                mm(kb, ps[:, j * 64:(j + 1) * 64], qhT[:, h, :], wuv[:, h * 64:(h + 1) * 64], j == 0, [qhTb, wuvb], [psb], sig=(j == nb - 1))
            P.op("act", lambda: nc.scalar.copy(out=catbf[:, h0 * 64:(h0 + nb) * 64], in_=ps[:, 0:nb * 64]), reads=[psb], writes=[catbfb])
        for h in range(4):
            ps, psb = kb.ps[at.sbank], kb.psb[at.sbank]
            at.sbank ^= 1
            for tl in range(2):
                mm(kb, ps[:, tl * 128:(tl + 1) * 128], kmT[:, h, tl * 128:(tl + 1) * 128], qmT[:, h, :], tl == 0, [kmTb, qmTb], [psb], sig=(tl == 1))
            P.op("act", lambda: nc.scalar.activation(out=pm[:], in_=ps[:, 0:256], func=AF.Exp), reads=[psb], writes=[pmb])
            bA = next_acc()
            O = kb.ps[bA][:, 0:65]
            for tl in range(2):
                mm(kb, O, pm[:, tl * 128:(tl + 1) * 128], vm1[:, tl, h, :], tl == 0, [pmb, vm1b], [kb.psb[bA]], sig=(tl == 1))
            P.op("dve", lambda: nc.vector.reciprocal(out=sm[:, 4:5], in_=O[:, 64:65]), reads=[kb.psb[bA]], writes=[smb])
            P.op("dve", lambda: nc.vector.tensor_scalar(out=catbf[:, 768 + h * 64:768 + (h + 1) * 64], in0=O[:, 0:64], scalar1=sm[:, 4:5], scalar2=None, op0=ALU.mult), reads=[kb.psb[bA], smb], writes=[catbfb])
        pt = kb.pt[kb.pti % 2]; ptb = kb.ptb[kb.pti % 2]; kb.pti += 1
        for j in range(8):
            P.op("pe", lambda j=j: nc.tensor.transpose(pt[:, j * 128:(j + 1) * 128], catbf[:, j * 128:(j + 1) * 128], kb.ident[:]),
                 reads=[catbfb, kb.identb], writes=[ptb], sig=(j == 7))
        P.op("dve", lambda: nc.vector.tensor_copy(out=catT[:], in_=pt[:, :].rearrange("p (a b) -> p a b", b=128)), reads=[ptb], writes=[catTb])
        for half in range(2):
            bA = next_acc()
            ps, psb = kb.ps[bA], kb.psb[bA]
            for k in range(8):
                mm(kb, ps[:, :], catT[:, k, :], wout[:, k, half * 512:(half + 1) * 512], k == 0, [catTb, woutb], [psb], sig=(k == 7))
            P.op("dve", lambda: nc.vector.tensor_tensor(out=big[:, half * 512:(half + 1) * 512], in0=ps[:, :], in1=big[:, half * 512:(half + 1) * 512], op=ALU.add), reads=[psb, bigb], writes=[bigb])
        P.dma("sp", hmid_d[tsl, :], big[:], reads=[bigb], final=True)
    return kb


def run_C(inputs, resF, stage=99, nslots=NT):
    kb = build_C(stage, nslots)
    ident = np.eye(128, dtype=np.float32)
    pw = np.tile((0.5 ** np.arange(1, NIT + 1)).astype(np.float32)[None, :], (128, 1))
    maps = []
    for core in range(8):
        b, c = core // 4, core % 4
        prs = [np.asarray(resF[4 * b + cc]["pr"]) for cc in range(4)]
        oh_s, _, _ = host_tables(c)
        z = np.arange(512)[None, :]
        q = np.arange(128)[:, None]
        cm = np.where(z <= 128 * c + q, 0.0, -1e30).astype(np.float32)
        mp = {"x": np.asarray(resF[core]["h"]), "pr": prs[c],
              "ckv_seq": gather_seq([np.ascontiguousarray(p[:, 256:384]) for p in prs], 0),
              "kidx_seq": gather_seq([np.ascontiguousarray(p[:, 384:448]) for p in prs], 0),
              "identb": ident.astype(NPBF), "antib": np.ascontiguousarray(ident[::-1]).astype(NPBF),
              "rel_bias": inputs["rel_bias"], "oh_s": oh_s, "cm": cm, "pw": pw,
              "q_norm": inputs["dsa_q_norm"][0:1], "kv_norm": inputs["dsa_kv_norm"][0:1], "kidx_norm": inputs["dsa_kidx_norm"][0:1],
              "w_q_up": inputs["dsa_w_q_up"][0], "w_uk": inputs["dsa_w_uk"][0].reshape(128, 768), "w_uv": inputs["dsa_w_uv"][0].reshape(128, 768),
              "w_q_idx": inputs["dsa_w_q_idx"][0],
              "mem": inputs["mem"][b], "g_mem": inputs["norm_mem"][1:2], "w_mem_kv": inputs["w_mem_kv"][1], "w_out": inputs["w_out"][1]}
        maps.append(mp)
    return kb.run(maps)


GROUPS = [[0, 1, 2, 3], [4, 5, 6, 7]]


def all_gather(kb, in_ap2d, out_ap2d):
    nc, P = kb.nc, kb.P
    P.barrier()
    kb.n += 1
    key = "cc%d" % kb.n
    sem = P.es.enter_context(nc.semaphore(key))
    P.sem[key] = sem
    P.cnt[key] = 0
    with nc.Block() as block:
        @block.gpsimd
        def _(g):
            g.collective_compute("AllGather", ALU.bypass, replica_groups=GROUPS, ins=[in_ap2d.opt()], outs=[out_ap2d.opt()]).then_inc(sem)
            g.wait_ge(sem, 1)
    P.cnt[key] = 1
    P.seen["pool"][key] = 1
    for e in P.eng:
        P._need(e, (key, 1))


def seq_rows(all_t, ncols_total, col0, ncol, m0, nm):
    return AP(tensor=all_t, offset=m0 * 128 * ncols_total + col0,
              ap=[[128 * ncols_total, nm], [2048 * ncols_total, 4], [ncols_total, 128], [1, ncol]])


def build_fused():
    kb = K("fused")
    nc, P = kb.nc, kb.P
    ext = {}

    def E(name, shape, dt=F32):
        ext[name] = nc.dram_tensor(name, list(shape), dt, kind="ExternalInput").ap()
        return ext[name]

    def S(name, shape, dt=F32):
        return nc.dram_tensor(name, list(shape), dt)

    x = E("x", [TOK, D])
    out = nc.dram_tensor("out", [TOK, D], F32, kind="ExternalOutput").ap()
    nm = {k: E(k, [1, D]) for k in ("norm_mix0", "norm_mix1", "norm_ffn0", "norm_ffn1", "norm_mem0", "norm_mem1", "norm_final")}
    nsa_w_in = E("nsa_w_in", [D, 1828]); gate_b = E("gate_b", [1, 36])
    ident = E("ident", [128, 128]); anti = E("anti", [128, 128])
    identb = E("identb", [128, 128], BF16); antib = E("antib", [128, 128], BF16)
    rel_bias = E("rel_bias", [32, 12])
    oh_s = E("oh_s", [33, Y_SLC]); oh_w = E("oh_w", [33, Y_WIN]); oh_c = E("oh_c", [33, Y_CMP])
    ew = E("ew", [128, T], BF16); ovl = E("ovl", [128, 4, 128], BF16); fb = E("fb", [NT, 128, 128])
    cmp_w = {k: E(k, shp) for k, shp in [("w1k", [2048, 256]), ("b1k", [256, 1]), ("w2k", [256, 64]), ("b2k", [64, 1]),
                                         ("w1v", [2048, 256]), ("b1v", [256, 1]), ("w2v", [256, 64]), ("b2v", [1, 64]),
                                         ("posk", [32, 64]), ("posv", [32, 64])]}
    mem = E("mem", [256, D])
    wkv = [E("w_mem_kv%d" % i, [D, 512]) for i in range(2)]
    wout = [E("w_out%d" % i, [D, D]) for i in range(2)]
    wg = [E("wg%d" % i, [D, DFF]) for i in range(2)]
    wu = [E("wu%d" % i, [D, DFF]) for i in range(2)]
    wd = [E("wd%d" % i, [DFF, D]) for i in range(2)]
    dsa_w_in = E("dsa_w_in", [D, 712])
    cm = E("cm", [128, 512]); pw = E("pw", [128, NIT])
    q_norm = E("q_norm", [1, 256]); kv_norm = E("kv_norm", [1, 128]); kidx_norm = E("kidx_norm", [1, 64])
    w_q_up = E("w_q_up", [256, 768]); w_uk = E("w_uk", [128, 768]); w_uv = E("w_uv", [128, 768]); w_q_idx = E("w_q_idx", [256, 512])

    fm_loc = S("fm_loc", [64, 24, TOK], BF16)
    tm_loc = S("tm_loc", [TOK, 292])
    kfm_loc = S("kfm_loc", [8, 64, TOK], BF16)
    kfm_all = S("kfm_all", [4, 4 * 128, TOK], BF16)
    vt_loc = S("vt_loc", [TOK, 256])
    vt_all = S("vt_all", [4, 4 * 512, 256])
    qT2_s = S("qT2_s", [128, 6, TOK], BF16)
    kseq = {k: S(k + "_s", [128, T], BF16) for k in ("kcrT2", "vcrT2", "ksT2", "kwT2")}
    vs_s = S("vs_s", [T, 128]); vw_s = S("vw_s", [T, 128])
    hmid0 = S("hmid0", [TOK, D]); h0 = S("h0", [TOK, D]); hmid1 = S("hmid1", [TOK, D])
    pr_loc = S("pr_loc", [TOK, 712])
    kv_loc = S("kv_loc", [TOK, 192]); kv_all = S("kv_all", [4, 4 * 512, 192])
    ckv_s = S("ckv_s", [T, 128]); kidx_s = S("kidx_s", [T, 64])

    kb.alias = {"x": x, "g": nm["norm_mix0"], "w_in": nsa_w_in, "gate_b": gate_b, "ident": ident, "anti": anti,
                "fmT": fm_loc.ap(), "tm": tm_loc.ap()}
    with kb.scope():
        build_A(kb)
    P.barrier()
    P.dma("sp", kfm_loc.ap(), AP(tensor=fm_loc, offset=12 * TOK, ap=[[TOK, 8], [24 * TOK, 64], [1, TOK]]))
    P.dma("pool", vt_loc.ap(), tm_loc.ap()[:, 0:256])
    for ch in range(4):
        all_gather(kb, kfm_loc.ap()[2 * ch:2 * ch + 2].rearrange("a d t -> (a d) t"), kfm_all.ap()[ch])
    for r4 in range(4):
        all_gather(kb, vt_loc.ap()[r4 * 512:(r4 + 1) * 512, :], vt_all.ap()[r4])
    for g in range(2):
        P.dma(kb.dq(), qT2_s.ap()[g * 64:(g + 1) * 64, :, :], fm_loc.ap()[:, 6 * g:6 * g + 6, :])
    for ch, k in enumerate(("kcrT2", "vcrT2", "ksT2", "kwT2")):
        for g in range(2):
            for cc in range(4):
                dst = AP(tensor=kseq[k], offset=(g * 64) * T + cc * 128, ap=[[T, 64], [512, 16], [1, 128]])
                src = AP(tensor=kfm_all, offset=((ch * 4 + cc) * 128 + g * 64) * TOK, ap=[[TOK, 64], [128, 16], [1, 128]])
                P.dma(kb.dq(), dst, src)
    for (dst_t, c0) in ((vs_s, 0), (vw_s, 128)):
        for cc in range(4):
            for r4 in range(4):
                dst = AP(tensor=dst_t, offset=(r4 * 16 * 128 + cc * 128) * 128, ap=[[512 * 128, 4], [128, 128], [1, 128]])
                src = AP(tensor=vt_all, offset=((r4 * 4 + cc) * 512) * 256 + c0, ap=[[128 * 256, 4], [256, 128], [1, 128]])
                P.dma(kb.dq(), dst, src)
    P.barrier()
    kb.alias = dict(cmp_w)
    kb.alias.update({"x": x, "identb": identb, "antib": antib, "rel_bias": rel_bias, "oh_s": oh_s, "oh_w": oh_w, "oh_c": oh_c,
                     "qT2": qT2_s.ap(), "qmT": fm_loc.ap()[:, 20:24, :], "gates": tm_loc.ap()[:, 256:292],
                     "ksT2": kseq["ksT2"].ap(), "kwT2": kseq["kwT2"].ap(), "kcrT2": kseq["kcrT2"].ap(), "vcrT2": kseq["vcrT2"].ap(),
                     "vs": vs_s.ap(), "vw": vw_s.ap(), "ew": ew, "ovl": ovl, "fb": fb,
                     "mem": mem, "g_mem": nm["norm_mem0"], "w_mem_kv": wkv[0], "w_out": wout[0], "hmid": hmid0.ap()})
    with kb.scope():
        build_B(0, kb)
    P.barrier()
    kb.alias = {"hmid": hmid0.ap(), "identb": identb, "g_ffn": nm["norm_ffn0"], "wg": wg[0], "wu": wu[0], "wd": wd[0],
                "g2": nm["norm_mix1"], "h": h0.ap(), "w_in2": dsa_w_in, "pr": pr_loc.ap()}
    with kb.scope():
        build_F(False, 712, kb)
    P.barrier()
    P.dma("sp", kv_loc.ap(), pr_loc.ap()[:, 256:448])
    for r4 in range(4):
        all_gather(kb, kv_loc.ap()[r4 * 512:(r4 + 1) * 512, :], kv_all.ap()[r4])
    for (dst_t, c0, nc_) in ((ckv_s, 0, 128), (kidx_s, 128, 64)):
        for cc in range(4):
            for r4 in range(4):
                dst = AP(tensor=dst_t, offset=(r4 * 16 * 128 + cc * 128) * nc_, ap=[[512 * nc_, 4], [nc_, 128], [1, nc_]])
                src = AP(tensor=kv_all, offset=((r4 * 4 + cc) * 512) * 192 + c0, ap=[[128 * 192, 4], [192, 128], [1, nc_]])
                P.dma(kb.dq(), dst, src)
    P.barrier()
    kb.alias = {"x": h0.ap(), "pr": pr_loc.ap(), "ckv_seq": ckv_s.ap(), "kidx_seq": kidx_s.ap(), "identb": identb, "antib": antib,
                "rel_bias": rel_bias, "oh_s": oh_s, "cm": cm, "pw": pw, "q_norm": q_norm, "kv_norm": kv_norm, "kidx_norm": kidx_norm,
                "w_q_up": w_q_up, "w_uk": w_uk, "w_uv": w_uv, "w_q_idx": w_q_idx, "mem": mem, "g_mem": nm["norm_mem1"],
                "w_mem_kv": wkv[1], "w_out": wout[1], "hmid": hmid1.ap()}
    with kb.scope():
        build_C(99, NT, kb)
    P.barrier()
    kb.alias = {"hmid": hmid1.ap(), "identb": identb, "g_ffn": nm["norm_ffn1"], "wg": wg[1], "wu": wu[1], "wd": wd[1],
                "g2": nm["norm_final"], "h": out}
    with kb.scope():
        build_F(True, 712, kb)
    kb.ext = ext
    return kb


def fused_maps(inputs):
    ident = np.eye(128, dtype=np.float32)
    anti = np.ascontiguousarray(ident[::-1])
    pw = np.tile((0.5 ** np.arange(1, NIT + 1)).astype(np.float32)[None, :], (128, 1))
    maps = []
    for core in range(8):
        b, c = core // 4, core % 4
        mp = consts_B(c)
        z = np.arange(512)[None, :]
        q = np.arange(128)[:, None]
        mp.update({
            "x": np.ascontiguousarray(inputs["x"][b][core_tokens(c)]),
            "norm_mix0": inputs["norm_mix"][0:1], "norm_mix1": inputs["norm_mix"][1:2],
            "norm_ffn0": inputs["norm_ffn"][0:1], "norm_ffn1": inputs["norm_ffn"][1:2],
            "norm_mem0": inputs["norm_mem"][0:1], "norm_mem1": inputs["norm_mem"][1:2],
            "norm_final": inputs["norm_final"].reshape(1, D),
            "nsa_w_in": inputs["nsa_w_in"][0], "gate_b": inputs["nsa_gate_b"][0:1],
            "ident": ident, "anti": anti, "rel_bias": inputs["rel_bias"],
            "w1k": inputs["nsa_cmp_k_w1"][0], "b1k": inputs["nsa_cmp_k_b1"][0].reshape(256, 1),
            "w2k": inputs["nsa_cmp_k_w2"][0], "b2k": inputs["nsa_cmp_k_b2"][0].reshape(64, 1),
            "w1v": inputs["nsa_cmp_v_w1"][0], "b1v": inputs["nsa_cmp_v_b1"][0].reshape(256, 1),
            "w2v": inputs["nsa_cmp_v_w2"][0], "b2v": inputs["nsa_cmp_v_b2"][0].reshape(1, 64),
            "posk": inputs["nsa_cmp_pos_k"][0], "posv": inputs["nsa_cmp_pos_v"][0],
            "mem": inputs["mem"][b],
            "w_mem_kv0": inputs["w_mem_kv"][0], "w_mem_kv1": inputs["w_mem_kv"][1],
            "w_out0": inputs["w_out"][0], "w_out1": inputs["w_out"][1],
            "wg0": inputs["ffn_gate"][0], "wg1": inputs["ffn_gate"][1],
            "wu0": inputs["ffn_up"][0], "wu1": inputs["ffn_up"][1],
            "wd0": inputs["ffn_down"][0], "wd1": inputs["ffn_down"][1],
            "dsa_w_in": inputs["dsa_w_in"][0],
            "cm": np.where(z <= 128 * c + q, 0.0, -1e30).astype(np.float32), "pw": pw,
            "q_norm": inputs["dsa_q_norm"][0:1], "kv_norm": inputs["dsa_kv_norm"][0:1], "kidx_norm": inputs["dsa_kidx_norm"][0:1],
            "w_q_up": inputs["dsa_w_q_up"][0], "w_uk": inputs["dsa_w_uk"][0].reshape(128, 768),
            "w_uv": inputs["dsa_w_uv"][0].reshape(128, 768), "w_q_idx": inputs["dsa_w_q_idx"][0],
        })
        maps.append(mp)
    return maps


def kernel(**inputs):
    inputs = {k: np.asarray(v) for k, v in inputs.items()}
    kb = build_fused()
    res = kb.run(fused_maps(inputs))
    return scatter_tokens(res, "out")
```

```python
import math
from contextlib import ExitStack
import numpy as np
import ml_dtypes
import concourse.bass as bass
import concourse.mybir as mybir
from concourse.bass import AP
from concourse.bass_utils import run_bass_kernel_spmd

F32 = mybir.dt.float32
BF16 = mybir.dt.bfloat16
AF = mybir.ActivationFunctionType
ALU = mybir.AluOpType
AX = mybir.AxisListType
NPBF = ml_dtypes.bfloat16

D = 1024
T = 8192
NT = 16
TOK = 2048
DFF = 2816
EPS = 1e-6
NEGB = -30000.0


class Buf:
    __slots__ = ("name", "w", "r")

    def __init__(self, name=""):
        self.name = name
        self.w = None
        self.r = {}


class Prog:
    NSLOT = 12

    def __init__(self, nc):
        self.nc = nc
        self.eng = {"pe": nc.tensor, "act": nc.scalar, "dve": nc.vector,
                    "pool": nc.gpsimd, "sp": nc.sync}
        self.es = ExitStack()
        self.sem = {}
        self.cnt = {}
        for e in ("pe", "act", "dve", "pool"):
            self.sem[e] = self.es.enter_context(nc.semaphore("c_" + e))
            self.cnt[e] = 0
        self.slots = {}
        self.slot_i = {}
        for q in ("sp", "pool"):
            lst = []
            for i in range(self.NSLOT):
                key = "d_%s%d" % (q, i)
                self.sem[key] = self.es.enter_context(nc.semaphore(key))
                self.cnt[key] = 0
                lst.append(key)
            self.slots[q] = lst
            self.slot_i[q] = 0
        self.seen = {e: {} for e in self.eng}
        self.outtoks = []

    def _need(self, e, tok):
        if tok is None:
            return
        key, val = tok
        if self.seen[e].get(key, 0) >= val:
            return
        if key in ("pe", "act", "dve", "pool"):
            assert self.cnt[key] >= val, "missing signal on %s" % key
        self.eng[e].wait_ge(self.sem[key], val)
        self.seen[e][key] = val

    def _deps(self, e, reads, writes):
        for b in reads:
            if b.w is not None and not (e == "pe" and b.w[0] == "pe"):
                self._need(e, b.w)
        for b in writes:
            if b.w is not None and b.w[0] != e:
                self._need(e, b.w)
            for k, v in b.r.items():
                if k != e:
                    self._need(e, (k, v))

    def _mark(self, tok, reads, writes):
        for b in reads:
            if b.r.get(tok[0], 0) < tok[1]:
                b.r[tok[0]] = tok[1]
        for b in writes:
            b.w = tok
            b.r = {}

    def op(self, e, fn, reads=(), writes=(), sig=True):
        self._deps(e, reads, writes)
        ins = fn()
        if sig:
            self.cnt[e] += 1
            ins.then_inc(self.sem[e], 1)
            tok = (e, self.cnt[e])
        else:
            tok = (e, self.cnt[e] + 1)
        self._mark(tok, reads, writes)
        return ins

    def dma(self, q, out, in_, reads=(), writes=(), final=False):
        lst = self.slots[q]
        key = lst[self.slot_i[q] % self.NSLOT]
        self.slot_i[q] += 1
        if self.cnt[key] > 0:
            self._need(q, (key, self.cnt[key]))
        self._deps(q, reads, writes)
        ins = self.eng[q].dma_start(out=out, in_=in_)
        self.cnt[key] += 16
        ins.then_inc(self.sem[key], 16)
        tok = (key, self.cnt[key])
        self._mark(tok, reads, writes)
        if final:
            self.outtoks.append(tok)
        return tok

    def barrier(self):
        toks = [(e, self.cnt[e]) for e in ("pe", "act", "dve", "pool") if self.cnt[e] > 0]
        for q in ("sp", "pool"):
            toks += [(k, self.cnt[k]) for k in self.slots[q] if self.cnt[k] > 0]
        for e in self.eng:
            for t in toks:
                if t[0] != e:
                    self._need(e, t)

    def finish(self):
        for tok in self.outtoks:
            self._need("sp", tok)
        for q in ("sp", "pool"):
            for key in self.slots[q]:
                if self.cnt[key] > 0:
                    self._need("sp", (key, self.cnt[key]))
        self.es.close()


class K:
    def __init__(self, name):
        self.nc = bass.Bass("TRN2", target_bir_lowering=False)
        self.P = Prog(self.nc)
        self.es = ExitStack()
        self.n = 0
        self.rr = 0
        nc = self.nc
        self.es.enter_context(nc.allow_non_contiguous_dma(reason="small strided parameter loads"))
        self.ps = [self.es.enter_context(nc.psum_tensor("ps%d" % i, [128, 512], F32)) for i in range(7)]
        self.psb = [Buf("ps%d" % i) for i in range(7)]
        pt0 = self.es.enter_context(nc.psum_tensor("pt0", [128, 1024], BF16))
        ptb0 = Buf("pt0")
        self.pt = [pt0, pt0]
        self.ptb = [ptb0, ptb0]
        self.pti = 0
        self.ident, self.identb = self.sb("ident", [128, 128], BF16)
        self.anti, self.antib = self.sb("anti", [128, 128], BF16)

    def sb(self, name, shape, dt):
        self.n += 1
        t = self.es.enter_context(self.nc.sbuf_tensor("%s_%d" % (name, self.n), shape, dt))
        return t, Buf(name)

    def scope(self):
        kb = self

        class _S:
            def __enter__(self_):
                self_.old = kb.es
                kb.es = ExitStack()
                return kb.es

            def __exit__(self_, *a):
                kb.P.barrier()
                kb.es.close()
                kb.es = self_.old
        return _S()

    alias = None

    def din(self, name, shape, dt=F32):
        if self.alias is not None:
            ap = self.alias[name]
            assert list(ap.shape) == list(shape), (name, ap.shape, shape)
            return ap
        return self.nc.dram_tensor(name, list(shape), dt, kind="ExternalInput").ap()

    def dout(self, name, shape, dt=F32):
        if self.alias is not None:
            ap = self.alias[name]
            assert list(ap.shape) == list(shape), (name, ap.shape, shape)
            return ap
        return self.nc.dram_tensor(name, list(shape), dt, kind="ExternalOutput").ap()

    def dscr(self, name, shape, dt=F32):
        self.n += 1
        return self.nc.dram_tensor("%s_%d" % (name, self.n), list(shape), dt, kind="Internal")

    def dq(self):
        self.rr += 1
        return "sp" if self.rr % 2 else "pool"

    def load_consts(self, ident_d, anti_d):
        st, stb = self.sb("cst", [128, 256], F32)
        self.P.dma("sp", st[:, 0:128], ident_d, writes=[stb])
        self.P.dma("sp", st[:, 128:256], anti_d, writes=[stb])
        nc = self.nc
        self.P.op("dve", lambda: nc.vector.tensor_copy(out=self.ident[:], in_=st[:, 0:128]), reads=[stb], writes=[self.identb])
        self.P.op("dve", lambda: nc.vector.tensor_copy(out=self.anti[:], in_=st[:, 128:256]), reads=[stb], writes=[self.antib])

    def load_w(self, dram, kdim, ncol, name, eng="pool", stage=None):
        nc, P = self.nc, self.P
        kp = min(128, kdim)
        nk = max(1, kdim // 128)
        w, wb = self.sb(name, [kp, nk, ncol], BF16)
        CH = 2048
        if stage is None:
            stage = [self.sb("wst", [128, CH], F32) for _ in range(2)]
        self._wst = stage
        i = 0
        for k in range(nk):
            for c0 in range(0, ncol, CH):
                cw = min(CH, ncol - c0)
                st, stb = stage[i % 2]
                i += 1
                P.dma(self.dq(), st[0:kp, 0:cw], dram[k * 128:k * 128 + kp, c0:c0 + cw], writes=[stb])
                self.cast(eng, w[:, k, c0:c0 + cw], st[0:kp, 0:cw], [stb], [wb])
        return w, wb

    def cast(self, eng, out, in_, reads, writes, sig=True):
        nc = self.nc
        if eng == "pool":
            self.P.op("pool", lambda: nc.gpsimd.tensor_copy(out=out, in_=in_), reads=reads, writes=writes, sig=sig)
        elif eng == "dve":
            self.P.op("dve", lambda: nc.vector.tensor_copy(out=out, in_=in_), reads=reads, writes=writes, sig=sig)
        else:
            self.P.op("act", lambda: nc.scalar.copy(out=out, in_=in_), reads=reads, writes=writes, sig=sig)

    def bcast_row(self, dram_row, ncol, name):
        t, tb = self.sb(name, [128, ncol], F32)
        src = AP(tensor=dram_row.tensor, offset=dram_row.offset, ap=[[0, 128], [1, ncol]])
        self.P.dma("sp", t[:], src, writes=[tb])
        return t, tb

    def rmsnorm(self, x, xb, g, gb, out, outb, dim, scr, scrb, np_=128):
        nc, P = self.nc, self.P
        P.op("act", lambda: nc.scalar.activation(out=out, in_=x, func=AF.Square, accum_out=scr[0:np_, 0:1]),
             reads=[xb], writes=[outb, scrb])
        P.op("act", lambda: nc.scalar.activation(out=scr[0:np_, 1:2], in_=scr[0:np_, 0:1], func=AF.Ln, scale=1.0 / dim, bias=self.eps_t[0:np_, 0:1]),
             reads=[scrb, self.eps_b], writes=[scrb])
        P.op("act", lambda: nc.scalar.activation(out=scr[0:np_, 2:3], in_=scr[0:np_, 1:2], func=AF.Exp, scale=-0.5),
             reads=[scrb], writes=[scrb])
        P.op("dve", lambda: nc.vector.scalar_tensor_tensor(out=out, in0=x, scalar=scr[0:np_, 2:3], in1=g, op0=ALU.mult, op1=ALU.mult),
             reads=[xb, scrb, gb], writes=[outb])

    def mk_eps(self):
        self.eps_t, self.eps_b = self.sb("eps", [128, 1], F32)
        nc = self.nc
        self.P.op("dve", lambda: nc.vector.memset(self.eps_t[:], EPS), writes=[self.eps_b])

    def transpose_to(self, src_list, dst, dstb, reads, evac="dve", np_in=128):
        nc, P = self.nc, self.P
        n = len(src_list)
        i0 = 0
        while i0 < n:
            nb = min(8, n - i0)
            pt = self.pt[self.pti % 2]
            ptb = self.ptb[self.pti % 2]
            self.pti += 1
            w = src_list[i0].shape[-1]
            for j in range(nb):
                s = src_list[i0 + j]
                P.op("pe", lambda s=s, j=j: nc.tensor.transpose(pt[0:w, j * 128:j * 128 + np_in], s, self.ident[0:np_in, 0:np_in]),
                     reads=list(reads) + [self.identb], writes=[ptb], sig=(j == nb - 1))
            src = pt[0:w, 0:nb * 128].rearrange("p (a b) -> p a b", b=128)[:, :, 0:np_in]
            o = dst[0:w, i0:i0 + nb, :]
            if evac == "dve":
                P.op("dve", lambda: nc.vector.tensor_copy(out=o, in_=src), reads=[ptb], writes=[dstb])
            else:
                P.op("act", lambda: nc.scalar.copy(out=o, in_=src), reads=[ptb], writes=[dstb])
            i0 += nb

    def run(self, in_maps):
        self.P.finish()
        self.es.close()
        res = run_bass_kernel_spmd(self.nc, in_maps, core_ids=list(range(8)))
        return res.results


def rel_bucket_np(dist):
    n = np.maximum(dist, 0)
    nf = np.maximum(n, 16).astype(np.float32)
    large = 16 + (np.log(nf / np.float32(16)) / np.float32(math.log(2048 / 16)) * np.float32(16)).astype(np.int32)
    large = np.minimum(large, 31)
    return np.where(n < 16, n, large)


def onehot_table(dist, masked):
    Y = dist.shape[0]
    oh = np.zeros((33, Y), np.float32)
    b = rel_bucket_np(dist)
    ok = ~masked
    oh[b[ok], np.nonzero(ok)[0]] = 1.0
    oh[32, masked] = 1.0
    return oh


Y_SLC = 2304
Y_WIN = 1280
Y_CMP = 5376


def host_tables(c):
    y = np.arange(Y_SLC)
    d = y - 511 + 128 * c
    oh_s = onehot_table(d, d < 0)
    y = np.arange(Y_WIN)
    d = y - 511 + 128 * c
    oh_w = onehot_table(d, (d < 0) | (d >= 512))
    y = np.arange(Y_CMP)
    d = y + 128 * c - 2063
    oh_c = onehot_table(d, d < 0)
    return oh_s, oh_w, oh_c


def build_tables(kb, rel_bias_d, ohs, names_Y):
    nc, P = kb.nc, kb.P
    bt, btb = kb.sb("biasT", [33, 12], F32)
    P.dma("sp", bt[0:32, :], rel_bias_d, writes=[btb])
    b31, b31b = kb.sb("b31", [33, 12], F32)
    src = AP(tensor=rel_bias_d.tensor, offset=rel_bias_d.offset + 31 * 12, ap=[[0, 32], [1, 12]])
    P.dma("sp", b31[0:32, :], src, writes=[b31b])
    bb, bbb = kb.sb("biasb", [33, 12], BF16)
    P.op("dve", lambda: nc.vector.memset(bb[:], NEGB), writes=[bbb])
    P.op("dve", lambda: nc.vector.tensor_tensor(out=bb[0:32, :], in0=bt[0:32, :], in1=b31[0:32, :], op=ALU.subtract),
         reads=[btb, b31b], writes=[bbb])
    outs = []
    for (oh_d, Y, nm) in [(o, y, n) for o, (n, y) in zip(ohs, names_Y)]:
        oh, ohb = kb.sb("oh" + nm, [33, Y], BF16)
        st, stb = kb.sb("ohst" + nm, [33, Y], F32)
        P.dma("sp", st[:], oh_d, writes=[stb])
        P.op("dve", lambda: nc.vector.tensor_copy(out=oh[:], in_=st[:]), reads=[stb], writes=[ohb])
        tt, ttb = kb.sb("tt" + nm, [12, Y], BF16)
        for c0 in range(0, Y, 512):
            cw = min(512, Y - c0)
            ps, psb = kb.ps[0], kb.psb[0]
            P.op("pe", lambda: nc.tensor.matmul(ps[0:12, 0:cw], lhsT=bb[:], rhs=oh[:, c0:c0 + cw], start=True, stop=True),
                 reads=[bbb, ohb], writes=[psb])
            P.op("dve", lambda: nc.vector.tensor_copy(out=tt[:, c0:c0 + cw], in_=ps[0:12, 0:cw]), reads=[psb], writes=[ttb])
        scr = kb.dscr("ttd" + nm, [12, Y], BF16)
        scrb = Buf("ttd" + nm)
        P.dma("sp", scr.ap(), tt[:], reads=[ttb], writes=[scrb])
        outs.append((scr, scrb, Y))
    return outs


def load_band(kb, scr, scrb, Y, Z, pstep, name, zoff=0, dst=None):
    if dst is None:
        dst = kb.sb(name, [128, 12, Z], BF16)
    t, tb = dst
    src = AP(tensor=scr, offset=zoff, ap=[[pstep, 128], [Y, 12], [1, Z]])
    kb.P.dma(kb.dq(), t[:, :, 0:Z], src, reads=[scrb], writes=[tb])
    return t, tb


FM_A = [(0 + 64 * i, 0.125) for i in range(12)] + [(768 + 64 * i, 1.0) for i in range(4)] + \
       [(1024, 1.0), (1088, 1.0), (1280, 1.0), (1344, 1.0)] + [(1572 + 64 * i, 0.125) for i in range(4)]


def proj_phase(kb, x_d, g_d, w, wb, ncols, fm_groups, tm_ranges, fmT_d, tm_d, post_tm=None):
    nc, P = kb.nc, kb.P
    g, gb = kb.bcast_row(g_d, D, "gain")
    xt = [kb.sb("xt", [128, D], F32) for _ in range(2)]
    xn = [kb.sb("xn", [128, D], BF16) for _ in range(2)]
    scr, scrb = kb.sb("scr", [128, 4], F32)
    xnT, xnTb = kb.sb("xnT", [128, 8, 512], BF16)
    ng = len(fm_groups)
    fst, fstb = kb.sb("fst", [64, ng, 512], BF16)
    ntm = sum(n for _, n in tm_ranges)
    tst = [kb.sb("tst", [128, max(ntm, 1)], F32) for _ in range(2)]
    ei = 0
    for blk in range(4):
        for tt in range(4):
            ti = blk * 4 + tt
            x_, xb_ = xt[ti % 2]
            n_, nb_ = xn[ti % 2]
            P.dma(kb.dq(), x_[:], x_d[ti * 128:(ti + 1) * 128, :], writes=[xb_])
            kb.rmsnorm(x_[:], xb_, g[:], gb, n_[:], nb_, D, scr, scrb)
            for half in range(1):
                pt = kb.pt[kb.pti % 2]
                ptb = kb.ptb[kb.pti % 2]
                kb.pti += 1
                for j in range(8):
                    P.op("pe", lambda j=j: nc.tensor.transpose(pt[:, j * 128:(j + 1) * 128], n_[:, j * 128:(j + 1) * 128], kb.ident[:]),
                         reads=[nb_, kb.identb], writes=[ptb], sig=(j == 7))
                P.op("dve", lambda: nc.vector.tensor_copy(out=xnT[:, :, tt * 128:(tt + 1) * 128],
                                                          in_=pt[:, :].rearrange("p (a b) -> p a b", b=128)),
                     reads=[ptb], writes=[xnTb])
            if ntm:
                ts_, tsb_ = tst[ti % 2]
                ps, psb = kb.ps[4], kb.psb[4]
                o = 0
                for (c0, n) in tm_ranges:
                    for k in range(8):
                        P.op("pe", lambda k=k, o=o, c0=c0, n=n: nc.tensor.matmul(ps[:, o:o + n], lhsT=xnT[:, k, tt * 128:(tt + 1) * 128], rhs=w[:, k, c0:c0 + n],
                                                                       start=(k == 0 and o == 0), stop=(k == 7)),
                             reads=[xnTb, wb], writes=[psb], sig=(k == 7))
                    o += n
                P.op("act", lambda: nc.scalar.copy(out=ts_[:, 0:ntm], in_=ps[:, 0:ntm]), reads=[psb], writes=[tsb_])
                if post_tm is not None:
                    post_tm(ts_, tsb_)
                P.dma(kb.dq(), tm_d[ti * 128:(ti + 1) * 128, :], ts_[:, 0:ntm], reads=[tsb_], final=True)
        for gi, (c0, sc) in enumerate(fm_groups):
            ps, psb = kb.ps[gi % 4], kb.psb[gi % 4]
            for k in range(8):
                P.op("pe", lambda k=k, c0=c0: nc.tensor.matmul(ps[0:64, :], lhsT=w[:, k, c0:c0 + 64], rhs=xnT[:, k, :], start=(k == 0), stop=(k == 7)),
                     reads=[xnTb, wb], writes=[psb], sig=(k == 7))
            if ei % 2 == 0:
                P.op("act", lambda: nc.scalar.activation(out=fst[:, gi, :], in_=ps[0:64, :], func=AF.Copy, scale=sc), reads=[psb], writes=[fstb])
            else:
                P.op("dve", lambda: nc.vector.tensor_scalar(out=fst[:, gi, :], in0=ps[0:64, :], scalar1=sc, scalar2=None, op0=ALU.mult), reads=[psb], writes=[fstb])
            ei += 1
        P.dma(kb.dq(), fmT_d[:, :, blk * 512:(blk + 1) * 512], fst[:], reads=[fstb], final=True)


def build_A(kb=None):
    kb = kb or K("A")
    nc, P = kb.nc, kb.P
    x_d = kb.din("x", [TOK, D])
    g_d = kb.din("g", [1, D])
    w_d = kb.din("w_in", [D, 1828])
    gb_d = kb.din("gate_b", [1, 36])
    id_d = kb.din("ident", [128, 128])
    an_d = kb.din("anti", [128, 128])
    fm_d = kb.dout("fmT", [64, 24, TOK], BF16)
    tm_d = kb.dout("tm", [TOK, 256 + 36])
    kb.load_consts(id_d, an_d)
    kb.mk_eps()
    w, wb = kb.load_w(w_d, D, 1828, "w_in")
    gbt, gbb = kb.bcast_row(gb_d, 36, "gateb")

    def post(ts_, tsb_):
        P.op("dve", lambda: nc.vector.tensor_tensor(out=ts_[:, 256:292], in0=ts_[:, 256:292], in1=gbt[:], op=ALU.add), reads=[tsb_, gbb], writes=[tsb_])
        P.op("act", lambda: nc.scalar.activation(out=ts_[:, 256:292], in_=ts_[:, 256:292], func=AF.Exp, scale=-1.0), reads=[tsb_], writes=[tsb_])
        P.op("dve", lambda: nc.vector.tensor_scalar(out=ts_[:, 256:292], in0=ts_[:, 256:292], scalar1=1.0, scalar2=None, op0=ALU.add), reads=[tsb_], writes=[tsb_])
        P.op("dve", lambda: nc.vector.reciprocal(out=ts_[:, 256:292], in_=ts_[:, 256:292]), reads=[tsb_], writes=[tsb_])

    proj_phase(kb, x_d, g_d, w, wb, 1828, FM_A, [(1152, 128), (1408, 128), (1536, 36)], fm_d, tm_d, post_tm=post)
    return kb


def core_tokens(c):
    return np.concatenate([np.arange(128 * (4 * m + c), 128 * (4 * m + c) + 128) for m in range(NT)])


def run_A(inputs):
    kb = build_A()
    ident = np.eye(128, dtype=np.float32)
    anti = np.ascontiguousarray(ident[::-1])
    maps = []
    for core in range(8):
        b, c = core // 4, core % 4
        maps.append({"x": np.ascontiguousarray(inputs["x"][b][core_tokens(c)]),
                     "g": inputs["norm_mix"][0:1], "w_in": inputs["nsa_w_in"][0],
                     "gate_b": inputs["nsa_gate_b"][0:1], "ident": ident, "anti": anti})
    return kb.run(maps)


def mm(kb, out, lhsT, rhs, start, reads, writes, sig=False, stop=True):
    nc = kb.nc
    kb.P.op("pe", lambda: nc.tensor.matmul(out, lhsT=lhsT, rhs=rhs, start=start, stop=stop), reads=reads, writes=writes, sig=sig)


def bc3(ap2d):
    return AP(tensor=ap2d.tensor, offset=ap2d.offset, ap=[list(ap2d.ap[0]), [0, 3], list(ap2d.ap[1])])


class Attn:
    def __init__(self, kb):
        self.kb = kb
        self.pT = [kb.sb("pT", [128, 384], BF16) for _ in range(4)]
        self.i = 0
        self.sb_list = [0, 1, 6]
        self.sb_i = 0
        self.pending = []

    def next_sbank(self):
        b = self.sb_list[self.sb_i % 3]
        self.sb_i += 1
        return b

    def unit(self, s_terms, near_terms, accs, accb, v_ap, vbufs, first):
        kb = self.kb
        nc, P = kb.nc, kb.P
        sbk = self.next_sbank()
        ps, psb = kb.ps[sbk], kb.psb[sbk]
        n = len(s_terms)
        for t, (l, r, bufs) in enumerate(s_terms):
            mm(kb, ps[:, 0:384], l, r, t == 0, bufs, [psb], sig=(t == n - 1 and not near_terms))
        if near_terms:
            l, r, bufs = near_terms
            mm(kb, ps[:, 0:384].rearrange("p (a b) -> p a b", b=128), l, r, False, bufs, [psb], sig=True)
        pT, pTb = self.pT[self.i % 4]
        self.i += 1
        P.op("act", lambda: nc.scalar.activation(out=pT[:], in_=ps[:, 0:384], func=AF.Exp), reads=[psb], writes=[pTb])
        self.pending.append((pT, pTb, accs, accb, v_ap, vbufs, first))
        if len(self.pending) > 2:
            self._pv(*self.pending.pop(0))

    def _pv(self, pT, pTb, accs, accb, v_ap, vbufs, first):
        kb = self.kb
        for hh in range(3):
            mm(kb, accs[hh], pT[:, hh * 128:(hh + 1) * 128], v_ap, first[hh], [pTb] + vbufs, [accb[hh]], sig=(hh == 2))

    def flush(self):
        while self.pending:
            self._pv(*self.pending.pop(0))


def build_B(layer, kb=None):
    kb = kb or K("B")
    nc, P = kb.nc, kb.P
    x_d = kb.din("x", [TOK, D])
    id_d = kb.din("identb", [128, 128], BF16)
    an_d = kb.din("antib", [128, 128], BF16)
    relb_d = kb.din("rel_bias", [32, 12])
    oh_s_d = kb.din("oh_s", [33, Y_SLC])
    oh_w_d = kb.din("oh_w", [33, Y_WIN])
    oh_c_d = kb.din("oh_c", [33, Y_CMP])
    qT_d = kb.din("qT2", [128, 6, TOK], BF16)
    qm_d = kb.din("qmT", [64, 4, TOK], BF16)
    gates_d = kb.din("gates", [TOK, 36])
    ks_d = kb.din("ksT2", [128, T], BF16)
    kw_d = kb.din("kwT2", [128, T], BF16)
    kcr_d = kb.din("kcrT2", [128, T], BF16)
    vcr_d = kb.din("vcrT2", [128, T], BF16)
    vs_d = kb.din("vs", [T, 128])
    vw_d = kb.din("vw", [T, 128])
    ew_d = kb.din("ew", [128, T], BF16)
    ov_d = kb.din("ovl", [128, 4, 128], BF16)
    fb_d = kb.din("fb", [NT, 128, 128])
    w1k_d = kb.din("w1k", [2048, 256]); b1k_d = kb.din("b1k", [256, 1]); w2k_d = kb.din("w2k", [256, 64]); b2k_d = kb.din("b2k", [64, 1])
    w1v_d = kb.din("w1v", [2048, 256]); b1v_d = kb.din("b1v", [256, 1]); w2v_d = kb.din("w2v", [256, 64]); b2v_d = kb.din("b2v", [1, 64])
    posk_d = kb.din("posk", [32, 64]); posv_d = kb.din("posv", [32, 64])
    mem_d = kb.din("mem", [256, D]); gmem_d = kb.din("g_mem", [1, D]); wkv_d = kb.din("w_mem_kv", [D, 512])
    wout_d = kb.din("w_out", [D, D])
    hmid_d = kb.dout("hmid", [TOK, D])

    P.dma("sp", kb.ident[:], id_d, writes=[kb.identb])
    P.dma("sp", kb.anti[:], an_d, writes=[kb.antib])
    kb.mk_eps()
    with kb.scope():
        tabs = build_tables(kb, relb_d, [oh_s_d, oh_w_d, oh_c_d], [("s", Y_SLC), ("w", Y_WIN), ("c", Y_CMP)])
    band_s, band_sb = load_band(kb, tabs[0][0], tabs[0][1], Y_SLC, 2048, 1, "band_s")
    band_w, band_wb = load_band(kb, tabs[1][0], tabs[1][1], Y_WIN, 1024, 1, "band_w")
    bandc = [kb.sb("bandc", [128, 12, 128], BF16) for _ in range(4)]
    bandc_i = 0

    ksT, ksTb = kb.sb("ksT", [128, T], BF16)
    P.dma("sp", ksT[:, 0:4096], ks_d[:, 0:4096], writes=[ksTb])
    P.dma("pool", ksT[:, 4096:T], ks_d[:, 4096:T], writes=[ksTb])
    ew, ewb = kb.sb("ew", [128, T], BF16)
    P.dma("sp", ew[:, 0:4096], ew_d[:, 0:4096], writes=[ewb])
    P.dma("pool", ew[:, 4096:T], ew_d[:, 4096:T], writes=[ewb])
    vs1, vs1b = kb.sb("vs1", [128, 64, 2, 65], BF16)
    P.op("pool", lambda: nc.gpsimd.memset(vs1[:], 1.0), writes=[vs1b])
    with kb.scope():
        vst = [kb.sb("vst", [128, 8, 128], F32) for _ in range(2)]
        for i in range(8):
            st, stb = vst[i % 2]
            P.dma(kb.dq(), st[:], vs_d[i * 1024:(i + 1) * 1024, :].rearrange("(a p) c -> p a c", p=128), writes=[stb])
            P.op("pool", lambda: nc.gpsimd.tensor_copy(out=vs1[:, i * 8:(i + 1) * 8, :, 0:64], in_=st[:].rearrange("p a (g d) -> p a g d", g=2)),
                 reads=[stb], writes=[vs1b])
    RC, RCb = kb.sb("RC", [128, 4, 2, 193], BF16)
    P.op("pool", lambda: nc.gpsimd.memset(RC[:], 1.0), writes=[RCb])
    ovt, ovtb = kb.sb("ovt", [128, 4, 128], BF16)
    P.dma("sp", ovt[:], ov_d, writes=[ovtb])
    for g in range(2):
        P.op("pool", lambda: nc.gpsimd.tensor_copy(out=RC[:, :, g, 0:128], in_=ovt[:]), reads=[ovtb], writes=[RCb])
    kcT, kcTb = kb.sb("kcT", [128, 512], BF16)

    with ExitStack() as es2:
        def tmp(name, shape, dt):
            kb.n += 1
            return es2.enter_context(nc.sbuf_tensor("%s_%d" % (name, kb.n), shape, dt)), Buf(name)
        rawT, rawTb = tmp("rawT", [128, T + 32], BF16)
        w1, w1b = tmp("w1", [128, 32, 256], BF16)
        w1st, w1stb = tmp("w1st", [128, 8, 256], F32)
        w2, w2b = tmp("w2", [128, 2, 128], BF16)
        w2st, w2stb = tmp("w2st", [128, 2, 64], F32)
        h1T, h1Tb = tmp("h1T", [128, 2, 512], BF16)
        u_, ub = tmp("u", [128, 512], F32)
        t1, t1b = tmp("t1", [128, 512], F32)
        t2, t2b = tmp("t2", [128, 512], F32)
        cb, cbb = tmp("cb", [128, 2], F32)
        b1t, b1tb = tmp("b1t", [128, 2], F32)
        b2d, b2db = tmp("b2d", [128, 1], F32)
        b2r, b2rb = tmp("b2r", [128, 64], F32)
        pst, pstb = tmp("pst", [32, 64], F32)
        psb16, psb16b = tmp("psb16", [32, 64], BF16)
        posT, posTb = tmp("posT", [64, 1, 32], BF16)
        for kv in range(2):
            raw_d = kcr_d if kv == 0 else vcr_d
            w1_d = w1k_d if kv == 0 else w1v_d
            b1_d = b1k_d if kv == 0 else b1v_d
            w2_d = w2k_d if kv == 0 else w2v_d
            pos_d = posk_d if kv == 0 else posv_d
            P.op("dve", lambda: nc.vector.memset(rawT[:, T:T + 32], 0.0), writes=[rawTb])
            P.dma("sp", rawT[:, 0:4096], raw_d[:, 0:4096], writes=[rawTb])
            P.dma("pool", rawT[:, 4096:T], raw_d[:, 4096:T], writes=[rawTb])
            for q4 in range(4):
                for half in range(2):
                    P.dma(kb.dq(), w1st[half * 64:(half + 1) * 64, :, :], w1_d[q4 * 512:(q4 + 1) * 512, :].rearrange("(l d) h -> d l h", d=64), writes=[w1stb])
                P.op("dve", lambda: nc.vector.tensor_copy(out=w1[:, q4 * 8:(q4 + 1) * 8, :], in_=w1st[:]), reads=[w1stb], writes=[w1b])
            P.dma("sp", w2st[:], w2_d.rearrange("(c p) d -> p c d", p=128), writes=[w2stb])
            for half in range(2):
                P.op("dve", lambda: nc.vector.tensor_copy(out=w2[:, :, half * 64:(half + 1) * 64], in_=w2st[:]), reads=[w2stb], writes=[w2b])
            P.dma("sp", b1t[:], b1_d.rearrange("(c p) o -> p (c o)", p=128), writes=[b1tb])
            P.dma("sp", pst[:], pos_d, writes=[pstb])
            P.op("dve", lambda: nc.vector.tensor_copy(out=psb16[:], in_=pst[:]), reads=[pstb], writes=[psb16b])
            kb.transpose_to([psb16[:, :]], posT, posTb, [psb16b], np_in=32)
            for hc in range(2):
                ps, psb = kb.ps[2], kb.psb[2]
                for l in range(32):
                    mm(kb, ps[:, 0:1], w1[0:64, l, hc * 128:(hc + 1) * 128], posT[:, 0, l:l + 1], l == 0, [w1b, posTb], [psb], sig=(l == 31))
                P.op("dve", lambda: nc.vector.tensor_tensor(out=cb[:, hc:hc + 1], in0=ps[:, 0:1], in1=b1t[:, hc:hc + 1], op=ALU.add), reads=[psb, b1tb], writes=[cbb])
            if kv == 0:
                for half in range(2):
                    P.dma("sp", b2d[half * 64:(half + 1) * 64, :], b2k_d, writes=[b2db])
            else:
                src = AP(tensor=b2v_d.tensor, offset=b2v_d.offset, ap=[[0, 128], [1, 64]])
                P.dma("sp", b2r[:], src, writes=[b2rb])
            for g in range(2):
                gs = slice(g * 64, (g + 1) * 64)
                for hc in range(2):
                    ps, psb = kb.ps[2 + hc], kb.psb[2 + hc]
                    for l in range(32):
                        rhs = AP(tensor=rawT.tensor if hasattr(rawT, "tensor") else rawT[:].tensor, offset=rawT[gs, l:l + 1].offset,
                                 ap=[list(rawT[gs, :].ap[0]), [16, 512]])
                        mm(kb, ps[:, :], w1[gs, l, hc * 128:(hc + 1) * 128], rhs, l == 0, [w1b, rawTb], [psb], sig=(l == 31))
                    P.op("act", lambda: nc.scalar.activation(out=u_[:], in_=ps[:, :], func=AF.Identity, bias=cb[:, hc:hc + 1]), reads=[psb, cbb], writes=[ub])
                    P.op("dve", lambda: nc.vector.tensor_tensor(out=t1[:], in0=u_[:], in1=u_[:], op=ALU.mult), reads=[ub], writes=[t1b])
                    P.op("dve", lambda: nc.vector.tensor_scalar(out=t1[:], in0=t1[:], scalar1=0.044715, scalar2=1.0, op0=ALU.mult, op1=ALU.add), reads=[t1b], writes=[t1b])
                    P.op("dve", lambda: nc.vector.tensor_tensor(out=t1[:], in0=t1[:], in1=u_[:], op=ALU.mult), reads=[t1b, ub], writes=[t1b])
                    P.op("act", lambda: nc.scalar.activation(out=t2[:], in_=t1[:], func=AF.Tanh, scale=0.7978845608028654), reads=[t1b], writes=[t2b])
                    P.op("dve", lambda: nc.vector.tensor_scalar(out=t2[:], in0=t2[:], scalar1=0.5, scalar2=0.5, op0=ALU.mult, op1=ALU.add), reads=[t2b], writes=[t2b])
                    P.op("dve", lambda: nc.vector.tensor_tensor(out=h1T[:, hc, :], in0=t2[:], in1=u_[:], op=ALU.mult), reads=[t2b, ub], writes=[h1Tb])
                if kv == 0:
                    ps, psb = kb.ps[4], kb.psb[4]
                    for hc in range(2):
                        mm(kb, ps[:, :], w2[:, hc, :], h1T[:, hc, :], hc == 0, [w2b, h1Tb], [psb], sig=(hc == 1))
                    P.op("act", lambda: nc.scalar.activation(out=kcT[gs, :], in_=ps[gs, :], func=AF.Identity, bias=b2d[gs, 0:1]), reads=[psb, b2db], writes=[kcTb])
                else:
                    for j in range(4):
                        ps, psb = kb.ps[4], kb.psb[4]
                        for hc in range(2):
                            mm(kb, ps[:, 0:64], h1T[:, hc, j * 128:(j + 1) * 128], w2[:, hc, 0:64], hc == 0, [w2b, h1Tb], [psb], sig=(hc == 1))
                        P.op("dve", lambda: nc.vector.tensor_tensor(out=RC[:, j, g, 129:193], in0=ps[:, 0:64], in1=b2r[:], op=ALU.add), reads=[psb, b2rb], writes=[RCb])

    P.barrier()
    kmT, kmTb = kb.sb("kmT", [64, 4, 256], BF16)
    vm1, vm1b = kb.sb("vm1", [128, 2, 4, 65], BF16)
    P.op("pool", lambda: nc.gpsimd.memset(vm1[:], 1.0), writes=[vm1b])
    gm, gmb = kb.bcast_row(gmem_d, D, "gmem")
    wkv, wkvb = kb.sb("wkv", [128, 8, 512], BF16)
    with ExitStack() as es2:
        def tmp(name, shape, dt):
            kb.n += 1
            return es2.enter_context(nc.sbuf_tensor("%s_%d" % (name, kb.n), shape, dt)), Buf(name)
        mt, mtb = tmp("mt", [128, D], F32)
        mn, mnb = tmp("mn", [128, D], BF16)
        mnT, mnTb = tmp("mnT", [128, 8, 256], BF16)
        scr, scrb = tmp("mscr", [128, 4], F32)
        wst = [tmp("wst2", [128, 512], F32) for _ in range(2)]
        for k in range(8):
            st, stb = wst[k % 2]
            P.dma(kb.dq(), st[:], wkv_d[k * 128:(k + 1) * 128, :], writes=[stb])
            P.op("dve", lambda: nc.vector.tensor_copy(out=wkv[:, k, :], in_=st[:]), reads=[stb], writes=[wkvb])
        for tl in range(2):
            P.dma("sp", mt[:], mem_d[tl * 128:(tl + 1) * 128, :], writes=[mtb])
            kb.rmsnorm(mt[:], mtb, gm[:], gmb, mn[:], mnb, D, scr, scrb)
            pt = kb.pt[kb.pti % 2]; ptb = kb.ptb[kb.pti % 2]; kb.pti += 1
            for j in range(8):
                P.op("pe", lambda j=j: nc.tensor.transpose(pt[:, j * 128:(j + 1) * 128], mn[:, j * 128:(j + 1) * 128], kb.ident[:]),
                     reads=[mnb, kb.identb], writes=[ptb], sig=(j == 7))
            P.op("dve", lambda: nc.vector.tensor_copy(out=mnT[:, :, tl * 128:(tl + 1) * 128], in_=pt[:, :].rearrange("p (a b) -> p a b", b=128)), reads=[ptb], writes=[mnTb])
        for h in range(4):
            ps, psb = kb.ps[2], kb.psb[2]
            for k in range(8):
                mm(kb, ps[0:64, 0:256], wkv[:, k, h * 64:(h + 1) * 64], mnT[:, k, :], k == 0, [wkvb, mnTb], [psb], sig=(k == 7))
            P.op("dve", lambda: nc.vector.tensor_copy(out=kmT[:, h, :], in_=ps[0:64, 0:256]), reads=[psb], writes=[kmTb])
        for tl in range(2):
            ps, psb = kb.ps[3], kb.psb[3]
            for k in range(8):
                mm(kb, ps[:, 0:256], mnT[:, k, tl * 128:(tl + 1) * 128], wkv[:, k, 256:512], k == 0, [wkvb, mnTb], [psb], sig=(k == 7))
            P.op("dve", lambda: nc.vector.tensor_copy(out=vm1[:, tl, :, 0:64], in_=ps[:, 0:256].rearrange("p (h d) -> p h d", d=64)), reads=[psb], writes=[vm1b])
    P.barrier()
    wout, woutb = kb.sb("wout", [128, 8, D], BF16)
    with kb.scope():
        wst = [kb.sb("wst3", [128, 1024], F32) for _ in range(2)]
        for k in range(8):
            st, stb = wst[k % 2]
            P.dma(kb.dq(), st[:], wout_d[k * 128:(k + 1) * 128, :], writes=[stb])
            P.op("dve", lambda: nc.vector.tensor_copy(out=wout[:, k, :], in_=st[:]), reads=[stb], writes=[woutb])

    at = Attn(kb)
    qs = [kb.sb("qs", [128, 6, 128], BF16) for _ in range(2)]
    qms = [kb.sb("qms", [64, 4, 128], BF16) for _ in range(2)]
    gts = [kb.sb("gts", [128, 12, 3], F32) for _ in range(2)]
    kws = [kb.sb("kws", [128, 1024], BF16) for _ in range(2)]
    vws = [kb.sb("vws", [128, 8, 2, 65], BF16) for _ in range(2)]
    vwst = [kb.sb("vwst", [128, 8, 128], F32)] * 2
    for (t_, b_) in vws:
        P.op("pool", lambda: nc.gpsimd.memset(t_[:], 1.0), writes=[b_])
    fbs = [kb.sb("fbs", [128, 128], F32) for _ in range(2)]
    xts = [kb.sb("xts", [128, D], F32)] * 2
    cat, catb = kb.sb("cat", [128, D], F32)
    catbf, catbfb = kb.sb("catbf", [128, D], BF16)
    catT, catTb = kb.sb("catT", [128, 8, 128], BF16)
    pslc, pslcb = kb.sb("pslc", [128, 2, 128], F32)
    wk, wkb = kb.sb("wk", [128, 128], F32)
    m8, m8b = kb.sb("m8", [128, 16], F32)
    sbf, sbfb = kb.sb("sbf", [128, 128], BF16)
    sbT, sbTb = kb.sb("sbT", [128, 2, 128], BF16)
    sm, smb = kb.sb("sm", [128, 16], F32)
    pm, pmb = kb.sb("pm", [128, 256], BF16)
    accbank = 2

    def next_acc():
        nonlocal accbank
        b = accbank
        accbank = 2 + (accbank - 2 + 1) % 4
        return b

    for m in range(NT):
        q_, qb_ = qs[m % 2]
        qm_, qmb_ = qms[m % 2]
        gt_, gtb_ = gts[m % 2]
        kw_, kwb_ = kws[m % 2]
        vw_, vwb_ = vws[m % 2]
        vwst_, vwstb_ = vwst[m % 2]
        fb_, fbb_ = fbs[m % 2]
        xt_, xtb_ = xts[m % 2]
        tsl = slice(m * 128, (m + 1) * 128)
        P.dma("sp", q_[:], qT_d[:, :, tsl], writes=[qb_])
        P.dma("sp", qm_[:], qm_d[:, :, tsl], writes=[qmb_])
        P.dma("sp", gt_[:], gates_d[tsl, :].rearrange("p (h b) -> p h b", b=3), writes=[gtb_])
        P.dma("sp", fb_[:], fb_d[m], writes=[fbb_])
        P.dma("pool", xt_[:], x_d[tsl, :], writes=[xtb_])
        kt0 = max(0, 4 * m - 4)
        nkw = 4 * m + 4 - kt0
        P.dma("pool", kw_[:, 0:nkw * 128], kw_d[:, kt0 * 128:(4 * m + 4) * 128], writes=[kwb_])
        P.dma("sp", vwst_[:, 0:nkw, :], vw_d[kt0 * 128:(4 * m + 4) * 128, :].rearrange("(a p) c -> p a c", p=128), writes=[vwstb_])
        P.op("pool", lambda: nc.gpsimd.tensor_copy(out=vw_[:, 0:nkw, :, 0:64], in_=vwst_[:, 0:nkw, :].rearrange("p a (g d) -> p a g d", g=2)),
             reads=[vwstb_], writes=[vwb_])
        cband = {}
        for j in range(4):
            v = m - 4 * j
            if 0 <= v <= 6:
                dst = bandc[bandc_i % 4]
                bandc_i += 1
                load_band(kb, tabs[2][0], tabs[2][1], Y_CMP, 128, 16, "bc", zoff=512 * v, dst=dst)
                cband[j] = dst
        for g in range(2):
            gs = slice(g * 64, (g + 1) * 64)
            for tr in range(2):
                bA, bB = next_acc(), next_acc()
                accs = [kb.ps[bA][:, 0:193], kb.ps[bA][:, 193:386], kb.ps[bB][:, 0:193]]
                accb = [kb.psb[bA], kb.psb[bA], kb.psb[bB]]
                js = [j for j in range(4) if m - 4 * j >= 0]
                for ji, j in enumerate(js):
                    s_terms = [(kcT[gs, j * 128:(j + 1) * 128], q_[gs, 3 * tr:3 * tr + 3, :], [kcTb, qb_])]
                    near = None
                    if j in cband:
                        bt, btb = cband[j]
                        near = (kb.anti[:], bt[:, 6 * g + 3 * tr:6 * g + 3 * tr + 3, :], [kb.antib, btb])
                    at.unit(s_terms, near, accs, accb, RC[:, j, g, :], [RCb], [ji == 0, False, ji == 0])
                at.flush()
                for hh in range(3):
                    h = 6 * g + 3 * tr + hh
                    U = accs[hh]
                    P.op("dve", lambda: nc.vector.tensor_scalar(out=sm[:, 0:1], in0=U[:, 128:129], scalar1=1e-30, scalar2=None, op0=ALU.max), reads=[accb[hh]], writes=[smb])
                    P.op("dve", lambda: nc.vector.reciprocal(out=sm[:, 1:2], in_=sm[:, 0:1]), reads=[smb], writes=[smb])
                    if tr == 0 and hh == 0:
                        P.op("dve", lambda: nc.vector.tensor_scalar(out=pslc[:, g, :], in0=U[:, 0:128], scalar1=sm[:, 1:2], scalar2=None, op0=ALU.mult), reads=[accb[hh], smb], writes=[pslcb])
                    else:
                        P.op("dve", lambda: nc.vector.scalar_tensor_tensor(out=pslc[:, g, :], in0=U[:, 0:128], scalar=sm[:, 1:2], in1=pslc[:, g, :], op0=ALU.mult, op1=ALU.add), reads=[accb[hh], smb, pslcb], writes=[pslcb])
                    P.op("dve", lambda: nc.vector.tensor_tensor(out=sm[:, 2:3], in0=sm[:, 1:2], in1=gt_[:, h, 0:1], op=ALU.mult), reads=[smb, gtb_], writes=[smb])
                    P.op("dve", lambda: nc.vector.tensor_scalar(out=cat[:, h * 64:(h + 1) * 64], in0=U[:, 129:193], scalar1=sm[:, 2:3], scalar2=None, op0=ALU.mult), reads=[accb[hh], smb], writes=[catb])
            P.op("dve", lambda: nc.vector.tensor_tensor(out=wk[:], in0=pslc[:, g, :], in1=fb_[:], op=ALU.add), reads=[pslcb, fbb_], writes=[wkb])
            P.op("dve", lambda: nc.vector.max(out=m8[:, 0:8], in_=wk[:]), reads=[wkb], writes=[m8b])
            P.op("dve", lambda: nc.vector.match_replace(out=pslc[:, g, :], in_to_replace=m8[:, 0:8], in_values=wk[:], imm_value=-3e30), reads=[wkb, m8b], writes=[pslcb])
            P.op("dve", lambda: nc.vector.max(out=m8[:, 8:16], in_=pslc[:, g, :]), reads=[pslcb], writes=[m8b])
            P.op("dve", lambda: nc.vector.tensor_scalar(out=wk[:], in0=wk[:], scalar1=m8[:, 15:16], scalar2=-NEGB, op0=ALU.is_ge, op1=ALU.mult), reads=[wkb, m8b], writes=[wkb])
            P.op("dve", lambda: nc.vector.tensor_scalar(out=sbf[:], in0=wk[:], scalar1=NEGB, scalar2=None, op0=ALU.add), reads=[wkb], writes=[sbfb])
            pt = kb.pt[kb.pti % 2]; ptb = kb.ptb[kb.pti % 2]; kb.pti += 1
            P.op("pe", lambda: nc.tensor.transpose(pt[:, 0:128], sbf[:], kb.ident[:]), reads=[sbfb, kb.identb], writes=[ptb])
            P.op("dve", lambda: nc.vector.tensor_copy(out=sbT[:, g, :], in_=pt[:, 0:128]), reads=[ptb], writes=[sbTb])
        for br in range(2):
            for g in range(2):
                gs = slice(g * 64, (g + 1) * 64)
                for tr in range(2):
                    bA = next_acc()
                    accs = [kb.ps[bA][:, 65 * hh:65 * hh + 65] for hh in range(3)]
                    accb = [kb.psb[bA]] * 3
                    kts = list(range(0, 4 * m + 4)) if br == 0 else list(range(kt0, 4 * m + 4))
                    for ki, kt in enumerate(kts):
                        u = kt - 4 * m
                        if br == 0:
                            s_terms = [(ksT[gs, kt * 128:(kt + 1) * 128], q_[gs, 3 * tr:3 * tr + 3, :], [ksTb, qb_]),
                                       (ew[:, kt * 128:(kt + 1) * 128], bc3(sbT[:, g, :]), [ewb, sbTb])]
                            near = None
                            if u >= -12:
                                z0 = 128 * (3 - u)
                                near = (kb.anti[:], band_s[:, 6 * g + 3 * tr:6 * g + 3 * tr + 3, z0:z0 + 128], [kb.antib, band_sb])
                            v_ap, vb_ = vs1[:, kt, g, :], [vs1b]
                        else:
                            kk = kt - kt0
                            s_terms = [(kw_[gs, kk * 128:(kk + 1) * 128], q_[gs, 3 * tr:3 * tr + 3, :], [kwb_, qb_])]
                            z0 = 128 * (3 - u)
                            near = (kb.anti[:], band_w[:, 6 * g + 3 * tr:6 * g + 3 * tr + 3, z0:z0 + 128], [kb.antib, band_wb])
                            v_ap, vb_ = vw_[:, kk, g, :], [vwb_]
                        at.unit(s_terms, near, accs, accb, v_ap, vb_, [ki == 0, False, False])
                    at.flush()
                    for hh in range(3):
                        h = 6 * g + 3 * tr + hh
                        O = accs[hh]
                        P.op("dve", lambda: nc.vector.tensor_scalar(out=sm[:, 0:1], in0=O[:, 64:65], scalar1=1e-30, scalar2=None, op0=ALU.max), reads=[accb[hh]], writes=[smb])
                        P.op("dve", lambda: nc.vector.reciprocal(out=sm[:, 1:2], in_=sm[:, 0:1]), reads=[smb], writes=[smb])
                        P.op("dve", lambda: nc.vector.tensor_tensor(out=sm[:, 2:3], in0=sm[:, 1:2], in1=gt_[:, h, 1 + br:2 + br], op=ALU.mult), reads=[smb, gtb_], writes=[smb])
                        P.op("dve", lambda: nc.vector.scalar_tensor_tensor(out=cat[:, h * 64:(h + 1) * 64], in0=O[:, 0:64], scalar=sm[:, 2:3], in1=cat[:, h * 64:(h + 1) * 64], op0=ALU.mult, op1=ALU.add),
                             reads=[accb[hh], smb, catb], writes=[catb])
        for h in range(4):
            sbk = at.next_sbank()
            ps, psb = kb.ps[sbk], kb.psb[sbk]
            for tl in range(2):
                mm(kb, ps[:, tl * 128:(tl + 1) * 128], kmT[:, h, tl * 128:(tl + 1) * 128], qm_[:, h, :], tl == 0, [kmTb, qmb_], [psb], sig=(tl == 1))
            P.op("act", lambda: nc.scalar.activation(out=pm[:], in_=ps[:, 0:256], func=AF.Exp), reads=[psb], writes=[pmb])
            bA = next_acc()
            O = kb.ps[bA][:, 0:65]
            for tl in range(2):
                mm(kb, O, pm[:, tl * 128:(tl + 1) * 128], vm1[:, tl, h, :], tl == 0, [pmb, vm1b], [kb.psb[bA]], sig=(tl == 1))
            P.op("dve", lambda: nc.vector.reciprocal(out=sm[:, 4:5], in_=O[:, 64:65]), reads=[kb.psb[bA]], writes=[smb])
            P.op("dve", lambda: nc.vector.tensor_scalar(out=cat[:, 768 + h * 64:768 + (h + 1) * 64], in0=O[:, 0:64], scalar1=sm[:, 4:5], scalar2=None, op0=ALU.mult), reads=[kb.psb[bA], smb], writes=[catb])
        P.op("act", lambda: nc.scalar.copy(out=catbf[:], in_=cat[:]), reads=[catb], writes=[catbfb])
        pt = kb.pt[kb.pti % 2]; ptb = kb.ptb[kb.pti % 2]; kb.pti += 1
        for j in range(8):
            P.op("pe", lambda j=j: nc.tensor.transpose(pt[:, j * 128:(j + 1) * 128], catbf[:, j * 128:(j + 1) * 128], kb.ident[:]),
                 reads=[catbfb, kb.identb], writes=[ptb], sig=(j == 7))
        P.op("dve", lambda: nc.vector.tensor_copy(out=catT[:], in_=pt[:, :].rearrange("p (a b) -> p a b", b=128)), reads=[ptb], writes=[catTb])
        for half in range(2):
            bA = next_acc()
            ps, psb = kb.ps[bA], kb.psb[bA]
            for k in range(8):
                mm(kb, ps[:, :], catT[:, k, :], wout[:, k, half * 512:(half + 1) * 512], k == 0, [catTb, woutb], [psb], sig=(k == 7))
            P.op("dve", lambda: nc.vector.tensor_tensor(out=xt_[:, half * 512:(half + 1) * 512], in0=ps[:, :], in1=xt_[:, half * 512:(half + 1) * 512], op=ALU.add), reads=[psb, xtb_], writes=[xtb_])
        P.dma("sp", hmid_d[tsl, :], xt_[:], reads=[xtb_], final=True)
    return kb


def gather_seq(arrs, axis):
    shp = list(arrs[0].shape)
    shp[axis] = T
    out = np.zeros(shp, arrs[0].dtype)
    for c in range(4):
        idx = [slice(None)] * len(shp)
        idx[axis] = core_tokens(c)
        out[tuple(idx)] = arrs[c]
    return out


def consts_B(c):
    ident = np.eye(128, dtype=np.float32)
    oh_s, oh_w, oh_c = host_tables(c)
    ew = (np.arange(T)[None, :] // 64 == np.arange(128)[:, None]).astype(NPBF)
    n = np.arange(512)
    cs, ce = n * 16, n * 16 + 31
    ss = np.arange(128) * 64
    ov = ((cs[:, None] <= ss[None, :] + 63) & (ce[:, None] >= ss[None, :])).astype(np.float32)
    ov[511] = 0
    ovl = np.ascontiguousarray(ov.reshape(4, 128, 128).transpose(1, 0, 2)).astype(NPBF)
    fb = np.zeros((NT, 128, 128), np.float32)
    blk = np.arange(128)[None, :]
    for m in range(NT):
        t = 128 * (4 * m + c) + np.arange(128)[:, None]
        cur = t // 64
        forced = (blk == 0) | (blk == cur) | (blk == cur - 1)
        adm = (blk * 64) <= t
        fb[m] = np.where(adm, 1e4 * forced, -1e30)
    return {"identb": ident.astype(NPBF), "antib": np.ascontiguousarray(ident[::-1]).astype(NPBF),
            "oh_s": oh_s, "oh_w": oh_w, "oh_c": oh_c, "ew": ew, "ovl": ovl, "fb": fb}


def to2(fm, lo):
    return np.ascontiguousarray(np.concatenate([fm[:, lo, :], fm[:, lo + 1, :]], axis=0))


def run_B(inputs, resA):
    kb = build_B(0)
    maps = []
    for core in range(8):
        b, c = core // 4, core % 4
        grp = [resA[4 * b + cc] for cc in range(4)]
        fms = [np.asarray(r["fmT"]) for r in grp]
        tms = [np.asarray(r["tm"]) for r in grp]
        fm = fms[c]
        mp = consts_B(c)
        mp["x"] = np.ascontiguousarray(inputs["x"][b][core_tokens(c)])
        mp["rel_bias"] = inputs["rel_bias"]
        q = fm[:, 0:12, :]
        mp["qT2"] = np.ascontiguousarray(np.concatenate([q[:, 0:6, :], q[:, 6:12, :]], axis=0))
        mp["qmT"] = np.ascontiguousarray(fm[:, 20:24, :])
        mp["gates"] = np.ascontiguousarray(tms[c][:, 256:292])
        mp["kcrT2"] = gather_seq([to2(f, 12) for f in fms], 1)
        mp["vcrT2"] = gather_seq([to2(f, 14) for f in fms], 1)
        mp["ksT2"] = gather_seq([to2(f, 16) for f in fms], 1)
        mp["kwT2"] = gather_seq([to2(f, 18) for f in fms], 1)
        mp["vs"] = gather_seq([np.ascontiguousarray(t_[:, 0:128]) for t_ in tms], 0)
        mp["vw"] = gather_seq([np.ascontiguousarray(t_[:, 128:256]) for t_ in tms], 0)
        mp["w1k"] = inputs["nsa_cmp_k_w1"][0]; mp["b1k"] = inputs["nsa_cmp_k_b1"][0].reshape(256, 1)
        mp["w2k"] = inputs["nsa_cmp_k_w2"][0]; mp["b2k"] = inputs["nsa_cmp_k_b2"][0].reshape(64, 1)
        mp["w1v"] = inputs["nsa_cmp_v_w1"][0]; mp["b1v"] = inputs["nsa_cmp_v_b1"][0].reshape(256, 1)
        mp["w2v"] = inputs["nsa_cmp_v_w2"][0]; mp["b2v"] = inputs["nsa_cmp_v_b2"][0].reshape(1, 64)
        mp["posk"] = inputs["nsa_cmp_pos_k"][0]; mp["posv"] = inputs["nsa_cmp_pos_v"][0]
        mp["mem"] = inputs["mem"][b]; mp["g_mem"] = inputs["norm_mem"][0:1]; mp["w_mem_kv"] = inputs["w_mem_kv"][0]
        mp["w_out"] = inputs["w_out"][0]
        maps.append(mp)
    return kb.run(maps)


def scatter_tokens(per_core, key):
    out = np.zeros((2, T, D), np.float32)
    for core in range(8):
        b, c = core // 4, core % 4
        out[b][core_tokens(c)] = np.asarray(per_core[core][key])
    return out


def kernel_unfused(**inputs):
    inputs = {k: np.asarray(v) for k, v in inputs.items()}
    resA = run_A(inputs)
    resB = run_B(inputs, resA)
    resF = run_F(inputs, [r["hmid"] for r in resB], 0, False)
    resC = run_C(inputs, resF)
    resG = run_F(inputs, [r["hmid"] for r in resC], 1, True)
    return scatter_tokens(resG, "h")


def build_F(final, nproj, kb=None):
    kb = kb or K("F")
    nc, P = kb.nc, kb.P
    hm_d = kb.din("hmid", [TOK, D])
    id_d = kb.din("identb", [128, 128], BF16)
    g_d = kb.din("g_ffn", [1, D])
    wg_d = kb.din("wg", [D, DFF]); wu_d = kb.din("wu", [D, DFF]); wd_d = kb.din("wd", [DFF, D])
    g2_d = kb.din("g2", [1, D])
    h_d = kb.dout("h", [TOK, D])
    if not final:
        win_d = kb.din("w_in2", [D, nproj])
        pr_d = kb.dout("pr", [TOK, nproj])
    P.dma("sp", kb.ident[:], id_d, writes=[kb.identb])
    kb.mk_eps()
    g, gb = kb.bcast_row(g_d, D, "gffn")
    g2, g2b = kb.bcast_row(g2_d, D, "g2")
    H, Hb = kb.sb("H", [128, NT, D], F32)
    Hbs = [Buf("H%d" % i) for i in range(NT)]
    hnT, hnTb = kb.sb("hnT", [128, 8, TOK], BF16)
    hn, hnb = kb.sb("hn", [128, D], BF16)
    scr, scrb = kb.sb("scr", [128, 4], F32)

    def norm_T(gt, gtb):
        for ti in range(NT):
            kb.rmsnorm(H[:, ti, :], Hbs[ti], gt[:], gtb, hn[:], hnb, D, scr, scrb)
            pt = kb.pt[kb.pti % 2]; ptb = kb.ptb[kb.pti % 2]; kb.pti += 1
            for j in range(8):
                P.op("pe", lambda j=j: nc.tensor.transpose(pt[:, j * 128:(j + 1) * 128], hn[:, j * 128:(j + 1) * 128], kb.ident[:]),
                     reads=[hnb, kb.identb], writes=[ptb], sig=(j == 7))
            P.op("dve", lambda: nc.vector.tensor_copy(out=hnT[:, :, ti * 128:(ti + 1) * 128], in_=pt[:, :].rearrange("p (a b) -> p a b", b=128)), reads=[ptb], writes=[hnTb])

    for ti in range(NT):
        P.dma(kb.dq(), H[:, ti, :], hm_d[ti * 128:(ti + 1) * 128, :], writes=[Hbs[ti]])
    norm_T(g, gb)
    wgs, wgsb = kb.sb("wgs", [128, 8, 512], BF16)
    wus, wusb = kb.sb("wus", [128, 8, 512], BF16)
    wds, wdsb = kb.sb("wds", [128, 4, D], BF16)
    stg = [kb.sb("stg", [128, 1024], F32) for _ in range(2)]
    sg, sgb = kb.sb("sg", [128, 512], F32)
    ab, abb = kb.sb("ab", [128, 512], BF16)
    aT, aTb = kb.sb("aT", [128, 4, 128], BF16)
    si = 0
    for fg in range(6):
        c0 = fg * 512
        cw = min(512, DFF - c0)
        nch = cw // 128
        for (wsrc, wdst, wdstb) in ((wg_d, wgs, wgsb), (wu_d, wus, wusb)):
            for k in range(8):
                st, stb = stg[si % 2]; si += 1
                P.dma(kb.dq(), st[:, 0:cw], wsrc[k * 128:(k + 1) * 128, c0:c0 + cw], writes=[stb])
                kb.cast("pool" if k % 2 else "act", wdst[:, k, 0:cw], st[:, 0:cw], [stb], [wdstb])
        for ch in range(nch):
            st, stb = stg[si % 2]; si += 1
            P.dma(kb.dq(), st[:], wd_d[c0 + ch * 128:c0 + (ch + 1) * 128, :], writes=[stb])
            kb.cast("pool" if ch % 2 else "act", wds[:, ch, :], st[:], [stb], [wdsb])
        for ti in range(NT):
            tsl = slice(ti * 128, (ti + 1) * 128)
            pg, pgb = kb.ps[0], kb.psb[0]
            pu, pub = kb.ps[1], kb.psb[1]
            for k in range(8):
                mm(kb, pg[:, 0:cw], hnT[:, k, tsl], wgs[:, k, 0:cw], k == 0, [hnTb, wgsb], [pgb], sig=(k == 7))
            for k in range(8):
                mm(kb, pu[:, 0:cw], hnT[:, k, tsl], wus[:, k, 0:cw], k == 0, [hnTb, wusb], [pub], sig=(k == 7))
            P.op("act", lambda: nc.scalar.activation(out=sg[:, 0:cw], in_=pg[:, 0:cw], func=AF.Silu), reads=[pgb], writes=[sgb])
            P.op("dve", lambda: nc.vector.tensor_tensor(out=ab[:, 0:cw], in0=sg[:, 0:cw], in1=pu[:, 0:cw], op=ALU.mult), reads=[sgb, pub], writes=[abb])
            pt = kb.pt[kb.pti % 2]; ptb = kb.ptb[kb.pti % 2]; kb.pti += 1
            for j in range(nch):
                P.op("pe", lambda j=j: nc.tensor.transpose(pt[:, j * 128:(j + 1) * 128], ab[:, j * 128:(j + 1) * 128], kb.ident[:]),
                     reads=[abb, kb.identb], writes=[ptb], sig=(j == nch - 1))
            P.op("act", lambda: nc.scalar.copy(out=aT[:, 0:nch, :], in_=pt[:, 0:nch * 128].rearrange("p (a b) -> p a b", b=128)), reads=[ptb], writes=[aTb])
            for half in range(2):
                py, pyb = kb.ps[2 + half], kb.psb[2 + half]
                for ch in range(nch):
                    mm(kb, py[:, :], aT[:, ch, :], wds[:, ch, half * 512:(half + 1) * 512], ch == 0, [aTb, wdsb], [pyb], sig=(ch == nch - 1))
                P.op("dve", lambda: nc.vector.tensor_tensor(out=H[:, ti, half * 512:(half + 1) * 512], in0=py[:, :], in1=H[:, ti, half * 512:(half + 1) * 512], op=ALU.add),
                     reads=[pyb, Hbs[ti]], writes=[Hbs[ti]])
    if final:
        o, ob = kb.sb("o", [128, D], F32)
        for ti in range(NT):
            kb.rmsnorm(H[:, ti, :], Hbs[ti], g2[:], g2b, o[:], ob, D, scr, scrb)
            P.dma(kb.dq(), h_d[ti * 128:(ti + 1) * 128, :], o[:], reads=[ob], final=True)
    else:
        for ti in range(NT):
            P.dma(kb.dq(), h_d[ti * 128:(ti + 1) * 128, :], H[:, ti, :], reads=[Hbs[ti]], final=True)
        norm_T(g2, g2b)
        win, winb = kb.sb("win", [128, 8, nproj], BF16)
        for k in range(8):
            st, stb = stg[si % 2]; si += 1
            P.dma(kb.dq(), st[:, 0:nproj], win_d[k * 128:(k + 1) * 128, :], writes=[stb])
            kb.cast("pool" if k % 2 else "act", win[:, k, :], st[:, 0:nproj], [stb], [winb])
        pro, prob = kb.sb("pro", [128, nproj], F32)
        for ti in range(NT):
            tsl = slice(ti * 128, (ti + 1) * 128)
            o0 = 0
            bi = 0
            while o0 < nproj:
                n = min(512, nproj - o0)
                ps, psb = kb.ps[bi % 2], kb.psb[bi % 2]
                for k in range(8):
                    mm(kb, ps[:, 0:n], hnT[:, k, tsl], win[:, k, o0:o0 + n], k == 0, [hnTb, winb], [psb], sig=(k == 7))
                P.op("act", lambda: nc.scalar.copy(out=pro[:, o0:o0 + n], in_=ps[:, 0:n]), reads=[psb], writes=[prob])
                o0 += n
                bi += 1
            P.dma(kb.dq(), pr_d[tsl, :], pro[:], reads=[prob], final=True)
    return kb


def run_F(inputs, hmids, layer, final):
    kb = build_F(final, 712)
    ident = np.eye(128, dtype=np.float32).astype(NPBF)
    maps = []
    for core in range(8):
        mp = {"hmid": np.asarray(hmids[core]), "identb": ident, "g_ffn": inputs["norm_ffn"][layer:layer + 1],
              "wg": inputs["ffn_gate"][layer], "wu": inputs["ffn_up"][layer], "wd": inputs["ffn_down"][layer]}
        if final:
            mp["g2"] = inputs["norm_final"].reshape(1, D)
        else:
            mp["g2"] = inputs["norm_mix"][layer + 1:layer + 2]
            mp["w_in2"] = inputs["dsa_w_in"][0]
        maps.append(mp)
    return kb.run(maps)


NIT = 16


def mem_setup(kb, mem_d, gmem_d, wkv_d):
    nc, P = kb.nc, kb.P
    kmT, kmTb = kb.sb("kmT", [64, 4, 256], BF16)
    vm1, vm1b = kb.sb("vm1", [128, 2, 4, 65], BF16)
    P.op("pool", lambda: nc.gpsimd.memset(vm1[:], 1.0), writes=[vm1b])
    with kb.scope():
        gm, gmb = kb.bcast_row(gmem_d, D, "gmem")
        wkv, wkvb = kb.sb("wkv", [128, 8, 512], BF16)
        mt, mtb = kb.sb("mt", [128, D], F32)
        mn, mnb = kb.sb("mn", [128, D], BF16)
        mnT, mnTb = kb.sb("mnT", [128, 8, 256], BF16)
        scr, scrb = kb.sb("mscr", [128, 4], F32)
        wst = [kb.sb("wst2", [128, 512], F32) for _ in range(2)]
        for k in range(8):
            st, stb = wst[k % 2]
            P.dma(kb.dq(), st[:], wkv_d[k * 128:(k + 1) * 128, :], writes=[stb])
            P.op("dve", lambda: nc.vector.tensor_copy(out=wkv[:, k, :], in_=st[:]), reads=[stb], writes=[wkvb])
        for tl in range(2):
            P.dma("sp", mt[:], mem_d[tl * 128:(tl + 1) * 128, :], writes=[mtb])
            kb.rmsnorm(mt[:], mtb, gm[:], gmb, mn[:], mnb, D, scr, scrb)
            pt = kb.pt[kb.pti % 2]; ptb = kb.ptb[kb.pti % 2]; kb.pti += 1
            for j in range(8):
                P.op("pe", lambda j=j: nc.tensor.transpose(pt[:, j * 128:(j + 1) * 128], mn[:, j * 128:(j + 1) * 128], kb.ident[:]),
                     reads=[mnb, kb.identb], writes=[ptb], sig=(j == 7))
            P.op("dve", lambda: nc.vector.tensor_copy(out=mnT[:, :, tl * 128:(tl + 1) * 128], in_=pt[:, :].rearrange("p (a b) -> p a b", b=128)), reads=[ptb], writes=[mnTb])
        for h in range(4):
            ps, psb = kb.ps[2], kb.psb[2]
            for k in range(8):
                mm(kb, ps[0:64, 0:256], wkv[:, k, h * 64:(h + 1) * 64], mnT[:, k, :], k == 0, [wkvb, mnTb], [psb], sig=(k == 7))
            P.op("dve", lambda: nc.vector.tensor_copy(out=kmT[:, h, :], in_=ps[0:64, 0:256]), reads=[psb], writes=[kmTb])
        for tl in range(2):
            ps, psb = kb.ps[3], kb.psb[3]
            for k in range(8):
                mm(kb, ps[:, 0:256], mnT[:, k, tl * 128:(tl + 1) * 128], wkv[:, k, 256:512], k == 0, [wkvb, mnTb], [psb], sig=(k == 7))
            P.op("dve", lambda: nc.vector.tensor_copy(out=vm1[:, tl, :, 0:64], in_=ps[:, 0:256].rearrange("p (h d) -> p h d", d=64)), reads=[psb], writes=[vm1b])
    return kmT, kmTb, vm1, vm1b


def load_wout(kb, wout_d):
    nc, P = kb.nc, kb.P
    wout, woutb = kb.sb("wout", [128, 8, D], BF16)
    with kb.scope():
        wst = [kb.sb("wst3", [128, 1024], F32) for _ in range(2)]
        for k in range(8):
            st, stb = wst[k % 2]
            P.dma(kb.dq(), st[:], wout_d[k * 128:(k + 1) * 128, :], writes=[stb])
            P.op("dve", lambda: nc.vector.tensor_copy(out=wout[:, k, :], in_=st[:]), reads=[stb], writes=[woutb])
    return wout, woutb


def rms_small(kb, x, xb, A, d, g, gb, out, outb, tmp, tmpb, ss, ssb):
    nc, P = kb.nc, kb.P
    P.op("dve", lambda: nc.vector.tensor_tensor(out=tmp, in0=x, in1=x, op=ALU.mult), reads=[xb], writes=[tmpb])
    P.op("dve", lambda: nc.vector.tensor_reduce(out=ss[:, 0:A], in_=tmp, axis=AX.X, op=ALU.add), reads=[tmpb], writes=[ssb])
    P.op("act", lambda: nc.scalar.activation(out=ss[:, A:2 * A], in_=ss[:, 0:A], func=AF.Ln, scale=1.0 / d, bias=kb.eps_t[:, 0:1]), reads=[ssb, kb.eps_b], writes=[ssb])
    P.op("act", lambda: nc.scalar.activation(out=ss[:, 0:A], in_=ss[:, A:2 * A], func=AF.Exp, scale=-0.5), reads=[ssb], writes=[ssb])
    r = ss[:, 0:A]
    rb_ = AP(tensor=r.tensor, offset=r.offset, ap=[list(r.ap[0]), list(r.ap[1]), [0, d]])
    g2 = g[:, 0:d]
    gb_ = AP(tensor=g2.tensor, offset=g2.offset, ap=[list(g2.ap[0]), [0, A], list(g2.ap[1])])
    P.op("dve", lambda: nc.vector.tensor_tensor(out=tmp, in0=x, in1=rb_, op=ALU.mult), reads=[xb, ssb], writes=[tmpb])
    P.op("dve", lambda: nc.vector.tensor_tensor(out=out, in0=tmp, in1=gb_, op=ALU.mult), reads=[tmpb, gb], writes=[outb])


def build_C(stage=99, nslots=NT, kb=None):
    kb = kb or K("C")
    nc, P = kb.nc, kb.P
    x_d = kb.din("x", [TOK, D])
    pr_d = kb.din("pr", [TOK, 712])
    ckv_d = kb.din("ckv_seq", [T, 128])
    kidx_d = kb.din("kidx_seq", [T, 64])
    id_d = kb.din("identb", [128, 128], BF16)
    an_d = kb.din("antib", [128, 128], BF16)
    relb_d = kb.din("rel_bias", [32, 12])
    oh_s_d = kb.din("oh_s", [33, Y_SLC])
    cm_d = kb.din("cm", [128, 512])
    pw_d = kb.din("pw", [128, NIT])
    qn_d = kb.din("q_norm", [1, 256]); kvn_d = kb.din("kv_norm", [1, 128]); kin_d = kb.din("kidx_norm", [1, 64])
    wqup_d = kb.din("w_q_up", [256, 768]); wuk_d = kb.din("w_uk", [128, 768]); wuv_d = kb.din("w_uv", [128, 768])
    wqi_d = kb.din("w_q_idx", [256, 512])
    mem_d = kb.din("mem", [256, D]); gmem_d = kb.din("g_mem", [1, D]); wkv_d = kb.din("w_mem_kv", [D, 512])
    wout_d = kb.din("w_out", [D, D])
    hmid_d = kb.dout("hmid", [TOK, D])

    P.dma("sp", kb.ident[:], id_d, writes=[kb.identb])
    P.dma("sp", kb.anti[:], an_d, writes=[kb.antib])
    kb.mk_eps()
    with kb.scope():
        tabs = build_tables(kb, relb_d, [oh_s_d], [("s", Y_SLC)])
    band_s, band_sb = load_band(kb, tabs[0][0], tabs[0][1], Y_SLC, 2048, 1, "band_s")
    ckvT, ckvTb = kb.sb("ckvT", [128, T], BF16)
    ckv1, ckv1b = kb.sb("ckv1", [128, 64, 129], BF16)
    kidxT, kidxTb = kb.sb("kidxT", [64, T], BF16)
    P.op("pool", lambda: nc.gpsimd.memset(ckv1[:], 1.0), writes=[ckv1b])
    gq, gqb = kb.bcast_row(qn_d, 256, "gq")
    with kb.scope():
        gkv, gkvb = kb.bcast_row(kvn_d, 128, "gkv")
        gki, gkib = kb.bcast_row(kin_d, 64, "gki")
        st, stb = kb.sb("kst", [128, 8, 128], F32)
        tmp, tmpb = kb.sb("ktmp", [128, 8, 128], F32)
        ss, ssb = kb.sb("kss", [128, 16], F32)
        kin, kinb = kb.sb("kin", [128, 8, 64], BF16)
        for i in range(8):
            P.dma(kb.dq(), st[:], ckv_d[i * 1024:(i + 1) * 1024, :].rearrange("(a p) c -> p a c", p=128), writes=[stb])
            rms_small(kb, st[:], stb, 8, 128, gkv, gkvb, ckv1[:, i * 8:(i + 1) * 8, 0:128], ckv1b, tmp[:], tmpb, ss, ssb)
            pt = kb.pt[kb.pti % 2]; ptb = kb.ptb[kb.pti % 2]; kb.pti += 1
            for j in range(8):
                P.op("pe", lambda j=j: nc.tensor.transpose(pt[:, j * 128:(j + 1) * 128], ckv1[:, i * 8 + j, 0:128], kb.ident[:]),
                     reads=[ckv1b, kb.identb], writes=[ptb], sig=(j == 7))
            P.op("act", lambda: nc.scalar.copy(out=ckvT[:, i * 1024:(i + 1) * 1024], in_=pt[:, :]), reads=[ptb], writes=[ckvTb])
        for i in range(8):
            P.dma(kb.dq(), st[:, :, 0:64], kidx_d[i * 1024:(i + 1) * 1024, :].rearrange("(a p) c -> p a c", p=128), writes=[stb])
            rms_small(kb, st[:, :, 0:64], stb, 8, 64, gki, gkib, kin[:], kinb, tmp[:, :, 0:64], tmpb, ss, ssb)
            pt = kb.pt[kb.pti % 2]; ptb = kb.ptb[kb.pti % 2]; kb.pti += 1
            for j in range(8):
                P.op("pe", lambda j=j: nc.tensor.transpose(pt[0:64, j * 128:(j + 1) * 128], kin[:, j, :], kb.ident[:]),
                     reads=[kinb, kb.identb], writes=[ptb], sig=(j == 7))
            P.op("act", lambda: nc.scalar.copy(out=kidxT[:, i * 1024:(i + 1) * 1024], in_=pt[0:64, :]), reads=[ptb], writes=[kidxTb])
    wqup, wqupb = kb.sb("wqup", [128, 2, 768], BF16)
    wqi, wqib = kb.sb("wqi", [128, 2, 512], BF16)
    wuv, wuvb = kb.sb("wuv", [128, 768], BF16)
    wukT, wukTb = kb.sb("wukT", [64, 12, 128], BF16)
    with kb.scope():
        wst = [kb.sb("wst4", [128, 768], F32) for _ in range(2)]
        wukb, wukbb = kb.sb("wukb", [128, 768], BF16)
        i = 0
        for (src, rows, ncol, dstf) in [(wqup_d, 0, 768, lambda: wqup[:, 0, :]), (wqup_d, 128, 768, lambda: wqup[:, 1, :]),
                                        (wqi_d, 0, 512, lambda: wqi[:, 0, :]), (wqi_d, 128, 512, lambda: wqi[:, 1, :]),
                                        (wuv_d, 0, 768, lambda: wuv[:]), (wuk_d, 0, 768, lambda: wukb[:])]:
            st, stb = wst[i % 2]; i += 1
            P.dma(kb.dq(), st[:, 0:ncol], src[rows:rows + 128, :], writes=[stb])
            dst = dstf()
            P.op("dve", lambda: nc.vector.tensor_copy(out=dst, in_=st[:, 0:ncol]), reads=[stb], writes=[wqupb, wqib, wuvb, wukbb])
        for h0 in (0, 8):
            nb = min(8, 12 - h0)
            pt = kb.pt[kb.pti % 2]; ptb = kb.ptb[kb.pti % 2]; kb.pti += 1
            for j in range(nb):
                P.op("pe", lambda j=j: nc.tensor.transpose(pt[0:64, j * 128:(j + 1) * 128], wukb[:, (h0 + j) * 64:(h0 + j + 1) * 64], kb.ident[:]),
                     reads=[wukbb, kb.identb], writes=[ptb], sig=(j == nb - 1))
            P.op("dve", lambda: nc.vector.tensor_copy(out=wukT[:, h0:h0 + nb, :], in_=pt[0:64, 0:nb * 128].rearrange("p (a b) -> p a b", b=128)), reads=[ptb], writes=[wukTb])
    kmT, kmTb, vm1, vm1b = mem_setup(kb, mem_d, gmem_d, wkv_d)
    wout, woutb = load_wout(kb, wout_d)
    cm, cmb = kb.sb("cm", [128, 512], F32)
    P.dma("sp", cm[:], cm_d, writes=[cmb])
    pw, pwb = kb.sb("pw", [128, NIT], F32)
    P.dma("sp", pw[:], pw_d, writes=[pwb])

    at = Attn(kb)
    score, scoreb = kb.sb("score", [128, T], F32)
    mb, mbb = kb.sb("mb", [128, T], BF16)
    mTs = [kb.sb("mT", [128, 8, 128], BF16) for _ in range(2)]
    big, bigb = kb.sb("big", [128, D], F32)
    cqn, cqnb = kb.sb("cqn", [128, 256], BF16)
    cqT, cqTb = kb.sb("cqT", [128, 2, 128], BF16)
    qhT, qhTb = kb.sb("qhT", [128, 12, 128], BF16)
    qabs, qabsb = kb.sb("qabs", [128, 12, 128], BF16)
    qiT, qiTb = kb.sb("qiT", [64, 8, 128], BF16)
    qmb16, qmb16b = kb.sb("qmb16", [128, 256], BF16)
    qmT, qmTb = kb.sb("qmT", [64, 4, 128], BF16)
    rts = [kb.sb("rt", [128, 512], F32) for _ in range(2)]
    catbf, catbfb = kb.sb("catbf", [128, D], BF16)
    catT, catTb = kb.sb("catT", [128, 8, 128], BF16)
    pm, pmb = kb.sb("pm", [128, 256], BF16)
    sm, smb = kb.sb("sm", [128, 16], F32)
    wv, wvb = kb.sb("wv", [128, 32], F32)
    bs, bsb = kb.sb("bs", [128, 8 + 2 * NIT], F32)
    accbank = 2

    def next_acc():
        nonlocal accbank
        b = accbank
        accbank = 2 + (accbank - 2 + 1) % 4
        return b

    for m in range(nslots):
        tsl = slice(m * 128, (m + 1) * 128)
        L = 128 * (4 * m + 4)
        nkt = 4 * m + 4
        P.dma("sp", big[:, 0:712], pr_d[tsl, :], writes=[bigb])
        if stage == 0:
            P.dma("sp", hmid_d[tsl, :], big[:], reads=[bigb], final=True)
            continue
        ssq = wv[:, 16:18]
        P.op("act", lambda: nc.scalar.activation(out=cqn[:], in_=big[:, 0:256], func=AF.Square, accum_out=wv[:, 16:17]), reads=[bigb], writes=[cqnb, wvb])
        P.op("act", lambda: nc.scalar.activation(out=wv[:, 17:18], in_=wv[:, 16:17], func=AF.Ln, scale=1.0 / 256, bias=kb.eps_t[:, 0:1]), reads=[wvb, kb.eps_b], writes=[wvb])
        P.op("act", lambda: nc.scalar.activation(out=wv[:, 18:19], in_=wv[:, 17:18], func=AF.Exp, scale=-0.5), reads=[wvb], writes=[wvb])
        P.op("dve", lambda: nc.vector.scalar_tensor_tensor(out=cqn[:], in0=big[:, 0:256], scalar=wv[:, 18:19], in1=gq[:], op0=ALU.mult, op1=ALU.mult), reads=[bigb, wvb, gqb], writes=[cqnb])
        pt = kb.pt[kb.pti % 2]; ptb = kb.ptb[kb.pti % 2]; kb.pti += 1
        for j in range(2):
            P.op("pe", lambda j=j: nc.tensor.transpose(pt[:, j * 128:(j + 1) * 128], cqn[:, j * 128:(j + 1) * 128], kb.ident[:]), reads=[cqnb, kb.identb], writes=[ptb], sig=(j == 1))
        P.op("dve", lambda: nc.vector.tensor_copy(out=cqT[:], in_=pt[:, 0:256].rearrange("p (a b) -> p a b", b=128)), reads=[ptb], writes=[cqTb])
        for b4 in range(3):
            ps, psb = kb.ps[b4 % 2], kb.psb[b4 % 2]
            for hh in range(4):
                h = 4 * b4 + hh
                for c in range(2):
                    mm(kb, ps[0:64, hh * 128:(hh + 1) * 128], wqup[:, c, h * 64:(h + 1) * 64], cqT[:, c, :], hh == 0 and c == 0, [wqupb, cqTb], [psb], sig=(hh == 3 and c == 1))
            P.op("act", lambda: nc.scalar.activation(out=qhT[0:64, 4 * b4:4 * b4 + 4, :], in_=ps[0:64, :].rearrange("p (a b) -> p a b", b=128), func=AF.Copy, scale=0.125), reads=[psb], writes=[qhTb])
        for b4 in range(2):
            ps, psb = kb.ps[b4 % 2], kb.psb[b4 % 2]
            for hh in range(4):
                h = 4 * b4 + hh
                for c in range(2):
                    mm(kb, ps[0:64, hh * 128:(hh + 1) * 128], wqi[:, c, h * 64:(h + 1) * 64], cqT[:, c, :], hh == 0 and c == 0, [wqib, cqTb], [psb], sig=(hh == 3 and c == 1))
            P.op("dve", lambda: nc.vector.tensor_copy(out=qiT[:, 4 * b4:4 * b4 + 4, :], in_=ps[0:64, :].rearrange("p (a b) -> p a b", b=128)), reads=[psb], writes=[qiTb])
        for b4 in range(3):
            ps, psb = kb.ps[b4 % 2], kb.psb[b4 % 2]
            for hh in range(4):
                h = 4 * b4 + hh
                mm(kb, ps[:, hh * 128:(hh + 1) * 128], wukT[:, h, :], qhT[0:64, h, :], hh == 0, [wukTb, qhTb], [psb], sig=(hh == 3))
            P.op("dve", lambda: nc.vector.tensor_copy(out=qabs[:, 4 * b4:4 * b4 + 4, :], in_=ps[:, :].rearrange("p (a b) -> p a b", b=128)), reads=[psb], writes=[qabsb])
        P.op("dve", lambda: nc.vector.tensor_scalar(out=wv[:, 0:8], in0=big[:, 448:456], scalar1=0.04419417382415922, scalar2=None, op0=ALU.mult), reads=[bigb], writes=[wvb])
        P.op("dve", lambda: nc.vector.tensor_scalar(out=wv[:, 8:16], in0=wv[:, 0:8], scalar1=0.0, scalar2=2.0, op0=ALU.is_ge, op1=ALU.mult), reads=[wvb], writes=[wvb])
        P.op("dve", lambda: nc.vector.tensor_scalar(out=wv[:, 8:16], in0=wv[:, 8:16], scalar1=-1.0, scalar2=None, op0=ALU.add), reads=[wvb], writes=[wvb])
        P.op("dve", lambda: nc.vector.tensor_tensor(out=wv[:, 0:8], in0=wv[:, 0:8], in1=wv[:, 8:16], op=ALU.mult), reads=[wvb], writes=[wvb])
        P.op("act", lambda: nc.scalar.activation(out=qmb16[:], in_=big[:, 456:712], func=AF.Copy, scale=0.125), reads=[bigb], writes=[qmb16b])
        pt = kb.pt[kb.pti % 2]; ptb = kb.ptb[kb.pti % 2]; kb.pti += 1
        for j in range(4):
            P.op("pe", lambda j=j: nc.tensor.transpose(pt[0:64, j * 128:(j + 1) * 128], qmb16[:, j * 64:(j + 1) * 64], kb.ident[:]), reads=[qmb16b, kb.identb], writes=[ptb], sig=(j == 3))
        P.op("dve", lambda: nc.vector.tensor_copy(out=qmT[:], in_=pt[0:64, 0:512].rearrange("p (a b) -> p a b", b=128)), reads=[ptb], writes=[qmTb])
        P.dma("pool", big[:], x_d[tsl, :], reads=[], writes=[bigb])
        if stage == 1:
            P.dma("sp", hmid_d[tsl, :], big[:], reads=[bigb], final=True)
            continue
        for kc in range(m + 1):
            csl = slice(kc * 512, (kc + 1) * 512)
            for h in range(8):
                ps, psb = kb.ps[h % 2], kb.psb[h % 2]
                rt, rtb = rts[h % 2]
                mm(kb, ps[:, :], qiT[:, h, :], kidxT[:, csl], True, [qiTb, kidxTb], [psb], sig=True)
                P.op("act", lambda: nc.scalar.activation(out=rt[:], in_=ps[:, :], func=AF.Relu, scale=wv[:, h:h + 1]), reads=[psb, wvb], writes=[rtb])
                if h == 0:
                    P.op("dve", lambda: nc.vector.tensor_scalar(out=score[:, csl], in0=rt[:], scalar1=wv[:, 8:9], scalar2=None, op0=ALU.mult), reads=[rtb, wvb], writes=[scoreb])
                else:
                    P.op("dve", lambda: nc.vector.scalar_tensor_tensor(out=score[:, csl], in0=rt[:], scalar=wv[:, 8 + h:9 + h], in1=score[:, csl], op0=ALU.mult, op1=ALU.add), reads=[rtb, wvb, scoreb], writes=[scoreb])
        if stage == 2:
            P.dma("sp", hmid_d[tsl, 0:512], score[:, 0:512], reads=[scoreb], final=True)
            continue
        P.op("dve", lambda: nc.vector.tensor_reduce(out=bs[:, 0:1], in_=score[:, 0:L], axis=AX.X, op=ALU.min), reads=[scoreb], writes=[bsb])
        P.op("dve", lambda: nc.vector.tensor_reduce(out=bs[:, 1:2], in_=score[:, 0:L], axis=AX.X, op=ALU.max), reads=[scoreb], writes=[bsb])
        P.op("dve", lambda: nc.vector.tensor_tensor(out=score[:, L - 512:L], in0=score[:, L - 512:L], in1=cm[:], op=ALU.add), reads=[scoreb, cmb], writes=[scoreb])
        P.op("dve", lambda: nc.vector.tensor_tensor(out=bs[:, 2:3], in0=bs[:, 1:2], in1=bs[:, 0:1], op=ALU.subtract), reads=[bsb], writes=[bsb])
        P.op("dve", lambda: nc.vector.tensor_scalar(out=bs[:, 8:8 + NIT], in0=pw[:], scalar1=bs[:, 2:3], scalar2=None, op0=ALU.mult), reads=[bsb, pwb], writes=[bsb])
        for it in range(NIT):
            hw = bs[:, 8 + it:9 + it]
            P.op("dve", lambda: nc.vector.tensor_tensor(out=bs[:, 3:4], in0=bs[:, 0:1], in1=hw, op=ALU.add), reads=[bsb], writes=[bsb])
            P.op("dve", lambda: nc.vector.tensor_scalar(out=mb[:, 0:L], in0=score[:, 0:L], scalar1=bs[:, 3:4], scalar2=None, op0=ALU.is_ge, op1=ALU.add, accum_out=bs[:, 4:5]),
                 reads=[scoreb, bsb], writes=[mbb, bsb])
            P.op("dve", lambda: nc.vector.tensor_scalar(out=bs[:, 5:6], in0=bs[:, 4:5], scalar1=255.5, scalar2=hw, op0=ALU.is_ge, op1=ALU.mult), reads=[bsb], writes=[bsb])
            P.op("dve", lambda: nc.vector.tensor_tensor(out=bs[:, 0:1], in0=bs[:, 0:1], in1=bs[:, 5:6], op=ALU.add), reads=[bsb], writes=[bsb])
        P.op("dve", lambda: nc.vector.tensor_scalar(out=mb[:, 0:L], in0=score[:, 0:L], scalar1=bs[:, 0:1], scalar2=NEGB, op0=ALU.is_lt, op1=ALU.mult), reads=[scoreb, bsb], writes=[mbb])
        if stage == 3:
            P.dma("sp", hmid_d[tsl, 0:512], score[:, 0:512], reads=[scoreb], final=True)
            P.dma("sp", hmid_d[tsl, 512:512 + 8 + 2 * NIT], bs[:], reads=[bsb], final=True)
            continue
        banks = [next_acc() for _ in range(4)]
        for g8 in range(0, nkt, 8):
            nb = min(8, nkt - g8)
            mT, mTb = mTs[(g8 // 8) % 2]
            pt = kb.pt[kb.pti % 2]; ptb = kb.ptb[kb.pti % 2]; kb.pti += 1
            for j in range(nb):
                P.op("pe", lambda j=j: nc.tensor.transpose(pt[:, j * 128:(j + 1) * 128], mb[:, (g8 + j) * 128:(g8 + j + 1) * 128], kb.ident[:]), reads=[mbb, kb.identb], writes=[ptb], sig=(j == nb - 1))
            P.op("dve", lambda: nc.vector.tensor_copy(out=mT[:, 0:nb, :], in_=pt[:, 0:nb * 128].rearrange("p (a b) -> p a b", b=128)), reads=[ptb], writes=[mTb])
            for j in range(nb):
                kt = g8 + j
                u = kt - 4 * m
                for tr in range(4):
                    bA = banks[tr]
                    accs = [kb.ps[bA][:, 129 * hh:129 * hh + 129] for hh in range(3)]
                    accb = [kb.psb[bA]] * 3
                    s_terms = [(ckvT[:, kt * 128:(kt + 1) * 128], qabs[:, 3 * tr:3 * tr + 3, :], [ckvTb, qabsb]),
                               (kb.ident[:], bc3(mT[:, j, :]), [kb.identb, mTb])]
                    near = None
                    if u >= -12:
                        z0 = 128 * (3 - u)
                        near = (kb.anti[:], band_s[:, 3 * tr:3 * tr + 3, z0:z0 + 128], [kb.antib, band_sb])
                    at.unit(s_terms, near, accs, accb, ckv1[:, kt, :], [ckv1b], [kt == 0, False, False])
        at.flush()
        for tr in range(4):
            bA = banks[tr]
            for hh in range(3):
                h = 3 * tr + hh
                O = kb.ps[bA][:, 129 * hh:129 * hh + 129]
                P.op("dve", lambda: nc.vector.tensor_scalar(out=sm[:, 0:1], in0=O[:, 128:129], scalar1=1e-30, scalar2=None, op0=ALU.max), reads=[kb.psb[bA]], writes=[smb])
                P.op("dve", lambda: nc.vector.reciprocal(out=sm[:, 1:2], in_=sm[:, 0:1]), reads=[smb], writes=[smb])
                P.op("dve", lambda: nc.vector.tensor_scalar(out=qabs[:, h, :], in0=O[:, 0:128], scalar1=sm[:, 1:2], scalar2=None, op0=ALU.mult), reads=[kb.psb[bA], smb], writes=[qabsb])
        for h0 in (0, 8):
            nb = min(8, 12 - h0)
            pt = kb.pt[kb.pti % 2]; ptb = kb.ptb[kb.pti % 2]; kb.pti += 1
            for j in range(nb):
                P.op("pe", lambda j=j: nc.tensor.transpose(pt[:, j * 128:(j + 1) * 128], qabs[:, h0 + j, :], kb.ident[:]), reads=[qabsb, kb.identb], writes=[ptb], sig=(j == nb - 1))
            P.op("dve", lambda: nc.vector.tensor_copy(out=qhT[:, h0:h0 + nb, :], in_=pt[:, 0:nb * 128].rearrange("p (a b) -> p a b", b=128)), reads=[ptb], writes=[qhTb])
        for h0 in (0, 8):
            nb = min(8, 12 - h0)
            bA = next_acc()
            ps, psb = kb.ps[bA], kb.psb[bA]
            for j in range(nb):
                h = h0 + j
                mm(kb, ps[:, j * 64:(j + 1) * 64], qhT[:, h, :], wuv[:, h * 64:(h + 1) * 64], j == 0, [qhTb, wuvb], [psb], sig=(j == nb - 1))
            P.op("act", lambda: nc.scalar.copy(out=catbf[:, h0 * 64:(h0 + nb) * 64], in_=ps[:, 0:nb * 64]), reads=[psb], writes=[catbfb])
        for h in range(4):
            sbk = at.next_sbank()
            ps, psb = kb.ps[sbk], kb.psb[sbk]
            for tl in range(2):
                mm(kb, ps[:, tl * 128:(tl + 1) * 128], kmT[:, h, tl * 128:(tl + 1) * 128], qmT[:, h, :], tl == 0, [kmTb, qmTb], [psb], sig=(tl == 1))
            P.op("act", lambda: nc.scalar.activation(out=pm[:], in_=ps[:, 0:256], func=AF.Exp), reads=[psb], writes=[pmb])
            bA = next_acc()
            O = kb.ps[bA][:, 0:65]
            for tl in range(2):
                mm(kb, O, pm[:, tl * 128:(tl + 1) * 128], vm1[:, tl, h, :], tl == 0, [pmb, vm1b], [kb.psb[bA]], sig=(tl == 1))
            P.op("dve", lambda: nc.vector.reciprocal(out=sm[:, 4:5], in_=O[:, 64:65]), reads=[kb.psb[bA]], writes=[smb])
            P.op("dve", lambda: nc.vector.tensor_scalar(out=catbf[:, 768 + h * 64:768 + (h + 1) * 64], in0=O[:, 0:64], scalar1=sm[:, 4:5], scalar2=None, op0=ALU.mult), reads=[kb.psb[bA], smb], writes=[catbfb])
        pt = kb.pt[kb.pti % 2]; ptb = kb.ptb[kb.pti % 2]; kb.pti += 1
        for j in range(8):
            P.op("pe", lambda j=j: nc.tensor.transpose(pt[:, j * 128:(j + 1) * 128], catbf[:, j * 128:(j + 1) * 128], kb.ident[:]),
                 reads=[catbfb, kb.identb], writes=[ptb], sig=(j == 7))
        P.op("dve", lambda: nc.vector.tensor_copy(out=catT[:], in_=pt[:, :].rearrange("p (a b) -> p a b", b=128)), reads=[ptb], writes=[catTb])
        for half in range(2):
            bA = next_acc()
            ps, psb = kb.ps[bA], kb.psb[bA]
            for k in range(8):
                mm(kb, ps[:, :], catT[:, k, :], wout[:, k, half * 512:(half + 1) * 512], k == 0, [catTb, woutb], [psb], sig=(k == 7))
            P.op("dve", lambda: nc.vector.tensor_tensor(out=big[:, half * 512:(half + 1) * 512], in0=ps[:, :], in1=big[:, half * 512:(half + 1) * 512], op=ALU.add), reads=[psb, bigb], writes=[bigb])
        P.dma("sp", hmid_d[tsl, :], big[:], reads=[bigb], final=True)
    return kb


def run_C(inputs, resF, stage=99, nslots=NT):
    kb = build_C(stage, nslots)
    ident = np.eye(128, dtype=np.float32)
    pw = np.tile((0.5 ** np.arange(1, NIT + 1)).astype(np.float32)[None, :], (128, 1))
    maps = []
    for core in range(8):
        b, c = core // 4, core % 4
        prs = [np.asarray(resF[4 * b + cc]["pr"]) for cc in range(4)]
        oh_s, _, _ = host_tables(c)
        z = np.arange(512)[None, :]
        q = np.arange(128)[:, None]
        cm = np.where(z <= 128 * c + q, 0.0, -1e30).astype(np.float32)
        mp = {"x": np.asarray(resF[core]["h"]), "pr": prs[c],
              "ckv_seq": gather_seq([np.ascontiguousarray(p[:, 256:384]) for p in prs], 0),
              "kidx_seq": gather_seq([np.ascontiguousarray(p[:, 384:448]) for p in prs], 0),
              "identb": ident.astype(NPBF), "antib": np.ascontiguousarray(ident[::-1]).astype(NPBF),
              "rel_bias": inputs["rel_bias"], "oh_s": oh_s, "cm": cm, "pw": pw,
              "q_norm": inputs["dsa_q_norm"][0:1], "kv_norm": inputs["dsa_kv_norm"][0:1], "kidx_norm": inputs["dsa_kidx_norm"][0:1],
              "w_q_up": inputs["dsa_w_q_up"][0], "w_uk": inputs["dsa_w_uk"][0].reshape(128, 768), "w_uv": inputs["dsa_w_uv"][0].reshape(128, 768),
              "w_q_idx": inputs["dsa_w_q_idx"][0],
              "mem": inputs["mem"][b], "g_mem": inputs["norm_mem"][1:2], "w_mem_kv": inputs["w_mem_kv"][1], "w_out": inputs["w_out"][1]}
        maps.append(mp)
    return kb.run(maps)


GROUPS = [[0, 1, 2, 3], [4, 5, 6, 7]]


def all_gather(kb, in_ap2d, out_ap2d):
    nc, P = kb.nc, kb.P
    P.barrier()
    kb.n += 1
    key = "cc%d" % kb.n
    sem = P.es.enter_context(nc.semaphore(key))
    P.sem[key] = sem
    P.cnt[key] = 0
    with nc.Block() as block:
        @block.gpsimd
        def _(g):
            g.collective_compute("AllGather", ALU.bypass, replica_groups=GROUPS, ins=[in_ap2d.opt()], outs=[out_ap2d.opt()]).then_inc(sem)
            g.wait_ge(sem, 1)
    P.cnt[key] = 1
    P.seen["pool"][key] = 1
    for e in P.eng:
        P._need(e, (key, 1))


def seq_rows(all_t, ncols_total, col0, ncol, m0, nm):
    return AP(tensor=all_t, offset=m0 * 128 * ncols_total + col0,
              ap=[[128 * ncols_total, nm], [2048 * ncols_total, 4], [ncols_total, 128], [1, ncol]])


def build_fused():
    kb = K("fused")
    nc, P = kb.nc, kb.P
    ext = {}

    def E(name, shape, dt=F32):
        ext[name] = nc.dram_tensor(name, list(shape), dt, kind="ExternalInput").ap()
        return ext[name]

    def S(name, shape, dt=F32):
        return nc.dram_tensor(name, list(shape), dt)

    x = E("x", [TOK, D])
    out = nc.dram_tensor("out", [TOK, D], F32, kind="ExternalOutput").ap()
    nm = {k: E(k, [1, D]) for k in ("norm_mix0", "norm_mix1", "norm_ffn0", "norm_ffn1", "norm_mem0", "norm_mem1", "norm_final")}
    nsa_w_in = E("nsa_w_in", [D, 1828]); gate_b = E("gate_b", [1, 36])
    ident = E("ident", [128, 128]); anti = E("anti", [128, 128])
    identb = E("identb", [128, 128], BF16); antib = E("antib", [128, 128], BF16)
    rel_bias = E("rel_bias", [32, 12])
    oh_s = E("oh_s", [33, Y_SLC]); oh_w = E("oh_w", [33, Y_WIN]); oh_c = E("oh_c", [33, Y_CMP])
    ew = E("ew", [128, T], BF16); ovl = E("ovl", [128, 4, 128], BF16); fb = E("fb", [NT, 128, 128])
    cmp_w = {k: E(k, shp) for k, shp in [("w1k", [2048, 256]), ("b1k", [256, 1]), ("w2k", [256, 64]), ("b2k", [64, 1]),
                                         ("w1v", [2048, 256]), ("b1v", [256, 1]), ("w2v", [256, 64]), ("b2v", [1, 64]),
                                         ("posk", [32, 64]), ("posv", [32, 64])]}
    mem = E("mem", [256, D])
    wkv = [E("w_mem_kv%d" % i, [D, 512]) for i in range(2)]
    wout = [E("w_out%d" % i, [D, D]) for i in range(2)]
    wg = [E("wg%d" % i, [D, DFF]) for i in range(2)]
    wu = [E("wu%d" % i, [D, DFF]) for i in range(2)]
    wd = [E("wd%d" % i, [DFF, D]) for i in range(2)]
    dsa_w_in = E("dsa_w_in", [D, 712])
    cm = E("cm", [128, 512]); pw = E("pw", [128, NIT])
    q_norm = E("q_norm", [1, 256]); kv_norm = E("kv_norm", [1, 128]); kidx_norm = E("kidx_norm", [1, 64])
    w_q_up = E("w_q_up", [256, 768]); w_uk = E("w_uk", [128, 768]); w_uv = E("w_uv", [128, 768]); w_q_idx = E("w_q_idx", [256, 512])

    fm_loc = S("fm_loc", [64, 24, TOK], BF16)
    tm_loc = S("tm_loc", [TOK, 292])
    kfm_loc = S("kfm_loc", [8, 64, TOK], BF16)
    kfm_all = S("kfm_all", [4, 4 * 128, TOK], BF16)
    vt_loc = S("vt_loc", [TOK, 256])
    vt_all = S("vt_all", [4, 4 * 512, 256])
    qT2_s = S("qT2_s", [128, 6, TOK], BF16)
    kseq = {k: S(k + "_s", [128, T], BF16) for k in ("kcrT2", "vcrT2", "ksT2", "kwT2")}
    vs_s = S("vs_s", [T, 128]); vw_s = S("vw_s", [T, 128])
    hmid0 = S("hmid0", [TOK, D]); h0 = S("h0", [TOK, D]); hmid1 = S("hmid1", [TOK, D])
    pr_loc = S("pr_loc", [TOK, 712])
    kv_loc = S("kv_loc", [TOK, 192]); kv_all = S("kv_all", [4, 4 * 512, 192])
    ckv_s = S("ckv_s", [T, 128]); kidx_s = S("kidx_s", [T, 64])

    kb.alias = {"x": x, "g": nm["norm_mix0"], "w_in": nsa_w_in, "gate_b": gate_b, "ident": ident, "anti": anti,
                "fmT": fm_loc.ap(), "tm": tm_loc.ap()}
    with kb.scope():
        build_A(kb)
    P.barrier()
    P.dma("sp", kfm_loc.ap(), AP(tensor=fm_loc, offset=12 * TOK, ap=[[TOK, 8], [24 * TOK, 64], [1, TOK]]))
    P.dma("pool", vt_loc.ap(), tm_loc.ap()[:, 0:256])
    for ch in range(4):
        all_gather(kb, kfm_loc.ap()[2 * ch:2 * ch + 2].rearrange("a d t -> (a d) t"), kfm_all.ap()[ch])
    for r4 in range(4):
        all_gather(kb, vt_loc.ap()[r4 * 512:(r4 + 1) * 512, :], vt_all.ap()[r4])
    for g in range(2):
        P.dma(kb.dq(), qT2_s.ap()[g * 64:(g + 1) * 64, :, :], fm_loc.ap()[:, 6 * g:6 * g + 6, :])
    for ch, k in enumerate(("kcrT2", "vcrT2", "ksT2", "kwT2")):
        for g in range(2):
            for cc in range(4):
                dst = AP(tensor=kseq[k], offset=(g * 64) * T + cc * 128, ap=[[T, 64], [512, 16], [1, 128]])
                src = AP(tensor=kfm_all, offset=((ch * 4 + cc) * 128 + g * 64) * TOK, ap=[[TOK, 64], [128, 16], [1, 128]])
                P.dma(kb.dq(), dst, src)
    for (dst_t, c0) in ((vs_s, 0), (vw_s, 128)):
        for cc in range(4):
            for r4 in range(4):
                dst = AP(tensor=dst_t, offset=(r4 * 16 * 128 + cc * 128) * 128, ap=[[512 * 128, 4], [128, 128], [1, 128]])
                src = AP(tensor=vt_all, offset=((r4 * 4 + cc) * 512) * 256 + c0, ap=[[128 * 256, 4], [256, 128], [1, 128]])
                P.dma(kb.dq(), dst, src)
    P.barrier()
    kb.alias = dict(cmp_w)
    kb.alias.update({"x": x, "identb": identb, "antib": antib, "rel_bias": rel_bias, "oh_s": oh_s, "oh_w": oh_w, "oh_c": oh_c,
                     "qT2": qT2_s.ap(), "qmT": fm_loc.ap()[:, 20:24, :], "gates": tm_loc.ap()[:, 256:292],
                     "ksT2": kseq["ksT2"].ap(), "kwT2": kseq["kwT2"].ap(), "kcrT2": kseq["kcrT2"].ap(), "vcrT2": kseq["vcrT2"].ap(),
                     "vs": vs_s.ap(), "vw": vw_s.ap(), "ew": ew, "ovl": ovl, "fb": fb,
                     "mem": mem, "g_mem": nm["norm_mem0"], "w_mem_kv": wkv[0], "w_out": wout[0], "hmid": hmid0.ap()})
    with kb.scope():
        build_B(0, kb)
    P.barrier()
    kb.alias = {"hmid": hmid0.ap(), "identb": identb, "g_ffn": nm["norm_ffn0"], "wg": wg[0], "wu": wu[0], "wd": wd[0],
                "g2": nm["norm_mix1"], "h": h0.ap(), "w_in2": dsa_w_in, "pr": pr_loc.ap()}
    with kb.scope():
        build_F(False, 712, kb)
    P.barrier()
    P.dma("sp", kv_loc.ap(), pr_loc.ap()[:, 256:448])
    for r4 in range(4):
        all_gather(kb, kv_loc.ap()[r4 * 512:(r4 + 1) * 512, :], kv_all.ap()[r4])
    for (dst_t, c0, nc_) in ((ckv_s, 0, 128), (kidx_s, 128, 64)):
        for cc in range(4):
            for r4 in range(4):
                dst = AP(tensor=dst_t, offset=(r4 * 16 * 128 + cc * 128) * nc_, ap=[[512 * nc_, 4], [nc_, 128], [1, nc_]])
                src = AP(tensor=kv_all, offset=((r4 * 4 + cc) * 512) * 192 + c0, ap=[[128 * 192, 4], [192, 128], [1, nc_]])
                P.dma(kb.dq(), dst, src)
    P.barrier()
    kb.alias = {"x": h0.ap(), "pr": pr_loc.ap(), "ckv_seq": ckv_s.ap(), "kidx_seq": kidx_s.ap(), "identb": identb, "antib": antib,
                "rel_bias": rel_bias, "oh_s": oh_s, "cm": cm, "pw": pw, "q_norm": q_norm, "kv_norm": kv_norm, "kidx_norm": kidx_norm,
                "w_q_up": w_q_up, "w_uk": w_uk, "w_uv": w_uv, "w_q_idx": w_q_idx, "mem": mem, "g_mem": nm["norm_mem1"],
                "w_mem_kv": wkv[1], "w_out": wout[1], "hmid": hmid1.ap()}
    with kb.scope():
        build_C(99, NT, kb)
    P.barrier()
    kb.alias = {"hmid": hmid1.ap(), "identb": identb, "g_ffn": nm["norm_ffn1"], "wg": wg[1], "wu": wu[1], "wd": wd[1],
                "g2": nm["norm_final"], "h": out}
    with kb.scope():
        build_F(True, 712, kb)
    kb.ext = ext
    return kb


def fused_maps(inputs):
    ident = np.eye(128, dtype=np.float32)
    anti = np.ascontiguousarray(ident[::-1])
    pw = np.tile((0.5 ** np.arange(1, NIT + 1)).astype(np.float32)[None, :], (128, 1))
    maps = []
    for core in range(8):
        b, c = core // 4, core % 4
        mp = consts_B(c)
        z = np.arange(512)[None, :]
        q = np.arange(128)[:, None]
        mp.update({
            "x": np.ascontiguousarray(inputs["x"][b][core_tokens(c)]),
            "norm_mix0": inputs["norm_mix"][0:1], "norm_mix1": inputs["norm_mix"][1:2],
            "norm_ffn0": inputs["norm_ffn"][0:1], "norm_ffn1": inputs["norm_ffn"][1:2],
            "norm_mem0": inputs["norm_mem"][0:1], "norm_mem1": inputs["norm_mem"][1:2],
            "norm_final": inputs["norm_final"].reshape(1, D),
            "nsa_w_in": inputs["nsa_w_in"][0], "gate_b": inputs["nsa_gate_b"][0:1],
            "ident": ident, "anti": anti, "rel_bias": inputs["rel_bias"],
            "w1k": inputs["nsa_cmp_k_w1"][0], "b1k": inputs["nsa_cmp_k_b1"][0].reshape(256, 1),
            "w2k": inputs["nsa_cmp_k_w2"][0], "b2k": inputs["nsa_cmp_k_b2"][0].reshape(64, 1),
            "w1v": inputs["nsa_cmp_v_w1"][0], "b1v": inputs["nsa_cmp_v_b1"][0].reshape(256, 1),
            "w2v": inputs["nsa_cmp_v_w2"][0], "b2v": inputs["nsa_cmp_v_b2"][0].reshape(1, 64),
            "posk": inputs["nsa_cmp_pos_k"][0], "posv": inputs["nsa_cmp_pos_v"][0],
            "mem": inputs["mem"][b],
            "w_mem_kv0": inputs["w_mem_kv"][0], "w_mem_kv1": inputs["w_mem_kv"][1],
            "w_out0": inputs["w_out"][0], "w_out1": inputs["w_out"][1],
            "wg0": inputs["ffn_gate"][0], "wg1": inputs["ffn_gate"][1],
            "wu0": inputs["ffn_up"][0], "wu1": inputs["ffn_up"][1],
            "wd0": inputs["ffn_down"][0], "wd1": inputs["ffn_down"][1],
            "dsa_w_in": inputs["dsa_w_in"][0],
            "cm": np.where(z <= 128 * c + q, 0.0, -1e30).astype(np.float32), "pw": pw,
            "q_norm": inputs["dsa_q_norm"][0:1], "kv_norm": inputs["dsa_kv_norm"][0:1], "kidx_norm": inputs["dsa_kidx_norm"][0:1],
            "w_q_up": inputs["dsa_w_q_up"][0], "w_uk": inputs["dsa_w_uk"][0].reshape(128, 768),
            "w_uv": inputs["dsa_w_uv"][0].reshape(128, 768), "w_q_idx": inputs["dsa_w_q_idx"][0],
        })
        maps.append(mp)
    return maps


def kernel(**inputs):
    inputs = {k: np.asarray(v) for k, v in inputs.items()}
    kb = build_fused()
    res = kb.run(fused_maps(inputs))
    return scatter_tokens(res, "out")
```

```python
import math
from contextlib import ExitStack
import numpy as np
import ml_dtypes
import concourse.bass as bass
import concourse.mybir as mybir
from concourse.bass import AP
from concourse.bass_utils import run_bass_kernel_spmd

F32 = mybir.dt.float32
BF16 = mybir.dt.bfloat16
AF = mybir.ActivationFunctionType
ALU = mybir.AluOpType
AX = mybir.AxisListType
NPBF = ml_dtypes.bfloat16

D = 1024
T = 8192
NT = 16
TOK = 2048
DFF = 2816
EPS = 1e-6
NEGB = -30000.0


class Buf:
    __slots__ = ("name", "w", "r")

    def __init__(self, name=""):
        self.name = name
        self.w = None
        self.r = {}


class Prog:
    NSLOT = 12

    def __init__(self, nc):
        self.nc = nc
        self.eng = {"pe": nc.tensor, "act": nc.scalar, "dve": nc.vector,
                    "pool": nc.gpsimd, "sp": nc.sync}
        self.es = ExitStack()
        self.sem = {}
        self.cnt = {}
        for e in ("pe", "act", "dve", "pool"):
            self.sem[e] = self.es.enter_context(nc.semaphore("c_" + e))
            self.cnt[e] = 0
        self.slots = {}
        self.slot_i = {}
        for q in ("sp", "pool"):
            lst = []
            for i in range(self.NSLOT):
                key = "d_%s%d" % (q, i)
                self.sem[key] = self.es.enter_context(nc.semaphore(key))
                self.cnt[key] = 0
                lst.append(key)
            self.slots[q] = lst
            self.slot_i[q] = 0
        self.seen = {e: {} for e in self.eng}
        self.outtoks = []

    def _need(self, e, tok):
        if tok is None:
            return
        key, val = tok
        if self.seen[e].get(key, 0) >= val:
            return
        if key in ("pe", "act", "dve", "pool"):
            assert self.cnt[key] >= val, "missing signal on %s" % key
        self.eng[e].wait_ge(self.sem[key], val)
        self.seen[e][key] = val

    def _deps(self, e, reads, writes):
        for b in reads:
            if b.w is not None and not (e == "pe" and b.w[0] == "pe"):
                self._need(e, b.w)
        for b in writes:
            if b.w is not None and b.w[0] != e:
                self._need(e, b.w)
            for k, v in b.r.items():
                if k != e:
                    self._need(e, (k, v))

    def _mark(self, tok, reads, writes):
        for b in reads:
            if b.r.get(tok[0], 0) < tok[1]:
                b.r[tok[0]] = tok[1]
        for b in writes:
            b.w = tok
            b.r = {}

    def op(self, e, fn, reads=(), writes=(), sig=True):
        self._deps(e, reads, writes)
        ins = fn()
        if sig:
            self.cnt[e] += 1
            ins.then_inc(self.sem[e], 1)
            tok = (e, self.cnt[e])
        else:
            tok = (e, self.cnt[e] + 1)
        self._mark(tok, reads, writes)
        return ins

    def dma(self, q, out, in_, reads=(), writes=(), final=False):
        lst = self.slots[q]
        key = lst[self.slot_i[q] % self.NSLOT]
        self.slot_i[q] += 1
        if self.cnt[key] > 0:
            self._need(q, (key, self.cnt[key]))
        self._deps(q, reads, writes)
        ins = self.eng[q].dma_start(out=out, in_=in_)
        self.cnt[key] += 16
        ins.then_inc(self.sem[key], 16)
        tok = (key, self.cnt[key])
        self._mark(tok, reads, writes)
        if final:
            self.outtoks.append(tok)
        return tok

    def barrier(self):
        toks = [(e, self.cnt[e]) for e in ("pe", "act", "dve", "pool") if self.cnt[e] > 0]
        for q in ("sp", "pool"):
            toks += [(k, self.cnt[k]) for k in self.slots[q] if self.cnt[k] > 0]
        for e in self.eng:
            for t in toks:
                if t[0] != e:
                    self._need(e, t)

    def finish(self):
        for tok in self.outtoks:
            self._need("sp", tok)
        for q in ("sp", "pool"):
            for key in self.slots[q]:
                if self.cnt[key] > 0:
                    self._need("sp", (key, self.cnt[key]))
        self.es.close()


class K:
    def __init__(self, name):
        self.nc = bass.Bass("TRN2", target_bir_lowering=False)
        self.P = Prog(self.nc)
        self.es = ExitStack()
        self.n = 0
        self.rr = 0
        nc = self.nc
        self.es.enter_context(nc.allow_non_contiguous_dma(reason="small strided parameter loads"))
        self.ps = [self.es.enter_context(nc.psum_tensor("ps%d" % i, [128, 512], F32)) for i in range(7)]
        self.psb = [Buf("ps%d" % i) for i in range(7)]
        pt0 = self.es.enter_context(nc.psum_tensor("pt0", [128, 1024], BF16))
        ptb0 = Buf("pt0")
        self.pt = [pt0, pt0]
        self.ptb = [ptb0, ptb0]
        self.pti = 0
        self.ident, self.identb = self.sb("ident", [128, 128], BF16)
        self.anti, self.antib = self.sb("anti", [128, 128], BF16)

    def sb(self, name, shape, dt):
        self.n += 1
        t = self.es.enter_context(self.nc.sbuf_tensor("%s_%d" % (name, self.n), shape, dt))
        return t, Buf(name)

    def scope(self):
        kb = self

        class _S:
            def __enter__(self_):
                self_.old = kb.es
                kb.es = ExitStack()
                return kb.es

            def __exit__(self_, *a):
                kb.P.barrier()
                kb.es.close()
                kb.es = self_.old
        return _S()

    alias = None

    def din(self, name, shape, dt=F32):
        if self.alias is not None:
            ap = self.alias[name]
            assert list(ap.shape) == list(shape), (name, ap.shape, shape)
            return ap
        return self.nc.dram_tensor(name, list(shape), dt, kind="ExternalInput").ap()

    def dout(self, name, shape, dt=F32):
        if self.alias is not None:
            ap = self.alias[name]
            assert list(ap.shape) == list(shape), (name, ap.shape, shape)
            return ap
        return self.nc.dram_tensor(name, list(shape), dt, kind="ExternalOutput").ap()

    def dscr(self, name, shape, dt=F32):
        self.n += 1
        return self.nc.dram_tensor("%s_%d" % (name, self.n), list(shape), dt, kind="Internal")

    def dq(self):
        self.rr += 1
        return "sp" if self.rr % 2 else "pool"

    def load_consts(self, ident_d, anti_d):
        st, stb = self.sb("cst", [128, 256], F32)
        self.P.dma("sp", st[:, 0:128], ident_d, writes=[stb])
        self.P.dma("sp", st[:, 128:256], anti_d, writes=[stb])
        nc = self.nc
        self.P.op("dve", lambda: nc.vector.tensor_copy(out=self.ident[:], in_=st[:, 0:128]), reads=[stb], writes=[self.identb])
        self.P.op("dve", lambda: nc.vector.tensor_copy(out=self.anti[:], in_=st[:, 128:256]), reads=[stb], writes=[self.antib])

    def load_w(self, dram, kdim, ncol, name, eng="pool", stage=None):
        nc, P = self.nc, self.P
        kp = min(128, kdim)
        nk = max(1, kdim // 128)
        w, wb = self.sb(name, [kp, nk, ncol], BF16)
        CH = 2048
        if stage is None:
            stage = [self.sb("wst", [128, CH], F32) for _ in range(2)]
        self._wst = stage
        i = 0
        for k in range(nk):
            for c0 in range(0, ncol, CH):
                cw = min(CH, ncol - c0)
                st, stb = stage[i % 2]
                i += 1
                P.dma(self.dq(), st[0:kp, 0:cw], dram[k * 128:k * 128 + kp, c0:c0 + cw], writes=[stb])
                self.cast(eng, w[:, k, c0:c0 + cw], st[0:kp, 0:cw], [stb], [wb])
        return w, wb

    def cast(self, eng, out, in_, reads, writes, sig=True):
        nc = self.nc
        if eng == "pool":
            self.P.op("pool", lambda: nc.gpsimd.tensor_copy(out=out, in_=in_), reads=reads, writes=writes, sig=sig)
        elif eng == "dve":
            self.P.op("dve", lambda: nc.vector.tensor_copy(out=out, in_=in_), reads=reads, writes=writes, sig=sig)
        else:
            self.P.op("act", lambda: nc.scalar.copy(out=out, in_=in_), reads=reads, writes=writes, sig=sig)

    def bcast_row(self, dram_row, ncol, name):
        t, tb = self.sb(name, [128, ncol], F32)
        src = AP(tensor=dram_row.tensor, offset=dram_row.offset, ap=[[0, 128], [1, ncol]])
        self.P.dma("sp", t[:], src, writes=[tb])
        return t, tb

    def rmsnorm(self, x, xb, g, gb, out, outb, dim, scr, scrb, np_=128):
        nc, P = self.nc, self.P
        P.op("act", lambda: nc.scalar.activation(out=out, in_=x, func=AF.Square, accum_out=scr[0:np_, 0:1]),
             reads=[xb], writes=[outb, scrb])
        P.op("act", lambda: nc.scalar.activation(out=scr[0:np_, 1:2], in_=scr[0:np_, 0:1], func=AF.Ln, scale=1.0 / dim, bias=self.eps_t[0:np_, 0:1]),
             reads=[scrb, self.eps_b], writes=[scrb])
        P.op("act", lambda: nc.scalar.activation(out=scr[0:np_, 2:3], in_=scr[0:np_, 1:2], func=AF.Exp, scale=-0.5),
             reads=[scrb], writes=[scrb])
        P.op("dve", lambda: nc.vector.scalar_tensor_tensor(out=out, in0=x, scalar=scr[0:np_, 2:3], in1=g, op0=ALU.mult, op1=ALU.mult),
             reads=[xb, scrb, gb], writes=[outb])

    def mk_eps(self):
        self.eps_t, self.eps_b = self.sb("eps", [128, 1], F32)
        nc = self.nc
        self.P.op("dve", lambda: nc.vector.memset(self.eps_t[:], EPS), writes=[self.eps_b])

    def transpose_to(self, src_list, dst, dstb, reads, evac="dve", np_in=128):
        nc, P = self.nc, self.P
        n = len(src_list)
        i0 = 0
        while i0 < n:
            nb = min(8, n - i0)
            pt = self.pt[self.pti % 2]
            ptb = self.ptb[self.pti % 2]
            self.pti += 1
            w = src_list[i0].shape[-1]
            for j in range(nb):
                s = src_list[i0 + j]
                P.op("pe", lambda s=s, j=j: nc.tensor.transpose(pt[0:w, j * 128:j * 128 + np_in], s, self.ident[0:np_in, 0:np_in]),
                     reads=list(reads) + [self.identb], writes=[ptb], sig=(j == nb - 1))
            src = pt[0:w, 0:nb * 128].rearrange("p (a b) -> p a b", b=128)[:, :, 0:np_in]
            o = dst[0:w, i0:i0 + nb, :]
            if evac == "dve":
                P.op("dve", lambda: nc.vector.tensor_copy(out=o, in_=src), reads=[ptb], writes=[dstb])
            else:
                P.op("act", lambda: nc.scalar.copy(out=o, in_=src), reads=[ptb], writes=[dstb])
            i0 += nb

    def run(self, in_maps):
        self.P.finish()
        self.es.close()
        res = run_bass_kernel_spmd(self.nc, in_maps, core_ids=list(range(8)))
        return res.results


def rel_bucket_np(dist):
    n = np.maximum(dist, 0)
    nf = np.maximum(n, 16).astype(np.float32)
    large = 16 + (np.log(nf / np.float32(16)) / np.float32(math.log(2048 / 16)) * np.float32(16)).astype(np.int32)
    large = np.minimum(large, 31)
    return np.where(n < 16, n, large)


def onehot_table(dist, masked):
    Y = dist.shape[0]
    oh = np.zeros((33, Y), np.float32)
    b = rel_bucket_np(dist)
    ok = ~masked
    oh[b[ok], np.nonzero(ok)[0]] = 1.0
    oh[32, masked] = 1.0
    return oh


Y_SLC = 2304
Y_WIN = 1280
Y_CMP = 5376


def host_tables(c):
    y = np.arange(Y_SLC)
    d = y - 511 + 128 * c
    oh_s = onehot_table(d, d < 0)
    y = np.arange(Y_WIN)
    d = y - 511 + 128 * c
    oh_w = onehot_table(d, (d < 0) | (d >= 512))
    y = np.arange(Y_CMP)
    d = y + 128 * c - 2063
    oh_c = onehot_table(d, d < 0)
    return oh_s, oh_w, oh_c


def build_tables(kb, rel_bias_d, ohs, names_Y):
    nc, P = kb.nc, kb.P
    bt, btb = kb.sb("biasT", [33, 12], F32)
    P.dma("sp", bt[0:32, :], rel_bias_d, writes=[btb])
    b31, b31b = kb.sb("b31", [33, 12], F32)
    src = AP(tensor=rel_bias_d.tensor, offset=rel_bias_d.offset + 31 * 12, ap=[[0, 32], [1, 12]])
    P.dma("sp", b31[0:32, :], src, writes=[b31b])
    bb, bbb = kb.sb("biasb", [33, 12], BF16)
    P.op("dve", lambda: nc.vector.memset(bb[:], NEGB), writes=[bbb])
    P.op("dve", lambda: nc.vector.tensor_tensor(out=bb[0:32, :], in0=bt[0:32, :], in1=b31[0:32, :], op=ALU.subtract),
         reads=[btb, b31b], writes=[bbb])
    outs = []
    for (oh_d, Y, nm) in [(o, y, n) for o, (n, y) in zip(ohs, names_Y)]:
        oh, ohb = kb.sb("oh" + nm, [33, Y], BF16)
        st, stb = kb.sb("ohst" + nm, [33, Y], F32)
        P.dma("sp", st[:], oh_d, writes=[stb])
        P.op("dve", lambda: nc.vector.tensor_copy(out=oh[:], in_=st[:]), reads=[stb], writes=[ohb])
        tt, ttb = kb.sb("tt" + nm, [12, Y], BF16)
        for c0 in range(0, Y, 512):
            cw = min(512, Y - c0)
            ps, psb = kb.ps[0], kb.psb[0]
            P.op("pe", lambda: nc.tensor.matmul(ps[0:12, 0:cw], lhsT=bb[:], rhs=oh[:, c0:c0 + cw], start=True, stop=True),
                 reads=[bbb, ohb], writes=[psb])
            P.op("dve", lambda: nc.vector.tensor_copy(out=tt[:, c0:c0 + cw], in_=ps[0:12, 0:cw]), reads=[psb], writes=[ttb])
        scr = kb.dscr("ttd" + nm, [12, Y], BF16)
        scrb = Buf("ttd" + nm)
        P.dma("sp", scr.ap(), tt[:], reads=[ttb], writes=[scrb])
        outs.append((scr, scrb, Y))
    return outs


def load_band(kb, scr, scrb, Y, Z, pstep, name, zoff=0, dst=None):
    if dst is None:
        dst = kb.sb(name, [128, 12, Z], BF16)
    t, tb = dst
    src = AP(tensor=scr, offset=zoff, ap=[[pstep, 128], [Y, 12], [1, Z]])
    kb.P.dma(kb.dq(), t[:, :, 0:Z], src, reads=[scrb], writes=[tb])
    return t, tb


FM_A = [(0 + 64 * i, 0.125) for i in range(12)] + [(768 + 64 * i, 1.0) for i in range(4)] + \
       [(1024, 1.0), (1088, 1.0), (1280, 1.0), (1344, 1.0)] + [(1572 + 64 * i, 0.125) for i in range(4)]


def proj_phase(kb, x_d, g_d, w, wb, ncols, fm_groups, tm_ranges, fmT_d, tm_d, post_tm=None):
    nc, P = kb.nc, kb.P
    g, gb = kb.bcast_row(g_d, D, "gain")
    xt = [kb.sb("xt", [128, D], F32) for _ in range(2)]
    xn = [kb.sb("xn", [128, D], BF16) for _ in range(2)]
    scr, scrb = kb.sb("scr", [128, 4], F32)
    xnT, xnTb = kb.sb("xnT", [128, 8, 512], BF16)
    ng = len(fm_groups)
    fst, fstb = kb.sb("fst", [64, ng, 512], BF16)
    ntm = sum(n for _, n in tm_ranges)
    tst = [kb.sb("tst", [128, max(ntm, 1)], F32) for _ in range(2)]
    ei = 0
    for blk in range(4):
        for tt in range(4):
            ti = blk * 4 + tt
            x_, xb_ = xt[ti % 2]
            n_, nb_ = xn[ti % 2]
            P.dma(kb.dq(), x_[:], x_d[ti * 128:(ti + 1) * 128, :], writes=[xb_])
            kb.rmsnorm(x_[:], xb_, g[:], gb, n_[:], nb_, D, scr, scrb)
            for half in range(1):
                pt = kb.pt[kb.pti % 2]
                ptb = kb.ptb[kb.pti % 2]
                kb.pti += 1
                for j in range(8):
                    P.op("pe", lambda j=j: nc.tensor.transpose(pt[:, j * 128:(j + 1) * 128], n_[:, j * 128:(j + 1) * 128], kb.ident[:]),
                         reads=[nb_, kb.identb], writes=[ptb], sig=(j == 7))
                P.op("dve", lambda: nc.vector.tensor_copy(out=xnT[:, :, tt * 128:(tt + 1) * 128],
                                                          in_=pt[:, :].rearrange("p (a b) -> p a b", b=128)),
                     reads=[ptb], writes=[xnTb])
            if ntm:
                ts_, tsb_ = tst[ti % 2]
                ps, psb = kb.ps[4], kb.psb[4]
                o = 0
                for (c0, n) in tm_ranges:
                    for k in range(8):
                        P.op("pe", lambda k=k, o=o, c0=c0, n=n: nc.tensor.matmul(ps[:, o:o + n], lhsT=xnT[:, k, tt * 128:(tt + 1) * 128], rhs=w[:, k, c0:c0 + n],
                                                                       start=(k == 0 and o == 0), stop=(k == 7)),
                             reads=[xnTb, wb], writes=[psb], sig=(k == 7))
                    o += n
                P.op("act", lambda: nc.scalar.copy(out=ts_[:, 0:ntm], in_=ps[:, 0:ntm]), reads=[psb], writes=[tsb_])
                if post_tm is not None:
                    post_tm(ts_, tsb_)
                P.dma(kb.dq(), tm_d[ti * 128:(ti + 1) * 128, :], ts_[:, 0:ntm], reads=[tsb_], final=True)
        for gi, (c0, sc) in enumerate(fm_groups):
            ps, psb = kb.ps[gi % 4], kb.psb[gi % 4]
            for k in range(8):
                P.op("pe", lambda k=k, c0=c0: nc.tensor.matmul(ps[0:64, :], lhsT=w[:, k, c0:c0 + 64], rhs=xnT[:, k, :], start=(k == 0), stop=(k == 7)),
                     reads=[xnTb, wb], writes=[psb], sig=(k == 7))
            if ei % 2 == 0:
                P.op("act", lambda: nc.scalar.activation(out=fst[:, gi, :], in_=ps[0:64, :], func=AF.Copy, scale=sc), reads=[psb], writes=[fstb])
            else:
                P.op("dve", lambda: nc.vector.tensor_scalar(out=fst[:, gi, :], in0=ps[0:64, :], scalar1=sc, scalar2=None, op0=ALU.mult), reads=[psb], writes=[fstb])
            ei += 1
        P.dma(kb.dq(), fmT_d[:, :, blk * 512:(blk + 1) * 512], fst[:], reads=[fstb], final=True)


def build_A(kb=None):
    kb = kb or K("A")
    nc, P = kb.nc, kb.P
    x_d = kb.din("x", [TOK, D])
    g_d = kb.din("g", [1, D])
    w_d = kb.din("w_in", [D, 1828])
    gb_d = kb.din("gate_b", [1, 36])
    id_d = kb.din("ident", [128, 128])
    an_d = kb.din("anti", [128, 128])
    fm_d = kb.dout("fmT", [64, 24, TOK], BF16)
    tm_d = kb.dout("tm", [TOK, 256 + 36])
    kb.load_consts(id_d, an_d)
    kb.mk_eps()
    w, wb = kb.load_w(w_d, D, 1828, "w_in")
    gbt, gbb = kb.bcast_row(gb_d, 36, "gateb")

    def post(ts_, tsb_):
        P.op("dve", lambda: nc.vector.tensor_tensor(out=ts_[:, 256:292], in0=ts_[:, 256:292], in1=gbt[:], op=ALU.add), reads=[tsb_, gbb], writes=[tsb_])
        P.op("act", lambda: nc.scalar.activation(out=ts_[:, 256:292], in_=ts_[:, 256:292], func=AF.Exp, scale=-1.0), reads=[tsb_], writes=[tsb_])
        P.op("dve", lambda: nc.vector.tensor_scalar(out=ts_[:, 256:292], in0=ts_[:, 256:292], scalar1=1.0, scalar2=None, op0=ALU.add), reads=[tsb_], writes=[tsb_])
        P.op("dve", lambda: nc.vector.reciprocal(out=ts_[:, 256:292], in_=ts_[:, 256:292]), reads=[tsb_], writes=[tsb_])

    proj_phase(kb, x_d, g_d, w, wb, 1828, FM_A, [(1152, 128), (1408, 128), (1536, 36)], fm_d, tm_d, post_tm=post)
    return kb


def core_tokens(c):
    return np.concatenate([np.arange(128 * (4 * m + c), 128 * (4 * m + c) + 128) for m in range(NT)])


def run_A(inputs):
    kb = build_A()
    ident = np.eye(128, dtype=np.float32)
    anti = np.ascontiguousarray(ident[::-1])
    maps = []
    for core in range(8):
        b, c = core // 4, core % 4
        maps.append({"x": np.ascontiguousarray(inputs["x"][b][core_tokens(c)]),
                     "g": inputs["norm_mix"][0:1], "w_in": inputs["nsa_w_in"][0],
                     "gate_b": inputs["nsa_gate_b"][0:1], "ident": ident, "anti": anti})
    return kb.run(maps)


def mm(kb, out, lhsT, rhs, start, reads, writes, sig=False, stop=True):
    nc = kb.nc
    kb.P.op("pe", lambda: nc.tensor.matmul(out, lhsT=lhsT, rhs=rhs, start=start, stop=stop), reads=reads, writes=writes, sig=sig)


def bc3(ap2d):
    return AP(tensor=ap2d.tensor, offset=ap2d.offset, ap=[list(ap2d.ap[0]), [0, 3], list(ap2d.ap[1])])


class Attn:
    def __init__(self, kb):
        self.kb = kb
        self.pT = [kb.sb("pT", [128, 384], BF16) for _ in range(4)]
        self.i = 0
        self.sb_list = [0, 1, 6]
        self.sb_i = 0
        self.pending = []

    def next_sbank(self):
        b = self.sb_list[self.sb_i % 3]
        self.sb_i += 1
        return b

    def unit(self, s_terms, near_terms, accs, accb, v_ap, vbufs, first):
        kb = self.kb
        nc, P = kb.nc, kb.P
        sbk = self.next_sbank()
        ps, psb = kb.ps[sbk], kb.psb[sbk]
        n = len(s_terms)
        for t, (l, r, bufs) in enumerate(s_terms):
            mm(kb, ps[:, 0:384], l, r, t == 0, bufs, [psb], sig=(t == n - 1 and not near_terms))
        if near_terms:
            l, r, bufs = near_terms
            mm(kb, ps[:, 0:384].rearrange("p (a b) -> p a b", b=128), l, r, False, bufs, [psb], sig=True)
        pT, pTb = self.pT[self.i % 4]
        self.i += 1
        P.op("act", lambda: nc.scalar.activation(out=pT[:], in_=ps[:, 0:384], func=AF.Exp), reads=[psb], writes=[pTb])
        self.pending.append((pT, pTb, accs, accb, v_ap, vbufs, first))
        if len(self.pending) > 2:
            self._pv(*self.pending.pop(0))

    def _pv(self, pT, pTb, accs, accb, v_ap, vbufs, first):
        kb = self.kb
        for hh in range(3):
            mm(kb, accs[hh], pT[:, hh * 128:(hh + 1) * 128], v_ap, first[hh], [pTb] + vbufs, [accb[hh]], sig=(hh == 2))

    def flush(self):
        while self.pending:
            self._pv(*self.pending.pop(0))


def build_B(layer, kb=None):
    kb = kb or K("B")
    nc, P = kb.nc, kb.P
    x_d = kb.din("x", [TOK, D])
    id_d = kb.din("identb", [128, 128], BF16)
    an_d = kb.din("antib", [128, 128], BF16)
    relb_d = kb.din("rel_bias", [32, 12])
    oh_s_d = kb.din("oh_s", [33, Y_SLC])
    oh_w_d = kb.din("oh_w", [33, Y_WIN])
    oh_c_d = kb.din("oh_c", [33, Y_CMP])
    qT_d = kb.din("qT2", [128, 6, TOK], BF16)
    qm_d = kb.din("qmT", [64, 4, TOK], BF16)
    gates_d = kb.din("gates", [TOK, 36])
    ks_d = kb.din("ksT2", [128, T], BF16)
    kw_d = kb.din("kwT2", [128, T], BF16)
    kcr_d = kb.din("kcrT2", [128, T], BF16)
    vcr_d = kb.din("vcrT2", [128, T], BF16)
    vs_d = kb.din("vs", [T, 128])
    vw_d = kb.din("vw", [T, 128])
    ew_d = kb.din("ew", [128, T], BF16)
    ov_d = kb.din("ovl", [128, 4, 128], BF16)
    fb_d = kb.din("fb", [NT, 128, 128])
    w1k_d = kb.din("w1k", [2048, 256]); b1k_d = kb.din("b1k", [256, 1]); w2k_d = kb.din("w2k", [256, 64]); b2k_d = kb.din("b2k", [64, 1])
    w1v_d = kb.din("w1v", [2048, 256]); b1v_d = kb.din("b1v", [256, 1]); w2v_d = kb.din("w2v", [256, 64]); b2v_d = kb.din("b2v", [1, 64])
    posk_d = kb.din("posk", [32, 64]); posv_d = kb.din("posv", [32, 64])
    mem_d = kb.din("mem", [256, D]); gmem_d = kb.din("g_mem", [1, D]); wkv_d = kb.din("w_mem_kv", [D, 512])
    wout_d = kb.din("w_out", [D, D])
    hmid_d = kb.dout("hmid", [TOK, D])

    P.dma("sp", kb.ident[:], id_d, writes=[kb.identb])
    P.dma("sp", kb.anti[:], an_d, writes=[kb.antib])
    kb.mk_eps()
    with kb.scope():
        tabs = build_tables(kb, relb_d, [oh_s_d, oh_w_d, oh_c_d], [("s", Y_SLC), ("w", Y_WIN), ("c", Y_CMP)])
    band_s, band_sb = load_band(kb, tabs[0][0], tabs[0][1], Y_SLC, 2048, 1, "band_s")
    band_w, band_wb = load_band(kb, tabs[1][0], tabs[1][1], Y_WIN, 1024, 1, "band_w")
    bandc = [kb.sb("bandc", [128, 12, 128], BF16) for _ in range(4)]
    bandc_i = 0

    ksT, ksTb = kb.sb("ksT", [128, T], BF16)
    P.dma("sp", ksT[:, 0:4096], ks_d[:, 0:4096], writes=[ksTb])
    P.dma("pool", ksT[:, 4096:T], ks_d[:, 4096:T], writes=[ksTb])
    ew, ewb = kb.sb("ew", [128, T], BF16)
    P.dma("sp", ew[:, 0:4096], ew_d[:, 0:4096], writes=[ewb])
    P.dma("pool", ew[:, 4096:T], ew_d[:, 4096:T], writes=[ewb])
    vs1, vs1b = kb.sb("vs1", [128, 64, 2, 65], BF16)
    P.op("pool", lambda: nc.gpsimd.memset(vs1[:], 1.0), writes=[vs1b])
    with kb.scope():
        vst = [kb.sb("vst", [128, 8, 128], F32) for _ in range(2)]
        for i in range(8):
            st, stb = vst[i % 2]
            P.dma(kb.dq(), st[:], vs_d[i * 1024:(i + 1) * 1024, :].rearrange("(a p) c -> p a c", p=128), writes=[stb])
            P.op("pool", lambda: nc.gpsimd.tensor_copy(out=vs1[:, i * 8:(i + 1) * 8, :, 0:64], in_=st[:].rearrange("p a (g d) -> p a g d", g=2)),
                 reads=[stb], writes=[vs1b])
    RC, RCb = kb.sb("RC", [128, 4, 2, 193], BF16)
    P.op("pool", lambda: nc.gpsimd.memset(RC[:], 1.0), writes=[RCb])
    ovt, ovtb = kb.sb("ovt", [128, 4, 128], BF16)
    P.dma("sp", ovt[:], ov_d, writes=[ovtb])
    for g in range(2):
        P.op("pool", lambda: nc.gpsimd.tensor_copy(out=RC[:, :, g, 0:128], in_=ovt[:]), reads=[ovtb], writes=[RCb])
    kcT, kcTb = kb.sb("kcT", [128, 512], BF16)

    with ExitStack() as es2:
        def tmp(name, shape, dt):
            kb.n += 1
            return es2.enter_context(nc.sbuf_tensor("%s_%d" % (name, kb.n), shape, dt)), Buf(name)
        rawT, rawTb = tmp("rawT", [128, T + 32], BF16)
        w1, w1b = tmp("w1", [128, 32, 256], BF16)
        w1st, w1stb = tmp("w1st", [128, 8, 256], F32)
        w2, w2b = tmp("w2", [128, 2, 128], BF16)
        w2st, w2stb = tmp("w2st", [128, 2, 64], F32)
        h1T, h1Tb = tmp("h1T", [128, 2, 512], BF16)
        u_, ub = tmp("u", [128, 512], F32)
        t1, t1b = tmp("t1", [128, 512], F32)
        t2, t2b = tmp("t2", [128, 512], F32)
        cb, cbb = tmp("cb", [128, 2], F32)
        b1t, b1tb = tmp("b1t", [128, 2], F32)
        b2d, b2db = tmp("b2d", [128, 1], F32)
        b2r, b2rb = tmp("b2r", [128, 64], F32)
        pst, pstb = tmp("pst", [32, 64], F32)
        psb16, psb16b = tmp("psb16", [32, 64], BF16)
        posT, posTb = tmp("posT", [64, 1, 32], BF16)
        for kv in range(2):
            raw_d = kcr_d if kv == 0 else vcr_d
            w1_d = w1k_d if kv == 0 else w1v_d
            b1_d = b1k_d if kv == 0 else b1v_d
            w2_d = w2k_d if kv == 0 else w2v_d
            pos_d = posk_d if kv == 0 else posv_d
            P.op("dve", lambda: nc.vector.memset(rawT[:, T:T + 32], 0.0), writes=[rawTb])
            P.dma("sp", rawT[:, 0:4096], raw_d[:, 0:4096], writes=[rawTb])
            P.dma("pool", rawT[:, 4096:T], raw_d[:, 4096:T], writes=[rawTb])
            for q4 in range(4):
                for half in range(2):
                    P.dma(kb.dq(), w1st[half * 64:(half + 1) * 64, :, :], w1_d[q4 * 512:(q4 + 1) * 512, :].rearrange("(l d) h -> d l h", d=64), writes=[w1stb])
                P.op("dve", lambda: nc.vector.tensor_copy(out=w1[:, q4 * 8:(q4 + 1) * 8, :], in_=w1st[:]), reads=[w1stb], writes=[w1b])
            P.dma("sp", w2st[:], w2_d.rearrange("(c p) d -> p c d", p=128), writes=[w2stb])
            for half in range(2):
                P.op("dve", lambda: nc.vector.tensor_copy(out=w2[:, :, half * 64:(half + 1) * 64], in_=w2st[:]), reads=[w2stb], writes=[w2b])
            P.dma("sp", b1t[:], b1_d.rearrange("(c p) o -> p (c o)", p=128), writes=[b1tb])
            P.dma("sp", pst[:], pos_d, writes=[pstb])
            P.op("dve", lambda: nc.vector.tensor_copy(out=psb16[:], in_=pst[:]), reads=[pstb], writes=[psb16b])
            kb.transpose_to([psb16[:, :]], posT, posTb, [psb16b], np_in=32)
            for hc in range(2):
                ps, psb = kb.ps[2], kb.psb[2]
                for l in range(32):
                    mm(kb, ps[:, 0:1], w1[0:64, l, hc * 128:(hc + 1) * 128], posT[:, 0, l:l + 1], l == 0, [w1b, posTb], [psb], sig=(l == 31))
                P.op("dve", lambda: nc.vector.tensor_tensor(out=cb[:, hc:hc + 1], in0=ps[:, 0:1], in1=b1t[:, hc:hc + 1], op=ALU.add), reads=[psb, b1tb], writes=[cbb])
            if kv == 0:
                for half in range(2):
                    P.dma("sp", b2d[half * 64:(half + 1) * 64, :], b2k_d, writes=[b2db])
            else:
                src = AP(tensor=b2v_d.tensor, offset=b2v_d.offset, ap=[[0, 128], [1, 64]])
                P.dma("sp", b2r[:], src, writes=[b2rb])
            for g in range(2):
                gs = slice(g * 64, (g + 1) * 64)
                for hc in range(2):
                    ps, psb = kb.ps[2 + hc], kb.psb[2 + hc]
                    for l in range(32):
                        rhs = AP(tensor=rawT.tensor if hasattr(rawT, "tensor") else rawT[:].tensor, offset=rawT[gs, l:l + 1].offset,
                                 ap=[list(rawT[gs, :].ap[0]), [16, 512]])
                        mm(kb, ps[:, :], w1[gs, l, hc * 128:(hc + 1) * 128], rhs, l == 0, [w1b, rawTb], [psb], sig=(l == 31))
                    P.op("act", lambda: nc.scalar.activation(out=u_[:], in_=ps[:, :], func=AF.Identity, bias=cb[:, hc:hc + 1]), reads=[psb, cbb], writes=[ub])
                    P.op("dve", lambda: nc.vector.tensor_tensor(out=t1[:], in0=u_[:], in1=u_[:], op=ALU.mult), reads=[ub], writes=[t1b])
                    P.op("dve", lambda: nc.vector.tensor_scalar(out=t1[:], in0=t1[:], scalar1=0.044715, scalar2=1.0, op0=ALU.mult, op1=ALU.add), reads=[t1b], writes=[t1b])
                    P.op("dve", lambda: nc.vector.tensor_tensor(out=t1[:], in0=t1[:], in1=u_[:], op=ALU.mult), reads=[t1b, ub], writes=[t1b])
                    P.op("act", lambda: nc.scalar.activation(out=t2[:], in_=t1[:], func=AF.Tanh, scale=0.7978845608028654), reads=[t1b], writes=[t2b])
                    P.op("dve", lambda: nc.vector.tensor_scalar(out=t2[:], in0=t2[:], scalar1=0.5, scalar2=0.5, op0=ALU.mult, op1=ALU.add), reads=[t2b], writes=[t2b])
                    P.op("dve", lambda: nc.vector.tensor_tensor(out=h1T[:, hc, :], in0=t2[:], in1=u_[:], op=ALU.mult), reads=[t2b, ub], writes=[h1Tb])
                if kv == 0:
                    ps, psb = kb.ps[4], kb.psb[4]
                    for hc in range(2):
                        mm(kb, ps[:, :], w2[:, hc, :], h1T[:, hc, :], hc == 0, [w2b, h1Tb], [psb], sig=(hc == 1))
                    P.op("act", lambda: nc.scalar.activation(out=kcT[gs, :], in_=ps[gs, :], func=AF.Identity, bias=b2d[gs, 0:1]), reads=[psb, b2db], writes=[kcTb])
                else:
                    for j in range(4):
                        ps, psb = kb.ps[4], kb.psb[4]
                        for hc in range(2):
                            mm(kb, ps[:, 0:64], h1T[:, hc, j * 128:(j + 1) * 128], w2[:, hc, 0:64], hc == 0, [w2b, h1Tb], [psb], sig=(hc == 1))
                        P.op("dve", lambda: nc.vector.tensor_tensor(out=RC[:, j, g, 129:193], in0=ps[:, 0:64], in1=b2r[:], op=ALU.add), reads=[psb, b2rb], writes=[RCb])

    P.barrier()
    kmT, kmTb = kb.sb("kmT", [64, 4, 256], BF16)
    vm1, vm1b = kb.sb("vm1", [128, 2, 4, 65], BF16)
    P.op("pool", lambda: nc.gpsimd.memset(vm1[:], 1.0), writes=[vm1b])
    gm, gmb = kb.bcast_row(gmem_d, D, "gmem")
    wkv, wkvb = kb.sb("wkv", [128, 8, 512], BF16)
    with ExitStack() as es2:
        def tmp(name, shape, dt):
            kb.n += 1
            return es2.enter_context(nc.sbuf_tensor("%s_%d" % (name, kb.n), shape, dt)), Buf(name)
        mt, mtb = tmp("mt", [128, D], F32)
        mn, mnb = tmp("mn", [128, D], BF16)
        mnT, mnTb = tmp("mnT", [128, 8, 256], BF16)
        scr, scrb = tmp("mscr", [128, 4], F32)
        wst = [tmp("wst2", [128, 512], F32) for _ in range(2)]
        for k in range(8):
            st, stb = wst[k % 2]
            P.dma(kb.dq(), st[:], wkv_d[k * 128:(k + 1) * 128, :], writes=[stb])
            P.op("dve", lambda: nc.vector.tensor_copy(out=wkv[:, k, :], in_=st[:]), reads=[stb], writes=[wkvb])
        for tl in range(2):
            P.dma("sp", mt[:], mem_d[tl * 128:(tl + 1) * 128, :], writes=[mtb])
            kb.rmsnorm(mt[:], mtb, gm[:], gmb, mn[:], mnb, D, scr, scrb)
            pt = kb.pt[kb.pti % 2]; ptb = kb.ptb[kb.pti % 2]; kb.pti += 1
            for j in range(8):
                P.op("pe", lambda j=j: nc.tensor.transpose(pt[:, j * 128:(j + 1) * 128], mn[:, j * 128:(j + 1) * 128], kb.ident[:]),
                     reads=[mnb, kb.identb], writes=[ptb], sig=(j == 7))
            P.op("dve", lambda: nc.vector.tensor_copy(out=mnT[:, :, tl * 128:(tl + 1) * 128], in_=pt[:, :].rearrange("p (a b) -> p a b", b=128)), reads=[ptb], writes=[mnTb])
        for h in range(4):
            ps, psb = kb.ps[2], kb.psb[2]
            for k in range(8):
                mm(kb, ps[0:64, 0:256], wkv[:, k, h * 64:(h + 1) * 64], mnT[:, k, :], k == 0, [wkvb, mnTb], [psb], sig=(k == 7))
            P.op("dve", lambda: nc.vector.tensor_copy(out=kmT[:, h, :], in_=ps[0:64, 0:256]), reads=[psb], writes=[kmTb])
        for tl in range(2):
            ps, psb = kb.ps[3], kb.psb[3]
            for k in range(8):
                mm(kb, ps[:, 0:256], mnT[:, k, tl * 128:(tl + 1) * 128], wkv[:, k, 256:512], k == 0, [wkvb, mnTb], [psb], sig=(k == 7))
            P.op("dve", lambda: nc.vector.tensor_copy(out=vm1[:, tl, :, 0:64], in_=ps[:, 0:256].rearrange("p (h d) -> p h d", d=64)), reads=[psb], writes=[vm1b])
    P.barrier()
    wout, woutb = kb.sb("wout", [128, 8, D], BF16)
    with kb.scope():
        wst = [kb.sb("wst3", [128, 1024], F32) for _ in range(2)]
        for k in range(8):
            st, stb = wst[k % 2]
            P.dma(kb.dq(), st[:], wout_d[k * 128:(k + 1) * 128, :], writes=[stb])
            P.op("dve", lambda: nc.vector.tensor_copy(out=wout[:, k, :], in_=st[:]), reads=[stb], writes=[woutb])

    at = Attn(kb)
    qs = [kb.sb("qs", [128, 6, 128], BF16) for _ in range(2)]
    qms = [kb.sb("qms", [64, 4, 128], BF16) for _ in range(2)]
    gts = [kb.sb("gts", [128, 12, 3], F32) for _ in range(2)]
    kws = [kb.sb("kws", [128, 1024], BF16) for _ in range(2)]
    vws = [kb.sb("vws", [128, 8, 2, 65], BF16) for _ in range(2)]
    vwst = [kb.sb("vwst", [128, 8, 128], F32)] * 2
    for (t_, b_) in vws:
        P.op("pool", lambda: nc.gpsimd.memset(t_[:], 1.0), writes=[b_])
    fbs = [kb.sb("fbs", [128, 128], F32) for _ in range(2)]
    xts = [kb.sb("xts", [128, D], F32)] * 2
    cat, catb = kb.sb("cat", [128, D], F32)
    catbf, catbfb = kb.sb("catbf", [128, D], BF16)
    catT, catTb = kb.sb("catT", [128, 8, 128], BF16)
    pslc, pslcb = kb.sb("pslc", [128, 2, 128], F32)
    wk, wkb = kb.sb("wk", [128, 128], F32)
    m8, m8b = kb.sb("m8", [128, 16], F32)
    sbf, sbfb = kb.sb("sbf", [128, 128], BF16)
    sbT, sbTb = kb.sb("sbT", [128, 2, 128], BF16)
    sm, smb = kb.sb("sm", [128, 16], F32)
    pm, pmb = kb.sb("pm", [128, 256], BF16)
    accbank = 2

    def next_acc():
        nonlocal accbank
        b = accbank
        accbank = 2 + (accbank - 2 + 1) % 4
        return b

    for m in range(NT):
        q_, qb_ = qs[m % 2]
        qm_, qmb_ = qms[m % 2]
        gt_, gtb_ = gts[m % 2]
        kw_, kwb_ = kws[m % 2]
        vw_, vwb_ = vws[m % 2]
        vwst_, vwstb_ = vwst[m % 2]
        fb_, fbb_ = fbs[m % 2]
        xt_, xtb_ = xts[m % 2]
        tsl = slice(m * 128, (m + 1) * 128)
        P.dma("sp", q_[:], qT_d[:, :, tsl], writes=[qb_])
        P.dma("sp", qm_[:], qm_d[:, :, tsl], writes=[qmb_])
        P.dma("sp", gt_[:], gates_d[tsl, :].rearrange("p (h b) -> p h b", b=3), writes=[gtb_])
        P.dma("sp", fb_[:], fb_d[m], writes=[fbb_])
        P.dma("pool", xt_[:], x_d[tsl, :], writes=[xtb_])
        kt0 = max(0, 4 * m - 4)
        nkw = 4 * m + 4 - kt0
        P.dma("pool", kw_[:, 0:nkw * 128], kw_d[:, kt0 * 128:(4 * m + 4) * 128], writes=[kwb_])
        P.dma("sp", vwst_[:, 0:nkw, :], vw_d[kt0 * 128:(4 * m + 4) * 128, :].rearrange("(a p) c -> p a c", p=128), writes=[vwstb_])
        P.op("pool", lambda: nc.gpsimd.tensor_copy(out=vw_[:, 0:nkw, :, 0:64], in_=vwst_[:, 0:nkw, :].rearrange("p a (g d) -> p a g d", g=2)),
             reads=[vwstb_], writes=[vwb_])
        cband = {}
        for j in range(4):
            v = m - 4 * j
            if 0 <= v <= 6:
                dst = bandc[bandc_i % 4]
                bandc_i += 1
                load_band(kb, tabs[2][0], tabs[2][1], Y_CMP, 128, 16, "bc", zoff=512 * v, dst=dst)
                cband[j] = dst
        for g in range(2):
            gs = slice(g * 64, (g + 1) * 64)
            for tr in range(2):
                bA, bB = next_acc(), next_acc()
                accs = [kb.ps[bA][:, 0:193], kb.ps[bA][:, 193:386], kb.ps[bB][:, 0:193]]
                accb = [kb.psb[bA], kb.psb[bA], kb.psb[bB]]
                js = [j for j in range(4) if m - 4 * j >= 0]
                for ji, j in enumerate(js):
                    s_terms = [(kcT[gs, j * 128:(j + 1) * 128], q_[gs, 3 * tr:3 * tr + 3, :], [kcTb, qb_])]
                    near = None
                    if j in cband:
                        bt, btb = cband[j]
                        near = (kb.anti[:], bt[:, 6 * g + 3 * tr:6 * g + 3 * tr + 3, :], [kb.antib, btb])
                    at.unit(s_terms, near, accs, accb, RC[:, j, g, :], [RCb], [ji == 0, False, ji == 0])
                at.flush()
                for hh in range(3):
                    h = 6 * g + 3 * tr + hh
                    U = accs[hh]
                    P.op("dve", lambda: nc.vector.tensor_scalar(out=sm[:, 0:1], in0=U[:, 128:129], scalar1=1e-30, scalar2=None, op0=ALU.max), reads=[accb[hh]], writes=[smb])
                    P.op("dve", lambda: nc.vector.reciprocal(out=sm[:, 1:2], in_=sm[:, 0:1]), reads=[smb], writes=[smb])
                    if tr == 0 and hh == 0:
                        P.op("dve", lambda: nc.vector.tensor_scalar(out=pslc[:, g, :], in0=U[:, 0:128], scalar1=sm[:, 1:2], scalar2=None, op0=ALU.mult), reads=[accb[hh], smb], writes=[pslcb])
                    else:
                        P.op("dve", lambda: nc.vector.scalar_tensor_tensor(out=pslc[:, g, :], in0=U[:, 0:128], scalar=sm[:, 1:2], in1=pslc[:, g, :], op0=ALU.mult, op1=ALU.add), reads=[accb[hh], smb, pslcb], writes=[pslcb])
                    P.op("dve", lambda: nc.vector.tensor_tensor(out=sm[:, 2:3], in0=sm[:, 1:2], in1=gt_[:, h, 0:1], op=ALU.mult), reads=[smb, gtb_], writes=[smb])
                    P.op("dve", lambda: nc.vector.tensor_scalar(out=cat[:, h * 64:(h + 1) * 64], in0=U[:, 129:193], scalar1=sm[:, 2:3], scalar2=None, op0=ALU.mult), reads=[accb[hh], smb], writes=[catb])
            P.op("dve", lambda: nc.vector.tensor_tensor(out=wk[:], in0=pslc[:, g, :], in1=fb_[:], op=ALU.add), reads=[pslcb, fbb_], writes=[wkb])
            P.op("dve", lambda: nc.vector.max(out=m8[:, 0:8], in_=wk[:]), reads=[wkb], writes=[m8b])
            P.op("dve", lambda: nc.vector.match_replace(out=pslc[:, g, :], in_to_replace=m8[:, 0:8], in_values=wk[:], imm_value=-3e30), reads=[wkb, m8b], writes=[pslcb])
            P.op("dve", lambda: nc.vector.max(out=m8[:, 8:16], in_=pslc[:, g, :]), reads=[pslcb], writes=[m8b])
            P.op("dve", lambda: nc.vector.tensor_scalar(out=wk[:], in0=wk[:], scalar1=m8[:, 15:16], scalar2=-NEGB, op0=ALU.is_ge, op1=ALU.mult), reads=[wkb, m8b], writes=[wkb])
            P.op("dve", lambda: nc.vector.tensor_scalar(out=sbf[:], in0=wk[:], scalar1=NEGB, scalar2=None, op0=ALU.add), reads=[wkb], writes=[sbfb])
            pt = kb.pt[kb.pti % 2]; ptb = kb.ptb[kb.pti % 2]; kb.pti += 1
            P.op("pe", lambda: nc.tensor.transpose(pt[:, 0:128], sbf[:], kb.ident[:]), reads=[sbfb, kb.identb], writes=[ptb])
            P.op("dve", lambda: nc.vector.tensor_copy(out=sbT[:, g, :], in_=pt[:, 0:128]), reads=[ptb], writes=[sbTb])
        for br in range(2):
            for g in range(2):
                gs = slice(g * 64, (g + 1) * 64)
                for tr in range(2):
                    bA = next_acc()
                    accs = [kb.ps[bA][:, 65 * hh:65 * hh + 65] for hh in range(3)]
                    accb = [kb.psb[bA]] * 3
                    kts = list(range(0, 4 * m + 4)) if br == 0 else list(range(kt0, 4 * m + 4))
                    for ki, kt in enumerate(kts):
                        u = kt - 4 * m
                        if br == 0:
                            s_terms = [(ksT[gs, kt * 128:(kt + 1) * 128], q_[gs, 3 * tr:3 * tr + 3, :], [ksTb, qb_]),
                                       (ew[:, kt * 128:(kt + 1) * 128], bc3(sbT[:, g, :]), [ewb, sbTb])]
                            near = None
                            if u >= -12:
                                z0 = 128 * (3 - u)
                                near = (kb.anti[:], band_s[:, 6 * g + 3 * tr:6 * g + 3 * tr + 3, z0:z0 + 128], [kb.antib, band_sb])
                            v_ap, vb_ = vs1[:, kt, g, :], [vs1b]
                        else:
                            kk = kt - kt0
                            s_terms = [(kw_[gs, kk * 128:(kk + 1) * 128], q_[gs, 3 * tr:3 * tr + 3, :], [kwb_, qb_])]
                            z0 = 128 * (3 - u)
                            near = (kb.anti[:], band_w[:, 6 * g + 3 * tr:6 * g + 3 * tr + 3, z0:z0 + 128], [kb.antib, band_wb])
                            v_ap, vb_ = vw_[:, kk, g, :], [vwb_]
                        at.unit(s_terms, near, accs, accb, v_ap, vb_, [ki == 0, False, False])
                    at.flush()
                    for hh in range(3):
                        h = 6 * g + 3 * tr + hh
                        O = accs[hh]
                        P.op("dve", lambda: nc.vector.tensor_scalar(out=sm[:, 0:1], in0=O[:, 64:65], scalar1=1e-30, scalar2=None, op0=ALU.max), reads=[accb[hh]], writes=[smb])
                        P.op("dve", lambda: nc.vector.reciprocal(out=sm[:, 1:2], in_=sm[:, 0:1]), reads=[smb], writes=[smb])
                        P.op("dve", lambda: nc.vector.tensor_tensor(out=sm[:, 2:3], in0=sm[:, 1:2], in1=gt_[:, h, 1 + br:2 + br], op=ALU.mult), reads=[smb, gtb_], writes=[smb])
                        P.op("dve", lambda: nc.vector.scalar_tensor_tensor(out=cat[:, h * 64:(h + 1) * 64], in0=O[:, 0:64], scalar=sm[:, 2:3], in1=cat[:, h * 64:(h + 1) * 64], op0=ALU.mult, op1=ALU.add),
                             reads=[accb[hh], smb, catb], writes=[catb])
        for h in range(4):
            sbk = at.next_sbank()
            ps, psb = kb.ps[sbk], kb.psb[sbk]
            for tl in range(2):
                mm(kb, ps[:, tl * 128:(tl + 1) * 128], kmT[:, h, tl * 128:(tl + 1) * 128], qm_[:, h, :], tl == 0, [kmTb, qmb_], [psb], sig=(tl == 1))
            P.op("act", lambda: nc.scalar.activation(out=pm[:], in_=ps[:, 0:256], func=AF.Exp), reads=[psb], writes=[pmb])
            bA = next_acc()
            O = kb.ps[bA][:, 0:65]
            for tl in range(2):
                mm(kb, O, pm[:, tl * 128:(tl + 1) * 128], vm1[:, tl, h, :], tl == 0, [pmb, vm1b], [kb.psb[bA]], sig=(tl == 1))
            P.op("dve", lambda: nc.vector.reciprocal(out=sm[:, 4:5], in_=O[:, 64:65]), reads=[kb.psb[bA]], writes=[smb])
            P.op("dve", lambda: nc.vector.tensor_scalar(out=cat[:, 768 + h * 64:768 + (h + 1) * 64], in0=O[:, 0:64], scalar1=sm[:, 4:5], scalar2=None, op0=ALU.mult), reads=[kb.psb[bA], smb], writes=[catb])
        P.op("act", lambda: nc.scalar.copy(out=catbf[:], in_=cat[:]), reads=[catb], writes=[catbfb])
        pt = kb.pt[kb.pti % 2]; ptb = kb.ptb[kb.pti % 2]; kb.pti += 1
        for j in range(8):
            P.op("pe", lambda j=j: nc.tensor.transpose(pt[:, j * 128:(j + 1) * 128], catbf[:, j * 128:(j + 1) * 128], kb.ident[:]),
                 reads=[catbfb, kb.identb], writes=[ptb], sig=(j == 7))
        P.op("dve", lambda: nc.vector.tensor_copy(out=catT[:], in_=pt[:, :].rearrange("p (a b) -> p a b", b=128)), reads=[ptb], writes=[catTb])
        for half in range(2):
            bA = next_acc()
            ps, psb = kb.ps[bA], kb.psb[bA]
            for k in range(8):
                mm(kb, ps[:, :], catT[:, k, :], wout[:, k, half * 512:(half + 1) * 512], k == 0, [catTb, woutb], [psb], sig=(k == 7))
            P.op("dve", lambda: nc.vector.tensor_tensor(out=xt_[:, half * 512:(half + 1) * 512], in0=ps[:, :], in1=xt_[:, half * 512:(half + 1) * 512], op=ALU.add), reads=[psb, xtb_], writes=[xtb_])
        P.dma("sp", hmid_d[tsl, :], xt_[:], reads=[xtb_], final=True)
    return kb


def gather_seq(arrs, axis):
    shp = list(arrs[0].shape)
    shp[axis] = T
    out = np.zeros(shp, arrs[0].dtype)
    for c in range(4):
        idx = [slice(None)] * len(shp)
        idx[axis] = core_tokens(c)
        out[tuple(idx)] = arrs[c]
    return out


def consts_B(c):
    ident = np.eye(128, dtype=np.float32)
    oh_s, oh_w, oh_c = host_tables(c)
    ew = (np.arange(T)[None, :] // 64 == np.arange(128)[:, None]).astype(NPBF)
    n = np.arange(512)
    cs, ce = n * 16, n * 16 + 31
    ss = np.arange(128) * 64
    ov = ((cs[:, None] <= ss[None, :] + 63) & (ce[:, None] >= ss[None, :])).astype(np.float32)
    ov[511] = 0
    ovl = np.ascontiguousarray(ov.reshape(4, 128, 128).transpose(1, 0, 2)).astype(NPBF)
    fb = np.zeros((NT, 128, 128), np.float32)
    blk = np.arange(128)[None, :]
    for m in range(NT):
        t = 128 * (4 * m + c) + np.arange(128)[:, None]
        cur = t // 64
        forced = (blk == 0) | (blk == cur) | (blk == cur - 1)
        adm = (blk * 64) <= t
        fb[m] = np.where(adm, 1e4 * forced, -1e30)
    return {"identb": ident.astype(NPBF), "antib": np.ascontiguousarray(ident[::-1]).astype(NPBF),
            "oh_s": oh_s, "oh_w": oh_w, "oh_c": oh_c, "ew": ew, "ovl": ovl, "fb": fb}


def to2(fm, lo):
    return np.ascontiguousarray(np.concatenate([fm[:, lo, :], fm[:, lo + 1, :]], axis=0))


def run_B(inputs, resA):
    kb = build_B(0)
    maps = []
    for core in range(8):
        b, c = core // 4, core % 4
        grp = [resA[4 * b + cc] for cc in range(4)]
        fms = [np.asarray(r["fmT"]) for r in grp]
        tms = [np.asarray(r["tm"]) for r in grp]
        fm = fms[c]
        mp = consts_B(c)
        mp["x"] = np.ascontiguousarray(inputs["x"][b][core_tokens(c)])
        mp["rel_bias"] = inputs["rel_bias"]
        q = fm[:, 0:12, :]
        mp["qT2"] = np.ascontiguousarray(np.concatenate([q[:, 0:6, :], q[:, 6:12, :]], axis=0))
        mp["qmT"] = np.ascontiguousarray(fm[:, 20:24, :])
        mp["gates"] = np.ascontiguousarray(tms[c][:, 256:292])
        mp["kcrT2"] = gather_seq([to2(f, 12) for f in fms], 1)
        mp["vcrT2"] = gather_seq([to2(f, 14) for f in fms], 1)
        mp["ksT2"] = gather_seq([to2(f, 16) for f in fms], 1)
        mp["kwT2"] = gather_seq([to2(f, 18) for f in fms], 1)
        mp["vs"] = gather_seq([np.ascontiguousarray(t_[:, 0:128]) for t_ in tms], 0)
        mp["vw"] = gather_seq([np.ascontiguousarray(t_[:, 128:256]) for t_ in tms], 0)
        mp["w1k"] = inputs["nsa_cmp_k_w1"][0]; mp["b1k"] = inputs["nsa_cmp_k_b1"][0].reshape(256, 1)
        mp["w2k"] = inputs["nsa_cmp_k_w2"][0]; mp["b2k"] = inputs["nsa_cmp_k_b2"][0].reshape(64, 1)
        mp["w1v"] = inputs["nsa_cmp_v_w1"][0]; mp["b1v"] = inputs["nsa_cmp_v_b1"][0].reshape(256, 1)
        mp["w2v"] = inputs["nsa_cmp_v_w2"][0]; mp["b2v"] = inputs["nsa_cmp_v_b2"][0].reshape(1, 64)
        mp["posk"] = inputs["nsa_cmp_pos_k"][0]; mp["posv"] = inputs["nsa_cmp_pos_v"][0]
        mp["mem"] = inputs["mem"][b]; mp["g_mem"] = inputs["norm_mem"][0:1]; mp["w_mem_kv"] = inputs["w_mem_kv"][0]
        mp["w_out"] = inputs["w_out"][0]
        maps.append(mp)
    return kb.run(maps)


def scatter_tokens(per_core, key):
    out = np.zeros((2, T, D), np.float32)
    for core in range(8):
        b, c = core // 4, core % 4
        out[b][core_tokens(c)] = np.asarray(per_core[core][key])
    return out


def kernel_unfused(**inputs):
    inputs = {k: np.asarray(v) for k, v in inputs.items()}
    resA = run_A(inputs)
    resB = run_B(inputs, resA)
    resF = run_F(inputs, [r["hmid"] for r in resB], 0, False)
    resC = run_C(inputs, resF)
    resG = run_F(inputs, [r["hmid"] for r in resC], 1, True)
    return scatter_tokens(resG, "h")


def build_F(final, nproj, kb=None):
    kb = kb or K("F")
    nc, P = kb.nc, kb.P
    hm_d = kb.din("hmid", [TOK, D])
    id_d = kb.din("identb", [128, 128], BF16)
    g_d = kb.din("g_ffn", [1, D])
    wg_d = kb.din("wg", [D, DFF]); wu_d = kb.din("wu", [D, DFF]); wd_d = kb.din("wd", [DFF, D])
    g2_d = kb.din("g2", [1, D])
    h_d = kb.dout("h", [TOK, D])
    if not final:
        win_d = kb.din("w_in2", [D, nproj])
        pr_d = kb.dout("pr", [TOK, nproj])
    P.dma("sp", kb.ident[:], id_d, writes=[kb.identb])
    kb.mk_eps()
    g, gb = kb.bcast_row(g_d, D, "gffn")
    g2, g2b = kb.bcast_row(g2_d, D, "g2")
    H, Hb = kb.sb("H", [128, NT, D], F32)
    Hbs = [Buf("H%d" % i) for i in range(NT)]
    hnT, hnTb = kb.sb("hnT", [128, 8, TOK], BF16)
    hn, hnb = kb.sb("hn", [128, D], BF16)
    scr, scrb = kb.sb("scr", [128, 4], F32)

    def norm_T(gt, gtb):
        for ti in range(NT):
            kb.rmsnorm(H[:, ti, :], Hbs[ti], gt[:], gtb, hn[:], hnb, D, scr, scrb)
            pt = kb.pt[kb.pti % 2]; ptb = kb.ptb[kb.pti % 2]; kb.pti += 1
            for j in range(8):
                P.op("pe", lambda j=j: nc.tensor.transpose(pt[:, j * 128:(j + 1) * 128], hn[:, j * 128:(j + 1) * 128], kb.ident[:]),
                     reads=[hnb, kb.identb], writes=[ptb], sig=(j == 7))
            P.op("dve", lambda: nc.vector.tensor_copy(out=hnT[:, :, ti * 128:(ti + 1) * 128], in_=pt[:, :].rearrange("p (a b) -> p a b", b=128)), reads=[ptb], writes=[hnTb])

    for ti in range(NT):
        P.dma(kb.dq(), H[:, ti, :], hm_d[ti * 128:(ti + 1) * 128, :], writes=[Hbs[ti]])
    norm_T(g, gb)
    wbuf = [(kb.sb("wgs", [128, 8, 512], BF16), kb.sb("wus", [128, 8, 512], BF16), kb.sb("wds", [128, 4, D], BF16)) for _ in range(2)]
    stg = [kb.sb("stg", [128, 1024], F32) for _ in range(3)]
    sgs = [kb.sb("sg", [128, 512], F32) for _ in range(2)]
    abs_ = [kb.sb("ab", [128, 512], BF16) for _ in range(2)]
    aTs = [kb.sb("aT", [128, 4, 128], BF16) for _ in range(2)]
    si = 0

    def load_fg(fg):
        nonlocal si
        (wgs, wgsb), (wus, wusb), (wds, wdsb) = wbuf[fg % 2]
        c0 = fg * 512
        cw = min(512, DFF - c0)
        nch = cw // 128
        for (wsrc, wdst, wdstb) in ((wg_d, wgs, wgsb), (wu_d, wus, wusb)):
            for k in range(8):
                st, stb = stg[si % 3]; si += 1
                P.dma(kb.dq(), st[:, 0:cw], wsrc[k * 128:(k + 1) * 128, c0:c0 + cw], writes=[stb])
                kb.cast("pool", wdst[:, k, 0:cw], st[:, 0:cw], [stb], [wdstb])
        for ch in range(nch):
            st, stb = stg[si % 3]; si += 1
            P.dma(kb.dq(), st[:], wd_d[c0 + ch * 128:c0 + (ch + 1) * 128, :], writes=[stb])
            kb.cast("pool", wds[:, ch, :], st[:], [stb], [wdsb])

    def stage1(fg, ti):
        (wgs, wgsb), (wus, wusb), _ = wbuf[fg % 2]
        cw = min(512, DFF - fg * 512)
        tsl = slice(ti * 128, (ti + 1) * 128)
        b0 = 0 if ti % 2 == 0 else 4
        pg, pgb = kb.ps[b0], kb.psb[b0]
        pu, pub = kb.ps[b0 + 1], kb.psb[b0 + 1]
        sg, sgb = sgs[ti % 2]
        ab, abb = abs_[ti % 2]
        for k in range(8):
            mm(kb, pg[:, 0:cw], hnT[:, k, tsl], wgs[:, k, 0:cw], k == 0, [hnTb, wgsb], [pgb], sig=(k == 7))
        for k in range(8):
            mm(kb, pu[:, 0:cw], hnT[:, k, tsl], wus[:, k, 0:cw], k == 0, [hnTb, wusb], [pub], sig=(k == 7))
        P.op("act", lambda: nc.scalar.activation(out=sg[:, 0:cw], in_=pg[:, 0:cw], func=AF.Silu), reads=[pgb], writes=[sgb])
        P.op("dve", lambda: nc.vector.tensor_tensor(out=ab[:, 0:cw], in0=sg[:, 0:cw], in1=pu[:, 0:cw], op=ALU.mult), reads=[sgb, pub], writes=[abb])

    def stage2(fg, ti):
        _, _, (wds, wdsb) = wbuf[fg % 2]
        cw = min(512, DFF - fg * 512)
        nch = cw // 128
        ab, abb = abs_[ti % 2]
        aT, aTb = aTs[ti % 2]
        pt = kb.pt[kb.pti % 2]; ptb = kb.ptb[kb.pti % 2]; kb.pti += 1
        for j in range(nch):
            P.op("pe", lambda j=j: nc.tensor.transpose(pt[:, j * 128:(j + 1) * 128], ab[:, j * 128:(j + 1) * 128], kb.ident[:]),
                 reads=[abb, kb.identb], writes=[ptb], sig=(j == nch - 1))
        P.op("act", lambda: nc.scalar.copy(out=aT[:, 0:nch, :], in_=pt[:, 0:nch * 128].rearrange("p (a b) -> p a b", b=128)), reads=[ptb], writes=[aTb])
        for half in range(2):
            py, pyb = kb.ps[2 + half], kb.psb[2 + half]
            for ch in range(nch):
                mm(kb, py[:, :], aT[:, ch, :], wds[:, ch, half * 512:(half + 1) * 512], ch == 0, [aTb, wdsb], [pyb], sig=(ch == nch - 1))
            P.op("dve", lambda: nc.vector.tensor_tensor(out=H[:, ti, half * 512:(half + 1) * 512], in0=py[:, :], in1=H[:, ti, half * 512:(half + 1) * 512], op=ALU.add),
                 reads=[pyb, Hbs[ti]], writes=[Hbs[ti]])

    load_fg(0)
    for fg in range(6):
        if fg + 1 < 6:
            load_fg(fg + 1)
        stage1(fg, 0)
        for ti in range(NT):
            if ti + 1 < NT:
                stage1(fg, ti + 1)
            stage2(fg, ti)
    if final:
        o, ob = kb.sb("o", [128, D], F32)
        for ti in range(NT):
            kb.rmsnorm(H[:, ti, :], Hbs[ti], g2[:], g2b, o[:], ob, D, scr, scrb)
            P.dma(kb.dq(), h_d[ti * 128:(ti + 1) * 128, :], o[:], reads=[ob], final=True)
    else:
        for ti in range(NT):
            P.dma(kb.dq(), h_d[ti * 128:(ti + 1) * 128, :], H[:, ti, :], reads=[Hbs[ti]], final=True)
        norm_T(g2, g2b)
        win, winb = kb.sb("win", [128, 8, nproj], BF16)
        for k in range(8):
            st, stb = stg[si % 3]; si += 1
            P.dma(kb.dq(), st[:, 0:nproj], win_d[k * 128:(k + 1) * 128, :], writes=[stb])
            kb.cast("pool" if k % 2 else "act", win[:, k, :], st[:, 0:nproj], [stb], [winb])
        pro, prob = kb.sb("pro", [128, nproj], F32)
        for ti in range(NT):
            tsl = slice(ti * 128, (ti + 1) * 128)
            o0 = 0
            bi = 0
            while o0 < nproj:
                n = min(512, nproj - o0)
                ps, psb = kb.ps[bi % 2], kb.psb[bi % 2]
                for k in range(8):
                    mm(kb, ps[:, 0:n], hnT[:, k, tsl], win[:, k, o0:o0 + n], k == 0, [hnTb, winb], [psb], sig=(k == 7))
                P.op("act", lambda: nc.scalar.copy(out=pro[:, o0:o0 + n], in_=ps[:, 0:n]), reads=[psb], writes=[prob])
                o0 += n
                bi += 1
            P.dma(kb.dq(), pr_d[tsl, :], pro[:], reads=[prob], final=True)
    return kb


def run_F(inputs, hmids, layer, final):
    kb = build_F(final, 712)
    ident = np.eye(128, dtype=np.float32).astype(NPBF)
    maps = []
    for core in range(8):
        mp = {"hmid": np.asarray(hmids[core]), "identb": ident, "g_ffn": inputs["norm_ffn"][layer:layer + 1],
              "wg": inputs["ffn_gate"][layer], "wu": inputs["ffn_up"][layer], "wd": inputs["ffn_down"][layer]}
        if final:
            mp["g2"] = inputs["norm_final"].reshape(1, D)
        else:
            mp["g2"] = inputs["norm_mix"][layer + 1:layer + 2]
            mp["w_in2"] = inputs["dsa_w_in"][0]
        maps.append(mp)
    return kb.run(maps)


NIT = 16


def mem_setup(kb, mem_d, gmem_d, wkv_d):
    nc, P = kb.nc, kb.P
    kmT, kmTb = kb.sb("kmT", [64, 4, 256], BF16)
    vm1, vm1b = kb.sb("vm1", [128, 2, 4, 65], BF16)
    P.op("pool", lambda: nc.gpsimd.memset(vm1[:], 1.0), writes=[vm1b])
    with kb.scope():
        gm, gmb = kb.bcast_row(gmem_d, D, "gmem")
        wkv, wkvb = kb.sb("wkv", [128, 8, 512], BF16)
        mt, mtb = kb.sb("mt", [128, D], F32)
        mn, mnb = kb.sb("mn", [128, D], BF16)
        mnT, mnTb = kb.sb("mnT", [128, 8, 256], BF16)
        scr, scrb = kb.sb("mscr", [128, 4], F32)
        wst = [kb.sb("wst2", [128, 512], F32) for _ in range(2)]
        for k in range(8):
            st, stb = wst[k % 2]
            P.dma(kb.dq(), st[:], wkv_d[k * 128:(k + 1) * 128, :], writes=[stb])
            P.op("dve", lambda: nc.vector.tensor_copy(out=wkv[:, k, :], in_=st[:]), reads=[stb], writes=[wkvb])
        for tl in range(2):
            P.dma("sp", mt[:], mem_d[tl * 128:(tl + 1) * 128, :], writes=[mtb])
            kb.rmsnorm(mt[:], mtb, gm[:], gmb, mn[:], mnb, D, scr, scrb)
            pt = kb.pt[kb.pti % 2]; ptb = kb.ptb[kb.pti % 2]; kb.pti += 1
            for j in range(8):
                P.op("pe", lambda j=j: nc.tensor.transpose(pt[:, j * 128:(j + 1) * 128], mn[:, j * 128:(j + 1) * 128], kb.ident[:]),
                     reads=[mnb, kb.identb], writes=[ptb], sig=(j == 7))
            P.op("dve", lambda: nc.vector.tensor_copy(out=mnT[:, :, tl * 128:(tl + 1) * 128], in_=pt[:, :].rearrange("p (a b) -> p a b", b=128)), reads=[ptb], writes=[mnTb])
        for h in range(4):
            ps, psb = kb.ps[2], kb.psb[2]
            for k in range(8):
                mm(kb, ps[0:64, 0:256], wkv[:, k, h * 64:(h + 1) * 64], mnT[:, k, :], k == 0, [wkvb, mnTb], [psb], sig=(k == 7))
            P.op("dve", lambda: nc.vector.tensor_copy(out=kmT[:, h, :], in_=ps[0:64, 0:256]), reads=[psb], writes=[kmTb])
        for tl in range(2):
            ps, psb = kb.ps[3], kb.psb[3]
            for k in range(8):
                mm(kb, ps[:, 0:256], mnT[:, k, tl * 128:(tl + 1) * 128], wkv[:, k, 256:512], k == 0, [wkvb, mnTb], [psb], sig=(k == 7))
            P.op("dve", lambda: nc.vector.tensor_copy(out=vm1[:, tl, :, 0:64], in_=ps[:, 0:256].rearrange("p (h d) -> p h d", d=64)), reads=[psb], writes=[vm1b])
    return kmT, kmTb, vm1, vm1b


def load_wout(kb, wout_d):
    nc, P = kb.nc, kb.P
    wout, woutb = kb.sb("wout", [128, 8, D], BF16)
    with kb.scope():
        wst = [kb.sb("wst3", [128, 1024], F32) for _ in range(2)]
        for k in range(8):
            st, stb = wst[k % 2]
            P.dma(kb.dq(), st[:], wout_d[k * 128:(k + 1) * 128, :], writes=[stb])
            P.op("dve", lambda: nc.vector.tensor_copy(out=wout[:, k, :], in_=st[:]), reads=[stb], writes=[woutb])
    return wout, woutb


def rms_small(kb, x, xb, A, d, g, gb, out, outb, tmp, tmpb, ss, ssb):
    nc, P = kb.nc, kb.P
    P.op("dve", lambda: nc.vector.tensor_tensor(out=tmp, in0=x, in1=x, op=ALU.mult), reads=[xb], writes=[tmpb])
    P.op("dve", lambda: nc.vector.tensor_reduce(out=ss[:, 0:A], in_=tmp, axis=AX.X, op=ALU.add), reads=[tmpb], writes=[ssb])
    P.op("act", lambda: nc.scalar.activation(out=ss[:, A:2 * A], in_=ss[:, 0:A], func=AF.Ln, scale=1.0 / d, bias=kb.eps_t[:, 0:1]), reads=[ssb, kb.eps_b], writes=[ssb])
    P.op("act", lambda: nc.scalar.activation(out=ss[:, 0:A], in_=ss[:, A:2 * A], func=AF.Exp, scale=-0.5), reads=[ssb], writes=[ssb])
    r = ss[:, 0:A]
    rb_ = AP(tensor=r.tensor, offset=r.offset, ap=[list(r.ap[0]), list(r.ap[1]), [0, d]])
    g2 = g[:, 0:d]
    gb_ = AP(tensor=g2.tensor, offset=g2.offset, ap=[list(g2.ap[0]), [0, A], list(g2.ap[1])])
    P.op("dve", lambda: nc.vector.tensor_tensor(out=tmp, in0=x, in1=rb_, op=ALU.mult), reads=[xb, ssb], writes=[tmpb])
    P.op("dve", lambda: nc.vector.tensor_tensor(out=out, in0=tmp, in1=gb_, op=ALU.mult), reads=[tmpb, gb], writes=[outb])


def build_C(stage=99, nslots=NT, kb=None):
    kb = kb or K("C")
    nc, P = kb.nc, kb.P
    x_d = kb.din("x", [TOK, D])
    pr_d = kb.din("pr", [TOK, 712])
    ckv_d = kb.din("ckv_seq", [T, 128])
    kidx_d = kb.din("kidx_seq", [T, 64])
    id_d = kb.din("identb", [128, 128], BF16)
    an_d = kb.din("antib", [128, 128], BF16)
    relb_d = kb.din("rel_bias", [32, 12])
    oh_s_d = kb.din("oh_s", [33, Y_SLC])
    cm_d = kb.din("cm", [128, 512])
    pw_d = kb.din("pw", [128, NIT])
    qn_d = kb.din("q_norm", [1, 256]); kvn_d = kb.din("kv_norm", [1, 128]); kin_d = kb.din("kidx_norm", [1, 64])
    wqup_d = kb.din("w_q_up", [256, 768]); wuk_d = kb.din("w_uk", [128, 768]); wuv_d = kb.din("w_uv", [128, 768])
    wqi_d = kb.din("w_q_idx", [256, 512])
    mem_d = kb.din("mem", [256, D]); gmem_d = kb.din("g_mem", [1, D]); wkv_d = kb.din("w_mem_kv", [D, 512])
    wout_d = kb.din("w_out", [D, D])
    hmid_d = kb.dout("hmid", [TOK, D])

    P.dma("sp", kb.ident[:], id_d, writes=[kb.identb])
    P.dma("sp", kb.anti[:], an_d, writes=[kb.antib])
    kb.mk_eps()
    with kb.scope():
        tabs = build_tables(kb, relb_d, [oh_s_d], [("s", Y_SLC)])
    band_s, band_sb = load_band(kb, tabs[0][0], tabs[0][1], Y_SLC, 2048, 1, "band_s")
    ckvT, ckvTb = kb.sb("ckvT", [128, T], BF16)
    ckv1, ckv1b = kb.sb("ckv1", [128, 64, 129], BF16)
    kidxT, kidxTb = kb.sb("kidxT", [64, T], BF16)
    P.op("pool", lambda: nc.gpsimd.memset(ckv1[:], 1.0), writes=[ckv1b])
    gq, gqb = kb.bcast_row(qn_d, 256, "gq")
    with kb.scope():
        gkv, gkvb = kb.bcast_row(kvn_d, 128, "gkv")
        gki, gkib = kb.bcast_row(kin_d, 64, "gki")
        st, stb = kb.sb("kst", [128, 8, 128], F32)
        tmp, tmpb = kb.sb("ktmp", [128, 8, 128], F32)
        ss, ssb = kb.sb("kss", [128, 16], F32)
        kin, kinb = kb.sb("kin", [128, 8, 64], BF16)
        for i in range(8):
            P.dma(kb.dq(), st[:], ckv_d[i * 1024:(i + 1) * 1024, :].rearrange("(a p) c -> p a c", p=128), writes=[stb])
            rms_small(kb, st[:], stb, 8, 128, gkv, gkvb, ckv1[:, i * 8:(i + 1) * 8, 0:128], ckv1b, tmp[:], tmpb, ss, ssb)
            pt = kb.pt[kb.pti % 2]; ptb = kb.ptb[kb.pti % 2]; kb.pti += 1
            for j in range(8):
                P.op("pe", lambda j=j: nc.tensor.transpose(pt[:, j * 128:(j + 1) * 128], ckv1[:, i * 8 + j, 0:128], kb.ident[:]),
                     reads=[ckv1b, kb.identb], writes=[ptb], sig=(j == 7))
            P.op("act", lambda: nc.scalar.copy(out=ckvT[:, i * 1024:(i + 1) * 1024], in_=pt[:, :]), reads=[ptb], writes=[ckvTb])
        for i in range(8):
            P.dma(kb.dq(), st[:, :, 0:64], kidx_d[i * 1024:(i + 1) * 1024, :].rearrange("(a p) c -> p a c", p=128), writes=[stb])
            rms_small(kb, st[:, :, 0:64], stb, 8, 64, gki, gkib, kin[:], kinb, tmp[:, :, 0:64], tmpb, ss, ssb)
            pt = kb.pt[kb.pti % 2]; ptb = kb.ptb[kb.pti % 2]; kb.pti += 1
            for j in range(8):
                P.op("pe", lambda j=j: nc.tensor.transpose(pt[0:64, j * 128:(j + 1) * 128], kin[:, j, :], kb.ident[:]),
                     reads=[kinb, kb.identb], writes=[ptb], sig=(j == 7))
            P.op("act", lambda: nc.scalar.copy(out=kidxT[:, i * 1024:(i + 1) * 1024], in_=pt[0:64, :]), reads=[ptb], writes=[kidxTb])
    wqup, wqupb = kb.sb("wqup", [128, 2, 768], BF16)
    wqi, wqib = kb.sb("wqi", [128, 2, 512], BF16)
    wuv, wuvb = kb.sb("wuv", [128, 768], BF16)
    wukT, wukTb = kb.sb("wukT", [64, 12, 128], BF16)
    with kb.scope():
        wst = [kb.sb("wst4", [128, 768], F32) for _ in range(2)]
        wukb, wukbb = kb.sb("wukb", [128, 768], BF16)
        i = 0
        for (src, rows, ncol, dstf) in [(wqup_d, 0, 768, lambda: wqup[:, 0, :]), (wqup_d, 128, 768, lambda: wqup[:, 1, :]),
                                        (wqi_d, 0, 512, lambda: wqi[:, 0, :]), (wqi_d, 128, 512, lambda: wqi[:, 1, :]),
                                        (wuv_d, 0, 768, lambda: wuv[:]), (wuk_d, 0, 768, lambda: wukb[:])]:
            st, stb = wst[i % 2]; i += 1
            P.dma(kb.dq(), st[:, 0:ncol], src[rows:rows + 128, :], writes=[stb])
            dst = dstf()
            P.op("dve", lambda: nc.vector.tensor_copy(out=dst, in_=st[:, 0:ncol]), reads=[stb], writes=[wqupb, wqib, wuvb, wukbb])
        for h0 in (0, 8):
            nb = min(8, 12 - h0)
            pt = kb.pt[kb.pti % 2]; ptb = kb.ptb[kb.pti % 2]; kb.pti += 1
            for j in range(nb):
                P.op("pe", lambda j=j: nc.tensor.transpose(pt[0:64, j * 128:(j + 1) * 128], wukb[:, (h0 + j) * 64:(h0 + j + 1) * 64], kb.ident[:]),
                     reads=[wukbb, kb.identb], writes=[ptb], sig=(j == nb - 1))
            P.op("dve", lambda: nc.vector.tensor_copy(out=wukT[:, h0:h0 + nb, :], in_=pt[0:64, 0:nb * 128].rearrange("p (a b) -> p a b", b=128)), reads=[ptb], writes=[wukTb])
    kmT, kmTb, vm1, vm1b = mem_setup(kb, mem_d, gmem_d, wkv_d)
    wout, woutb = load_wout(kb, wout_d)
    cm, cmb = kb.sb("cm", [128, 512], F32)
    P.dma("sp", cm[:], cm_d, writes=[cmb])
    pw, pwb = kb.sb("pw", [128, NIT], F32)
    P.dma("sp", pw[:], pw_d, writes=[pwb])

    at = Attn(kb)
    score, scoreb = kb.sb("score", [128, T], F32)
    mb, mbb = kb.sb("mb", [128, T], BF16)
    mTs = [kb.sb("mT", [128, 8, 128], BF16) for _ in range(2)]
    big, bigb = kb.sb("big", [128, D], F32)
    cqn, cqnb = kb.sb("cqn", [128, 256], BF16)
    cqT, cqTb = kb.sb("cqT", [128, 2, 128], BF16)
    qhT, qhTb = kb.sb("qhT", [128, 12, 128], BF16)
    qabs, qabsb = kb.sb("qabs", [128, 12, 128], BF16)
    qiT, qiTb = kb.sb("qiT", [64, 8, 128], BF16)
    qmb16, qmb16b = kb.sb("qmb16", [128, 256], BF16)
    qmT, qmTb = kb.sb("qmT", [64, 4, 128], BF16)
    rts = [kb.sb("rt", [128, 512], F32) for _ in range(2)]
    catbf, catbfb = kb.sb("catbf", [128, D], BF16)
    catT, catTb = kb.sb("catT", [128, 8, 128], BF16)
    pm, pmb = kb.sb("pm", [128, 256], BF16)
    sm, smb = kb.sb("sm", [128, 16], F32)
    wv, wvb = kb.sb("wv", [128, 32], F32)
    bs, bsb = kb.sb("bs", [128, 8 + 2 * NIT], F32)
    accbank = 2

    def next_acc():
        nonlocal accbank
        b = accbank
        accbank = 2 + (accbank - 2 + 1) % 4
        return b

    for m in range(nslots):
        tsl = slice(m * 128, (m + 1) * 128)
        L = 128 * (4 * m + 4)
        nkt = 4 * m + 4
        P.dma("sp", big[:, 0:712], pr_d[tsl, :], writes=[bigb])
        if stage == 0:
            P.dma("sp", hmid_d[tsl, :], big[:], reads=[bigb], final=True)
            continue
        ssq = wv[:, 16:18]
        P.op("act", lambda: nc.scalar.activation(out=cqn[:], in_=big[:, 0:256], func=AF.Square, accum_out=wv[:, 16:17]), reads=[bigb], writes=[cqnb, wvb])
        P.op("act", lambda: nc.scalar.activation(out=wv[:, 17:18], in_=wv[:, 16:17], func=AF.Ln, scale=1.0 / 256, bias=kb.eps_t[:, 0:1]), reads=[wvb, kb.eps_b], writes=[wvb])
        P.op("act", lambda: nc.scalar.activation(out=wv[:, 18:19], in_=wv[:, 17:18], func=AF.Exp, scale=-0.5), reads=[wvb], writes=[wvb])
        P.op("dve", lambda: nc.vector.scalar_tensor_tensor(out=cqn[:], in0=big[:, 0:256], scalar=wv[:, 18:19], in1=gq[:], op0=ALU.mult, op1=ALU.mult), reads=[bigb, wvb, gqb], writes=[cqnb])
        pt = kb.pt[kb.pti % 2]; ptb = kb.ptb[kb.pti % 2]; kb.pti += 1
        for j in range(2):
            P.op("pe", lambda j=j: nc.tensor.transpose(pt[:, j * 128:(j + 1) * 128], cqn[:, j * 128:(j + 1) * 128], kb.ident[:]), reads=[cqnb, kb.identb], writes=[ptb], sig=(j == 1))
        P.op("dve", lambda: nc.vector.tensor_copy(out=cqT[:], in_=pt[:, 0:256].rearrange("p (a b) -> p a b", b=128)), reads=[ptb], writes=[cqTb])
        for b4 in range(3):
            ps, psb = kb.ps[b4 % 2], kb.psb[b4 % 2]
            for hh in range(4):
                h = 4 * b4 + hh
                for c in range(2):
                    mm(kb, ps[0:64, hh * 128:(hh + 1) * 128], wqup[:, c, h * 64:(h + 1) * 64], cqT[:, c, :], hh == 0 and c == 0, [wqupb, cqTb], [psb], sig=(hh == 3 and c == 1))
            P.op("act", lambda: nc.scalar.activation(out=qhT[0:64, 4 * b4:4 * b4 + 4, :], in_=ps[0:64, :].rearrange("p (a b) -> p a b", b=128), func=AF.Copy, scale=0.125), reads=[psb], writes=[qhTb])
        for b4 in range(2):
            ps, psb = kb.ps[b4 % 2], kb.psb[b4 % 2]
            for hh in range(4):
                h = 4 * b4 + hh
                for c in range(2):
                    mm(kb, ps[0:64, hh * 128:(hh + 1) * 128], wqi[:, c, h * 64:(h + 1) * 64], cqT[:, c, :], hh == 0 and c == 0, [wqib, cqTb], [psb], sig=(hh == 3 and c == 1))
            P.op("dve", lambda: nc.vector.tensor_copy(out=qiT[:, 4 * b4:4 * b4 + 4, :], in_=ps[0:64, :].rearrange("p (a b) -> p a b", b=128)), reads=[psb], writes=[qiTb])
        for b4 in range(3):
            ps, psb = kb.ps[b4 % 2], kb.psb[b4 % 2]
            for hh in range(4):
                h = 4 * b4 + hh
                mm(kb, ps[:, hh * 128:(hh + 1) * 128], wukT[:, h, :], qhT[0:64, h, :], hh == 0, [wukTb, qhTb], [psb], sig=(hh == 3))
            P.op("dve", lambda: nc.vector.tensor_copy(out=qabs[:, 4 * b4:4 * b4 + 4, :], in_=ps[:, :].rearrange("p (a b) -> p a b", b=128)), reads=[psb], writes=[qabsb])
        P.op("dve", lambda: nc.vector.tensor_scalar(out=wv[:, 0:8], in0=big[:, 448:456], scalar1=0.04419417382415922, scalar2=None, op0=ALU.mult), reads=[bigb], writes=[wvb])
        P.op("dve", lambda: nc.vector.tensor_scalar(out=wv[:, 8:16], in0=wv[:, 0:8], scalar1=0.0, scalar2=2.0, op0=ALU.is_ge, op1=ALU.mult), reads=[wvb], writes=[wvb])
        P.op("dve", lambda: nc.vector.tensor_scalar(out=wv[:, 8:16], in0=wv[:, 8:16], scalar1=-1.0, scalar2=None, op0=ALU.add), reads=[wvb], writes=[wvb])
        P.op("dve", lambda: nc.vector.tensor_tensor(out=wv[:, 0:8], in0=wv[:, 0:8], in1=wv[:, 8:16], op=ALU.mult), reads=[wvb], writes=[wvb])
        P.op("act", lambda: nc.scalar.activation(out=qmb16[:], in_=big[:, 456:712], func=AF.Copy, scale=0.125), reads=[bigb], writes=[qmb16b])
        pt = kb.pt[kb.pti % 2]; ptb = kb.ptb[kb.pti % 2]; kb.pti += 1
        for j in range(4):
            P.op("pe", lambda j=j: nc.tensor.transpose(pt[0:64, j * 128:(j + 1) * 128], qmb16[:, j * 64:(j + 1) * 64], kb.ident[:]), reads=[qmb16b, kb.identb], writes=[ptb], sig=(j == 3))
        P.op("dve", lambda: nc.vector.tensor_copy(out=qmT[:], in_=pt[0:64, 0:512].rearrange("p (a b) -> p a b", b=128)), reads=[ptb], writes=[qmTb])
        P.dma("pool", big[:], x_d[tsl, :], reads=[], writes=[bigb])
        if stage == 1:
            P.dma("sp", hmid_d[tsl, :], big[:], reads=[bigb], final=True)
            continue
        for kc in range(m + 1):
            csl = slice(kc * 512, (kc + 1) * 512)
            for h in range(8):
                ps, psb = kb.ps[h % 2], kb.psb[h % 2]
                rt, rtb = rts[h % 2]
                mm(kb, ps[:, :], qiT[:, h, :], kidxT[:, csl], True, [qiTb, kidxTb], [psb], sig=True)
                P.op("act", lambda: nc.scalar.activation(out=rt[:], in_=ps[:, :], func=AF.Relu, scale=wv[:, h:h + 1]), reads=[psb, wvb], writes=[rtb])
                if h == 0:
                    P.op("dve", lambda: nc.vector.tensor_scalar(out=score[:, csl], in0=rt[:], scalar1=wv[:, 8:9], scalar2=None, op0=ALU.mult), reads=[rtb, wvb], writes=[scoreb])
                else:
                    P.op("dve", lambda: nc.vector.scalar_tensor_tensor(out=score[:, csl], in0=rt[:], scalar=wv[:, 8 + h:9 + h], in1=score[:, csl], op0=ALU.mult, op1=ALU.add), reads=[rtb, wvb, scoreb], writes=[scoreb])
        if stage == 2:
            P.dma("sp", hmid_d[tsl, 0:512], score[:, 0:512], reads=[scoreb], final=True)
            continue
        P.op("dve", lambda: nc.vector.tensor_reduce(out=bs[:, 0:1], in_=score[:, 0:L], axis=AX.X, op=ALU.min), reads=[scoreb], writes=[bsb])
        P.op("dve", lambda: nc.vector.tensor_reduce(out=bs[:, 1:2], in_=score[:, 0:L], axis=AX.X, op=ALU.max), reads=[scoreb], writes=[bsb])
        P.op("dve", lambda: nc.vector.tensor_tensor(out=score[:, L - 512:L], in0=score[:, L - 512:L], in1=cm[:], op=ALU.add), reads=[scoreb, cmb], writes=[scoreb])
        P.op("dve", lambda: nc.vector.tensor_tensor(out=bs[:, 2:3], in0=bs[:, 1:2], in1=bs[:, 0:1], op=ALU.subtract), reads=[bsb], writes=[bsb])
        P.op("dve", lambda: nc.vector.tensor_scalar(out=bs[:, 8:8 + NIT], in0=pw[:], scalar1=bs[:, 2:3], scalar2=None, op0=ALU.mult), reads=[bsb, pwb], writes=[bsb])
        for it in range(NIT):
            hw = bs[:, 8 + it:9 + it]
            P.op("dve", lambda: nc.vector.tensor_tensor(out=bs[:, 3:4], in0=bs[:, 0:1], in1=hw, op=ALU.add), reads=[bsb], writes=[bsb])
            P.op("dve", lambda: nc.vector.tensor_scalar(out=mb[:, 0:L], in0=score[:, 0:L], scalar1=bs[:, 3:4], scalar2=None, op0=ALU.is_ge, op1=ALU.add, accum_out=bs[:, 4:5]),
                 reads=[scoreb, bsb], writes=[mbb, bsb])
            P.op("dve", lambda: nc.vector.tensor_scalar(out=bs[:, 5:6], in0=bs[:, 4:5], scalar1=255.5, scalar2=hw, op0=ALU.is_ge, op1=ALU.mult), reads=[bsb], writes=[bsb])
            P.op("dve", lambda: nc.vector.tensor_tensor(out=bs[:, 0:1], in0=bs[:, 0:1], in1=bs[:, 5:6], op=ALU.add), reads=[bsb], writes=[bsb])
        P.op("dve", lambda: nc.vector.tensor_scalar(out=mb[:, 0:L], in0=score[:, 0:L], scalar1=bs[:, 0:1], scalar2=NEGB, op0=ALU.is_lt, op1=ALU.mult), reads=[scoreb, bsb], writes=[mbb])
        if stage == 3:
            P.dma("sp", hmid_d[tsl, 0:512], score[:, 0:512], reads=[scoreb], final=True)
            P.dma("sp", hmid_d[tsl, 512:512 + 8 + 2 * NIT], bs[:], reads=[bsb], final=True)
            continue
        banks = [next_acc() for _ in range(4)]
        for g8 in range(0, nkt, 8):
            nb = min(8, nkt - g8)
            mT, mTb = mTs[(g8 // 8) % 2]
            pt = kb.pt[kb.pti % 2]; ptb = kb.ptb[kb.pti % 2]; kb.pti += 1
            for j in range(nb):
                P.op("pe", lambda j=j: nc.tensor.transpose(pt[:, j * 128:(j + 1) * 128], mb[:, (g8 + j) * 128:(g8 + j + 1) * 128], kb.ident[:]), reads=[mbb, kb.identb], writes=[ptb], sig=(j == nb - 1))
            P.op("dve", lambda: nc.vector.tensor_copy(out=mT[:, 0:nb, :], in_=pt[:, 0:nb * 128].rearrange("p (a b) -> p a b", b=128)), reads=[ptb], writes=[mTb])
            for j in range(nb):
                kt = g8 + j
                u = kt - 4 * m
                for tr in range(4):
                    bA = banks[tr]
                    accs = [kb.ps[bA][:, 129 * hh:129 * hh + 129] for hh in range(3)]
                    accb = [kb.psb[bA]] * 3
                    s_terms = [(ckvT[:, kt * 128:(kt + 1) * 128], qabs[:, 3 * tr:3 * tr + 3, :], [ckvTb, qabsb]),
                               (kb.ident[:], bc3(mT[:, j, :]), [kb.identb, mTb])]
                    near = None
                    if u >= -12:
                        z0 = 128 * (3 - u)
                        near = (kb.anti[:], band_s[:, 3 * tr:3 * tr + 3, z0:z0 + 128], [kb.antib, band_sb])
                    at.unit(s_terms, near, accs, accb, ckv1[:, kt, :], [ckv1b], [kt == 0, False, False])
        at.flush()
        for tr in range(4):
            bA = banks[tr]
            for hh in range(3):
                h = 3 * tr + hh
                O = kb.ps[bA][:, 129 * hh:129 * hh + 129]
                P.op("dve", lambda: nc.vector.tensor_scalar(out=sm[:, 0:1], in0=O[:, 128:129], scalar1=1e-30, scalar2=None, op0=ALU.max), reads=[kb.psb[bA]], writes=[smb])
                P.op("dve", lambda: nc.vector.reciprocal(out=sm[:, 1:2], in_=sm[:, 0:1]), reads=[smb], writes=[smb])
                P.op("dve", lambda: nc.vector.tensor_scalar(out=qabs[:, h, :], in0=O[:, 0:128], scalar1=sm[:, 1:2], scalar2=None, op0=ALU.mult), reads=[kb.psb[bA], smb], writes=[qabsb])
        for h0 in (0, 8):
            nb = min(8, 12 - h0)
            pt = kb.pt[kb.pti % 2]; ptb = kb.ptb[kb.pti % 2]; kb.pti += 1
            for j in range(nb):
                P.op("pe", lambda j=j: nc.tensor.transpose(pt[:, j * 128:(j + 1) * 128], qabs[:, h0 + j, :], kb.ident[:]), reads=[qabsb, kb.identb], writes=[ptb], sig=(j == nb - 1))
            P.op("dve", lambda: nc.vector.tensor_copy(out=qhT[:, h0:h0 + nb, :], in_=pt[:, 0:nb * 128].rearrange("p (a b) -> p a b", b=128)), reads=[ptb], writes=[qhTb])
        for h0 in (0, 8):
            nb = min(8, 12 - h0)
            bA = next_acc()
            ps, psb = kb.ps[bA], kb.psb[bA]
            for j in range(nb):
                h = h0 + j
                mm(kb, ps[:, j * 64:(j + 1) * 64], qhT[:, h, :], wuv[:, h * 64:(h + 1) * 64], j == 0, [qhTb, wuvb], [psb], sig=(j == nb - 1))
            P.op("act", lambda: nc.scalar.copy(out=catbf[:, h0 * 64:(h0 + nb) * 64], in_=ps[:, 0:nb * 64]), reads=[psb], writes=[catbfb])
        for h in range(4):
            sbk = at.next_sbank()
            ps, psb = kb.ps[sbk], kb.psb[sbk]
            for tl in range(2):
                mm(kb, ps[:, tl * 128:(tl + 1) * 128], kmT[:, h, tl * 128:(tl + 1) * 128], qmT[:, h, :], tl == 0, [kmTb, qmTb], [psb], sig=(tl == 1))
            P.op("act", lambda: nc.scalar.activation(out=pm[:], in_=ps[:, 0:256], func=AF.Exp), reads=[psb], writes=[pmb])
            bA = next_acc()
            O = kb.ps[bA][:, 0:65]
            for tl in range(2):
                mm(kb, O, pm[:, tl * 128:(tl + 1) * 128], vm1[:, tl, h, :], tl == 0, [pmb, vm1b], [kb.psb[bA]], sig=(tl == 1))
            P.op("dve", lambda: nc.vector.reciprocal(out=sm[:, 4:5], in_=O[:, 64:65]), reads=[kb.psb[bA]], writes=[smb])
            P.op("dve", lambda: nc.vector.tensor_scalar(out=catbf[:, 768 + h * 64:768 + (h + 1) * 64], in0=O[:, 0:64], scalar1=sm[:, 4:5], scalar2=None, op0=ALU.mult), reads=[kb.psb[bA], smb], writes=[catbfb])
        pt = kb.pt[kb.pti % 2]; ptb = kb.ptb[kb.pti % 2]; kb.pti += 1
        for j in range(8):
            P.op("pe", lambda j=j: nc.tensor.transpose(pt[:, j * 128:(j + 1) * 128], catbf[:, j * 128:(j + 1) * 128], kb.ident[:]),
                 reads=[catbfb, kb.identb], writes=[ptb], sig=(j == 7))
        P.op("dve", lambda: nc.vector.tensor_copy(out=catT[:], in_=pt[:, :].rearrange("p (a b) -> p a b", b=128)), reads=[ptb], writes=[catTb])
        for half in range(2):
            bA = next_acc()
            ps, psb = kb.ps[bA], kb.psb[bA]
            for k in range(8):
                mm(kb, ps[:, :], catT[:, k, :], wout[:, k, half * 512:(half + 1) * 512], k == 0, [catTb, woutb], [psb], sig=(k == 7))
            P.op("dve", lambda: nc.vector.tensor_tensor(out=big[:, half * 512:(half + 1) * 512], in0=ps[:, :], in1=big[:, half * 512:(half + 1) * 512], op=ALU.add), reads=[psb, bigb], writes=[bigb])
        P.dma("sp", hmid_d[tsl, :], big[:], reads=[bigb], final=True)
    return kb


def run_C(inputs, resF, stage=99, nslots=NT):
    kb = build_C(stage, nslots)
    ident = np.eye(128, dtype=np.float32)
    pw = np.tile((0.5 ** np.arange(1, NIT + 1)).astype(np.float32)[None, :], (128, 1))
    maps = []
    for core in range(8):
        b, c = core // 4, core % 4
        prs = [np.asarray(resF[4 * b + cc]["pr"]) for cc in range(4)]
        oh_s, _, _ = host_tables(c)
        z = np.arange(512)[None, :]
        q = np.arange(128)[:, None]
        cm = np.where(z <= 128 * c + q, 0.0, -1e30).astype(np.float32)
        mp = {"x": np.asarray(resF[core]["h"]), "pr": prs[c],
              "ckv_seq": gather_seq([np.ascontiguousarray(p[:, 256:384]) for p in prs], 0),
              "kidx_seq": gather_seq([np.ascontiguousarray(p[:, 384:448]) for p in prs], 0),
              "identb": ident.astype(NPBF), "antib": np.ascontiguousarray(ident[::-1]).astype(NPBF),
              "rel_bias": inputs["rel_bias"], "oh_s": oh_s, "cm": cm, "pw": pw,
              "q_norm": inputs["dsa_q_norm"][0:1], "kv_norm": inputs["dsa_kv_norm"][0:1], "kidx_norm": inputs["dsa_kidx_norm"][0:1],
              "w_q_up": inputs["dsa_w_q_up"][0], "w_uk": inputs["dsa_w_uk"][0].reshape(128, 768), "w_uv": inputs["dsa_w_uv"][0].reshape(128, 768),
              "w_q_idx": inputs["dsa_w_q_idx"][0],
              "mem": inputs["mem"][b], "g_mem": inputs["norm_mem"][1:2], "w_mem_kv": inputs["w_mem_kv"][1], "w_out": inputs["w_out"][1]}
        maps.append(mp)
    return kb.run(maps)


GROUPS = [[0, 1, 2, 3], [4, 5, 6, 7]]


def all_gather(kb, in_ap2d, out_ap2d):
    nc, P = kb.nc, kb.P
    P.barrier()
    kb.n += 1
    key = "cc%d" % kb.n
    sem = P.es.enter_context(nc.semaphore(key))
    P.sem[key] = sem
    P.cnt[key] = 0
    with nc.Block() as block:
        @block.gpsimd
        def _(g):
            g.collective_compute("AllGather", ALU.bypass, replica_groups=GROUPS, ins=[in_ap2d.opt()], outs=[out_ap2d.opt()]).then_inc(sem)
            g.wait_ge(sem, 1)
    P.cnt[key] = 1
    P.seen["pool"][key] = 1
    for e in P.eng:
        P._need(e, (key, 1))


def seq_rows(all_t, ncols_total, col0, ncol, m0, nm):
    return AP(tensor=all_t, offset=m0 * 128 * ncols_total + col0,
              ap=[[128 * ncols_total, nm], [2048 * ncols_total, 4], [ncols_total, 128], [1, ncol]])


def build_fused():
    kb = K("fused")
    nc, P = kb.nc, kb.P
    ext = {}

    def E(name, shape, dt=F32):
        ext[name] = nc.dram_tensor(name, list(shape), dt, kind="ExternalInput").ap()
        return ext[name]

    def S(name, shape, dt=F32):
        return nc.dram_tensor(name, list(shape), dt)

    x = E("x", [TOK, D])
    out = nc.dram_tensor("out", [TOK, D], F32, kind="ExternalOutput").ap()
    nm = {k: E(k, [1, D]) for k in ("norm_mix0", "norm_mix1", "norm_ffn0", "norm_ffn1", "norm_mem0", "norm_mem1", "norm_final")}
    nsa_w_in = E("nsa_w_in", [D, 1828]); gate_b = E("gate_b", [1, 36])
    ident = E("ident", [128, 128]); anti = E("anti", [128, 128])
    identb = E("identb", [128, 128], BF16); antib = E("antib", [128, 128], BF16)
    rel_bias = E("rel_bias", [32, 12])
    oh_s = E("oh_s", [33, Y_SLC]); oh_w = E("oh_w", [33, Y_WIN]); oh_c = E("oh_c", [33, Y_CMP])
    ew = E("ew", [128, T], BF16); ovl = E("ovl", [128, 4, 128], BF16); fb = E("fb", [NT, 128, 128])
    cmp_w = {k: E(k, shp) for k, shp in [("w1k", [2048, 256]), ("b1k", [256, 1]), ("w2k", [256, 64]), ("b2k", [64, 1]),
                                         ("w1v", [2048, 256]), ("b1v", [256, 1]), ("w2v", [256, 64]), ("b2v", [1, 64]),
                                         ("posk", [32, 64]), ("posv", [32, 64])]}
    mem = E("mem", [256, D])
    wkv = [E("w_mem_kv%d" % i, [D, 512]) for i in range(2)]
    wout = [E("w_out%d" % i, [D, D]) for i in range(2)]
    wg = [E("wg%d" % i, [D, DFF]) for i in range(2)]
    wu = [E("wu%d" % i, [D, DFF]) for i in range(2)]
    wd = [E("wd%d" % i, [DFF, D]) for i in range(2)]
    dsa_w_in = E("dsa_w_in", [D, 712])
    cm = E("cm", [128, 512]); pw = E("pw", [128, NIT])
    q_norm = E("q_norm", [1, 256]); kv_norm = E("kv_norm", [1, 128]); kidx_norm = E("kidx_norm", [1, 64])
    w_q_up = E("w_q_up", [256, 768]); w_uk = E("w_uk", [128, 768]); w_uv = E("w_uv", [128, 768]); w_q_idx = E("w_q_idx", [256, 512])

    fm_loc = S("fm_loc", [64, 24, TOK], BF16)
    tm_loc = S("tm_loc", [TOK, 292])
    kfm_loc = S("kfm_loc", [8, 64, TOK], BF16)
    kfm_all = S("kfm_all", [4, 4 * 128, TOK], BF16)
    vt_loc = S("vt_loc", [TOK, 256])
    vt_all = S("vt_all", [4, 4 * 512, 256])
    qT2_s = S("qT2_s", [128, 6, TOK], BF16)
    kseq = {k: S(k + "_s", [128, T], BF16) for k in ("kcrT2", "vcrT2", "ksT2", "kwT2")}
    vs_s = S("vs_s", [T, 128]); vw_s = S("vw_s", [T, 128])
    hmid0 = S("hmid0", [TOK, D]); h0 = S("h0", [TOK, D]); hmid1 = S("hmid1", [TOK, D])
    pr_loc = S("pr_loc", [TOK, 712])
    kv_loc = S("kv_loc", [TOK, 192]); kv_all = S("kv_all", [4, 4 * 512, 192])
    ckv_s = S("ckv_s", [T, 128]); kidx_s = S("kidx_s", [T, 64])

    kb.alias = {"x": x, "g": nm["norm_mix0"], "w_in": nsa_w_in, "gate_b": gate_b, "ident": ident, "anti": anti,
                "fmT": fm_loc.ap(), "tm": tm_loc.ap()}
    with kb.scope():
        build_A(kb)
    P.barrier()
    P.dma("sp", kfm_loc.ap(), AP(tensor=fm_loc, offset=12 * TOK, ap=[[TOK, 8], [24 * TOK, 64], [1, TOK]]))
    P.dma("pool", vt_loc.ap(), tm_loc.ap()[:, 0:256])
    for ch in range(4):
        all_gather(kb, kfm_loc.ap()[2 * ch:2 * ch + 2].rearrange("a d t -> (a d) t"), kfm_all.ap()[ch])
    for r4 in range(4):
        all_gather(kb, vt_loc.ap()[r4 * 512:(r4 + 1) * 512, :], vt_all.ap()[r4])
    for g in range(2):
        P.dma(kb.dq(), qT2_s.ap()[g * 64:(g + 1) * 64, :, :], fm_loc.ap()[:, 6 * g:6 * g + 6, :])
    for ch, k in enumerate(("kcrT2", "vcrT2", "ksT2", "kwT2")):
        for g in range(2):
            for cc in range(4):
                dst = AP(tensor=kseq[k], offset=(g * 64) * T + cc * 128, ap=[[T, 64], [512, 16], [1, 128]])
                src = AP(tensor=kfm_all, offset=((ch * 4 + cc) * 128 + g * 64) * TOK, ap=[[TOK, 64], [128, 16], [1, 128]])
                P.dma(kb.dq(), dst, src)
    for (dst_t, c0) in ((vs_s, 0), (vw_s, 128)):
        for cc in range(4):
            for r4 in range(4):
                dst = AP(tensor=dst_t, offset=(r4 * 16 * 128 + cc * 128) * 128, ap=[[512 * 128, 4], [128, 128], [1, 128]])
                src = AP(tensor=vt_all, offset=((r4 * 4 + cc) * 512) * 256 + c0, ap=[[128 * 256, 4], [256, 128], [1, 128]])
                P.dma(kb.dq(), dst, src)
    P.barrier()
    kb.alias = dict(cmp_w)
    kb.alias.update({"x": x, "identb": identb, "antib": antib, "rel_bias": rel_bias, "oh_s": oh_s, "oh_w": oh_w, "oh_c": oh_c,
                     "qT2": qT2_s.ap(), "qmT": fm_loc.ap()[:, 20:24, :], "gates": tm_loc.ap()[:, 256:292],
                     "ksT2": kseq["ksT2"].ap(), "kwT2": kseq["kwT2"].ap(), "kcrT2": kseq["kcrT2"].ap(), "vcrT2": kseq["vcrT2"].ap(),
                     "vs": vs_s.ap(), "vw": vw_s.ap(), "ew": ew, "ovl": ovl, "fb": fb,
                     "mem": mem, "g_mem": nm["norm_mem0"], "w_mem_kv": wkv[0], "w_out": wout[0], "hmid": hmid0.ap()})
    with kb.scope():
        build_B(0, kb)
    P.barrier()
    kb.alias = {"hmid": hmid0.ap(), "identb": identb, "g_ffn": nm["norm_ffn0"], "wg": wg[0], "wu": wu[0], "wd": wd[0],
                "g2": nm["norm_mix1"], "h": h0.ap(), "w_in2": dsa_w_in, "pr": pr_loc.ap()}
    with kb.scope():
        build_F(False, 712, kb)
    P.barrier()
    P.dma("sp", kv_loc.ap(), pr_loc.ap()[:, 256:448])
    for r4 in range(4):
        all_gather(kb, kv_loc.ap()[r4 * 512:(r4 + 1) * 512, :], kv_all.ap()[r4])
    for (dst_t, c0, nc_) in ((ckv_s, 0, 128), (kidx_s, 128, 64)):
        for cc in range(4):
            for r4 in range(4):
                dst = AP(tensor=dst_t, offset=(r4 * 16 * 128 + cc * 128) * nc_, ap=[[512 * nc_, 4], [nc_, 128], [1, nc_]])
                src = AP(tensor=kv_all, offset=((r4 * 4 + cc) * 512) * 192 + c0, ap=[[128 * 192, 4], [192, 128], [1, nc_]])
                P.dma(kb.dq(), dst, src)
    P.barrier()
    kb.alias = {"x": h0.ap(), "pr": pr_loc.ap(), "ckv_seq": ckv_s.ap(), "kidx_seq": kidx_s.ap(), "identb": identb, "antib": antib,
                "rel_bias": rel_bias, "oh_s": oh_s, "cm": cm, "pw": pw, "q_norm": q_norm, "kv_norm": kv_norm, "kidx_norm": kidx_norm,
                "w_q_up": w_q_up, "w_uk": w_uk, "w_uv": w_uv, "w_q_idx": w_q_idx, "mem": mem, "g_mem": nm["norm_mem1"],
                "w_mem_kv": wkv[1], "w_out": wout[1], "hmid": hmid1.ap()}
    with kb.scope():
        build_C(99, NT, kb)
    P.barrier()
    kb.alias = {"hmid": hmid1.ap(), "identb": identb, "g_ffn": nm["norm_ffn1"], "wg": wg[1], "wu": wu[1], "wd": wd[1],
                "g2": nm["norm_final"], "h": out}
    with kb.scope():
        build_F(True, 712, kb)
    kb.ext = ext
    return kb


def fused_maps(inputs):
    ident = np.eye(128, dtype=np.float32)
    anti = np.ascontiguousarray(ident[::-1])
    pw = np.tile((0.5 ** np.arange(1, NIT + 1)).astype(np.float32)[None, :], (128, 1))
    maps = []
    for core in range(8):
        b, c = core // 4, core % 4
        mp = consts_B(c)
        z = np.arange(512)[None, :]
        q = np.arange(128)[:, None]
        mp.update({
            "x": np.ascontiguousarray(inputs["x"][b][core_tokens(c)]),
            "norm_mix0": inputs["norm_mix"][0:1], "norm_mix1": inputs["norm_mix"][1:2],
            "norm_ffn0": inputs["norm_ffn"][0:1], "norm_ffn1": inputs["norm_ffn"][1:2],
            "norm_mem0": inputs["norm_mem"][0:1], "norm_mem1": inputs["norm_mem"][1:2],
            "norm_final": inputs["norm_final"].reshape(1, D),
            "nsa_w_in": inputs["nsa_w_in"][0], "gate_b": inputs["nsa_gate_b"][0:1],
            "ident": ident, "anti": anti, "rel_bias": inputs["rel_bias"],
            "w1k": inputs["nsa_cmp_k_w1"][0], "b1k": inputs["nsa_cmp_k_b1"][0].reshape(256, 1),
            "w2k": inputs["nsa_cmp_k_w2"][0], "b2k": inputs["nsa_cmp_k_b2"][0].reshape(64, 1),
            "w1v": inputs["nsa_cmp_v_w1"][0], "b1v": inputs["nsa_cmp_v_b1"][0].reshape(256, 1),
            "w2v": inputs["nsa_cmp_v_w2"][0], "b2v": inputs["nsa_cmp_v_b2"][0].reshape(1, 64),
            "posk": inputs["nsa_cmp_pos_k"][0], "posv": inputs["nsa_cmp_pos_v"][0],
            "mem": inputs["mem"][b],
            "w_mem_kv0": inputs["w_mem_kv"][0], "w_mem_kv1": inputs["w_mem_kv"][1],
            "w_out0": inputs["w_out"][0], "w_out1": inputs["w_out"][1],
            "wg0": inputs["ffn_gate"][0], "wg1": inputs["ffn_gate"][1],
            "wu0": inputs["ffn_up"][0], "wu1": inputs["ffn_up"][1],
            "wd0": inputs["ffn_down"][0], "wd1": inputs["ffn_down"][1],
            "dsa_w_in": inputs["dsa_w_in"][0],
            "cm": np.where(z <= 128 * c + q, 0.0, -1e30).astype(np.float32), "pw": pw,
            "q_norm": inputs["dsa_q_norm"][0:1], "kv_norm": inputs["dsa_kv_norm"][0:1], "kidx_norm": inputs["dsa_kidx_norm"][0:1],
            "w_q_up": inputs["dsa_w_q_up"][0], "w_uk": inputs["dsa_w_uk"][0].reshape(128, 768),
            "w_uv": inputs["dsa_w_uv"][0].reshape(128, 768), "w_q_idx": inputs["dsa_w_q_idx"][0],
        })
        maps.append(mp)
    return maps


def kernel(**inputs):
    inputs = {k: np.asarray(v) for k, v in inputs.items()}
    kb = build_fused()
    res = kb.run(fused_maps(inputs))
    return scatter_tokens(res, "out")
```

```python
import math
from contextlib import ExitStack
import numpy as np
import ml_dtypes
import concourse.bass as bass
import concourse.mybir as mybir
from concourse.bass import AP
from concourse.bass_utils import run_bass_kernel_spmd

F32 = mybir.dt.float32
BF16 = mybir.dt.bfloat16
AF = mybir.ActivationFunctionType
ALU = mybir.AluOpType
AX = mybir.AxisListType
NPBF = ml_dtypes.bfloat16

D = 1024
T = 8192
NT = 16
TOK = 2048
DFF = 2816
EPS = 1e-6
NEGB = -30000.0


class Buf:
    __slots__ = ("name", "w", "r")

    def __init__(self, name=""):
        self.name = name
        self.w = None
        self.r = {}


class Prog:
    NSLOT = 12

    def __init__(self, nc):
        self.nc = nc
        self.eng = {"pe": nc.tensor, "act": nc.scalar, "dve": nc.vector,
                    "pool": nc.gpsimd, "sp": nc.sync}
        self.es = ExitStack()
        self.sem = {}
        self.cnt = {}
        for e in ("pe", "act", "dve", "pool"):
            self.sem[e] = self.es.enter_context(nc.semaphore("c_" + e))
            self.cnt[e] = 0
        self.slots = {}
        self.slot_i = {}
        for q in ("sp", "pool"):
            lst = []
            for i in range(self.NSLOT):
                key = "d_%s%d" % (q, i)
                self.sem[key] = self.es.enter_context(nc.semaphore(key))
                self.cnt[key] = 0
                lst.append(key)
            self.slots[q] = lst
            self.slot_i[q] = 0
        self.seen = {e: {} for e in self.eng}
        self.outtoks = []

    def _need(self, e, tok):
        if tok is None:
            return
        key, val = tok
        if self.seen[e].get(key, 0) >= val:
            return
        if key in ("pe", "act", "dve", "pool"):
            assert self.cnt[key] >= val, "missing signal on %s" % key
        self.eng[e].wait_ge(self.sem[key], val)
        self.seen[e][key] = val

    def _deps(self, e, reads, writes):
        for b in reads:
            if b.w is not None and not (e == "pe" and b.w[0] == "pe"):
                self._need(e, b.w)
        for b in writes:
            if b.w is not None and b.w[0] != e:
                self._need(e, b.w)
            for k, v in b.r.items():
                if k != e:
                    self._need(e, (k, v))

    def _mark(self, tok, reads, writes):
        for b in reads:
            if b.r.get(tok[0], 0) < tok[1]:
                b.r[tok[0]] = tok[1]
        for b in writes:
            b.w = tok
            b.r = {}

    def op(self, e, fn, reads=(), writes=(), sig=True):
        self._deps(e, reads, writes)
        ins = fn()
        if sig:
            self.cnt[e] += 1
            ins.then_inc(self.sem[e], 1)
            tok = (e, self.cnt[e])
        else:
            tok = (e, self.cnt[e] + 1)
        self._mark(tok, reads, writes)
        return ins

    def dma(self, q, out, in_, reads=(), writes=(), final=False):
        lst = self.slots[q]
        key = lst[self.slot_i[q] % self.NSLOT]
        self.slot_i[q] += 1
        if self.cnt[key] > 0:
            self._need(q, (key, self.cnt[key]))
        self._deps(q, reads, writes)
        ins = self.eng[q].dma_start(out=out, in_=in_)
        self.cnt[key] += 16
        ins.then_inc(self.sem[key], 16)
        tok = (key, self.cnt[key])
        self._mark(tok, reads, writes)
        if final:
            self.outtoks.append(tok)
        return tok

    def barrier(self):
        toks = [(e, self.cnt[e]) for e in ("pe", "act", "dve", "pool") if self.cnt[e] > 0]
        for q in ("sp", "pool"):
            toks += [(k, self.cnt[k]) for k in self.slots[q] if self.cnt[k] > 0]
        for e in self.eng:
            for t in toks:
                if t[0] != e:
                    self._need(e, t)

    def finish(self):
        for tok in self.outtoks:
            self._need("sp", tok)
        for q in ("sp", "pool"):
            for key in self.slots[q]:
                if self.cnt[key] > 0:
                    self._need("sp", (key, self.cnt[key]))
        self.es.close()


class K:
    def __init__(self, name):
        self.nc = bass.Bass("TRN2", target_bir_lowering=False)
        self.P = Prog(self.nc)
        self.es = ExitStack()
        self.n = 0
        self.rr = 0
        nc = self.nc
        self.es.enter_context(nc.allow_non_contiguous_dma(reason="small strided parameter loads"))
        self.ps = [self.es.enter_context(nc.psum_tensor("ps%d" % i, [128, 512], F32)) for i in range(7)]
        self.psb = [Buf("ps%d" % i) for i in range(7)]
        pt0 = self.es.enter_context(nc.psum_tensor("pt0", [128, 1024], BF16))
        ptb0 = Buf("pt0")
        self.pt = [pt0, pt0]
        self.ptb = [ptb0, ptb0]
        self.pti = 0
        self.ident, self.identb = self.sb("ident", [128, 128], BF16)
        self.anti, self.antib = self.sb("anti", [128, 128], BF16)

    def sb(self, name, shape, dt):
        self.n += 1
        t = self.es.enter_context(self.nc.sbuf_tensor("%s_%d" % (name, self.n), shape, dt))
        return t, Buf(name)

    def scope(self):
        kb = self

        class _S:
            def __enter__(self_):
                self_.old = kb.es
                kb.es = ExitStack()
                return kb.es

            def __exit__(self_, *a):
                kb.P.barrier()
                kb.es.close()
                kb.es = self_.old
        return _S()

    alias = None

    def din(self, name, shape, dt=F32):
        if self.alias is not None:
            ap = self.alias[name]
            assert list(ap.shape) == list(shape), (name, ap.shape, shape)
            return ap
        return self.nc.dram_tensor(name, list(shape), dt, kind="ExternalInput").ap()

    def dout(self, name, shape, dt=F32):
        if self.alias is not None:
            ap = self.alias[name]
            assert list(ap.shape) == list(shape), (name, ap.shape, shape)
            return ap
        return self.nc.dram_tensor(name, list(shape), dt, kind="ExternalOutput").ap()

    def dscr(self, name, shape, dt=F32):
        self.n += 1
        return self.nc.dram_tensor("%s_%d" % (name, self.n), list(shape), dt, kind="Internal")

    def dq(self):
        self.rr += 1
        return "sp" if self.rr % 2 else "pool"

    def load_consts(self, ident_d, anti_d):
        st, stb = self.sb("cst", [128, 256], F32)
        self.P.dma("sp", st[:, 0:128], ident_d, writes=[stb])
        self.P.dma("sp", st[:, 128:256], anti_d, writes=[stb])
        nc = self.nc
        self.P.op("dve", lambda: nc.vector.tensor_copy(out=self.ident[:], in_=st[:, 0:128]), reads=[stb], writes=[self.identb])
        self.P.op("dve", lambda: nc.vector.tensor_copy(out=self.anti[:], in_=st[:, 128:256]), reads=[stb], writes=[self.antib])

    def load_w(self, dram, kdim, ncol, name, eng="pool", stage=None):
        nc, P = self.nc, self.P
        kp = min(128, kdim)
        nk = max(1, kdim // 128)
        w, wb = self.sb(name, [kp, nk, ncol], BF16)
        CH = 2048
        if stage is None:
            stage = [self.sb("wst", [128, CH], F32) for _ in range(2)]
        self._wst = stage
        i = 0
        for k in range(nk):
            for c0 in range(0, ncol, CH):
                cw = min(CH, ncol - c0)
                st, stb = stage[i % 2]
                i += 1
                P.dma(self.dq(), st[0:kp, 0:cw], dram[k * 128:k * 128 + kp, c0:c0 + cw], writes=[stb])
                self.cast(eng, w[:, k, c0:c0 + cw], st[0:kp, 0:cw], [stb], [wb])
        return w, wb

    def cast(self, eng, out, in_, reads, writes, sig=True):
        nc = self.nc
        if eng == "pool":
            self.P.op("pool", lambda: nc.gpsimd.tensor_copy(out=out, in_=in_), reads=reads, writes=writes, sig=sig)
        elif eng == "dve":
            self.P.op("dve", lambda: nc.vector.tensor_copy(out=out, in_=in_), reads=reads, writes=writes, sig=sig)
        else:
            self.P.op("act", lambda: nc.scalar.copy(out=out, in_=in_), reads=reads, writes=writes, sig=sig)

    def bcast_row(self, dram_row, ncol, name):
        t, tb = self.sb(name, [128, ncol], F32)
        src = AP(tensor=dram_row.tensor, offset=dram_row.offset, ap=[[0, 128], [1, ncol]])
        self.P.dma("sp", t[:], src, writes=[tb])
        return t, tb

    def rmsnorm(self, x, xb, g, gb, out, outb, dim, scr, scrb, np_=128):
        nc, P = self.nc, self.P
        P.op("act", lambda: nc.scalar.activation(out=out, in_=x, func=AF.Square, accum_out=scr[0:np_, 0:1]),
             reads=[xb], writes=[outb, scrb])
        P.op("act", lambda: nc.scalar.activation(out=scr[0:np_, 1:2], in_=scr[0:np_, 0:1], func=AF.Ln, scale=1.0 / dim, bias=self.eps_t[0:np_, 0:1]),
             reads=[scrb, self.eps_b], writes=[scrb])
        P.op("act", lambda: nc.scalar.activation(out=scr[0:np_, 2:3], in_=scr[0:np_, 1:2], func=AF.Exp, scale=-0.5),
             reads=[scrb], writes=[scrb])
        P.op("dve", lambda: nc.vector.scalar_tensor_tensor(out=out, in0=x, scalar=scr[0:np_, 2:3], in1=g, op0=ALU.mult, op1=ALU.mult),
             reads=[xb, scrb, gb], writes=[outb])

    def mk_eps(self):
        self.eps_t, self.eps_b = self.sb("eps", [128, 1], F32)
        nc = self.nc
        self.P.op("dve", lambda: nc.vector.memset(self.eps_t[:], EPS), writes=[self.eps_b])

    def transpose_to(self, src_list, dst, dstb, reads, evac="dve", np_in=128):
        nc, P = self.nc, self.P
        n = len(src_list)
        i0 = 0
        while i0 < n:
            nb = min(8, n - i0)
            pt = self.pt[self.pti % 2]
            ptb = self.ptb[self.pti % 2]
            self.pti += 1
            w = src_list[i0].shape[-1]
            for j in range(nb):
                s = src_list[i0 + j]
                P.op("pe", lambda s=s, j=j: nc.tensor.transpose(pt[0:w, j * 128:j * 128 + np_in], s, self.ident[0:np_in, 0:np_in]),
                     reads=list(reads) + [self.identb], writes=[ptb], sig=(j == nb - 1))
            src = pt[0:w, 0:nb * 128].rearrange("p (a b) -> p a b", b=128)[:, :, 0:np_in]
            o = dst[0:w, i0:i0 + nb, :]
            if evac == "dve":
                P.op("dve", lambda: nc.vector.tensor_copy(out=o, in_=src), reads=[ptb], writes=[dstb])
            else:
                P.op("act", lambda: nc.scalar.copy(out=o, in_=src), reads=[ptb], writes=[dstb])
            i0 += nb

    def run(self, in_maps):
        self.P.finish()
        self.es.close()
        res = run_bass_kernel_spmd(self.nc, in_maps, core_ids=list(range(8)))
        return res.results


def rel_bucket_np(dist):
    n = np.maximum(dist, 0)
    nf = np.maximum(n, 16).astype(np.float32)
    large = 16 + (np.log(nf / np.float32(16)) / np.float32(math.log(2048 / 16)) * np.float32(16)).astype(np.int32)
    large = np.minimum(large, 31)
    return np.where(n < 16, n, large)


def onehot_table(dist, masked):
    Y = dist.shape[0]
    oh = np.zeros((33, Y), np.float32)
    b = rel_bucket_np(dist)
    ok = ~masked
    oh[b[ok], np.nonzero(ok)[0]] = 1.0
    oh[32, masked] = 1.0
    return oh


Y_SLC = 2304
Y_WIN = 1280
Y_CMP = 5376


def host_tables(c):
    y = np.arange(Y_SLC)
    d = y - 511 + 128 * c
    oh_s = onehot_table(d, d < 0)
    y = np.arange(Y_WIN)
    d = y - 511 + 128 * c
    oh_w = onehot_table(d, (d < 0) | (d >= 512))
    y = np.arange(Y_CMP)
    d = y + 128 * c - 2063
    oh_c = onehot_table(d, d < 0)
    return oh_s, oh_w, oh_c


def build_tables(kb, rel_bias_d, ohs, names_Y):
    nc, P = kb.nc, kb.P
    bt, btb = kb.sb("biasT", [33, 12], F32)
    P.dma("sp", bt[0:32, :], rel_bias_d, writes=[btb])
    b31, b31b = kb.sb("b31", [33, 12], F32)
    src = AP(tensor=rel_bias_d.tensor, offset=rel_bias_d.offset + 31 * 12, ap=[[0, 32], [1, 12]])
    P.dma("sp", b31[0:32, :], src, writes=[b31b])
    bb, bbb = kb.sb("biasb", [33, 12], BF16)
    P.op("dve", lambda: nc.vector.memset(bb[:], NEGB), writes=[bbb])
    P.op("dve", lambda: nc.vector.tensor_tensor(out=bb[0:32, :], in0=bt[0:32, :], in1=b31[0:32, :], op=ALU.subtract),
         reads=[btb, b31b], writes=[bbb])
    outs = []
    for (oh_d, Y, nm) in [(o, y, n) for o, (n, y) in zip(ohs, names_Y)]:
        oh, ohb = kb.sb("oh" + nm, [33, Y], BF16)
        st, stb = kb.sb("ohst" + nm, [33, Y], F32)
        P.dma("sp", st[:], oh_d, writes=[stb])
        P.op("dve", lambda: nc.vector.tensor_copy(out=oh[:], in_=st[:]), reads=[stb], writes=[ohb])
        tt, ttb = kb.sb("tt" + nm, [12, Y], BF16)
        for c0 in range(0, Y, 512):
            cw = min(512, Y - c0)
            ps, psb = kb.ps[0], kb.psb[0]
            P.op("pe", lambda: nc.tensor.matmul(ps[0:12, 0:cw], lhsT=bb[:], rhs=oh[:, c0:c0 + cw], start=True, stop=True),
                 reads=[bbb, ohb], writes=[psb])
            P.op("dve", lambda: nc.vector.tensor_copy(out=tt[:, c0:c0 + cw], in_=ps[0:12, 0:cw]), reads=[psb], writes=[ttb])
        scr = kb.dscr("ttd" + nm, [12, Y], BF16)
        scrb = Buf("ttd" + nm)
        P.dma("sp", scr.ap(), tt[:], reads=[ttb], writes=[scrb])
        outs.append((scr, scrb, Y))
    return outs


def load_band(kb, scr, scrb, Y, Z, pstep, name, zoff=0, dst=None):
    if dst is None:
        dst = kb.sb(name, [128, 12, Z], BF16)
    t, tb = dst
    src = AP(tensor=scr, offset=zoff, ap=[[pstep, 128], [Y, 12], [1, Z]])
    kb.P.dma(kb.dq(), t[:, :, 0:Z], src, reads=[scrb], writes=[tb])
    return t, tb


FM_A = [(0 + 64 * i, 0.125) for i in range(12)] + [(768 + 64 * i, 1.0) for i in range(4)] + \
       [(1024, 1.0), (1088, 1.0), (1280, 1.0), (1344, 1.0)] + [(1572 + 64 * i, 0.125) for i in range(4)]


def proj_phase(kb, x_d, g_d, w, wb, ncols, fm_groups, tm_ranges, fmT_d, tm_d, post_tm=None):
    nc, P = kb.nc, kb.P
    g, gb = kb.bcast_row(g_d, D, "gain")
    xt = [kb.sb("xt", [128, D], F32) for _ in range(2)]
    xn = [kb.sb("xn", [128, D], BF16) for _ in range(2)]
    scr, scrb = kb.sb("scr", [128, 4], F32)
    xnT, xnTb = kb.sb("xnT", [128, 8, 512], BF16)
    ng = len(fm_groups)
    fst, fstb = kb.sb("fst", [64, ng, 512], BF16)
    ntm = sum(n for _, n in tm_ranges)
    tst = [kb.sb("tst", [128, max(ntm, 1)], F32) for _ in range(2)]
    ei = 0
    for blk in range(4):
        for tt in range(4):
            ti = blk * 4 + tt
            x_, xb_ = xt[ti % 2]
            n_, nb_ = xn[ti % 2]
            P.dma(kb.dq(), x_[:], x_d[ti * 128:(ti + 1) * 128, :], writes=[xb_])
            kb.rmsnorm(x_[:], xb_, g[:], gb, n_[:], nb_, D, scr, scrb)
            for half in range(1):
                pt = kb.pt[kb.pti % 2]
                ptb = kb.ptb[kb.pti % 2]
                kb.pti += 1
                for j in range(8):
                    P.op("pe", lambda j=j: nc.tensor.transpose(pt[:, j * 128:(j + 1) * 128], n_[:, j * 128:(j + 1) * 128], kb.ident[:]),
                         reads=[nb_, kb.identb], writes=[ptb], sig=(j == 7))
                P.op("dve", lambda: nc.vector.tensor_copy(out=xnT[:, :, tt * 128:(tt + 1) * 128],
                                                          in_=pt[:, :].rearrange("p (a b) -> p a b", b=128)),
                     reads=[ptb], writes=[xnTb])
            if ntm:
                ts_, tsb_ = tst[ti % 2]
                ps, psb = kb.ps[4], kb.psb[4]
                o = 0
                for (c0, n) in tm_ranges:
                    for k in range(8):
                        P.op("pe", lambda k=k, o=o, c0=c0, n=n: nc.tensor.matmul(ps[:, o:o + n], lhsT=xnT[:, k, tt * 128:(tt + 1) * 128], rhs=w[:, k, c0:c0 + n],
                                                                       start=(k == 0 and o == 0), stop=(k == 7)),
                             reads=[xnTb, wb], writes=[psb], sig=(k == 7))
                    o += n
                P.op("act", lambda: nc.scalar.copy(out=ts_[:, 0:ntm], in_=ps[:, 0:ntm]), reads=[psb], writes=[tsb_])
                if post_tm is not None:
                    post_tm(ts_, tsb_)
                P.dma(kb.dq(), tm_d[ti * 128:(ti + 1) * 128, :], ts_[:, 0:ntm], reads=[tsb_], final=True)
        for gi, (c0, sc) in enumerate(fm_groups):
            ps, psb = kb.ps[gi % 4], kb.psb[gi % 4]
            for k in range(8):
                P.op("pe", lambda k=k, c0=c0: nc.tensor.matmul(ps[0:64, :], lhsT=w[:, k, c0:c0 + 64], rhs=xnT[:, k, :], start=(k == 0), stop=(k == 7)),
                     reads=[xnTb, wb], writes=[psb], sig=(k == 7))
            if ei % 2 == 0:
                P.op("act", lambda: nc.scalar.activation(out=fst[:, gi, :], in_=ps[0:64, :], func=AF.Copy, scale=sc), reads=[psb], writes=[fstb])
            else:
                P.op("dve", lambda: nc.vector.tensor_scalar(out=fst[:, gi, :], in0=ps[0:64, :], scalar1=sc, scalar2=None, op0=ALU.mult), reads=[psb], writes=[fstb])
            ei += 1
        P.dma(kb.dq(), fmT_d[:, :, blk * 512:(blk + 1) * 512], fst[:], reads=[fstb], final=True)


def build_A(kb=None):
    kb = kb or K("A")
    nc, P = kb.nc, kb.P
    x_d = kb.din("x", [TOK, D])
    g_d = kb.din("g", [1, D])
    w_d = kb.din("w_in", [D, 1828])
    gb_d = kb.din("gate_b", [1, 36])
    id_d = kb.din("ident", [128, 128])
    an_d = kb.din("anti", [128, 128])
    fm_d = kb.dout("fmT", [64, 24, TOK], BF16)
    tm_d = kb.dout("tm", [TOK, 256 + 36])
    kb.load_consts(id_d, an_d)
    kb.mk_eps()
    w, wb = kb.load_w(w_d, D, 1828, "w_in")
    gbt, gbb = kb.bcast_row(gb_d, 36, "gateb")

    def post(ts_, tsb_):
        P.op("dve", lambda: nc.vector.tensor_tensor(out=ts_[:, 256:292], in0=ts_[:, 256:292], in1=gbt[:], op=ALU.add), reads=[tsb_, gbb], writes=[tsb_])
        P.op("act", lambda: nc.scalar.activation(out=ts_[:, 256:292], in_=ts_[:, 256:292], func=AF.Exp, scale=-1.0), reads=[tsb_], writes=[tsb_])
        P.op("dve", lambda: nc.vector.tensor_scalar(out=ts_[:, 256:292], in0=ts_[:, 256:292], scalar1=1.0, scalar2=None, op0=ALU.add), reads=[tsb_], writes=[tsb_])
        P.op("dve", lambda: nc.vector.reciprocal(out=ts_[:, 256:292], in_=ts_[:, 256:292]), reads=[tsb_], writes=[tsb_])

    proj_phase(kb, x_d, g_d, w, wb, 1828, FM_A, [(1152, 128), (1408, 128), (1536, 36)], fm_d, tm_d, post_tm=post)
    return kb


def core_tokens(c):
    return np.concatenate([np.arange(128 * (4 * m + c), 128 * (4 * m + c) + 128) for m in range(NT)])


def run_A(inputs):
    kb = build_A()
    ident = np.eye(128, dtype=np.float32)
    anti = np.ascontiguousarray(ident[::-1])
    maps = []
    for core in range(8):
        b, c = core // 4, core % 4
        maps.append({"x": np.ascontiguousarray(inputs["x"][b][core_tokens(c)]),
                     "g": inputs["norm_mix"][0:1], "w_in": inputs["nsa_w_in"][0],
                     "gate_b": inputs["nsa_gate_b"][0:1], "ident": ident, "anti": anti})
    return kb.run(maps)


def mm(kb, out, lhsT, rhs, start, reads, writes, sig=False, stop=True):
    nc = kb.nc
    kb.P.op("pe", lambda: nc.tensor.matmul(out, lhsT=lhsT, rhs=rhs, start=start, stop=stop), reads=reads, writes=writes, sig=sig)


def bc3(ap2d):
    return AP(tensor=ap2d.tensor, offset=ap2d.offset, ap=[list(ap2d.ap[0]), [0, 3], list(ap2d.ap[1])])


class Attn:
    def __init__(self, kb):
        self.kb = kb
        self.pT = [kb.sb("pT", [128, 384], BF16) for _ in range(4)]
        self.i = 0
        self.sb_list = [0, 1, 6]
        self.sb_i = 0
        self.pending = []

    def next_sbank(self):
        b = self.sb_list[self.sb_i % 3]
        self.sb_i += 1
        return b

    def unit(self, s_terms, near_terms, accs, accb, v_ap, vbufs, first):
        kb = self.kb
        nc, P = kb.nc, kb.P
        sbk = self.next_sbank()
        ps, psb = kb.ps[sbk], kb.psb[sbk]
        n = len(s_terms)
        for t, (l, r, bufs) in enumerate(s_terms):
            mm(kb, ps[:, 0:384], l, r, t == 0, bufs, [psb], sig=(t == n - 1 and not near_terms))
        if near_terms:
            l, r, bufs = near_terms
            mm(kb, ps[:, 0:384].rearrange("p (a b) -> p a b", b=128), l, r, False, bufs, [psb], sig=True)
        pT, pTb = self.pT[self.i % 4]
        self.i += 1
        P.op("act", lambda: nc.scalar.activation(out=pT[:], in_=ps[:, 0:384], func=AF.Exp), reads=[psb], writes=[pTb])
        self.pending.append((pT, pTb, accs, accb, v_ap, vbufs, first))
        if len(self.pending) > 2:
            self._pv(*self.pending.pop(0))

    def _pv(self, pT, pTb, accs, accb, v_ap, vbufs, first):
        kb = self.kb
        for hh in range(3):
            mm(kb, accs[hh], pT[:, hh * 128:(hh + 1) * 128], v_ap, first[hh], [pTb] + vbufs, [accb[hh]], sig=(hh == 2))

    def flush(self):
        while self.pending:
            self._pv(*self.pending.pop(0))


def build_B(layer, kb=None):
    kb = kb or K("B")
    nc, P = kb.nc, kb.P
    x_d = kb.din("x", [TOK, D])
    id_d = kb.din("identb", [128, 128], BF16)
    an_d = kb.din("antib", [128, 128], BF16)
    relb_d = kb.din("rel_bias", [32, 12])
    oh_s_d = kb.din("oh_s", [33, Y_SLC])
    oh_w_d = kb.din("oh_w", [33, Y_WIN])
    oh_c_d = kb.din("oh_c", [33, Y_CMP])
    qT_d = kb.din("qT2", [128, 6, TOK], BF16)
    qm_d = kb.din("qmT", [64, 4, TOK], BF16)
    gates_d = kb.din("gates", [TOK, 36])
    ks_d = kb.din("ksT2", [128, T], BF16)
    kw_d = kb.din("kwT2", [128, T], BF16)
    kcr_d = kb.din("kcrT2", [128, T], BF16)
    vcr_d = kb.din("vcrT2", [128, T], BF16)
    vs_d = kb.din("vs", [T, 128])
    vw_d = kb.din("vw", [T, 128])
    ew_d = kb.din("ew", [128, T], BF16)
    ov_d = kb.din("ovl", [128, 4, 128], BF16)
    fb_d = kb.din("fb", [NT, 128, 128])
    w1k_d = kb.din("w1k", [2048, 256]); b1k_d = kb.din("b1k", [256, 1]); w2k_d = kb.din("w2k", [256, 64]); b2k_d = kb.din("b2k", [64, 1])
    w1v_d = kb.din("w1v", [2048, 256]); b1v_d = kb.din("b1v", [256, 1]); w2v_d = kb.din("w2v", [256, 64]); b2v_d = kb.din("b2v", [1, 64])
    posk_d = kb.din("posk", [32, 64]); posv_d = kb.din("posv", [32, 64])
    mem_d = kb.din("mem", [256, D]); gmem_d = kb.din("g_mem", [1, D]); wkv_d = kb.din("w_mem_kv", [D, 512])
    wout_d = kb.din("w_out", [D, D])
    hmid_d = kb.dout("hmid", [TOK, D])

    P.dma("sp", kb.ident[:], id_d, writes=[kb.identb])
    P.dma("sp", kb.anti[:], an_d, writes=[kb.antib])
    kb.mk_eps()
    with kb.scope():
        tabs = build_tables(kb, relb_d, [oh_s_d, oh_w_d, oh_c_d], [("s", Y_SLC), ("w", Y_WIN), ("c", Y_CMP)])
    band_s, band_sb = load_band(kb, tabs[0][0], tabs[0][1], Y_SLC, 2048, 1, "band_s")
    band_w, band_wb = load_band(kb, tabs[1][0], tabs[1][1], Y_WIN, 1024, 1, "band_w")
    bandc = [kb.sb("bandc", [128, 12, 128], BF16) for _ in range(4)]
    bandc_i = 0

    ksT, ksTb = kb.sb("ksT", [128, T], BF16)
    P.dma("sp", ksT[:, 0:4096], ks_d[:, 0:4096], writes=[ksTb])
    P.dma("pool", ksT[:, 4096:T], ks_d[:, 4096:T], writes=[ksTb])
    ew, ewb = kb.sb("ew", [128, T], BF16)
    P.dma("sp", ew[:, 0:4096], ew_d[:, 0:4096], writes=[ewb])
    P.dma("pool", ew[:, 4096:T], ew_d[:, 4096:T], writes=[ewb])
    vs1, vs1b = kb.sb("vs1", [128, 64, 2, 65], BF16)
    P.op("pool", lambda: nc.gpsimd.memset(vs1[:], 1.0), writes=[vs1b])
    with kb.scope():
        vst = [kb.sb("vst", [128, 8, 128], F32) for _ in range(2)]
        for i in range(8):
            st, stb = vst[i % 2]
            P.dma(kb.dq(), st[:], vs_d[i * 1024:(i + 1) * 1024, :].rearrange("(a p) c -> p a c", p=128), writes=[stb])
            P.op("pool", lambda: nc.gpsimd.tensor_copy(out=vs1[:, i * 8:(i + 1) * 8, :, 0:64], in_=st[:].rearrange("p a (g d) -> p a g d", g=2)),
                 reads=[stb], writes=[vs1b])
    RC, RCb = kb.sb("RC", [128, 4, 2, 193], BF16)
    P.op("pool", lambda: nc.gpsimd.memset(RC[:], 1.0), writes=[RCb])
    ovt, ovtb = kb.sb("ovt", [128, 4, 128], BF16)
    P.dma("sp", ovt[:], ov_d, writes=[ovtb])
    for g in range(2):
        P.op("pool", lambda: nc.gpsimd.tensor_copy(out=RC[:, :, g, 0:128], in_=ovt[:]), reads=[ovtb], writes=[RCb])
    kcT, kcTb = kb.sb("kcT", [128, 512], BF16)

    with ExitStack() as es2:
        def tmp(name, shape, dt):
            kb.n += 1
            return es2.enter_context(nc.sbuf_tensor("%s_%d" % (name, kb.n), shape, dt)), Buf(name)
        rawT, rawTb = tmp("rawT", [128, T + 32], BF16)
        w1, w1b = tmp("w1", [128, 32, 256], BF16)
        w1st, w1stb = tmp("w1st", [128, 8, 256], F32)
        w2, w2b = tmp("w2", [128, 2, 128], BF16)
        w2st, w2stb = tmp("w2st", [128, 2, 64], F32)
        h1T, h1Tb = tmp("h1T", [128, 2, 512], BF16)
        u_, ub = tmp("u", [128, 512], F32)
        t1, t1b = tmp("t1", [128, 512], F32)
        t2, t2b = tmp("t2", [128, 512], F32)
        cb, cbb = tmp("cb", [128, 2], F32)
        b1t, b1tb = tmp("b1t", [128, 2], F32)
        b2d, b2db = tmp("b2d", [128, 1], F32)
        b2r, b2rb = tmp("b2r", [128, 64], F32)
        pst, pstb = tmp("pst", [32, 64], F32)
        psb16, psb16b = tmp("psb16", [32, 64], BF16)
        posT, posTb = tmp("posT", [64, 1, 32], BF16)
        for kv in range(2):
            raw_d = kcr_d if kv == 0 else vcr_d
            w1_d = w1k_d if kv == 0 else w1v_d
            b1_d = b1k_d if kv == 0 else b1v_d
            w2_d = w2k_d if kv == 0 else w2v_d
            pos_d = posk_d if kv == 0 else posv_d
            P.op("dve", lambda: nc.vector.memset(rawT[:, T:T + 32], 0.0), writes=[rawTb])
            P.dma("sp", rawT[:, 0:4096], raw_d[:, 0:4096], writes=[rawTb])
            P.dma("pool", rawT[:, 4096:T], raw_d[:, 4096:T], writes=[rawTb])
            for q4 in range(4):
                for half in range(2):
                    P.dma(kb.dq(), w1st[half * 64:(half + 1) * 64, :, :], w1_d[q4 * 512:(q4 + 1) * 512, :].rearrange("(l d) h -> d l h", d=64), writes=[w1stb])
                P.op("dve", lambda: nc.vector.tensor_copy(out=w1[:, q4 * 8:(q4 + 1) * 8, :], in_=w1st[:]), reads=[w1stb], writes=[w1b])
            P.dma("sp", w2st[:], w2_d.rearrange("(c p) d -> p c d", p=128), writes=[w2stb])
            for half in range(2):
                P.op("dve", lambda: nc.vector.tensor_copy(out=w2[:, :, half * 64:(half + 1) * 64], in_=w2st[:]), reads=[w2stb], writes=[w2b])
            P.dma("sp", b1t[:], b1_d.rearrange("(c p) o -> p (c o)", p=128), writes=[b1tb])
            P.dma("sp", pst[:], pos_d, writes=[pstb])
            P.op("dve", lambda: nc.vector.tensor_copy(out=psb16[:], in_=pst[:]), reads=[pstb], writes=[psb16b])
            kb.transpose_to([psb16[:, :]], posT, posTb, [psb16b], np_in=32)
            for hc in range(2):
                ps, psb = kb.ps[2], kb.psb[2]
                for l in range(32):
                    mm(kb, ps[:, 0:1], w1[0:64, l, hc * 128:(hc + 1) * 128], posT[:, 0, l:l + 1], l == 0, [w1b, posTb], [psb], sig=(l == 31))
                P.op("dve", lambda: nc.vector.tensor_tensor(out=cb[:, hc:hc + 1], in0=ps[:, 0:1], in1=b1t[:, hc:hc + 1], op=ALU.add), reads=[psb, b1tb], writes=[cbb])
            if kv == 0:
                for half in range(2):
                    P.dma("sp", b2d[half * 64:(half + 1) * 64, :], b2k_d, writes=[b2db])
            else:
                src = AP(tensor=b2v_d.tensor, offset=b2v_d.offset, ap=[[0, 128], [1, 64]])
                P.dma("sp", b2r[:], src, writes=[b2rb])
            for g in range(2):
                gs = slice(g * 64, (g + 1) * 64)
                for hc in range(2):
                    ps, psb = kb.ps[2 + hc], kb.psb[2 + hc]
                    for l in range(32):
                        rhs = AP(tensor=rawT.tensor if hasattr(rawT, "tensor") else rawT[:].tensor, offset=rawT[gs, l:l + 1].offset,
                                 ap=[list(rawT[gs, :].ap[0]), [16, 512]])
                        mm(kb, ps[:, :], w1[gs, l, hc * 128:(hc + 1) * 128], rhs, l == 0, [w1b, rawTb], [psb], sig=(l == 31))
                    P.op("act", lambda: nc.scalar.activation(out=u_[:], in_=ps[:, :], func=AF.Identity, bias=cb[:, hc:hc + 1]), reads=[psb, cbb], writes=[ub])
                    P.op("dve", lambda: nc.vector.tensor_tensor(out=t1[:], in0=u_[:], in1=u_[:], op=ALU.mult), reads=[ub], writes=[t1b])
                    P.op("dve", lambda: nc.vector.tensor_scalar(out=t1[:], in0=t1[:], scalar1=0.044715, scalar2=1.0, op0=ALU.mult, op1=ALU.add), reads=[t1b], writes=[t1b])
                    P.op("dve", lambda: nc.vector.tensor_tensor(out=t1[:], in0=t1[:], in1=u_[:], op=ALU.mult), reads=[t1b, ub], writes=[t1b])
                    P.op("act", lambda: nc.scalar.activation(out=t2[:], in_=t1[:], func=AF.Tanh, scale=0.7978845608028654), reads=[t1b], writes=[t2b])
                    P.op("dve", lambda: nc.vector.tensor_scalar(out=t2[:], in0=t2[:], scalar1=0.5, scalar2=0.5, op0=ALU.mult, op1=ALU.add), reads=[t2b], writes=[t2b])
                    P.op("dve", lambda: nc.vector.tensor_tensor(out=h1T[:, hc, :], in0=t2[:], in1=u_[:], op=ALU.mult), reads=[t2b, ub], writes=[h1Tb])
                if kv == 0:
                    ps, psb = kb.ps[4], kb.psb[4]
                    for hc in range(2):
                        mm(kb, ps[:, :], w2[:, hc, :], h1T[:, hc, :], hc == 0, [w2b, h1Tb], [psb], sig=(hc == 1))
                    P.op("act", lambda: nc.scalar.activation(out=kcT[gs, :], in_=ps[gs, :], func=AF.Identity, bias=b2d[gs, 0:1]), reads=[psb, b2db], writes=[kcTb])
                else:
                    for j in range(4):
                        ps, psb = kb.ps[4], kb.psb[4]
                        for hc in range(2):
                            mm(kb, ps[:, 0:64], h1T[:, hc, j * 128:(j + 1) * 128], w2[:, hc, 0:64], hc == 0, [w2b, h1Tb], [psb], sig=(hc == 1))
                        P.op("dve", lambda: nc.vector.tensor_tensor(out=RC[:, j, g, 129:193], in0=ps[:, 0:64], in1=b2r[:], op=ALU.add), reads=[psb, b2rb], writes=[RCb])

    P.barrier()
    kmT, kmTb = kb.sb("kmT", [64, 4, 256], BF16)
    vm1, vm1b = kb.sb("vm1", [128, 2, 4, 65], BF16)
    P.op("pool", lambda: nc.gpsimd.memset(vm1[:], 1.0), writes=[vm1b])
    gm, gmb = kb.bcast_row(gmem_d, D, "gmem")
    wkv, wkvb = kb.sb("wkv", [128, 8, 512], BF16)
    with ExitStack() as es2:
        def tmp(name, shape, dt):
            kb.n += 1
            return es2.enter_context(nc.sbuf_tensor("%s_%d" % (name, kb.n), shape, dt)), Buf(name)
        mt, mtb = tmp("mt", [128, D], F32)
        mn, mnb = tmp("mn", [128, D], BF16)
        mnT, mnTb = tmp("mnT", [128, 8, 256], BF16)
        scr, scrb = tmp("mscr", [128, 4], F32)
        wst = [tmp("wst2", [128, 512], F32) for _ in range(2)]
        for k in range(8):
            st, stb = wst[k % 2]
            P.dma(kb.dq(), st[:], wkv_d[k * 128:(k + 1) * 128, :], writes=[stb])
            P.op("dve", lambda: nc.vector.tensor_copy(out=wkv[:, k, :], in_=st[:]), reads=[stb], writes=[wkvb])
        for tl in range(2):
            P.dma("sp", mt[:], mem_d[tl * 128:(tl + 1) * 128, :], writes=[mtb])
            kb.rmsnorm(mt[:], mtb, gm[:], gmb, mn[:], mnb, D, scr, scrb)
            pt = kb.pt[kb.pti % 2]; ptb = kb.ptb[kb.pti % 2]; kb.pti += 1
            for j in range(8):
                P.op("pe", lambda j=j: nc.tensor.transpose(pt[:, j * 128:(j + 1) * 128], mn[:, j * 128:(j + 1) * 128], kb.ident[:]),
                     reads=[mnb, kb.identb], writes=[ptb], sig=(j == 7))
            P.op("dve", lambda: nc.vector.tensor_copy(out=mnT[:, :, tl * 128:(tl + 1) * 128], in_=pt[:, :].rearrange("p (a b) -> p a b", b=128)), reads=[ptb], writes=[mnTb])
        for h in range(4):
            ps, psb = kb.ps[2], kb.psb[2]
            for k in range(8):
                mm(kb, ps[0:64, 0:256], wkv[:, k, h * 64:(h + 1) * 64], mnT[:, k, :], k == 0, [wkvb, mnTb], [psb], sig=(k == 7))
            P.op("dve", lambda: nc.vector.tensor_copy(out=kmT[:, h, :], in_=ps[0:64, 0:256]), reads=[psb], writes=[kmTb])
        for tl in range(2):
            ps, psb = kb.ps[3], kb.psb[3]
            for k in range(8):
                mm(kb, ps[:, 0:256], mnT[:, k, tl * 128:(tl + 1) * 128], wkv[:, k, 256:512], k == 0, [wkvb, mnTb], [psb], sig=(k == 7))
            P.op("dve", lambda: nc.vector.tensor_copy(out=vm1[:, tl, :, 0:64], in_=ps[:, 0:256].rearrange("p (h d) -> p h d", d=64)), reads=[psb], writes=[vm1b])
    P.barrier()
    wout, woutb = kb.sb("wout", [128, 8, D], BF16)
    with kb.scope():
        wst = [kb.sb("wst3", [128, 1024], F32) for _ in range(2)]
        for k in range(8):
            st, stb = wst[k % 2]
            P.dma(kb.dq(), st[:], wout_d[k * 128:(k + 1) * 128, :], writes=[stb])
            P.op("dve", lambda: nc.vector.tensor_copy(out=wout[:, k, :], in_=st[:]), reads=[stb], writes=[woutb])

    at = Attn(kb)
    qs = [kb.sb("qs", [128, 6, 128], BF16) for _ in range(2)]
    qms = [kb.sb("qms", [64, 4, 128], BF16) for _ in range(2)]
    gts = [kb.sb("gts", [128, 12, 3], F32) for _ in range(2)]
    kws = [kb.sb("kws", [128, 1024], BF16) for _ in range(2)]
    vws = [kb.sb("vws", [128, 8, 2, 65], BF16) for _ in range(2)]
    vwst = [kb.sb("vwst", [128, 8, 128], F32)] * 2
    for (t_, b_) in vws:
        P.op("pool", lambda: nc.gpsimd.memset(t_[:], 1.0), writes=[b_])
    fbs = [kb.sb("fbs", [128, 128], F32) for _ in range(2)]
    xts = [kb.sb("xts", [128, D], F32)] * 2
    cat, catb = kb.sb("cat", [128, D], F32)
    catbf, catbfb = kb.sb("catbf", [128, D], BF16)
    catT, catTb = kb.sb("catT", [128, 8, 128], BF16)
    pslc, pslcb = kb.sb("pslc", [128, 2, 128], F32)
    wk, wkb = kb.sb("wk", [128, 128], F32)
    m8, m8b = kb.sb("m8", [128, 16], F32)
    sbf, sbfb = kb.sb("sbf", [128, 128], BF16)
    sbT, sbTb = kb.sb("sbT", [128, 2, 128], BF16)
    sm, smb = kb.sb("sm", [128, 16], F32)
    pm, pmb = kb.sb("pm", [128, 256], BF16)
    accbank = 2

    def next_acc():
        nonlocal accbank
        b = accbank
        accbank = 2 + (accbank - 2 + 1) % 4
        return b

    for m in range(NT):
        q_, qb_ = qs[m % 2]
        qm_, qmb_ = qms[m % 2]
        gt_, gtb_ = gts[m % 2]
        kw_, kwb_ = kws[m % 2]
        vw_, vwb_ = vws[m % 2]
        vwst_, vwstb_ = vwst[m % 2]
        fb_, fbb_ = fbs[m % 2]
        xt_, xtb_ = xts[m % 2]
        tsl = slice(m * 128, (m + 1) * 128)
        P.dma("sp", q_[:], qT_d[:, :, tsl], writes=[qb_])
        P.dma("sp", qm_[:], qm_d[:, :, tsl], writes=[qmb_])
        P.dma("sp", gt_[:], gates_d[tsl, :].rearrange("p (h b) -> p h b", b=3), writes=[gtb_])
        P.dma("sp", fb_[:], fb_d[m], writes=[fbb_])
        P.dma("pool", xt_[:], x_d[tsl, :], writes=[xtb_])
        kt0 = max(0, 4 * m - 4)
        nkw = 4 * m + 4 - kt0
        P.dma("pool", kw_[:, 0:nkw * 128], kw_d[:, kt0 * 128:(4 * m + 4) * 128], writes=[kwb_])
        P.dma("sp", vwst_[:, 0:nkw, :], vw_d[kt0 * 128:(4 * m + 4) * 128, :].rearrange("(a p) c -> p a c", p=128), writes=[vwstb_])
        P.op("pool", lambda: nc.gpsimd.tensor_copy(out=vw_[:, 0:nkw, :, 0:64], in_=vwst_[:, 0:nkw, :].rearrange("p a (g d) -> p a g d", g=2)),
             reads=[vwstb_], writes=[vwb_])
        cband = {}
        for j in range(4):
            v = m - 4 * j
            if 0 <= v <= 6:
                dst = bandc[bandc_i % 4]
                bandc_i += 1
                load_band(kb, tabs[2][0], tabs[2][1], Y_CMP, 128, 16, "bc", zoff=512 * v, dst=dst)
                cband[j] = dst
        for g in range(2):
            gs = slice(g * 64, (g + 1) * 64)
            for tr in range(2):
                bA, bB = next_acc(), next_acc()
                accs = [kb.ps[bA][:, 0:193], kb.ps[bA][:, 193:386], kb.ps[bB][:, 0:193]]
                accb = [kb.psb[bA], kb.psb[bA], kb.psb[bB]]
                js = [j for j in range(4) if m - 4 * j >= 0]
                for ji, j in enumerate(js):
                    s_terms = [(kcT[gs, j * 128:(j + 1) * 128], q_[gs, 3 * tr:3 * tr + 3, :], [kcTb, qb_])]
                    near = None
                    if j in cband:
                        bt, btb = cband[j]
                        near = (kb.anti[:], bt[:, 6 * g + 3 * tr:6 * g + 3 * tr + 3, :], [kb.antib, btb])
                    at.unit(s_terms, near, accs, accb, RC[:, j, g, :], [RCb], [ji == 0, False, ji == 0])
                at.flush()
                for hh in range(3):
                    h = 6 * g + 3 * tr + hh
                    U = accs[hh]
                    P.op("dve", lambda: nc.vector.tensor_scalar(out=sm[:, 0:1], in0=U[:, 128:129], scalar1=1e-30, scalar2=None, op0=ALU.max), reads=[accb[hh]], writes=[smb])
                    P.op("dve", lambda: nc.vector.reciprocal(out=sm[:, 1:2], in_=sm[:, 0:1]), reads=[smb], writes=[smb])
                    if tr == 0 and hh == 0:
                        P.op("dve", lambda: nc.vector.tensor_scalar(out=pslc[:, g, :], in0=U[:, 0:128], scalar1=sm[:, 1:2], scalar2=None, op0=ALU.mult), reads=[accb[hh], smb], writes=[pslcb])
                    else:
                        P.op("dve", lambda: nc.vector.scalar_tensor_tensor(out=pslc[:, g, :], in0=U[:, 0:128], scalar=sm[:, 1:2], in1=pslc[:, g, :], op0=ALU.mult, op1=ALU.add), reads=[accb[hh], smb, pslcb], writes=[pslcb])
                    P.op("dve", lambda: nc.vector.tensor_tensor(out=sm[:, 2:3], in0=sm[:, 1:2], in1=gt_[:, h, 0:1], op=ALU.mult), reads=[smb, gtb_], writes=[smb])
                    P.op("dve", lambda: nc.vector.tensor_scalar(out=cat[:, h * 64:(h + 1) * 64], in0=U[:, 129:193], scalar1=sm[:, 2:3], scalar2=None, op0=ALU.mult), reads=[accb[hh], smb], writes=[catb])
            P.op("dve", lambda: nc.vector.tensor_tensor(out=wk[:], in0=pslc[:, g, :], in1=fb_[:], op=ALU.add), reads=[pslcb, fbb_], writes=[wkb])
            P.op("dve", lambda: nc.vector.max(out=m8[:, 0:8], in_=wk[:]), reads=[wkb], writes=[m8b])
            P.op("dve", lambda: nc.vector.match_replace(out=pslc[:, g, :], in_to_replace=m8[:, 0:8], in_values=wk[:], imm_value=-3e30), reads=[wkb, m8b], writes=[pslcb])
            P.op("dve", lambda: nc.vector.max(out=m8[:, 8:16], in_=pslc[:, g, :]), reads=[pslcb], writes=[m8b])
            P.op("dve", lambda: nc.vector.tensor_scalar(out=wk[:], in0=wk[:], scalar1=m8[:, 15:16], scalar2=-NEGB, op0=ALU.is_ge, op1=ALU.mult), reads=[wkb, m8b], writes=[wkb])
            P.op("dve", lambda: nc.vector.tensor_scalar(out=sbf[:], in0=wk[:], scalar1=NEGB, scalar2=None, op0=ALU.add), reads=[wkb], writes=[sbfb])
            pt = kb.pt[kb.pti % 2]; ptb = kb.ptb[kb.pti % 2]; kb.pti += 1
            P.op("pe", lambda: nc.tensor.transpose(pt[:, 0:128], sbf[:], kb.ident[:]), reads=[sbfb, kb.identb], writes=[ptb])
            P.op("dve", lambda: nc.vector.tensor_copy(out=sbT[:, g, :], in_=pt[:, 0:128]), reads=[ptb], writes=[sbTb])
        for br in range(2):
            for g in range(2):
                gs = slice(g * 64, (g + 1) * 64)
                for tr in range(2):
                    bA = next_acc()
                    accs = [kb.ps[bA][:, 65 * hh:65 * hh + 65] for hh in range(3)]
                    accb = [kb.psb[bA]] * 3
                    kts = list(range(0, 4 * m + 4)) if br == 0 else list(range(kt0, 4 * m + 4))
                    for ki, kt in enumerate(kts):
                        u = kt - 4 * m
                        if br == 0:
                            s_terms = [(ksT[gs, kt * 128:(kt + 1) * 128], q_[gs, 3 * tr:3 * tr + 3, :], [ksTb, qb_]),
                                       (ew[:, kt * 128:(kt + 1) * 128], bc3(sbT[:, g, :]), [ewb, sbTb])]
                            near = None
                            if u >= -12:
                                z0 = 128 * (3 - u)
                                near = (kb.anti[:], band_s[:, 6 * g + 3 * tr:6 * g + 3 * tr + 3, z0:z0 + 128], [kb.antib, band_sb])
                            v_ap, vb_ = vs1[:, kt, g, :], [vs1b]
                        else:
                            kk = kt - kt0
                            s_terms = [(kw_[gs, kk * 128:(kk + 1) * 128], q_[gs, 3 * tr:3 * tr + 3, :], [kwb_, qb_])]
                            z0 = 128 * (3 - u)
                            near = (kb.anti[:], band_w[:, 6 * g + 3 * tr:6 * g + 3 * tr + 3, z0:z0 + 128], [kb.antib, band_wb])
                            v_ap, vb_ = vw_[:, kk, g, :], [vwb_]
                        at.unit(s_terms, near, accs, accb, v_ap, vb_, [ki == 0, False, False])
                    at.flush()
                    for hh in range(3):
                        h = 6 * g + 3 * tr + hh
                        O = accs[hh]
                        P.op("dve", lambda: nc.vector.tensor_scalar(out=sm[:, 0:1], in0=O[:, 64:65], scalar1=1e-30, scalar2=None, op0=ALU.max), reads=[accb[hh]], writes=[smb])
                        P.op("dve", lambda: nc.vector.reciprocal(out=sm[:, 1:2], in_=sm[:, 0:1]), reads=[smb], writes=[smb])
                        P.op("dve", lambda: nc.vector.tensor_tensor(out=sm[:, 2:3], in0=sm[:, 1:2], in1=gt_[:, h, 1 + br:2 + br], op=ALU.mult), reads=[smb, gtb_], writes=[smb])
                        P.op("dve", lambda: nc.vector.scalar_tensor_tensor(out=cat[:, h * 64:(h + 1) * 64], in0=O[:, 0:64], scalar=sm[:, 2:3], in1=cat[:, h * 64:(h + 1) * 64], op0=ALU.mult, op1=ALU.add),
                             reads=[accb[hh], smb, catb], writes=[catb])
        for h in range(4):
            sbk = at.next_sbank()
            ps, psb = kb.ps[sbk], kb.psb[sbk]
            for tl in range(2):
                mm(kb, ps[:, tl * 128:(tl + 1) * 128], kmT[:, h, tl * 128:(tl + 1) * 128], qm_[:, h, :], tl == 0, [kmTb, qmb_], [psb], sig=(tl == 1))
            P.op("act", lambda: nc.scalar.activation(out=pm[:], in_=ps[:, 0:256], func=AF.Exp), reads=[psb], writes=[pmb])
            bA = next_acc()
            O = kb.ps[bA][:, 0:65]
            for tl in range(2):
                mm(kb, O, pm[:, tl * 128:(tl + 1) * 128], vm1[:, tl, h, :], tl == 0, [pmb, vm1b], [kb.psb[bA]], sig=(tl == 1))
            P.op("dve", lambda: nc.vector.reciprocal(out=sm[:, 4:5], in_=O[:, 64:65]), reads=[kb.psb[bA]], writes=[smb])
            P.op("dve", lambda: nc.vector.tensor_scalar(out=cat[:, 768 + h * 64:768 + (h + 1) * 64], in0=O[:, 0:64], scalar1=sm[:, 4:5], scalar2=None, op0=ALU.mult), reads=[kb.psb[bA], smb], writes=[catb])
        P.op("act", lambda: nc.scalar.copy(out=catbf[:], in_=cat[:]), reads=[catb], writes=[catbfb])
        pt = kb.pt[kb.pti % 2]; ptb = kb.ptb[kb.pti % 2]; kb.pti += 1
        for j in range(8):
            P.op("pe", lambda j=j: nc.tensor.transpose(pt[:, j * 128:(j + 1) * 128], catbf[:, j * 128:(j + 1) * 128], kb.ident[:]),
                 reads=[catbfb, kb.identb], writes=[ptb], sig=(j == 7))
        P.op("dve", lambda: nc.vector.tensor_copy(out=catT[:], in_=pt[:, :].rearrange("p (a b) -> p a b", b=128)), reads=[ptb], writes=[catTb])
        for half in range(2):
            bA = next_acc()
            ps, psb = kb.ps[bA], kb.psb[bA]
            for k in range(8):
                mm(kb, ps[:, :], catT[:, k, :], wout[:, k, half * 512:(half + 1) * 512], k == 0, [catTb, woutb], [psb], sig=(k == 7))
            P.op("dve", lambda: nc.vector.tensor_tensor(out=xt_[:, half * 512:(half + 1) * 512], in0=ps[:, :], in1=xt_[:, half * 512:(half + 1) * 512], op=ALU.add), reads=[psb, xtb_], writes=[xtb_])
        P.dma("sp", hmid_d[tsl, :], xt_[:], reads=[xtb_], final=True)
    return kb


def gather_seq(arrs, axis):
    shp = list(arrs[0].shape)
    shp[axis] = T
    out = np.zeros(shp, arrs[0].dtype)
    for c in range(4):
        idx = [slice(None)] * len(shp)
        idx[axis] = core_tokens(c)
        out[tuple(idx)] = arrs[c]
    return out


def consts_B(c):
    ident = np.eye(128, dtype=np.float32)
    oh_s, oh_w, oh_c = host_tables(c)
    ew = (np.arange(T)[None, :] // 64 == np.arange(128)[:, None]).astype(NPBF)
    n = np.arange(512)
    cs, ce = n * 16, n * 16 + 31
    ss = np.arange(128) * 64
    ov = ((cs[:, None] <= ss[None, :] + 63) & (ce[:, None] >= ss[None, :])).astype(np.float32)
    ov[511] = 0
    ovl = np.ascontiguousarray(ov.reshape(4, 128, 128).transpose(1, 0, 2)).astype(NPBF)
    fb = np.zeros((NT, 128, 128), np.float32)
    blk = np.arange(128)[None, :]
    for m in range(NT):
        t = 128 * (4 * m + c) + np.arange(128)[:, None]
        cur = t // 64
        forced = (blk == 0) | (blk == cur) | (blk == cur - 1)
        adm = (blk * 64) <= t
        fb[m] = np.where(adm, 1e4 * forced, -1e30)
    return {"identb": ident.astype(NPBF), "antib": np.ascontiguousarray(ident[::-1]).astype(NPBF),
            "oh_s": oh_s, "oh_w": oh_w, "oh_c": oh_c, "ew": ew, "ovl": ovl, "fb": fb}


def to2(fm, lo):
    return np.ascontiguousarray(np.concatenate([fm[:, lo, :], fm[:, lo + 1, :]], axis=0))


def run_B(inputs, resA):
    kb = build_B(0)
    maps = []
    for core in range(8):
        b, c = core // 4, core % 4
        grp = [resA[4 * b + cc] for cc in range(4)]
        fms = [np.asarray(r["fmT"]) for r in grp]
        tms = [np.asarray(r["tm"]) for r in grp]
        fm = fms[c]
        mp = consts_B(c)
        mp["x"] = np.ascontiguousarray(inputs["x"][b][core_tokens(c)])
        mp["rel_bias"] = inputs["rel_bias"]
        q = fm[:, 0:12, :]
        mp["qT2"] = np.ascontiguousarray(np.concatenate([q[:, 0:6, :], q[:, 6:12, :]], axis=0))
        mp["qmT"] = np.ascontiguousarray(fm[:, 20:24, :])
        mp["gates"] = np.ascontiguousarray(tms[c][:, 256:292])
        mp["kcrT2"] = gather_seq([to2(f, 12) for f in fms], 1)
        mp["vcrT2"] = gather_seq([to2(f, 14) for f in fms], 1)
        mp["ksT2"] = gather_seq([to2(f, 16) for f in fms], 1)
        mp["kwT2"] = gather_seq([to2(f, 18) for f in fms], 1)
        mp["vs"] = gather_seq([np.ascontiguousarray(t_[:, 0:128]) for t_ in tms], 0)
        mp["vw"] = gather_seq([np.ascontiguousarray(t_[:, 128:256]) for t_ in tms], 0)
        mp["w1k"] = inputs["nsa_cmp_k_w1"][0]; mp["b1k"] = inputs["nsa_cmp_k_b1"][0].reshape(256, 1)
        mp["w2k"] = inputs["nsa_cmp_k_w2"][0]; mp["b2k"] = inputs["nsa_cmp_k_b2"][0].reshape(64, 1)
        mp["w1v"] = inputs["nsa_cmp_v_w1"][0]; mp["b1v"] = inputs["nsa_cmp_v_b1"][0].reshape(256, 1)
        mp["w2v"] = inputs["nsa_cmp_v_w2"][0]; mp["b2v"] = inputs["nsa_cmp_v_b2"][0].reshape(1, 64)
        mp["posk"] = inputs["nsa_cmp_pos_k"][0]; mp["posv"] = inputs["nsa_cmp_pos_v"][0]
        mp["mem"] = inputs["mem"][b]; mp["g_mem"] = inputs["norm_mem"][0:1]; mp["w_mem_kv"] = inputs["w_mem_kv"][0]
        mp["w_out"] = inputs["w_out"][0]
        maps.append(mp)
    return kb.run(maps)


def scatter_tokens(per_core, key):
    out = np.zeros((2, T, D), np.float32)
    for core in range(8):
        b, c = core // 4, core % 4
        out[b][core_tokens(c)] = np.asarray(per_core[core][key])
    return out


def kernel_unfused(**inputs):
    inputs = {k: np.asarray(v) for k, v in inputs.items()}
    resA = run_A(inputs)
    resB = run_B(inputs, resA)
    resF = run_F(inputs, [r["hmid"] for r in resB], 0, False)
    resC = run_C(inputs, resF)
    resG = run_F(inputs, [r["hmid"] for r in resC], 1, True)
    return scatter_tokens(resG, "h")


def build_F(final, nproj, kb=None):
    kb = kb or K("F")
    nc, P = kb.nc, kb.P
    hm_d = kb.din("hmid", [TOK, D])
    id_d = kb.din("identb", [128, 128], BF16)
    g_d = kb.din("g_ffn", [1, D])
    wg_d = kb.din("wg", [D, DFF]); wu_d = kb.din("wu", [D, DFF]); wd_d = kb.din("wd", [DFF, D])
    g2_d = kb.din("g2", [1, D])
    h_d = kb.dout("h", [TOK, D])
    if not final:
        win_d = kb.din("w_in2", [D, nproj])
        pr_d = kb.dout("pr", [TOK, nproj])
    P.dma("sp", kb.ident[:], id_d, writes=[kb.identb])
    kb.mk_eps()
    g, gb = kb.bcast_row(g_d, D, "gffn")
    g2, g2b = kb.bcast_row(g2_d, D, "g2")
    H, Hb = kb.sb("H", [128, NT, D], F32)
    Hbs = [Buf("H%d" % i) for i in range(NT)]
    hnT, hnTb = kb.sb("hnT", [128, 8, TOK], BF16)
    hn, hnb = kb.sb("hn", [128, D], BF16)
    scr, scrb = kb.sb("scr", [128, 4], F32)

    def norm_T(gt, gtb):
        for ti in range(NT):
            kb.rmsnorm(H[:, ti, :], Hbs[ti], gt[:], gtb, hn[:], hnb, D, scr, scrb)
            pt = kb.pt[kb.pti % 2]; ptb = kb.ptb[kb.pti % 2]; kb.pti += 1
            for j in range(8):
                P.op("pe", lambda j=j: nc.tensor.transpose(pt[:, j * 128:(j + 1) * 128], hn[:, j * 128:(j + 1) * 128], kb.ident[:]),
                     reads=[hnb, kb.identb], writes=[ptb], sig=(j == 7))
            P.op("dve", lambda: nc.vector.tensor_copy(out=hnT[:, :, ti * 128:(ti + 1) * 128], in_=pt[:, :].rearrange("p (a b) -> p a b", b=128)), reads=[ptb], writes=[hnTb])

    for ti in range(NT):
        P.dma(kb.dq(), H[:, ti, :], hm_d[ti * 128:(ti + 1) * 128, :], writes=[Hbs[ti]])
    norm_T(g, gb)
    wbuf = [(kb.sb("wgs", [128, 8, 512], BF16), kb.sb("wus", [128, 8, 512], BF16), kb.sb("wds", [128, 4, D], BF16)) for _ in range(2)]
    stg = [kb.sb("stg", [128, 1024], F32) for _ in range(3)]
    sgs = [kb.sb("sg", [128, 512], F32) for _ in range(2)]
    abs_ = [kb.sb("ab", [128, 512], BF16) for _ in range(2)]
    aTs = [kb.sb("aT", [128, 4, 128], BF16) for _ in range(2)]
    si = 0

    def load_fg(fg):
        nonlocal si
        (wgs, wgsb), (wus, wusb), (wds, wdsb) = wbuf[fg % 2]
        c0 = fg * 512
        cw = min(512, DFF - c0)
        nch = cw // 128
        for (wsrc, wdst, wdstb) in ((wg_d, wgs, wgsb), (wu_d, wus, wusb)):
            for k in range(8):
                st, stb = stg[si % 3]; si += 1
                P.dma(kb.dq(), st[:, 0:cw], wsrc[k * 128:(k + 1) * 128, c0:c0 + cw], writes=[stb])
                kb.cast("pool", wdst[:, k, 0:cw], st[:, 0:cw], [stb], [wdstb])
        for ch in range(nch):
            st, stb = stg[si % 3]; si += 1
            P.dma(kb.dq(), st[:], wd_d[c0 + ch * 128:c0 + (ch + 1) * 128, :], writes=[stb])
            kb.cast("pool", wds[:, ch, :], st[:], [stb], [wdsb])

    def stage1(fg, ti):
        (wgs, wgsb), (wus, wusb), _ = wbuf[fg % 2]
        cw = min(512, DFF - fg * 512)
        tsl = slice(ti * 128, (ti + 1) * 128)
        b0 = 0 if ti % 2 == 0 else 4
        pg, pgb = kb.ps[b0], kb.psb[b0]
        pu, pub = kb.ps[b0 + 1], kb.psb[b0 + 1]
        sg, sgb = sgs[ti % 2]
        ab, abb = abs_[ti % 2]
        for k in range(8):
            mm(kb, pg[:, 0:cw], hnT[:, k, tsl], wgs[:, k, 0:cw], k == 0, [hnTb, wgsb], [pgb], sig=(k == 7))
        for k in range(8):
            mm(kb, pu[:, 0:cw], hnT[:, k, tsl], wus[:, k, 0:cw], k == 0, [hnTb, wusb], [pub], sig=(k == 7))
        P.op("act", lambda: nc.scalar.activation(out=sg[:, 0:cw], in_=pg[:, 0:cw], func=AF.Silu), reads=[pgb], writes=[sgb])
        P.op("dve", lambda: nc.vector.tensor_tensor(out=ab[:, 0:cw], in0=sg[:, 0:cw], in1=pu[:, 0:cw], op=ALU.mult), reads=[sgb, pub], writes=[abb])

    def stage2(fg, ti):
        _, _, (wds, wdsb) = wbuf[fg % 2]
        cw = min(512, DFF - fg * 512)
        nch = cw // 128
        ab, abb = abs_[ti % 2]
        aT, aTb = aTs[ti % 2]
        pt = kb.pt[kb.pti % 2]; ptb = kb.ptb[kb.pti % 2]; kb.pti += 1
        for j in range(nch):
            P.op("pe", lambda j=j: nc.tensor.transpose(pt[:, j * 128:(j + 1) * 128], ab[:, j * 128:(j + 1) * 128], kb.ident[:]),
                 reads=[abb, kb.identb], writes=[ptb], sig=(j == nch - 1))
        P.op("act", lambda: nc.scalar.copy(out=aT[:, 0:nch, :], in_=pt[:, 0:nch * 128].rearrange("p (a b) -> p a b", b=128)), reads=[ptb], writes=[aTb])
        for half in range(2):
            py, pyb = kb.ps[2 + half], kb.psb[2 + half]
            for ch in range(nch):
                mm(kb, py[:, :], aT[:, ch, :], wds[:, ch, half * 512:(half + 1) * 512], ch == 0, [aTb, wdsb], [pyb], sig=(ch == nch - 1))
            P.op("dve", lambda: nc.vector.tensor_tensor(out=H[:, ti, half * 512:(half + 1) * 512], in0=py[:, :], in1=H[:, ti, half * 512:(half + 1) * 512], op=ALU.add),
                 reads=[pyb, Hbs[ti]], writes=[Hbs[ti]])

    load_fg(0)
    for fg in range(6):
        if fg + 1 < 6:
            load_fg(fg + 1)
        stage1(fg, 0)
        for ti in range(NT):
            if ti + 1 < NT:
                stage1(fg, ti + 1)
            stage2(fg, ti)
    if final:
        o, ob = kb.sb("o", [128, D], F32)
        for ti in range(NT):
            kb.rmsnorm(H[:, ti, :], Hbs[ti], g2[:], g2b, o[:], ob, D, scr, scrb)
            P.dma(kb.dq(), h_d[ti * 128:(ti + 1) * 128, :], o[:], reads=[ob], final=True)
    else:
        for ti in range(NT):
            P.dma(kb.dq(), h_d[ti * 128:(ti + 1) * 128, :], H[:, ti, :], reads=[Hbs[ti]], final=True)
        norm_T(g2, g2b)
        win, winb = kb.sb("win", [128, 8, nproj], BF16)
        for k in range(8):
            st, stb = stg[si % 3]; si += 1
            P.dma(kb.dq(), st[:, 0:nproj], win_d[k * 128:(k + 1) * 128, :], writes=[stb])
            kb.cast("pool" if k % 2 else "act", win[:, k, :], st[:, 0:nproj], [stb], [winb])
        pro, prob = kb.sb("pro", [128, nproj], F32)
        for ti in range(NT):
            tsl = slice(ti * 128, (ti + 1) * 128)
            o0 = 0
            bi = 0
            while o0 < nproj:
                n = min(512, nproj - o0)
                ps, psb = kb.ps[bi % 2], kb.psb[bi % 2]
                for k in range(8):
                    mm(kb, ps[:, 0:n], hnT[:, k, tsl], win[:, k, o0:o0 + n], k == 0, [hnTb, winb], [psb], sig=(k == 7))
                P.op("act", lambda: nc.scalar.copy(out=pro[:, o0:o0 + n], in_=ps[:, 0:n]), reads=[psb], writes=[prob])
                o0 += n
                bi += 1
            P.dma(kb.dq(), pr_d[tsl, :], pro[:], reads=[prob], final=True)
    return kb


def run_F(inputs, hmids, layer, final):
    kb = build_F(final, 712)
    ident = np.eye(128, dtype=np.float32).astype(NPBF)
    maps = []
    for core in range(8):
        mp = {"hmid": np.asarray(hmids[core]), "identb": ident, "g_ffn": inputs["norm_ffn"][layer:layer + 1],
              "wg": inputs["ffn_gate"][layer], "wu": inputs["ffn_up"][layer], "wd": inputs["ffn_down"][layer]}
        if final:
            mp["g2"] = inputs["norm_final"].reshape(1, D)
        else:
            mp["g2"] = inputs["norm_mix"][layer + 1:layer + 2]
            mp["w_in2"] = inputs["dsa_w_in"][0]
        maps.append(mp)
    return kb.run(maps)


NIT = 16


def mem_setup(kb, mem_d, gmem_d, wkv_d):
    nc, P = kb.nc, kb.P
    kmT, kmTb = kb.sb("kmT", [64, 4, 256], BF16)
    vm1, vm1b = kb.sb("vm1", [128, 2, 4, 65], BF16)
    P.op("pool", lambda: nc.gpsimd.memset(vm1[:], 1.0), writes=[vm1b])
    with kb.scope():
        gm, gmb = kb.bcast_row(gmem_d, D, "gmem")
        wkv, wkvb = kb.sb("wkv", [128, 8, 512], BF16)
        mt, mtb = kb.sb("mt", [128, D], F32)
        mn, mnb = kb.sb("mn", [128, D], BF16)
        mnT, mnTb = kb.sb("mnT", [128, 8, 256], BF16)
        scr, scrb = kb.sb("mscr", [128, 4], F32)
        wst = [kb.sb("wst2", [128, 512], F32) for _ in range(2)]
        for k in range(8):
            st, stb = wst[k % 2]
            P.dma(kb.dq(), st[:], wkv_d[k * 128:(k + 1) * 128, :], writes=[stb])
            P.op("dve", lambda: nc.vector.tensor_copy(out=wkv[:, k, :], in_=st[:]), reads=[stb], writes=[wkvb])
        for tl in range(2):
            P.dma("sp", mt[:], mem_d[tl * 128:(tl + 1) * 128, :], writes=[mtb])
            kb.rmsnorm(mt[:], mtb, gm[:], gmb, mn[:], mnb, D, scr, scrb)
            pt = kb.pt[kb.pti % 2]; ptb = kb.ptb[kb.pti % 2]; kb.pti += 1
            for j in range(8):
                P.op("pe", lambda j=j: nc.tensor.transpose(pt[:, j * 128:(j + 1) * 128], mn[:, j * 128:(j + 1) * 128], kb.ident[:]),
                     reads=[mnb, kb.identb], writes=[ptb], sig=(j == 7))
            P.op("dve", lambda: nc.vector.tensor_copy(out=mnT[:, :, tl * 128:(tl + 1) * 128], in_=pt[:, :].rearrange("p (a b) -> p a b", b=128)), reads=[ptb], writes=[mnTb])
        for h in range(4):
            ps, psb = kb.ps[2], kb.psb[2]
            for k in range(8):
                mm(kb, ps[0:64, 0:256], wkv[:, k, h * 64:(h + 1) * 64], mnT[:, k, :], k == 0, [wkvb, mnTb], [psb], sig=(k == 7))
            P.op("dve", lambda: nc.vector.tensor_copy(out=kmT[:, h, :], in_=ps[0:64, 0:256]), reads=[psb], writes=[kmTb])
        for tl in range(2):
            ps, psb = kb.ps[3], kb.psb[3]
            for k in range(8):
                mm(kb, ps[:, 0:256], mnT[:, k, tl * 128:(tl + 1) * 128], wkv[:, k, 256:512], k == 0, [wkvb, mnTb], [psb], sig=(k == 7))
            P.op("dve", lambda: nc.vector.tensor_copy(out=vm1[:, tl, :, 0:64], in_=ps[:, 0:256].rearrange("p (h d) -> p h d", d=64)), reads=[psb], writes=[vm1b])
    return kmT, kmTb, vm1, vm1b


def load_wout(kb, wout_d):
    nc, P = kb.nc, kb.P
    wout, woutb = kb.sb("wout", [128, 8, D], BF16)
    with kb.scope():
        wst = [kb.sb("wst3", [128, 1024], F32) for _ in range(2)]
        for k in range(8):
            st, stb = wst[k % 2]
            P.dma(kb.dq(), st[:], wout_d[k * 128:(k + 1) * 128, :], writes=[stb])
            P.op("dve", lambda: nc.vector.tensor_copy(out=wout[:, k, :], in_=st[:]), reads=[stb], writes=[woutb])
    return wout, woutb


def rms_small(kb, x, xb, A, d, g, gb, out, outb, tmp, tmpb, ss, ssb):
    nc, P = kb.nc, kb.P
    P.op("dve", lambda: nc.vector.tensor_tensor(out=tmp, in0=x, in1=x, op=ALU.mult), reads=[xb], writes=[tmpb])
    P.op("dve", lambda: nc.vector.tensor_reduce(out=ss[:, 0:A], in_=tmp, axis=AX.X, op=ALU.add), reads=[tmpb], writes=[ssb])
    P.op("act", lambda: nc.scalar.activation(out=ss[:, A:2 * A], in_=ss[:, 0:A], func=AF.Ln, scale=1.0 / d, bias=kb.eps_t[:, 0:1]), reads=[ssb, kb.eps_b], writes=[ssb])
    P.op("act", lambda: nc.scalar.activation(out=ss[:, 0:A], in_=ss[:, A:2 * A], func=AF.Exp, scale=-0.5), reads=[ssb], writes=[ssb])
    r = ss[:, 0:A]
    rb_ = AP(tensor=r.tensor, offset=r.offset, ap=[list(r.ap[0]), list(r.ap[1]), [0, d]])
    g2 = g[:, 0:d]
    gb_ = AP(tensor=g2.tensor, offset=g2.offset, ap=[list(g2.ap[0]), [0, A], list(g2.ap[1])])
    P.op("dve", lambda: nc.vector.tensor_tensor(out=tmp, in0=x, in1=rb_, op=ALU.mult), reads=[xb, ssb], writes=[tmpb])
    P.op("dve", lambda: nc.vector.tensor_tensor(out=out, in0=tmp, in1=gb_, op=ALU.mult), reads=[tmpb, gb], writes=[outb])


def build_C(stage=99, nslots=NT, kb=None):
    kb = kb or K("C")
    nc, P = kb.nc, kb.P
    x_d = kb.din("x", [TOK, D])
    pr_d = kb.din("pr", [TOK, 712])
    ckv_d = kb.din("ckv_seq", [T, 128])
    kidx_d = kb.din("kidx_seq", [T, 64])
    id_d = kb.din("identb", [128, 128], BF16)
    an_d = kb.din("antib", [128, 128], BF16)
    relb_d = kb.din("rel_bias", [32, 12])
    oh_s_d = kb.din("oh_s", [33, Y_SLC])
    cm_d = kb.din("cm", [128, 512])
    pw_d = kb.din("pw", [128, NIT])
    qn_d = kb.din("q_norm", [1, 256]); kvn_d = kb.din("kv_norm", [1, 128]); kin_d = kb.din("kidx_norm", [1, 64])
    wqup_d = kb.din("w_q_up", [256, 768]); wuk_d = kb.din("w_uk", [128, 768]); wuv_d = kb.din("w_uv", [128, 768])
    wqi_d = kb.din("w_q_idx", [256, 512])
    mem_d = kb.din("mem", [256, D]); gmem_d = kb.din("g_mem", [1, D]); wkv_d = kb.din("w_mem_kv", [D, 512])
    wout_d = kb.din("w_out", [D, D])
    hmid_d = kb.dout("hmid", [TOK, D])

    P.dma("sp", kb.ident[:], id_d, writes=[kb.identb])
    P.dma("sp", kb.anti[:], an_d, writes=[kb.antib])
    kb.mk_eps()
    with kb.scope():
        tabs = build_tables(kb, relb_d, [oh_s_d], [("s", Y_SLC)])
    band_s, band_sb = load_band(kb, tabs[0][0], tabs[0][1], Y_SLC, 2048, 1, "band_s")
    ckvT, ckvTb = kb.sb("ckvT", [128, T], BF16)
    ckv1, ckv1b = kb.sb("ckv1", [128, 64, 129], BF16)
    kidxT, kidxTb = kb.sb("kidxT", [64, T], BF16)
    P.op("pool", lambda: nc.gpsimd.memset(ckv1[:], 1.0), writes=[ckv1b])
    gq, gqb = kb.bcast_row(qn_d, 256, "gq")
    with kb.scope():
        gkv, gkvb = kb.bcast_row(kvn_d, 128, "gkv")
        gki, gkib = kb.bcast_row(kin_d, 64, "gki")
        st, stb = kb.sb("kst", [128, 8, 128], F32)
        tmp, tmpb = kb.sb("ktmp", [128, 8, 128], F32)
        ss, ssb = kb.sb("kss", [128, 16], F32)
        kin, kinb = kb.sb("kin", [128, 8, 64], BF16)
        for i in range(8):
            P.dma(kb.dq(), st[:], ckv_d[i * 1024:(i + 1) * 1024, :].rearrange("(a p) c -> p a c", p=128), writes=[stb])
            rms_small(kb, st[:], stb, 8, 128, gkv, gkvb, ckv1[:, i * 8:(i + 1) * 8, 0:128], ckv1b, tmp[:], tmpb, ss, ssb)
            pt = kb.pt[kb.pti % 2]; ptb = kb.ptb[kb.pti % 2]; kb.pti += 1
            for j in range(8):
                P.op("pe", lambda j=j: nc.tensor.transpose(pt[:, j * 128:(j + 1) * 128], ckv1[:, i * 8 + j, 0:128], kb.ident[:]),
                     reads=[ckv1b, kb.identb], writes=[ptb], sig=(j == 7))
            P.op("act", lambda: nc.scalar.copy(out=ckvT[:, i * 1024:(i + 1) * 1024], in_=pt[:, :]), reads=[ptb], writes=[ckvTb])
        for i in range(8):
            P.dma(kb.dq(), st[:, :, 0:64], kidx_d[i * 1024:(i + 1) * 1024, :].rearrange("(a p) c -> p a c", p=128), writes=[stb])
            rms_small(kb, st[:, :, 0:64], stb, 8, 64, gki, gkib, kin[:], kinb, tmp[:, :, 0:64], tmpb, ss, ssb)
            pt = kb.pt[kb.pti % 2]; ptb = kb.ptb[kb.pti % 2]; kb.pti += 1
            for j in range(8):
                P.op("pe", lambda j=j: nc.tensor.transpose(pt[0:64, j * 128:(j + 1) * 128], kin[:, j, :], kb.ident[:]),
                     reads=[kinb, kb.identb], writes=[ptb], sig=(j == 7))
            P.op("act", lambda: nc.scalar.copy(out=kidxT[:, i * 1024:(i + 1) * 1024], in_=pt[0:64, :]), reads=[ptb], writes=[kidxTb])
    wqup, wqupb = kb.sb("wqup", [128, 2, 768], BF16)
    wqi, wqib = kb.sb("wqi", [128, 2, 512], BF16)
    wuv, wuvb = kb.sb("wuv", [128, 768], BF16)
    wukT, wukTb = kb.sb("wukT", [64, 12, 128], BF16)
    with kb.scope():
        wst = [kb.sb("wst4", [128, 768], F32) for _ in range(2)]
        wukb, wukbb = kb.sb("wukb", [128, 768], BF16)
        i = 0
        for (src, rows, ncol, dstf) in [(wqup_d, 0, 768, lambda: wqup[:, 0, :]), (wqup_d, 128, 768, lambda: wqup[:, 1, :]),
                                        (wqi_d, 0, 512, lambda: wqi[:, 0, :]), (wqi_d, 128, 512, lambda: wqi[:, 1, :]),
                                        (wuv_d, 0, 768, lambda: wuv[:]), (wuk_d, 0, 768, lambda: wukb[:])]:
            st, stb = wst[i % 2]; i += 1
            P.dma(kb.dq(), st[:, 0:ncol], src[rows:rows + 128, :], writes=[stb])
            dst = dstf()
            P.op("dve", lambda: nc.vector.tensor_copy(out=dst, in_=st[:, 0:ncol]), reads=[stb], writes=[wqupb, wqib, wuvb, wukbb])
        for h0 in (0, 8):
            nb = min(8, 12 - h0)
            pt = kb.pt[kb.pti % 2]; ptb = kb.ptb[kb.pti % 2]; kb.pti += 1
            for j in range(nb):
                P.op("pe", lambda j=j: nc.tensor.transpose(pt[0:64, j * 128:(j + 1) * 128], wukb[:, (h0 + j) * 64:(h0 + j + 1) * 64], kb.ident[:]),
                     reads=[wukbb, kb.identb], writes=[ptb], sig=(j == nb - 1))
            P.op("dve", lambda: nc.vector.tensor_copy(out=wukT[:, h0:h0 + nb, :], in_=pt[0:64, 0:nb * 128].rearrange("p (a b) -> p a b", b=128)), reads=[ptb], writes=[wukTb])
    kmT, kmTb, vm1, vm1b = mem_setup(kb, mem_d, gmem_d, wkv_d)
    wout, woutb = load_wout(kb, wout_d)
    cm, cmb = kb.sb("cm", [128, 512], F32)
    P.dma("sp", cm[:], cm_d, writes=[cmb])
    pw, pwb = kb.sb("pw", [128, NIT], F32)
    P.dma("sp", pw[:], pw_d, writes=[pwb])

    at = Attn(kb)
    score, scoreb = kb.sb("score", [128, T], F32)
    mb, mbb = kb.sb("mb", [128, T], BF16)
    mTs = [kb.sb("mT", [128, 8, 128], BF16) for _ in range(2)]
    big, bigb = kb.sb("big", [128, D], F32)
    cqn, cqnb = kb.sb("cqn", [128, 256], BF16)
    cqT, cqTb = kb.sb("cqT", [128, 2, 128], BF16)
    qhT, qhTb = kb.sb("qhT", [128, 12, 128], BF16)
    qabs, qabsb = kb.sb("qabs", [128, 12, 128], BF16)
    qiT, qiTb = kb.sb("qiT", [64, 8, 128], BF16)
    qmb16, qmb16b = kb.sb("qmb16", [128, 256], BF16)
    qmT, qmTb = kb.sb("qmT", [64, 4, 128], BF16)
    rts = [kb.sb("rt", [128, 512], F32) for _ in range(2)]
    catbf, catbfb = kb.sb("catbf", [128, D], BF16)
    catT, catTb = kb.sb("catT", [128, 8, 128], BF16)
    pm, pmb = kb.sb("pm", [128, 256], BF16)
    sm, smb = kb.sb("sm", [128, 16], F32)
    wv, wvb = kb.sb("wv", [128, 32], F32)
    bs, bsb = kb.sb("bs", [128, 8 + 2 * NIT], F32)
    accbank = 2

    def next_acc():
        nonlocal accbank
        b = accbank
        accbank = 2 + (accbank - 2 + 1) % 4
        return b

    for m in range(nslots):
        tsl = slice(m * 128, (m + 1) * 128)
        L = 128 * (4 * m + 4)
        nkt = 4 * m + 4
        P.dma("sp", big[:, 0:712], pr_d[tsl, :], writes=[bigb])
        if stage == 0:
            P.dma("sp", hmid_d[tsl, :], big[:], reads=[bigb], final=True)
            continue
        ssq = wv[:, 16:18]
        P.op("act", lambda: nc.scalar.activation(out=cqn[:], in_=big[:, 0:256], func=AF.Square, accum_out=wv[:, 16:17]), reads=[bigb], writes=[cqnb, wvb])
        P.op("act", lambda: nc.scalar.activation(out=wv[:, 17:18], in_=wv[:, 16:17], func=AF.Ln, scale=1.0 / 256, bias=kb.eps_t[:, 0:1]), reads=[wvb, kb.eps_b], writes=[wvb])
        P.op("act", lambda: nc.scalar.activation(out=wv[:, 18:19], in_=wv[:, 17:18], func=AF.Exp, scale=-0.5), reads=[wvb], writes=[wvb])
        P.op("dve", lambda: nc.vector.scalar_tensor_tensor(out=cqn[:], in0=big[:, 0:256], scalar=wv[:, 18:19], in1=gq[:], op0=ALU.mult, op1=ALU.mult), reads=[bigb, wvb, gqb], writes=[cqnb])
        pt = kb.pt[kb.pti % 2]; ptb = kb.ptb[kb.pti % 2]; kb.pti += 1
        for j in range(2):
            P.op("pe", lambda j=j: nc.tensor.transpose(pt[:, j * 128:(j + 1) * 128], cqn[:, j * 128:(j + 1) * 128], kb.ident[:]), reads=[cqnb, kb.identb], writes=[ptb], sig=(j == 1))
        P.op("dve", lambda: nc.vector.tensor_copy(out=cqT[:], in_=pt[:, 0:256].rearrange("p (a b) -> p a b", b=128)), reads=[ptb], writes=[cqTb])
        for b4 in range(3):
            ps, psb = kb.ps[b4 % 2], kb.psb[b4 % 2]
            for hh in range(4):
                h = 4 * b4 + hh
                for c in range(2):
                    mm(kb, ps[0:64, hh * 128:(hh + 1) * 128], wqup[:, c, h * 64:(h + 1) * 64], cqT[:, c, :], hh == 0 and c == 0, [wqupb, cqTb], [psb], sig=(hh == 3 and c == 1))
            P.op("act", lambda: nc.scalar.activation(out=qhT[0:64, 4 * b4:4 * b4 + 4, :], in_=ps[0:64, :].rearrange("p (a b) -> p a b", b=128), func=AF.Copy, scale=0.125), reads=[psb], writes=[qhTb])
        for b4 in range(2):
            ps, psb = kb.ps[b4 % 2], kb.psb[b4 % 2]
            for hh in range(4):
                h = 4 * b4 + hh
                for c in range(2):
                    mm(kb, ps[0:64, hh * 128:(hh + 1) * 128], wqi[:, c, h * 64:(h + 1) * 64], cqT[:, c, :], hh == 0 and c == 0, [wqib, cqTb], [psb], sig=(hh == 3 and c == 1))
            P.op("dve", lambda: nc.vector.tensor_copy(out=qiT[:, 4 * b4:4 * b4 + 4, :], in_=ps[0:64, :].rearrange("p (a b) -> p a b", b=128)), reads=[psb], writes=[qiTb])
        for b4 in range(3):
            ps, psb = kb.ps[b4 % 2], kb.psb[b4 % 2]
            for hh in range(4):
                h = 4 * b4 + hh
                mm(kb, ps[:, hh * 128:(hh + 1) * 128], wukT[:, h, :], qhT[0:64, h, :], hh == 0, [wukTb, qhTb], [psb], sig=(hh == 3))
            P.op("dve", lambda: nc.vector.tensor_copy(out=qabs[:, 4 * b4:4 * b4 + 4, :], in_=ps[:, :].rearrange("p (a b) -> p a b", b=128)), reads=[psb], writes=[qabsb])
        P.op("dve", lambda: nc.vector.tensor_scalar(out=wv[:, 0:8], in0=big[:, 448:456], scalar1=0.04419417382415922, scalar2=None, op0=ALU.mult), reads=[bigb], writes=[wvb])
        P.op("dve", lambda: nc.vector.tensor_scalar(out=wv[:, 8:16], in0=wv[:, 0:8], scalar1=0.0, scalar2=2.0, op0=ALU.is_ge, op1=ALU.mult), reads=[wvb], writes=[wvb])
        P.op("dve", lambda: nc.vector.tensor_scalar(out=wv[:, 8:16], in0=wv[:, 8:16], scalar1=-1.0, scalar2=None, op0=ALU.add), reads=[wvb], writes=[wvb])
        P.op("dve", lambda: nc.vector.tensor_tensor(out=wv[:, 0:8], in0=wv[:, 0:8], in1=wv[:, 8:16], op=ALU.mult), reads=[wvb], writes=[wvb])
        P.op("act", lambda: nc.scalar.activation(out=qmb16[:], in_=big[:, 456:712], func=AF.Copy, scale=0.125), reads=[bigb], writes=[qmb16b])
        pt = kb.pt[kb.pti % 2]; ptb = kb.ptb[kb.pti % 2]; kb.pti += 1
        for j in range(4):
            P.op("pe", lambda j=j: nc.tensor.transpose(pt[0:64, j * 128:(j + 1) * 128], qmb16[:, j * 64:(j + 1) * 64], kb.ident[:]), reads=[qmb16b, kb.identb], writes=[ptb], sig=(j == 3))
        P.op("dve", lambda: nc.vector.tensor_copy(out=qmT[:], in_=pt[0:64, 0:512].rearrange("p (a b) -> p a b", b=128)), reads=[ptb], writes=[qmTb])
        P.dma("pool", big[:], x_d[tsl, :], reads=[], writes=[bigb])
        if stage == 1:
            P.dma("sp", hmid_d[tsl, :], big[:], reads=[bigb], final=True)
            continue
        for kc in range(m + 1):
            csl = slice(kc * 512, (kc + 1) * 512)
            for h in range(8):
                ps, psb = kb.ps[h % 2], kb.psb[h % 2]
                rp, rpb = kb.ps[2 + h % 2], kb.psb[2 + h % 2]
                mm(kb, ps[:, :], qiT[:, h, :], kidxT[:, csl], True, [qiTb, kidxTb], [psb], sig=True)
                P.op("act", lambda: nc.scalar.activation(out=rp[:, :], in_=ps[:, :], func=AF.Relu, scale=wv[:, h:h + 1]), reads=[psb, wvb], writes=[rpb])
                if h == 0:
                    P.op("dve", lambda: nc.vector.tensor_scalar(out=score[:, csl], in0=rp[:, :], scalar1=wv[:, 8:9], scalar2=None, op0=ALU.mult), reads=[rpb, wvb], writes=[scoreb])
                else:
                    P.op("dve", lambda: nc.vector.scalar_tensor_tensor(out=score[:, csl], in0=rp[:, :], scalar=wv[:, 8 + h:9 + h], in1=score[:, csl], op0=ALU.mult, op1=ALU.add), reads=[rpb, wvb, scoreb], writes=[scoreb])
        if stage == 2:
            P.dma("sp", hmid_d[tsl, 0:512], score[:, 0:512], reads=[scoreb], final=True)
            continue
        P.op("dve", lambda: nc.vector.tensor_reduce(out=bs[:, 0:1], in_=score[:, 0:L], axis=AX.X, op=ALU.min), reads=[scoreb], writes=[bsb])
        P.op("dve", lambda: nc.vector.tensor_reduce(out=bs[:, 1:2], in_=score[:, 0:L], axis=AX.X, op=ALU.max), reads=[scoreb], writes=[bsb])
        P.op("dve", lambda: nc.vector.tensor_tensor(out=score[:, L - 512:L], in0=score[:, L - 512:L], in1=cm[:], op=ALU.add), reads=[scoreb, cmb], writes=[scoreb])
        P.op("dve", lambda: nc.vector.tensor_tensor(out=bs[:, 2:3], in0=bs[:, 1:2], in1=bs[:, 0:1], op=ALU.subtract), reads=[bsb], writes=[bsb])
        P.op("dve", lambda: nc.vector.tensor_scalar(out=bs[:, 8:8 + NIT], in0=pw[:], scalar1=bs[:, 2:3], scalar2=None, op0=ALU.mult), reads=[bsb, pwb], writes=[bsb])
        for it in range(NIT):
            hw = bs[:, 8 + it:9 + it]
            P.op("dve", lambda: nc.vector.tensor_tensor(out=bs[:, 3:4], in0=bs[:, 0:1], in1=hw, op=ALU.add), reads=[bsb], writes=[bsb])
            P.op("dve", lambda: nc.vector.tensor_scalar(out=mb[:, 0:L], in0=score[:, 0:L], scalar1=bs[:, 3:4], scalar2=None, op0=ALU.is_ge, op1=ALU.add, accum_out=bs[:, 4:5]),
                 reads=[scoreb, bsb], writes=[mbb, bsb])
            P.op("dve", lambda: nc.vector.tensor_scalar(out=bs[:, 5:6], in0=bs[:, 4:5], scalar1=255.5, scalar2=hw, op0=ALU.is_ge, op1=ALU.mult), reads=[bsb], writes=[bsb])
            P.op("dve", lambda: nc.vector.tensor_tensor(out=bs[:, 0:1], in0=bs[:, 0:1], in1=bs[:, 5:6], op=ALU.add), reads=[bsb], writes=[bsb])
        P.op("dve", lambda: nc.vector.tensor_scalar(out=mb[:, 0:L], in0=score[:, 0:L], scalar1=bs[:, 0:1], scalar2=NEGB, op0=ALU.is_lt, op1=ALU.mult), reads=[scoreb, bsb], writes=[mbb])
        if stage == 3:
            P.dma("sp", hmid_d[tsl, 0:512], score[:, 0:512], reads=[scoreb], final=True)
            P.dma("sp", hmid_d[tsl, 512:512 + 8 + 2 * NIT], bs[:], reads=[bsb], final=True)
            continue
        banks = [next_acc() for _ in range(4)]
        for g8 in range(0, nkt, 8):
            nb = min(8, nkt - g8)
            mT, mTb = mTs[(g8 // 8) % 2]
            pt = kb.pt[kb.pti % 2]; ptb = kb.ptb[kb.pti % 2]; kb.pti += 1
            for j in range(nb):
                P.op("pe", lambda j=j: nc.tensor.transpose(pt[:, j * 128:(j + 1) * 128], mb[:, (g8 + j) * 128:(g8 + j + 1) * 128], kb.ident[:]), reads=[mbb, kb.identb], writes=[ptb], sig=(j == nb - 1))
            P.op("dve", lambda: nc.vector.tensor_copy(out=mT[:, 0:nb, :], in_=pt[:, 0:nb * 128].rearrange("p (a b) -> p a b", b=128)), reads=[ptb], writes=[mTb])
            for j in range(nb):
                kt = g8 + j
                u = kt - 4 * m
                for tr in range(4):
                    bA = banks[tr]
                    accs = [kb.ps[bA][:, 129 * hh:129 * hh + 129] for hh in range(3)]
                    accb = [kb.psb[bA]] * 3
                    s_terms = [(ckvT[:, kt * 128:(kt + 1) * 128], qabs[:, 3 * tr:3 * tr + 3, :], [ckvTb, qabsb]),
                               (kb.ident[:], bc3(mT[:, j, :]), [kb.identb, mTb])]
                    near = None
                    if u >= -12:
                        z0 = 128 * (3 - u)
                        near = (kb.anti[:], band_s[:, 3 * tr:3 * tr + 3, z0:z0 + 128], [kb.antib, band_sb])
                    at.unit(s_terms, near, accs, accb, ckv1[:, kt, :], [ckv1b], [kt == 0, False, False])
        at.flush()
        for tr in range(4):
            bA = banks[tr]
            for hh in range(3):
                h = 3 * tr + hh
                O = kb.ps[bA][:, 129 * hh:129 * hh + 129]
                P.op("dve", lambda: nc.vector.tensor_scalar(out=sm[:, 0:1], in0=O[:, 128:129], scalar1=1e-30, scalar2=None, op0=ALU.max), reads=[kb.psb[bA]], writes=[smb])
                P.op("dve", lambda: nc.vector.reciprocal(out=sm[:, 1:2], in_=sm[:, 0:1]), reads=[smb], writes=[smb])
                P.op("dve", lambda: nc.vector.tensor_scalar(out=qabs[:, h, :], in0=O[:, 0:128], scalar1=sm[:, 1:2], scalar2=None, op0=ALU.mult), reads=[kb.psb[bA], smb], writes=[qabsb])
        for h0 in (0, 8):
            nb = min(8, 12 - h0)
            pt = kb.pt[kb.pti % 2]; ptb = kb.ptb[kb.pti % 2]; kb.pti += 1
            for j in range(nb):
                P.op("pe", lambda j=j: nc.tensor.transpose(pt[:, j * 128:(j + 1) * 128], qabs[:, h0 + j, :], kb.ident[:]), reads=[qabsb, kb.identb], writes=[ptb], sig=(j == nb - 1))
            P.op("dve", lambda: nc.vector.tensor_copy(out=qhT[:, h0:h0 + nb, :], in_=pt[:, 0:nb * 128].rearrange("p (a b) -> p a b", b=128)), reads=[ptb], writes=[qhTb])
        for h0 in (0, 8):
            nb = min(8, 12 - h0)
            bA = next_acc()
            ps, psb = kb.ps[bA], kb.psb[bA]
            for j in range(nb):
                h = h0 + j
                mm(kb, ps[:, j * 64:(j + 1) * 64], qhT[:, h, :], wuv[:, h * 64:(h + 1) * 64], j == 0, [qhTb, wuvb], [psb], sig=(j == nb - 1))
            P.op("act", lambda: nc.scalar.copy(out=catbf[:, h0 * 64:(h0 + nb) * 64], in_=ps[:, 0:nb * 64]), reads=[psb], writes=[catbfb])
        for h in range(4):
            sbk = at.next_sbank()
            ps, psb = kb.ps[sbk], kb.psb[sbk]
            for tl in range(2):
                mm(kb, ps[:, tl * 128:(tl + 1) * 128], kmT[:, h, tl * 128:(tl + 1) * 128], qmT[:, h, :], tl == 0, [kmTb, qmTb], [psb], sig=(tl == 1))
            P.op("act", lambda: nc.scalar.activation(out=pm[:], in_=ps[:, 0:256], func=AF.Exp), reads=[psb], writes=[pmb])
            bA = next_acc()
            O = kb.ps[bA][:, 0:65]
            for tl in range(2):
                mm(kb, O, pm[:, tl * 128:(tl + 1) * 128], vm1[:, tl, h, :], tl == 0, [pmb, vm1b], [kb.psb[bA]], sig=(tl == 1))
            P.op("dve", lambda: nc.vector.reciprocal(out=sm[:, 4:5], in_=O[:, 64:65]), reads=[kb.psb[bA]], writes=[smb])
            P.op("dve", lambda: nc.vector.tensor_scalar(out=catbf[:, 768 + h * 64:768 + (h + 1) * 64], in0=O[:, 0:64], scalar1=sm[:, 4:5], scalar2=None, op0=ALU.mult), reads=[kb.psb[bA], smb], writes=[catbfb])
        pt = kb.pt[kb.pti % 2]; ptb = kb.ptb[kb.pti % 2]; kb.pti += 1
        for j in range(8):
            P.op("pe", lambda j=j: nc.tensor.transpose(pt[:, j * 128:(j + 1) * 128], catbf[:, j * 128:(j + 1) * 128], kb.ident[:]),
                 reads=[catbfb, kb.identb], writes=[ptb], sig=(j == 7))
        P.op("dve", lambda: nc.vector.tensor_copy(out=catT[:], in_=pt[:, :].rearrange("p (a b) -> p a b", b=128)), reads=[ptb], writes=[catTb])
        for half in range(2):
            bA = next_acc()
            ps, psb = kb.ps[bA], kb.psb[bA]
            for k in range(8):
                mm(kb, ps[:, :], catT[:, k, :], wout[:, k, half * 512:(half + 1) * 512], k == 0, [catTb, woutb], [psb], sig=(k == 7))
            P.op("dve", lambda: nc.vector.tensor_tensor(out=big[:, half * 512:(half + 1) * 512], in0=ps[:, :], in1=big[:, half * 512:(half + 1) * 512], op=ALU.add), reads=[psb, bigb], writes=[bigb])
        P.dma("sp", hmid_d[tsl, :], big[:], reads=[bigb], final=True)
    return kb


def run_C(inputs, resF, stage=99, nslots=NT):
    kb = build_C(stage, nslots)
    ident = np.eye(128, dtype=np.float32)
    pw = np.tile((0.5 ** np.arange(1, NIT + 1)).astype(np.float32)[None, :], (128, 1))
    maps = []
    for core in range(8):
        b, c = core // 4, core % 4
        prs = [np.asarray(resF[4 * b + cc]["pr"]) for cc in range(4)]
        oh_s, _, _ = host_tables(c)
        z = np.arange(512)[None, :]
        q = np.arange(128)[:, None]
        cm = np.where(z <= 128 * c + q, 0.0, -1e30).astype(np.float32)
        mp = {"x": np.asarray(resF[core]["h"]), "pr": prs[c],
              "ckv_seq": gather_seq([np.ascontiguousarray(p[:, 256:384]) for p in prs], 0),
              "kidx_seq": gather_seq([np.ascontiguousarray(p[:, 384:448]) for p in prs], 0),
              "identb": ident.astype(NPBF), "antib": np.ascontiguousarray(ident[::-1]).astype(NPBF),
              "rel_bias": inputs["rel_bias"], "oh_s": oh_s, "cm": cm, "pw": pw,
              "q_norm": inputs["dsa_q_norm"][0:1], "kv_norm": inputs["dsa_kv_norm"][0:1], "kidx_norm": inputs["dsa_kidx_norm"][0:1],
              "w_q_up": inputs["dsa_w_q_up"][0], "w_uk": inputs["dsa_w_uk"][0].reshape(128, 768), "w_uv": inputs["dsa_w_uv"][0].reshape(128, 768),
              "w_q_idx": inputs["dsa_w_q_idx"][0],
              "mem": inputs["mem"][b], "g_mem": inputs["norm_mem"][1:2], "w_mem_kv": inputs["w_mem_kv"][1], "w_out": inputs["w_out"][1]}
        maps.append(mp)
    return kb.run(maps)


GROUPS = [[0, 1, 2, 3], [4, 5, 6, 7]]


def all_gather(kb, in_ap2d, out_ap2d):
    nc, P = kb.nc, kb.P
    P.barrier()
    kb.n += 1
    key = "cc%d" % kb.n
    sem = P.es.enter_context(nc.semaphore(key))
    P.sem[key] = sem
    P.cnt[key] = 0
    with nc.Block() as block:
        @block.gpsimd
        def _(g):
            g.collective_compute("AllGather", ALU.bypass, replica_groups=GROUPS, ins=[in_ap2d.opt()], outs=[out_ap2d.opt()]).then_inc(sem)
            g.wait_ge(sem, 1)
    P.cnt[key] = 1
    P.seen["pool"][key] = 1
    for e in P.eng:
        P._need(e, (key, 1))


def seq_rows(all_t, ncols_total, col0, ncol, m0, nm):
    return AP(tensor=all_t, offset=m0 * 128 * ncols_total + col0,
              ap=[[128 * ncols_total, nm], [2048 * ncols_total, 4], [ncols_total, 128], [1, ncol]])


def build_fused():
    kb = K("fused")
    nc, P = kb.nc, kb.P
    ext = {}

    def E(name, shape, dt=F32):
        ext[name] = nc.dram_tensor(name, list(shape), dt, kind="ExternalInput").ap()
        return ext[name]

    def S(name, shape, dt=F32):
        return nc.dram_tensor(name, list(shape), dt)

    x = E("x", [TOK, D])
    out = nc.dram_tensor("out", [TOK, D], F32, kind="ExternalOutput").ap()
    nm = {k: E(k, [1, D]) for k in ("norm_mix0", "norm_mix1", "norm_ffn0", "norm_ffn1", "norm_mem0", "norm_mem1", "norm_final")}
    nsa_w_in = E("nsa_w_in", [D, 1828]); gate_b = E("gate_b", [1, 36])
    ident = E("ident", [128, 128]); anti = E("anti", [128, 128])
    identb = E("identb", [128, 128], BF16); antib = E("antib", [128, 128], BF16)
    rel_bias = E("rel_bias", [32, 12])
    oh_s = E("oh_s", [33, Y_SLC]); oh_w = E("oh_w", [33, Y_WIN]); oh_c = E("oh_c", [33, Y_CMP])
    ew = E("ew", [128, T], BF16); ovl = E("ovl", [128, 4, 128], BF16); fb = E("fb", [NT, 128, 128])
    cmp_w = {k: E(k, shp) for k, shp in [("w1k", [2048, 256]), ("b1k", [256, 1]), ("w2k", [256, 64]), ("b2k", [64, 1]),
                                         ("w1v", [2048, 256]), ("b1v", [256, 1]), ("w2v", [256, 64]), ("b2v", [1, 64]),
                                         ("posk", [32, 64]), ("posv", [32, 64])]}
    mem = E("mem", [256, D])
    wkv = [E("w_mem_kv%d" % i, [D, 512]) for i in range(2)]
    wout = [E("w_out%d" % i, [D, D]) for i in range(2)]
    wg = [E("wg%d" % i, [D, DFF]) for i in range(2)]
    wu = [E("wu%d" % i, [D, DFF]) for i in range(2)]
    wd = [E("wd%d" % i, [DFF, D]) for i in range(2)]
    dsa_w_in = E("dsa_w_in", [D, 712])
    cm = E("cm", [128, 512]); pw = E("pw", [128, NIT])
    q_norm = E("q_norm", [1, 256]); kv_norm = E("kv_norm", [1, 128]); kidx_norm = E("kidx_norm", [1, 64])
    w_q_up = E("w_q_up", [256, 768]); w_uk = E("w_uk", [128, 768]); w_uv = E("w_uv", [128, 768]); w_q_idx = E("w_q_idx", [256, 512])

    fm_loc = S("fm_loc", [64, 24, TOK], BF16)
    tm_loc = S("tm_loc", [TOK, 292])
    kfm_loc = S("kfm_loc", [8, 64, TOK], BF16)
    kfm_all = S("kfm_all", [4, 4 * 128, TOK], BF16)
    vt_loc = S("vt_loc", [TOK, 256])
    vt_all = S("vt_all", [4, 4 * 512, 256])
    qT2_s = S("qT2_s", [128, 6, TOK], BF16)
    kseq = {k: S(k + "_s", [128, T], BF16) for k in ("kcrT2", "vcrT2", "ksT2", "kwT2")}
    vs_s = S("vs_s", [T, 128]); vw_s = S("vw_s", [T, 128])
    hmid0 = S("hmid0", [TOK, D]); h0 = S("h0", [TOK, D]); hmid1 = S("hmid1", [TOK, D])
    pr_loc = S("pr_loc", [TOK, 712])
    kv_loc = S("kv_loc", [TOK, 192]); kv_all = S("kv_all", [4, 4 * 512, 192])
    ckv_s = S("ckv_s", [T, 128]); kidx_s = S("kidx_s", [T, 64])

    kb.alias = {"x": x, "g": nm["norm_mix0"], "w_in": nsa_w_in, "gate_b": gate_b, "ident": ident, "anti": anti,
                "fmT": fm_loc.ap(), "tm": tm_loc.ap()}
    with kb.scope():
        build_A(kb)
    P.barrier()
    P.dma("sp", kfm_loc.ap(), AP(tensor=fm_loc, offset=12 * TOK, ap=[[TOK, 8], [24 * TOK, 64], [1, TOK]]))
    P.dma("pool", vt_loc.ap(), tm_loc.ap()[:, 0:256])
    for ch in range(4):
        all_gather(kb, kfm_loc.ap()[2 * ch:2 * ch + 2].rearrange("a d t -> (a d) t"), kfm_all.ap()[ch])
    for r4 in range(4):
        all_gather(kb, vt_loc.ap()[r4 * 512:(r4 + 1) * 512, :], vt_all.ap()[r4])
    for g in range(2):
        P.dma(kb.dq(), qT2_s.ap()[g * 64:(g + 1) * 64, :, :], fm_loc.ap()[:, 6 * g:6 * g + 6, :])
    for ch, k in enumerate(("kcrT2", "vcrT2", "ksT2", "kwT2")):
        for g in range(2):
            for cc in range(4):
                dst = AP(tensor=kseq[k], offset=(g * 64) * T + cc * 128, ap=[[T, 64], [512, 16], [1, 128]])
                src = AP(tensor=kfm_all, offset=((ch * 4 + cc) * 128 + g * 64) * TOK, ap=[[TOK, 64], [128, 16], [1, 128]])
                P.dma(kb.dq(), dst, src)
    for (dst_t, c0) in ((vs_s, 0), (vw_s, 128)):
        for cc in range(4):
            for r4 in range(4):
                dst = AP(tensor=dst_t, offset=(r4 * 16 * 128 + cc * 128) * 128, ap=[[512 * 128, 4], [128, 128], [1, 128]])
                src = AP(tensor=vt_all, offset=((r4 * 4 + cc) * 512) * 256 + c0, ap=[[128 * 256, 4], [256, 128], [1, 128]])
                P.dma(kb.dq(), dst, src)
    P.barrier()
    kb.alias = dict(cmp_w)
    kb.alias.update({"x": x, "identb": identb, "antib": antib, "rel_bias": rel_bias, "oh_s": oh_s, "oh_w": oh_w, "oh_c": oh_c,
                     "qT2": qT2_s.ap(), "qmT": fm_loc.ap()[:, 20:24, :], "gates": tm_loc.ap()[:, 256:292],
                     "ksT2": kseq["ksT2"].ap(), "kwT2": kseq["kwT2"].ap(), "kcrT2": kseq["kcrT2"].ap(), "vcrT2": kseq["vcrT2"].ap(),
                     "vs": vs_s.ap(), "vw": vw_s.ap(), "ew": ew, "ovl": ovl, "fb": fb,
                     "mem": mem, "g_mem": nm["norm_mem0"], "w_mem_kv": wkv[0], "w_out": wout[0], "hmid": hmid0.ap()})
    with kb.scope():
        build_B(0, kb)
    P.barrier()
    kb.alias = {"hmid": hmid0.ap(), "identb": identb, "g_ffn": nm["norm_ffn0"], "wg": wg[0], "wu": wu[0], "wd": wd[0],
                "g2": nm["norm_mix1"], "h": h0.ap(), "w_in2": dsa_w_in, "pr": pr_loc.ap()}
    with kb.scope():
        build_F(False, 712, kb)
    P.barrier()
    P.dma("sp", kv_loc.ap(), pr_loc.ap()[:, 256:448])
    for r4 in range(4):
        all_gather(kb, kv_loc.ap()[r4 * 512:(r4 + 1) * 512, :], kv_all.ap()[r4])
    for (dst_t, c0, nc_) in ((ckv_s, 0, 128), (kidx_s, 128, 64)):
        for cc in range(4):
            for r4 in range(4):
                dst = AP(tensor=dst_t, offset=(r4 * 16 * 128 + cc * 128) * nc_, ap=[[512 * nc_, 4], [nc_, 128], [1, nc_]])
                src = AP(tensor=kv_all, offset=((r4 * 4 + cc) * 512) * 192 + c0, ap=[[128 * 192, 4], [192, 128], [1, nc_]])
                P.dma(kb.dq(), dst, src)
    P.barrier()
    kb.alias = {"x": h0.ap(), "pr": pr_loc.ap(), "ckv_seq": ckv_s.ap(), "kidx_seq": kidx_s.ap(), "identb": identb, "antib": antib,
                "rel_bias": rel_bias, "oh_s": oh_s, "cm": cm, "pw": pw, "q_norm": q_norm, "kv_norm": kv_norm, "kidx_norm": kidx_norm,
                "w_q_up": w_q_up, "w_uk": w_uk, "w_uv": w_uv, "w_q_idx": w_q_idx, "mem": mem, "g_mem": nm["norm_mem1"],
                "w_mem_kv": wkv[1], "w_out": wout[1], "hmid": hmid1.ap()}
    with kb.scope():
        build_C(99, NT, kb)
    P.barrier()
    kb.alias = {"hmid": hmid1.ap(), "identb": identb, "g_ffn": nm["norm_ffn1"], "wg": wg[1], "wu": wu[1], "wd": wd[1],
                "g2": nm["norm_final"], "h": out}
    with kb.scope():
        build_F(True, 712, kb)
    kb.ext = ext
    return kb


def fused_maps(inputs):
    ident = np.eye(128, dtype=np.float32)
    anti = np.ascontiguousarray(ident[::-1])
    pw = np.tile((0.5 ** np.arange(1, NIT + 1)).astype(np.float32)[None, :], (128, 1))
    maps = []
    for core in range(8):
        b, c = core // 4, core % 4
        mp = consts_B(c)
        z = np.arange(512)[None, :]
        q = np.arange(128)[:, None]
        mp.update({
            "x": np.ascontiguousarray(inputs["x"][b][core_tokens(c)]),
            "norm_mix0": inputs["norm_mix"][0:1], "norm_mix1": inputs["norm_mix"][1:2],
            "norm_ffn0": inputs["norm_ffn"][0:1], "norm_ffn1": inputs["norm_ffn"][1:2],
            "norm_mem0": inputs["norm_mem"][0:1], "norm_mem1": inputs["norm_mem"][1:2],
            "norm_final": inputs["norm_final"].reshape(1, D),
            "nsa_w_in": inputs["nsa_w_in"][0], "gate_b": inputs["nsa_gate_b"][0:1],
            "ident": ident, "anti": anti, "rel_bias": inputs["rel_bias"],
            "w1k": inputs["nsa_cmp_k_w1"][0], "b1k": inputs["nsa_cmp_k_b1"][0].reshape(256, 1),
            "w2k": inputs["nsa_cmp_k_w2"][0], "b2k": inputs["nsa_cmp_k_b2"][0].reshape(64, 1),
            "w1v": inputs["nsa_cmp_v_w1"][0], "b1v": inputs["nsa_cmp_v_b1"][0].reshape(256, 1),
            "w2v": inputs["nsa_cmp_v_w2"][0], "b2v": inputs["nsa_cmp_v_b2"][0].reshape(1, 64),
            "posk": inputs["nsa_cmp_pos_k"][0], "posv": inputs["nsa_cmp_pos_v"][0],
            "mem": inputs["mem"][b],
            "w_mem_kv0": inputs["w_mem_kv"][0], "w_mem_kv1": inputs["w_mem_kv"][1],
            "w_out0": inputs["w_out"][0], "w_out1": inputs["w_out"][1],
            "wg0": inputs["ffn_gate"][0], "wg1": inputs["ffn_gate"][1],
            "wu0": inputs["ffn_up"][0], "wu1": inputs["ffn_up"][1],
            "wd0": inputs["ffn_down"][0], "wd1": inputs["ffn_down"][1],
            "dsa_w_in": inputs["dsa_w_in"][0],
            "cm": np.where(z <= 128 * c + q, 0.0, -1e30).astype(np.float32), "pw": pw,
            "q_norm": inputs["dsa_q_norm"][0:1], "kv_norm": inputs["dsa_kv_norm"][0:1], "kidx_norm": inputs["dsa_kidx_norm"][0:1],
            "w_q_up": inputs["dsa_w_q_up"][0], "w_uk": inputs["dsa_w_uk"][0].reshape(128, 768),
            "w_uv": inputs["dsa_w_uv"][0].reshape(128, 768), "w_q_idx": inputs["dsa_w_q_idx"][0],
        })
        maps.append(mp)
    return maps


def kernel(**inputs):
    inputs = {k: np.asarray(v) for k, v in inputs.items()}
    kb = build_fused()
    res = kb.run(fused_maps(inputs))
    return scatter_tokens(res, "out")
```

```python
import math
from contextlib import ExitStack
import numpy as np
import ml_dtypes
import concourse.bass as bass
import concourse.mybir as mybir
from concourse.bass import AP
from concourse.bass_utils import run_bass_kernel_spmd

F32 = mybir.dt.float32
BF16 = mybir.dt.bfloat16
AF = mybir.ActivationFunctionType
ALU = mybir.AluOpType
AX = mybir.AxisListType
NPBF = ml_dtypes.bfloat16

D = 1024
T = 8192
NT = 16
TOK = 2048
DFF = 2816
EPS = 1e-6
NEGB = -30000.0


class Buf:
    __slots__ = ("name", "w", "r")

    def __init__(self, name=""):
        self.name = name
        self.w = None
        self.r = {}


class Prog:
    NSLOT = 12

    def __init__(self, nc):
        self.nc = nc
        self.eng = {"pe": nc.tensor, "act": nc.scalar, "dve": nc.vector,
                    "pool": nc.gpsimd, "sp": nc.sync}
        self.es = ExitStack()
        self.sem = {}
        self.cnt = {}
        for e in ("pe", "act", "dve", "pool"):
            self.sem[e] = self.es.enter_context(nc.semaphore("c_" + e))
            self.cnt[e] = 0
        self.slots = {}
        self.slot_i = {}
        for q in ("sp", "pool"):
            lst = []
            for i in range(self.NSLOT):
                key = "d_%s%d" % (q, i)
                self.sem[key] = self.es.enter_context(nc.semaphore(key))
                self.cnt[key] = 0
                lst.append(key)
            self.slots[q] = lst
            self.slot_i[q] = 0
        self.seen = {e: {} for e in self.eng}
        self.outtoks = []

    def _need(self, e, tok):
        if tok is None:
            return
        key, val = tok
        if self.seen[e].get(key, 0) >= val:
            return
        if key in ("pe", "act", "dve", "pool"):
            assert self.cnt[key] >= val, "missing signal on %s" % key
        self.eng[e].wait_ge(self.sem[key], val)
        self.seen[e][key] = val

    def _deps(self, e, reads, writes):
        for b in reads:
            if b.w is not None and not (e == "pe" and b.w[0] == "pe"):
                self._need(e, b.w)
        for b in writes:
            if b.w is not None and b.w[0] != e:
                self._need(e, b.w)
            for k, v in b.r.items():
                if k != e:
                    self._need(e, (k, v))

    def _mark(self, tok, reads, writes):
        for b in reads:
            if b.r.get(tok[0], 0) < tok[1]:
                b.r[tok[0]] = tok[1]
        for b in writes:
            b.w = tok
            b.r = {}

    def op(self, e, fn, reads=(), writes=(), sig=True):
        self._deps(e, reads, writes)
        ins = fn()
        if sig:
            self.cnt[e] += 1
            ins.then_inc(self.sem[e], 1)
            tok = (e, self.cnt[e])
        else:
            tok = (e, self.cnt[e] + 1)
        self._mark(tok, reads, writes)
        return ins

    def dma(self, q, out, in_, reads=(), writes=(), final=False):
        lst = self.slots[q]
        key = lst[self.slot_i[q] % self.NSLOT]
        self.slot_i[q] += 1
        if self.cnt[key] > 0:
            self._need(q, (key, self.cnt[key]))
        self._deps(q, reads, writes)
        ins = self.eng[q].dma_start(out=out, in_=in_)
        self.cnt[key] += 16
        ins.then_inc(self.sem[key], 16)
        tok = (key, self.cnt[key])
        self._mark(tok, reads, writes)
        if final:
            self.outtoks.append(tok)
        return tok

    def barrier(self):
        toks = [(e, self.cnt[e]) for e in ("pe", "act", "dve", "pool") if self.cnt[e] > 0]
        for q in ("sp", "pool"):
            toks += [(k, self.cnt[k]) for k in self.slots[q] if self.cnt[k] > 0]
        for e in self.eng:
            for t in toks:
                if t[0] != e:
                    self._need(e, t)

    def finish(self):
        for tok in self.outtoks:
            self._need("sp", tok)
        for q in ("sp", "pool"):
            for key in self.slots[q]:
                if self.cnt[key] > 0:
                    self._need("sp", (key, self.cnt[key]))
        self.es.close()


class K:
    def __init__(self, name):
        self.nc = bass.Bass("TRN2", target_bir_lowering=False)
        self.P = Prog(self.nc)
        self.es = ExitStack()
        self.n = 0
        self.rr = 0
        nc = self.nc
        self.es.enter_context(nc.allow_non_contiguous_dma(reason="small strided parameter loads"))
        self.ps = [self.es.enter_context(nc.psum_tensor("ps%d" % i, [128, 512], F32)) for i in range(7)]
        self.psb = [Buf("ps%d" % i) for i in range(7)]
        pt0 = self.es.enter_context(nc.psum_tensor("pt0", [128, 1024], BF16))
        ptb0 = Buf("pt0")
        self.pt = [pt0, pt0]
        self.ptb = [ptb0, ptb0]
        self.pti = 0
        self.ident, self.identb = self.sb("ident", [128, 128], BF16)
        self.anti, self.antib = self.sb("anti", [128, 128], BF16)

    def sb(self, name, shape, dt):
        self.n += 1
        t = self.es.enter_context(self.nc.sbuf_tensor("%s_%d" % (name, self.n), shape, dt))
        return t, Buf(name)

    def scope(self):
        kb = self

        class _S:
            def __enter__(self_):
                self_.old = kb.es
                kb.es = ExitStack()
                return kb.es

            def __exit__(self_, *a):
                kb.P.barrier()
                kb.es.close()
                kb.es = self_.old
        return _S()

    alias = None

    def din(self, name, shape, dt=F32):
        if self.alias is not None:
            ap = self.alias[name]
            assert list(ap.shape) == list(shape), (name, ap.shape, shape)
            return ap
        return self.nc.dram_tensor(name, list(shape), dt, kind="ExternalInput").ap()

    def dout(self, name, shape, dt=F32):
        if self.alias is not None:
            ap = self.alias[name]
            assert list(ap.shape) == list(shape), (name, ap.shape, shape)
            return ap
        return self.nc.dram_tensor(name, list(shape), dt, kind="ExternalOutput").ap()

    def dscr(self, name, shape, dt=F32):
        self.n += 1
        return self.nc.dram_tensor("%s_%d" % (name, self.n), list(shape), dt, kind="Internal")

    def dq(self):
        self.rr += 1
        return "sp" if self.rr % 2 else "pool"

    def load_consts(self, ident_d, anti_d):
        st, stb = self.sb("cst", [128, 256], F32)
        self.P.dma("sp", st[:, 0:128], ident_d, writes=[stb])
        self.P.dma("sp", st[:, 128:256], anti_d, writes=[stb])
        nc = self.nc
        self.P.op("dve", lambda: nc.vector.tensor_copy(out=self.ident[:], in_=st[:, 0:128]), reads=[stb], writes=[self.identb])
        self.P.op("dve", lambda: nc.vector.tensor_copy(out=self.anti[:], in_=st[:, 128:256]), reads=[stb], writes=[self.antib])

    def load_w(self, dram, kdim, ncol, name, eng="pool", stage=None):
        nc, P = self.nc, self.P
        kp = min(128, kdim)
        nk = max(1, kdim // 128)
        w, wb = self.sb(name, [kp, nk, ncol], BF16)
        CH = 2048
        if stage is None:
            stage = [self.sb("wst", [128, CH], F32) for _ in range(2)]
        self._wst = stage
        i = 0
        for k in range(nk):
            for c0 in range(0, ncol, CH):
                cw = min(CH, ncol - c0)
                st, stb = stage[i % 2]
                i += 1
                P.dma(self.dq(), st[0:kp, 0:cw], dram[k * 128:k * 128 + kp, c0:c0 + cw], writes=[stb])
                self.cast(eng, w[:, k, c0:c0 + cw], st[0:kp, 0:cw], [stb], [wb])
        return w, wb

    def cast(self, eng, out, in_, reads, writes, sig=True):
        nc = self.nc
        if eng == "pool":
            self.P.op("pool", lambda: nc.gpsimd.tensor_copy(out=out, in_=in_), reads=reads, writes=writes, sig=sig)
        elif eng == "dve":
            self.P.op("dve", lambda: nc.vector.tensor_copy(out=out, in_=in_), reads=reads, writes=writes, sig=sig)
        else:
            self.P.op("act", lambda: nc.scalar.copy(out=out, in_=in_), reads=reads, writes=writes, sig=sig)

    def bcast_row(self, dram_row, ncol, name):
        t, tb = self.sb(name, [128, ncol], F32)
        src = AP(tensor=dram_row.tensor, offset=dram_row.offset, ap=[[0, 128], [1, ncol]])
        self.P.dma("sp", t[:], src, writes=[tb])
        return t, tb

    def rmsnorm(self, x, xb, g, gb, out, outb, dim, scr, scrb, np_=128):
        nc, P = self.nc, self.P
        P.op("act", lambda: nc.scalar.activation(out=out, in_=x, func=AF.Square, accum_out=scr[0:np_, 0:1]),
             reads=[xb], writes=[outb, scrb])
        P.op("act", lambda: nc.scalar.activation(out=scr[0:np_, 1:2], in_=scr[0:np_, 0:1], func=AF.Ln, scale=1.0 / dim, bias=self.eps_t[0:np_, 0:1]),
             reads=[scrb, self.eps_b], writes=[scrb])
        P.op("act", lambda: nc.scalar.activation(out=scr[0:np_, 2:3], in_=scr[0:np_, 1:2], func=AF.Exp, scale=-0.5),
             reads=[scrb], writes=[scrb])
        P.op("dve", lambda: nc.vector.scalar_tensor_tensor(out=out, in0=x, scalar=scr[0:np_, 2:3], in1=g, op0=ALU.mult, op1=ALU.mult),
             reads=[xb, scrb, gb], writes=[outb])

    def mk_eps(self):
        self.eps_t, self.eps_b = self.sb("eps", [128, 1], F32)
        nc = self.nc
        self.P.op("dve", lambda: nc.vector.memset(self.eps_t[:], EPS), writes=[self.eps_b])

    def transpose_to(self, src_list, dst, dstb, reads, evac="dve", np_in=128):
        nc, P = self.nc, self.P
        n = len(src_list)
        i0 = 0
        while i0 < n:
            nb = min(8, n - i0)
            pt = self.pt[self.pti % 2]
            ptb = self.ptb[self.pti % 2]
            self.pti += 1
            w = src_list[i0].shape[-1]
            for j in range(nb):
                s = src_list[i0 + j]
                P.op("pe", lambda s=s, j=j: nc.tensor.transpose(pt[0:w, j * 128:j * 128 + np_in], s, self.ident[0:np_in, 0:np_in]),
                     reads=list(reads) + [self.identb], writes=[ptb], sig=(j == nb - 1))
            src = pt[0:w, 0:nb * 128].rearrange("p (a b) -> p a b", b=128)[:, :, 0:np_in]
            o = dst[0:w, i0:i0 + nb, :]
            if evac == "dve":
                P.op("dve", lambda: nc.vector.tensor_copy(out=o, in_=src), reads=[ptb], writes=[dstb])
            else:
                P.op("act", lambda: nc.scalar.copy(out=o, in_=src), reads=[ptb], writes=[dstb])
            i0 += nb

    def run(self, in_maps):
        self.P.finish()
        self.es.close()
        res = run_bass_kernel_spmd(self.nc, in_maps, core_ids=list(range(8)))
        return res.results


def rel_bucket_np(dist):
    n = np.maximum(dist, 0)
    nf = np.maximum(n, 16).astype(np.float32)
    large = 16 + (np.log(nf / np.float32(16)) / np.float32(math.log(2048 / 16)) * np.float32(16)).astype(np.int32)
    large = np.minimum(large, 31)
    return np.where(n < 16, n, large)


def onehot_table(dist, masked):
    Y = dist.shape[0]
    oh = np.zeros((33, Y), np.float32)
    b = rel_bucket_np(dist)
    ok = ~masked
    oh[b[ok], np.nonzero(ok)[0]] = 1.0
    oh[32, masked] = 1.0
    return oh


Y_SLC = 2304
Y_WIN = 1280
Y_CMP = 5376


def host_tables(c):
    y = np.arange(Y_SLC)
    d = y - 511 + 128 * c
    oh_s = onehot_table(d, d < 0)
    y = np.arange(Y_WIN)
    d = y - 511 + 128 * c
    oh_w = onehot_table(d, (d < 0) | (d >= 512))
    y = np.arange(Y_CMP)
    d = y + 128 * c - 2063
    oh_c = onehot_table(d, d < 0)
    return oh_s, oh_w, oh_c


def build_tables(kb, rel_bias_d, ohs, names_Y):
    nc, P = kb.nc, kb.P
    bt, btb = kb.sb("biasT", [33, 12], F32)
    P.dma("sp", bt[0:32, :], rel_bias_d, writes=[btb])
    b31, b31b = kb.sb("b31", [33, 12], F32)
    src = AP(tensor=rel_bias_d.tensor, offset=rel_bias_d.offset + 31 * 12, ap=[[0, 32], [1, 12]])
    P.dma("sp", b31[0:32, :], src, writes=[b31b])
    bb, bbb = kb.sb("biasb", [33, 12], BF16)
    P.op("dve", lambda: nc.vector.memset(bb[:], NEGB), writes=[bbb])
    P.op("dve", lambda: nc.vector.tensor_tensor(out=bb[0:32, :], in0=bt[0:32, :], in1=b31[0:32, :], op=ALU.subtract),
         reads=[btb, b31b], writes=[bbb])
    outs = []
    for (oh_d, Y, nm) in [(o, y, n) for o, (n, y) in zip(ohs, names_Y)]:
        oh, ohb = kb.sb("oh" + nm, [33, Y], BF16)
        st, stb = kb.sb("ohst" + nm, [33, Y], F32)
        P.dma("sp", st[:], oh_d, writes=[stb])
        P.op("dve", lambda: nc.vector.tensor_copy(out=oh[:], in_=st[:]), reads=[stb], writes=[ohb])
        tt, ttb = kb.sb("tt" + nm, [12, Y], BF16)
        for c0 in range(0, Y, 512):
            cw = min(512, Y - c0)
            ps, psb = kb.ps[0], kb.psb[0]
            P.op("pe", lambda: nc.tensor.matmul(ps[0:12, 0:cw], lhsT=bb[:], rhs=oh[:, c0:c0 + cw], start=True, stop=True),
                 reads=[bbb, ohb], writes=[psb])
            P.op("dve", lambda: nc.vector.tensor_copy(out=tt[:, c0:c0 + cw], in_=ps[0:12, 0:cw]), reads=[psb], writes=[ttb])
        scr = kb.dscr("ttd" + nm, [12, Y], BF16)
        scrb = Buf("ttd" + nm)
        P.dma("sp", scr.ap(), tt[:], reads=[ttb], writes=[scrb])
        outs.append((scr, scrb, Y))
    return outs


def load_band(kb, scr, scrb, Y, Z, pstep, name, zoff=0, dst=None):
    if dst is None:
        dst = kb.sb(name, [128, 12, Z], BF16)
    t, tb = dst
    src = AP(tensor=scr, offset=zoff, ap=[[pstep, 128], [Y, 12], [1, Z]])
    kb.P.dma(kb.dq(), t[:, :, 0:Z], src, reads=[scrb], writes=[tb])
    return t, tb


FM_A = [(0 + 64 * i, 0.125) for i in range(12)] + [(768 + 64 * i, 1.0) for i in range(4)] + \
       [(1024, 1.0), (1088, 1.0), (1280, 1.0), (1344, 1.0)] + [(1572 + 64 * i, 0.125) for i in range(4)]


def proj_phase(kb, x_d, g_d, w, wb, ncols, fm_groups, tm_ranges, fmT_d, tm_d, post_tm=None):
    nc, P = kb.nc, kb.P
    g, gb = kb.bcast_row(g_d, D, "gain")
    xt = [kb.sb("xt", [128, D], F32) for _ in range(2)]
    xn = [kb.sb("xn", [128, D], BF16) for _ in range(2)]
    scr, scrb = kb.sb("scr", [128, 4], F32)
    xnT, xnTb = kb.sb("xnT", [128, 8, 512], BF16)
    ng = len(fm_groups)
    fst, fstb = kb.sb("fst", [64, ng, 512], BF16)
    ntm = sum(n for _, n in tm_ranges)
    tst = [kb.sb("tst", [128, max(ntm, 1)], F32) for _ in range(2)]
    ei = 0
    for blk in range(4):
        for tt in range(4):
            ti = blk * 4 + tt
            x_, xb_ = xt[ti % 2]
            n_, nb_ = xn[ti % 2]
            P.dma(kb.dq(), x_[:], x_d[ti * 128:(ti + 1) * 128, :], writes=[xb_])
            kb.rmsnorm(x_[:], xb_, g[:], gb, n_[:], nb_, D, scr, scrb)
            for half in range(1):
                pt = kb.pt[kb.pti % 2]
                ptb = kb.ptb[kb.pti % 2]
                kb.pti += 1
                for j in range(8):
                    P.op("pe", lambda j=j: nc.tensor.transpose(pt[:, j * 128:(j + 1) * 128], n_[:, j * 128:(j + 1) * 128], kb.ident[:]),
                         reads=[nb_, kb.identb], writes=[ptb], sig=(j == 7))
                P.op("dve", lambda: nc.vector.tensor_copy(out=xnT[:, :, tt * 128:(tt + 1) * 128],
                                                          in_=pt[:, :].rearrange("p (a b) -> p a b", b=128)),
                     reads=[ptb], writes=[xnTb])
            if ntm:
                ts_, tsb_ = tst[ti % 2]
                ps, psb = kb.ps[4], kb.psb[4]
                o = 0
                for (c0, n) in tm_ranges:
                    for k in range(8):
                        P.op("pe", lambda k=k, o=o, c0=c0, n=n: nc.tensor.matmul(ps[:, o:o + n], lhsT=xnT[:, k, tt * 128:(tt + 1) * 128], rhs=w[:, k, c0:c0 + n],
                                                                       start=(k == 0 and o == 0), stop=(k == 7)),
                             reads=[xnTb, wb], writes=[psb], sig=(k == 7))
                    o += n
                P.op("act", lambda: nc.scalar.copy(out=ts_[:, 0:ntm], in_=ps[:, 0:ntm]), reads=[psb], writes=[tsb_])
                if post_tm is not None:
                    post_tm(ts_, tsb_)
                P.dma(kb.dq(), tm_d[ti * 128:(ti + 1) * 128, :], ts_[:, 0:ntm], reads=[tsb_], final=True)
        for gi, (c0, sc) in enumerate(fm_groups):
            ps, psb = kb.ps[gi % 4], kb.psb[gi % 4]
            for k in range(8):
                P.op("pe", lambda k=k, c0=c0: nc.tensor.matmul(ps[0:64, :], lhsT=w[:, k, c0:c0 + 64], rhs=xnT[:, k, :], start=(k == 0), stop=(k == 7)),
                     reads=[xnTb, wb], writes=[psb], sig=(k == 7))
            if ei % 2 == 0:
                P.op("act", lambda: nc.scalar.activation(out=fst[:, gi, :], in_=ps[0:64, :], func=AF.Copy, scale=sc), reads=[psb], writes=[fstb])
            else:
                P.op("dve", lambda: nc.vector.tensor_scalar(out=fst[:, gi, :], in0=ps[0:64, :], scalar1=sc, scalar2=None, op0=ALU.mult), reads=[psb], writes=[fstb])
            ei += 1
        P.dma(kb.dq(), fmT_d[:, :, blk * 512:(blk + 1) * 512], fst[:], reads=[fstb], final=True)


def build_A(kb=None):
    kb = kb or K("A")
    nc, P = kb.nc, kb.P
    x_d = kb.din("x", [TOK, D])
    g_d = kb.din("g", [1, D])
    w_d = kb.din("w_in", [D, 1828])
    gb_d = kb.din("gate_b", [1, 36])
    id_d = kb.din("ident", [128, 128])
    an_d = kb.din("anti", [128, 128])
    fm_d = kb.dout("fmT", [64, 24, TOK], BF16)
    tm_d = kb.dout("tm", [TOK, 256 + 36])
    kb.load_consts(id_d, an_d)
    kb.mk_eps()
    w, wb = kb.load_w(w_d, D, 1828, "w_in")
    gbt, gbb = kb.bcast_row(gb_d, 36, "gateb")

    def post(ts_, tsb_):
        P.op("dve", lambda: nc.vector.tensor_tensor(out=ts_[:, 256:292], in0=ts_[:, 256:292], in1=gbt[:], op=ALU.add), reads=[tsb_, gbb], writes=[tsb_])
        P.op("act", lambda: nc.scalar.activation(out=ts_[:, 256:292], in_=ts_[:, 256:292], func=AF.Exp, scale=-1.0), reads=[tsb_], writes=[tsb_])
        P.op("dve", lambda: nc.vector.tensor_scalar(out=ts_[:, 256:292], in0=ts_[:, 256:292], scalar1=1.0, scalar2=None, op0=ALU.add), reads=[tsb_], writes=[tsb_])
        P.op("dve", lambda: nc.vector.reciprocal(out=ts_[:, 256:292], in_=ts_[:, 256:292]), reads=[tsb_], writes=[tsb_])

    proj_phase(kb, x_d, g_d, w, wb, 1828, FM_A, [(1152, 128), (1408, 128), (1536, 36)], fm_d, tm_d, post_tm=post)
    return kb


def core_tokens(c):
    return np.concatenate([np.arange(128 * (4 * m + c), 128 * (4 * m + c) + 128) for m in range(NT)])


def run_A(inputs):
    kb = build_A()
    ident = np.eye(128, dtype=np.float32)
    anti = np.ascontiguousarray(ident[::-1])
    maps = []
    for core in range(8):
        b, c = core // 4, core % 4
        maps.append({"x": np.ascontiguousarray(inputs["x"][b][core_tokens(c)]),
                     "g": inputs["norm_mix"][0:1], "w_in": inputs["nsa_w_in"][0],
                     "gate_b": inputs["nsa_gate_b"][0:1], "ident": ident, "anti": anti})
    return kb.run(maps)


def mm(kb, out, lhsT, rhs, start, reads, writes, sig=False, stop=True):
    nc = kb.nc
    kb.P.op("pe", lambda: nc.tensor.matmul(out, lhsT=lhsT, rhs=rhs, start=start, stop=stop), reads=reads, writes=writes, sig=sig)


def bc3(ap2d):
    return AP(tensor=ap2d.tensor, offset=ap2d.offset, ap=[list(ap2d.ap[0]), [0, 3], list(ap2d.ap[1])])


class Attn:
    def __init__(self, kb, sb_list=(0, 1, 6), depth=2):
        self.kb = kb
        self.depth = depth
        self.npT = depth + 2
        self.pT = [kb.sb("pT", [128, 384], BF16) for _ in range(self.npT)]
        self.i = 0
        self.sb_list = list(sb_list)
        self.sb_i = 0
        self.pending = []

    def next_sbank(self):
        b = self.sb_list[self.sb_i % len(self.sb_list)]
        self.sb_i += 1
        return b

    def unit(self, s_terms, near_terms, accs, accb, v_ap, vbufs, first, mul_mask=None):
        kb = self.kb
        nc, P = kb.nc, kb.P
        sbk = self.next_sbank()
        ps, psb = kb.ps[sbk], kb.psb[sbk]
        n = len(s_terms)
        for t, (l, r, bufs) in enumerate(s_terms):
            mm(kb, ps[:, 0:384], l, r, t == 0, bufs, [psb], sig=(t == n - 1 and not near_terms))
        if near_terms:
            l, r, bufs = near_terms
            mm(kb, ps[:, 0:384].rearrange("p (a b) -> p a b", b=128), l, r, False, bufs, [psb], sig=True)
        pT, pTb = self.pT[self.i % self.npT]
        self.i += 1
        P.op("act", lambda: nc.scalar.activation(out=pT[:], in_=ps[:, 0:384], func=AF.Exp), reads=[psb], writes=[pTb])
        if mul_mask is not None:
            m_ap, m_bufs = mul_mask
            p3 = pT[:].rearrange("p (a b) -> p a b", b=128)
            P.op("dve", lambda: nc.vector.tensor_tensor(out=p3, in0=p3, in1=m_ap, op=ALU.mult), reads=[pTb] + m_bufs, writes=[pTb])
        self.pending.append((pT, pTb, accs, accb, v_ap, vbufs, first))
        if len(self.pending) > self.depth:
            self._pv(*self.pending.pop(0))

    def _pv(self, pT, pTb, accs, accb, v_ap, vbufs, first):
        kb = self.kb
        for hh in range(3):
            mm(kb, accs[hh], pT[:, hh * 128:(hh + 1) * 128], v_ap, first[hh], [pTb] + vbufs, [accb[hh]], sig=(hh == 2))

    def flush(self):
        while self.pending:
            self._pv(*self.pending.pop(0))


def build_B(layer, kb=None):
    kb = kb or K("B")
    nc, P = kb.nc, kb.P
    x_d = kb.din("x", [TOK, D])
    id_d = kb.din("identb", [128, 128], BF16)
    an_d = kb.din("antib", [128, 128], BF16)
    relb_d = kb.din("rel_bias", [32, 12])
    oh_s_d = kb.din("oh_s", [33, Y_SLC])
    oh_w_d = kb.din("oh_w", [33, Y_WIN])
    oh_c_d = kb.din("oh_c", [33, Y_CMP])
    qT_d = kb.din("qT2", [128, 6, TOK], BF16)
    qm_d = kb.din("qmT", [64, 4, TOK], BF16)
    gates_d = kb.din("gates", [TOK, 36])
    ks_d = kb.din("ksT2", [128, T], BF16)
    kw_d = kb.din("kwT2", [128, T], BF16)
    kcr_d = kb.din("kcrT2", [128, T], BF16)
    vcr_d = kb.din("vcrT2", [128, T], BF16)
    vs_d = kb.din("vs", [T, 128])
    vw_d = kb.din("vw", [T, 128])
    ew_d = kb.din("ew", [128, T], BF16)
    ov_d = kb.din("ovl", [128, 4, 128], BF16)
    fb_d = kb.din("fb", [NT, 128, 128])
    w1k_d = kb.din("w1k", [2048, 256]); b1k_d = kb.din("b1k", [256, 1]); w2k_d = kb.din("w2k", [256, 64]); b2k_d = kb.din("b2k", [64, 1])
    w1v_d = kb.din("w1v", [2048, 256]); b1v_d = kb.din("b1v", [256, 1]); w2v_d = kb.din("w2v", [256, 64]); b2v_d = kb.din("b2v", [1, 64])
    posk_d = kb.din("posk", [32, 64]); posv_d = kb.din("posv", [32, 64])
    mem_d = kb.din("mem", [256, D]); gmem_d = kb.din("g_mem", [1, D]); wkv_d = kb.din("w_mem_kv", [D, 512])
    wout_d = kb.din("w_out", [D, D])
    hmid_d = kb.dout("hmid", [TOK, D])

    P.dma("sp", kb.ident[:], id_d, writes=[kb.identb])
    P.dma("sp", kb.anti[:], an_d, writes=[kb.antib])
    kb.mk_eps()
    with kb.scope():
        tabs = build_tables(kb, relb_d, [oh_s_d, oh_w_d, oh_c_d], [("s", Y_SLC), ("w", Y_WIN), ("c", Y_CMP)])
    band_s, band_sb = load_band(kb, tabs[0][0], tabs[0][1], Y_SLC, 2048, 1, "band_s")
    band_w, band_wb = load_band(kb, tabs[1][0], tabs[1][1], Y_WIN, 1024, 1, "band_w")
    bandc = [kb.sb("bandc", [128, 12, 128], BF16) for _ in range(4)]
    bandc_i = 0

    ksT, ksTb = kb.sb("ksT", [128, T], BF16)
    P.dma("sp", ksT[:, 0:4096], ks_d[:, 0:4096], writes=[ksTb])
    P.dma("pool", ksT[:, 4096:T], ks_d[:, 4096:T], writes=[ksTb])
    ew, ewb = kb.sb("ew", [128, T], BF16)
    P.dma("sp", ew[:, 0:4096], ew_d[:, 0:4096], writes=[ewb])
    P.dma("pool", ew[:, 4096:T], ew_d[:, 4096:T], writes=[ewb])
    vs1, vs1b = kb.sb("vs1", [128, 64, 2, 65], BF16)
    P.op("pool", lambda: nc.gpsimd.memset(vs1[:], 1.0), writes=[vs1b])
    with kb.scope():
        vst = [kb.sb("vst", [128, 8, 128], F32) for _ in range(2)]
        for i in range(8):
            st, stb = vst[i % 2]
            P.dma(kb.dq(), st[:], vs_d[i * 1024:(i + 1) * 1024, :].rearrange("(a p) c -> p a c", p=128), writes=[stb])
            P.op("pool", lambda: nc.gpsimd.tensor_copy(out=vs1[:, i * 8:(i + 1) * 8, :, 0:64], in_=st[:].rearrange("p a (g d) -> p a g d", g=2)),
                 reads=[stb], writes=[vs1b])
    RC, RCb = kb.sb("RC", [128, 4, 2, 193], BF16)
    P.op("pool", lambda: nc.gpsimd.memset(RC[:], 1.0), writes=[RCb])
    ovt, ovtb = kb.sb("ovt", [128, 4, 128], BF16)
    P.dma("sp", ovt[:], ov_d, writes=[ovtb])
    for g in range(2):
        P.op("pool", lambda: nc.gpsimd.tensor_copy(out=RC[:, :, g, 0:128], in_=ovt[:]), reads=[ovtb], writes=[RCb])
    kcT, kcTb = kb.sb("kcT", [128, 512], BF16)

    with ExitStack() as es2:
        def tmp(name, shape, dt):
            kb.n += 1
            return es2.enter_context(nc.sbuf_tensor("%s_%d" % (name, kb.n), shape, dt)), Buf(name)
        rawT, rawTb = tmp("rawT", [128, T + 32], BF16)
        w1, w1b = tmp("w1", [128, 32, 256], BF16)
        w1st, w1stb = tmp("w1st", [128, 8, 256], F32)
        w2, w2b = tmp("w2", [128, 2, 128], BF16)
        w2st, w2stb = tmp("w2st", [128, 2, 64], F32)
        h1T, h1Tb = tmp("h1T", [128, 2, 512], BF16)
        u_, ub = tmp("u", [128, 512], F32)
        t1, t1b = tmp("t1", [128, 512], F32)
        t2, t2b = tmp("t2", [128, 512], F32)
        cb, cbb = tmp("cb", [128, 2], F32)
        b1t, b1tb = tmp("b1t", [128, 2], F32)
        b2d, b2db = tmp("b2d", [128, 1], F32)
        b2r, b2rb = tmp("b2r", [128, 64], F32)
        pst, pstb = tmp("pst", [32, 64], F32)
        psb16, psb16b = tmp("psb16", [32, 64], BF16)
        posT, posTb = tmp("posT", [64, 1, 32], BF16)
        for kv in range(2):
            raw_d = kcr_d if kv == 0 else vcr_d
            w1_d = w1k_d if kv == 0 else w1v_d
            b1_d = b1k_d if kv == 0 else b1v_d
            w2_d = w2k_d if kv == 0 else w2v_d
            pos_d = posk_d if kv == 0 else posv_d
            P.op("dve", lambda: nc.vector.memset(rawT[:, T:T + 32], 0.0), writes=[rawTb])
            P.dma("sp", rawT[:, 0:4096], raw_d[:, 0:4096], writes=[rawTb])
            P.dma("pool", rawT[:, 4096:T], raw_d[:, 4096:T], writes=[rawTb])
            for q4 in range(4):
                for half in range(2):
                    P.dma(kb.dq(), w1st[half * 64:(half + 1) * 64, :, :], w1_d[q4 * 512:(q4 + 1) * 512, :].rearrange("(l d) h -> d l h", d=64), writes=[w1stb])
                P.op("dve", lambda: nc.vector.tensor_copy(out=w1[:, q4 * 8:(q4 + 1) * 8, :], in_=w1st[:]), reads=[w1stb], writes=[w1b])
            P.dma("sp", w2st[:], w2_d.rearrange("(c p) d -> p c d", p=128), writes=[w2stb])
            for half in range(2):
                P.op("dve", lambda: nc.vector.tensor_copy(out=w2[:, :, half * 64:(half + 1) * 64], in_=w2st[:]), reads=[w2stb], writes=[w2b])
            P.dma("sp", b1t[:], b1_d.rearrange("(c p) o -> p (c o)", p=128), writes=[b1tb])
            P.dma("sp", pst[:], pos_d, writes=[pstb])
            P.op("dve", lambda: nc.vector.tensor_copy(out=psb16[:], in_=pst[:]), reads=[pstb], writes=[psb16b])
            kb.transpose_to([psb16[:, :]], posT, posTb, [psb16b], np_in=32)
            for hc in range(2):
                ps, psb = kb.ps[2], kb.psb[2]
                for l in range(32):
                    mm(kb, ps[:, 0:1], w1[0:64, l, hc * 128:(hc + 1) * 128], posT[:, 0, l:l + 1], l == 0, [w1b, posTb], [psb], sig=(l == 31))
                P.op("dve", lambda: nc.vector.tensor_tensor(out=cb[:, hc:hc + 1], in0=ps[:, 0:1], in1=b1t[:, hc:hc + 1], op=ALU.add), reads=[psb, b1tb], writes=[cbb])
            if kv == 0:
                for half in range(2):
                    P.dma("sp", b2d[half * 64:(half + 1) * 64, :], b2k_d, writes=[b2db])
            else:
                src = AP(tensor=b2v_d.tensor, offset=b2v_d.offset, ap=[[0, 128], [1, 64]])
                P.dma("sp", b2r[:], src, writes=[b2rb])
            for g in range(2):
                gs = slice(g * 64, (g + 1) * 64)
                for hc in range(2):
                    ps, psb = kb.ps[2 + hc], kb.psb[2 + hc]
                    for l in range(32):
                        rhs = AP(tensor=rawT.tensor if hasattr(rawT, "tensor") else rawT[:].tensor, offset=rawT[gs, l:l + 1].offset,
                                 ap=[list(rawT[gs, :].ap[0]), [16, 512]])
                        mm(kb, ps[:, :], w1[gs, l, hc * 128:(hc + 1) * 128], rhs, l == 0, [w1b, rawTb], [psb], sig=(l == 31))
                    P.op("act", lambda: nc.scalar.activation(out=u_[:], in_=ps[:, :], func=AF.Identity, bias=cb[:, hc:hc + 1]), reads=[psb, cbb], writes=[ub])
                    P.op("dve", lambda: nc.vector.tensor_tensor(out=t1[:], in0=u_[:], in1=u_[:], op=ALU.mult), reads=[ub], writes=[t1b])
                    P.op("dve", lambda: nc.vector.tensor_scalar(out=t1[:], in0=t1[:], scalar1=0.044715, scalar2=1.0, op0=ALU.mult, op1=ALU.add), reads=[t1b], writes=[t1b])
                    P.op("dve", lambda: nc.vector.tensor_tensor(out=t1[:], in0=t1[:], in1=u_[:], op=ALU.mult), reads=[t1b, ub], writes=[t1b])
                    P.op("act", lambda: nc.scalar.activation(out=t2[:], in_=t1[:], func=AF.Tanh, scale=0.7978845608028654), reads=[t1b], writes=[t2b])
                    P.op("dve", lambda: nc.vector.tensor_scalar(out=t2[:], in0=t2[:], scalar1=0.5, scalar2=0.5, op0=ALU.mult, op1=ALU.add), reads=[t2b], writes=[t2b])
                    P.op("dve", lambda: nc.vector.tensor_tensor(out=h1T[:, hc, :], in0=t2[:], in1=u_[:], op=ALU.mult), reads=[t2b, ub], writes=[h1Tb])
                if kv == 0:
                    ps, psb = kb.ps[4], kb.psb[4]
                    for hc in range(2):
                        mm(kb, ps[:, :], w2[:, hc, :], h1T[:, hc, :], hc == 0, [w2b, h1Tb], [psb], sig=(hc == 1))
                    P.op("act", lambda: nc.scalar.activation(out=kcT[gs, :], in_=ps[gs, :], func=AF.Identity, bias=b2d[gs, 0:1]), reads=[psb, b2db], writes=[kcTb])
                else:
                    for j in range(4):
                        ps, psb = kb.ps[4], kb.psb[4]
                        for hc in range(2):
                            mm(kb, ps[:, 0:64], h1T[:, hc, j * 128:(j + 1) * 128], w2[:, hc, 0:64], hc == 0, [w2b, h1Tb], [psb], sig=(hc == 1))
                        P.op("dve", lambda: nc.vector.tensor_tensor(out=RC[:, j, g, 129:193], in0=ps[:, 0:64], in1=b2r[:], op=ALU.add), reads=[psb, b2rb], writes=[RCb])

    P.barrier()
    kmT, kmTb = kb.sb("kmT", [64, 4, 256], BF16)
    vm1, vm1b = kb.sb("vm1", [128, 2, 4, 65], BF16)
    P.op("pool", lambda: nc.gpsimd.memset(vm1[:], 1.0), writes=[vm1b])
    gm, gmb = kb.bcast_row(gmem_d, D, "gmem")
    wkv, wkvb = kb.sb("wkv", [128, 8, 512], BF16)
    with ExitStack() as es2:
        def tmp(name, shape, dt):
            kb.n += 1
            return es2.enter_context(nc.sbuf_tensor("%s_%d" % (name, kb.n), shape, dt)), Buf(name)
        mt, mtb = tmp("mt", [128, D], F32)
        mn, mnb = tmp("mn", [128, D], BF16)
        mnT, mnTb = tmp("mnT", [128, 8, 256], BF16)
        scr, scrb = tmp("mscr", [128, 4], F32)
        wst = [tmp("wst2", [128, 512], F32) for _ in range(2)]
        for k in range(8):
            st, stb = wst[k % 2]
            P.dma(kb.dq(), st[:], wkv_d[k * 128:(k + 1) * 128, :], writes=[stb])
            P.op("dve", lambda: nc.vector.tensor_copy(out=wkv[:, k, :], in_=st[:]), reads=[stb], writes=[wkvb])
        for tl in range(2):
            P.dma("sp", mt[:], mem_d[tl * 128:(tl + 1) * 128, :], writes=[mtb])
            kb.rmsnorm(mt[:], mtb, gm[:], gmb, mn[:], mnb, D, scr, scrb)
            pt = kb.pt[kb.pti % 2]; ptb = kb.ptb[kb.pti % 2]; kb.pti += 1
            for j in range(8):
                P.op("pe", lambda j=j: nc.tensor.transpose(pt[:, j * 128:(j + 1) * 128], mn[:, j * 128:(j + 1) * 128], kb.ident[:]),
                     reads=[mnb, kb.identb], writes=[ptb], sig=(j == 7))
            P.op("dve", lambda: nc.vector.tensor_copy(out=mnT[:, :, tl * 128:(tl + 1) * 128], in_=pt[:, :].rearrange("p (a b) -> p a b", b=128)), reads=[ptb], writes=[mnTb])
        for h in range(4):
            ps, psb = kb.ps[2], kb.psb[2]
            for k in range(8):
                mm(kb, ps[0:64, 0:256], wkv[:, k, h * 64:(h + 1) * 64], mnT[:, k, :], k == 0, [wkvb, mnTb], [psb], sig=(k == 7))
            P.op("dve", lambda: nc.vector.tensor_copy(out=kmT[:, h, :], in_=ps[0:64, 0:256]), reads=[psb], writes=[kmTb])
        for tl in range(2):
            ps, psb = kb.ps[3], kb.psb[3]
            for k in range(8):
                mm(kb, ps[:, 0:256], mnT[:, k, tl * 128:(tl + 1) * 128], wkv[:, k, 256:512], k == 0, [wkvb, mnTb], [psb], sig=(k == 7))
            P.op("dve", lambda: nc.vector.tensor_copy(out=vm1[:, tl, :, 0:64], in_=ps[:, 0:256].rearrange("p (h d) -> p h d", d=64)), reads=[psb], writes=[vm1b])
    P.barrier()
    wout, woutb = kb.sb("wout", [128, 8, D], BF16)
    with kb.scope():
        wst = [kb.sb("wst3", [128, 1024], F32) for _ in range(2)]
        for k in range(8):
            st, stb = wst[k % 2]
            P.dma(kb.dq(), st[:], wout_d[k * 128:(k + 1) * 128, :], writes=[stb])
            P.op("dve", lambda: nc.vector.tensor_copy(out=wout[:, k, :], in_=st[:]), reads=[stb], writes=[woutb])

    at = Attn(kb, sb_list=(0, 1, 6, 5), depth=3)
    qs = [kb.sb("qs", [128, 6, 128], BF16) for _ in range(2)]
    qms = [kb.sb("qms", [64, 4, 128], BF16) for _ in range(2)]
    gts = [kb.sb("gts", [128, 12, 3], F32) for _ in range(2)]
    kws = [kb.sb("kws", [128, 1024], BF16) for _ in range(2)]
    vws = [kb.sb("vws", [128, 8, 2, 65], BF16) for _ in range(2)]
    vwst = [kb.sb("vwst", [128, 8, 128], F32)] * 2
    for (t_, b_) in vws:
        P.op("pool", lambda: nc.gpsimd.memset(t_[:], 1.0), writes=[b_])
    fbs = [kb.sb("fbs", [128, 128], F32) for _ in range(2)]
    xts = [kb.sb("xts", [128, D], F32)] * 2
    cat, catb = kb.sb("cat", [128, D], F32)
    catbf, catbfb = kb.sb("catbf", [128, D], BF16)
    catT, catTb = kb.sb("catT", [128, 8, 128], BF16)
    pslc, pslcb = kb.sb("pslc", [128, 2, 128], F32)
    wk, wkb = kb.sb("wk", [128, 128], F32)
    m8, m8b = kb.sb("m8", [128, 16], F32)
    sbf, sbfb = kb.sb("sbf", [128, 128], BF16)
    sbT, sbTb = kb.sb("sbT", [128, 2, 128], BF16)
    sm, smb = kb.sb("sm", [128, 16], F32)
    pm, pmb = kb.sb("pm", [128, 256], BF16)
    accbank = 2

    def next_acc():
        nonlocal accbank
        b = accbank
        accbank = 2 + (accbank - 2 + 1) % 3
        return b

    for m in range(NT):
        q_, qb_ = qs[m % 2]
        qm_, qmb_ = qms[m % 2]
        gt_, gtb_ = gts[m % 2]
        kw_, kwb_ = kws[m % 2]
        vw_, vwb_ = vws[m % 2]
        vwst_, vwstb_ = vwst[m % 2]
        fb_, fbb_ = fbs[m % 2]
        xt_, xtb_ = xts[m % 2]
        tsl = slice(m * 128, (m + 1) * 128)
        P.dma("sp", q_[:], qT_d[:, :, tsl], writes=[qb_])
        P.dma("sp", qm_[:], qm_d[:, :, tsl], writes=[qmb_])
        P.dma("sp", gt_[:], gates_d[tsl, :].rearrange("p (h b) -> p h b", b=3), writes=[gtb_])
        P.dma("sp", fb_[:], fb_d[m], writes=[fbb_])
        P.dma("pool", xt_[:], x_d[tsl, :], writes=[xtb_])
        kt0 = max(0, 4 * m - 4)
        nkw = 4 * m + 4 - kt0
        P.dma("pool", kw_[:, 0:nkw * 128], kw_d[:, kt0 * 128:(4 * m + 4) * 128], writes=[kwb_])
        P.dma("sp", vwst_[:, 0:nkw, :], vw_d[kt0 * 128:(4 * m + 4) * 128, :].rearrange("(a p) c -> p a c", p=128), writes=[vwstb_])
        P.op("pool", lambda: nc.gpsimd.tensor_copy(out=vw_[:, 0:nkw, :, 0:64], in_=vwst_[:, 0:nkw, :].rearrange("p a (g d) -> p a g d", g=2)),
             reads=[vwstb_], writes=[vwb_])
        cband = {}
        for j in range(4):
            v = m - 4 * j
            if 0 <= v <= 6:
                dst = bandc[bandc_i % 4]
                bandc_i += 1
                load_band(kb, tabs[2][0], tabs[2][1], Y_CMP, 128, 16, "bc", zoff=512 * v, dst=dst)
                cband[j] = dst
        for g in range(2):
            gs = slice(g * 64, (g + 1) * 64)
            for tr in range(2):
                bA, bB = next_acc(), next_acc()
                accs = [kb.ps[bA][:, 0:193], kb.ps[bA][:, 193:386], kb.ps[bB][:, 0:193]]
                accb = [kb.psb[bA], kb.psb[bA], kb.psb[bB]]
                js = [j for j in range(4) if m - 4 * j >= 0]
                for ji, j in enumerate(js):
                    s_terms = [(kcT[gs, j * 128:(j + 1) * 128], q_[gs, 3 * tr:3 * tr + 3, :], [kcTb, qb_])]
                    near = None
                    if j in cband:
                        bt, btb = cband[j]
                        near = (kb.anti[:], bt[:, 6 * g + 3 * tr:6 * g + 3 * tr + 3, :], [kb.antib, btb])
                    at.unit(s_terms, near, accs, accb, RC[:, j, g, :], [RCb], [ji == 0, False, ji == 0])
                at.flush()
                for hh in range(3):
                    h = 6 * g + 3 * tr + hh
                    U = accs[hh]
                    P.op("dve", lambda: nc.vector.tensor_scalar(out=sm[:, 0:1], in0=U[:, 128:129], scalar1=1e-30, scalar2=None, op0=ALU.max), reads=[accb[hh]], writes=[smb])
                    P.op("dve", lambda: nc.vector.reciprocal(out=sm[:, 1:2], in_=sm[:, 0:1]), reads=[smb], writes=[smb])
                    if tr == 0 and hh == 0:
                        P.op("dve", lambda: nc.vector.tensor_scalar(out=pslc[:, g, :], in0=U[:, 0:128], scalar1=sm[:, 1:2], scalar2=None, op0=ALU.mult), reads=[accb[hh], smb], writes=[pslcb])
                    else:
                        P.op("dve", lambda: nc.vector.scalar_tensor_tensor(out=pslc[:, g, :], in0=U[:, 0:128], scalar=sm[:, 1:2], in1=pslc[:, g, :], op0=ALU.mult, op1=ALU.add), reads=[accb[hh], smb, pslcb], writes=[pslcb])
                    P.op("dve", lambda: nc.vector.tensor_tensor(out=sm[:, 2:3], in0=sm[:, 1:2], in1=gt_[:, h, 0:1], op=ALU.mult), reads=[smb, gtb_], writes=[smb])
                    P.op("dve", lambda: nc.vector.tensor_scalar(out=cat[:, h * 64:(h + 1) * 64], in0=U[:, 129:193], scalar1=sm[:, 2:3], scalar2=None, op0=ALU.mult), reads=[accb[hh], smb], writes=[catb])
            P.op("dve", lambda: nc.vector.tensor_tensor(out=wk[:], in0=pslc[:, g, :], in1=fb_[:], op=ALU.add), reads=[pslcb, fbb_], writes=[wkb])
            P.op("dve", lambda: nc.vector.max(out=m8[:, 0:8], in_=wk[:]), reads=[wkb], writes=[m8b])
            P.op("dve", lambda: nc.vector.match_replace(out=pslc[:, g, :], in_to_replace=m8[:, 0:8], in_values=wk[:], imm_value=-3e30), reads=[wkb, m8b], writes=[pslcb])
            P.op("dve", lambda: nc.vector.max(out=m8[:, 8:16], in_=pslc[:, g, :]), reads=[pslcb], writes=[m8b])
            P.op("dve", lambda: nc.vector.tensor_scalar(out=wk[:], in0=wk[:], scalar1=m8[:, 15:16], scalar2=-NEGB, op0=ALU.is_ge, op1=ALU.mult), reads=[wkb, m8b], writes=[wkb])
            P.op("dve", lambda: nc.vector.tensor_scalar(out=sbf[:], in0=wk[:], scalar1=NEGB, scalar2=None, op0=ALU.add), reads=[wkb], writes=[sbfb])
            pt = kb.pt[kb.pti % 2]; ptb = kb.ptb[kb.pti % 2]; kb.pti += 1
            P.op("pe", lambda: nc.tensor.transpose(pt[:, 0:128], sbf[:], kb.ident[:]), reads=[sbfb, kb.identb], writes=[ptb])
            P.op("dve", lambda: nc.vector.tensor_copy(out=sbT[:, g, :], in_=pt[:, 0:128]), reads=[ptb], writes=[sbTb])
        for br in range(2):
            for g in range(2):
                gs = slice(g * 64, (g + 1) * 64)
                for tr in range(2):
                    bA = next_acc()
                    accs = [kb.ps[bA][:, 65 * hh:65 * hh + 65] for hh in range(3)]
                    accb = [kb.psb[bA]] * 3
                    kts = list(range(0, 4 * m + 4)) if br == 0 else list(range(kt0, 4 * m + 4))
                    for ki, kt in enumerate(kts):
                        u = kt - 4 * m
                        if br == 0:
                            s_terms = [(ksT[gs, kt * 128:(kt + 1) * 128], q_[gs, 3 * tr:3 * tr + 3, :], [ksTb, qb_]),
                                       (ew[:, kt * 128:(kt + 1) * 128], bc3(sbT[:, g, :]), [ewb, sbTb])]
                            near = None
                            if u >= -12:
                                z0 = 128 * (3 - u)
                                near = (kb.anti[:], band_s[:, 6 * g + 3 * tr:6 * g + 3 * tr + 3, z0:z0 + 128], [kb.antib, band_sb])
                            v_ap, vb_ = vs1[:, kt, g, :], [vs1b]
                        else:
                            kk = kt - kt0
                            s_terms = [(kw_[gs, kk * 128:(kk + 1) * 128], q_[gs, 3 * tr:3 * tr + 3, :], [kwb_, qb_])]
                            z0 = 128 * (3 - u)
                            near = (kb.anti[:], band_w[:, 6 * g + 3 * tr:6 * g + 3 * tr + 3, z0:z0 + 128], [kb.antib, band_wb])
                            v_ap, vb_ = vw_[:, kk, g, :], [vwb_]
                        at.unit(s_terms, near, accs, accb, v_ap, vb_, [ki == 0, False, False])
                    at.flush()
                    for hh in range(3):
                        h = 6 * g + 3 * tr + hh
                        O = accs[hh]
                        P.op("dve", lambda: nc.vector.tensor_scalar(out=sm[:, 0:1], in0=O[:, 64:65], scalar1=1e-30, scalar2=None, op0=ALU.max), reads=[accb[hh]], writes=[smb])
                        P.op("dve", lambda: nc.vector.reciprocal(out=sm[:, 1:2], in_=sm[:, 0:1]), reads=[smb], writes=[smb])
                        P.op("dve", lambda: nc.vector.tensor_tensor(out=sm[:, 2:3], in0=sm[:, 1:2], in1=gt_[:, h, 1 + br:2 + br], op=ALU.mult), reads=[smb, gtb_], writes=[smb])
                        P.op("dve", lambda: nc.vector.scalar_tensor_tensor(out=cat[:, h * 64:(h + 1) * 64], in0=O[:, 0:64], scalar=sm[:, 2:3], in1=cat[:, h * 64:(h + 1) * 64], op0=ALU.mult, op1=ALU.add),
                             reads=[accb[hh], smb, catb], writes=[catb])
        for h in range(4):
            sbk = at.next_sbank()
            ps, psb = kb.ps[sbk], kb.psb[sbk]
            for tl in range(2):
                mm(kb, ps[:, tl * 128:(tl + 1) * 128], kmT[:, h, tl * 128:(tl + 1) * 128], qm_[:, h, :], tl == 0, [kmTb, qmb_], [psb], sig=(tl == 1))
            P.op("act", lambda: nc.scalar.activation(out=pm[:], in_=ps[:, 0:256], func=AF.Exp), reads=[psb], writes=[pmb])
            bA = next_acc()
            O = kb.ps[bA][:, 0:65]
            for tl in range(2):
                mm(kb, O, pm[:, tl * 128:(tl + 1) * 128], vm1[:, tl, h, :], tl == 0, [pmb, vm1b], [kb.psb[bA]], sig=(tl == 1))
            P.op("dve", lambda: nc.vector.reciprocal(out=sm[:, 4:5], in_=O[:, 64:65]), reads=[kb.psb[bA]], writes=[smb])
            P.op("dve", lambda: nc.vector.tensor_scalar(out=cat[:, 768 + h * 64:768 + (h + 1) * 64], in0=O[:, 0:64], scalar1=sm[:, 4:5], scalar2=None, op0=ALU.mult), reads=[kb.psb[bA], smb], writes=[catb])
        P.op("act", lambda: nc.scalar.copy(out=catbf[:], in_=cat[:]), reads=[catb], writes=[catbfb])
        pt = kb.pt[kb.pti % 2]; ptb = kb.ptb[kb.pti % 2]; kb.pti += 1
        for j in range(8):
            P.op("pe", lambda j=j: nc.tensor.transpose(pt[:, j * 128:(j + 1) * 128], catbf[:, j * 128:(j + 1) * 128], kb.ident[:]),
                 reads=[catbfb, kb.identb], writes=[ptb], sig=(j == 7))
        P.op("dve", lambda: nc.vector.tensor_copy(out=catT[:], in_=pt[:, :].rearrange("p (a b) -> p a b", b=128)), reads=[ptb], writes=[catTb])
        for half in range(2):
            bA = next_acc()
            ps, psb = kb.ps[bA], kb.psb[bA]
            for k in range(8):
                mm(kb, ps[:, :], catT[:, k, :], wout[:, k, half * 512:(half + 1) * 512], k == 0, [catTb, woutb], [psb], sig=(k == 7))
            P.op("dve", lambda: nc.vector.tensor_tensor(out=xt_[:, half * 512:(half + 1) * 512], in0=ps[:, :], in1=xt_[:, half * 512:(half + 1) * 512], op=ALU.add), reads=[psb, xtb_], writes=[xtb_])
        P.dma("sp", hmid_d[tsl, :], xt_[:], reads=[xtb_], final=True)
    return kb


def gather_seq(arrs, axis):
    shp = list(arrs[0].shape)
    shp[axis] = T
    out = np.zeros(shp, arrs[0].dtype)
    for c in range(4):
        idx = [slice(None)] * len(shp)
        idx[axis] = core_tokens(c)
        out[tuple(idx)] = arrs[c]
    return out


def consts_B(c):
    ident = np.eye(128, dtype=np.float32)
    oh_s, oh_w, oh_c = host_tables(c)
    ew = (np.arange(T)[None, :] // 64 == np.arange(128)[:, None]).astype(NPBF)
    n = np.arange(512)
    cs, ce = n * 16, n * 16 + 31
    ss = np.arange(128) * 64
    ov = ((cs[:, None] <= ss[None, :] + 63) & (ce[:, None] >= ss[None, :])).astype(np.float32)
    ov[511] = 0
    ovl = np.ascontiguousarray(ov.reshape(4, 128, 128).transpose(1, 0, 2)).astype(NPBF)
    fb = np.zeros((NT, 128, 128), np.float32)
    blk = np.arange(128)[None, :]
    for m in range(NT):
        t = 128 * (4 * m + c) + np.arange(128)[:, None]
        cur = t // 64
        forced = (blk == 0) | (blk == cur) | (blk == cur - 1)
        adm = (blk * 64) <= t
        fb[m] = np.where(adm, 1e4 * forced, -1e30)
    return {"identb": ident.astype(NPBF), "antib": np.ascontiguousarray(ident[::-1]).astype(NPBF),
            "oh_s": oh_s, "oh_w": oh_w, "oh_c": oh_c, "ew": ew, "ovl": ovl, "fb": fb}


def to2(fm, lo):
    return np.ascontiguousarray(np.concatenate([fm[:, lo, :], fm[:, lo + 1, :]], axis=0))


def run_B(inputs, resA):
    kb = build_B(0)
    maps = []
    for core in range(8):
        b, c = core // 4, core % 4
        grp = [resA[4 * b + cc] for cc in range(4)]
        fms = [np.asarray(r["fmT"]) for r in grp]
        tms = [np.asarray(r["tm"]) for r in grp]
        fm = fms[c]
        mp = consts_B(c)
        mp["x"] = np.ascontiguousarray(inputs["x"][b][core_tokens(c)])
        mp["rel_bias"] = inputs["rel_bias"]
        q = fm[:, 0:12, :]
        mp["qT2"] = np.ascontiguousarray(np.concatenate([q[:, 0:6, :], q[:, 6:12, :]], axis=0))
        mp["qmT"] = np.ascontiguousarray(fm[:, 20:24, :])
        mp["gates"] = np.ascontiguousarray(tms[c][:, 256:292])
        mp["kcrT2"] = gather_seq([to2(f, 12) for f in fms], 1)
        mp["vcrT2"] = gather_seq([to2(f, 14) for f in fms], 1)
        mp["ksT2"] = gather_seq([to2(f, 16) for f in fms], 1)
        mp["kwT2"] = gather_seq([to2(f, 18) for f in fms], 1)
        mp["vs"] = gather_seq([np.ascontiguousarray(t_[:, 0:128]) for t_ in tms], 0)
        mp["vw"] = gather_seq([np.ascontiguousarray(t_[:, 128:256]) for t_ in tms], 0)
        mp["w1k"] = inputs["nsa_cmp_k_w1"][0]; mp["b1k"] = inputs["nsa_cmp_k_b1"][0].reshape(256, 1)
        mp["w2k"] = inputs["nsa_cmp_k_w2"][0]; mp["b2k"] = inputs["nsa_cmp_k_b2"][0].reshape(64, 1)
        mp["w1v"] = inputs["nsa_cmp_v_w1"][0]; mp["b1v"] = inputs["nsa_cmp_v_b1"][0].reshape(256, 1)
        mp["w2v"] = inputs["nsa_cmp_v_w2"][0]; mp["b2v"] = inputs["nsa_cmp_v_b2"][0].reshape(1, 64)
        mp["posk"] = inputs["nsa_cmp_pos_k"][0]; mp["posv"] = inputs["nsa_cmp_pos_v"][0]
        mp["mem"] = inputs["mem"][b]; mp["g_mem"] = inputs["norm_mem"][0:1]; mp["w_mem_kv"] = inputs["w_mem_kv"][0]
        mp["w_out"] = inputs["w_out"][0]
        maps.append(mp)
    return kb.run(maps)


def scatter_tokens(per_core, key):
    out = np.zeros((2, T, D), np.float32)
    for core in range(8):
        b, c = core // 4, core % 4
        out[b][core_tokens(c)] = np.asarray(per_core[core][key])
    return out


def kernel_unfused(**inputs):
    inputs = {k: np.asarray(v) for k, v in inputs.items()}
    resA = run_A(inputs)
    resB = run_B(inputs, resA)
    resF = run_F(inputs, [r["hmid"] for r in resB], 0, False)
    resC = run_C(inputs, resF)
    resG = run_F(inputs, [r["hmid"] for r in resC], 1, True)
    return scatter_tokens(resG, "h")


def build_F(final, nproj, kb=None):
    kb = kb or K("F")
    nc, P = kb.nc, kb.P
    hm_d = kb.din("hmid", [TOK, D])
    id_d = kb.din("identb", [128, 128], BF16)
    g_d = kb.din("g_ffn", [1, D])
    wg_d = kb.din("wg", [D, DFF]); wu_d = kb.din("wu", [D, DFF]); wd_d = kb.din("wd", [DFF, D])
    g2_d = kb.din("g2", [1, D])
    h_d = kb.dout("h", [TOK, D])
    if not final:
        win_d = kb.din("w_in2", [D, nproj])
        pr_d = kb.dout("pr", [TOK, nproj])
    P.dma("sp", kb.ident[:], id_d, writes=[kb.identb])
    kb.mk_eps()
    g, gb = kb.bcast_row(g_d, D, "gffn")
    g2, g2b = kb.bcast_row(g2_d, D, "g2")
    H, Hb = kb.sb("H", [128, NT, D], F32)
    Hbs = [Buf("H%d" % i) for i in range(NT)]
    hnT, hnTb = kb.sb("hnT", [128, 8, TOK], BF16)
    hn, hnb = kb.sb("hn", [128, D], BF16)
    scr, scrb = kb.sb("scr", [128, 4], F32)

    def norm_T(gt, gtb):
        for ti in range(NT):
            kb.rmsnorm(H[:, ti, :], Hbs[ti], gt[:], gtb, hn[:], hnb, D, scr, scrb)
            pt = kb.pt[kb.pti % 2]; ptb = kb.ptb[kb.pti % 2]; kb.pti += 1
            for j in range(8):
                P.op("pe", lambda j=j: nc.tensor.transpose(pt[:, j * 128:(j + 1) * 128], hn[:, j * 128:(j + 1) * 128], kb.ident[:]),
                     reads=[hnb, kb.identb], writes=[ptb], sig=(j == 7))
            P.op("dve", lambda: nc.vector.tensor_copy(out=hnT[:, :, ti * 128:(ti + 1) * 128], in_=pt[:, :].rearrange("p (a b) -> p a b", b=128)), reads=[ptb], writes=[hnTb])

    for ti in range(NT):
        P.dma(kb.dq(), H[:, ti, :], hm_d[ti * 128:(ti + 1) * 128, :], writes=[Hbs[ti]])
    norm_T(g, gb)
    wbuf = [(kb.sb("wgs", [128, 8, 512], BF16), kb.sb("wus", [128, 8, 512], BF16), kb.sb("wds", [128, 4, D], BF16)) for _ in range(2)]
    stg = [kb.sb("stg", [128, 1024], F32) for _ in range(3)]
    sgs = [kb.sb("sg", [128, 512], F32) for _ in range(2)]
    abs_ = [kb.sb("ab", [128, 512], BF16) for _ in range(2)]
    aTs = [kb.sb("aT", [128, 4, 128], BF16) for _ in range(2)]
    si = 0

    def load_fg(fg):
        nonlocal si
        (wgs, wgsb), (wus, wusb), (wds, wdsb) = wbuf[fg % 2]
        c0 = fg * 512
        cw = min(512, DFF - c0)
        nch = cw // 128
        for (wsrc, wdst, wdstb) in ((wg_d, wgs, wgsb), (wu_d, wus, wusb)):
            for k in range(8):
                st, stb = stg[si % 3]; si += 1
                P.dma(kb.dq(), st[:, 0:cw], wsrc[k * 128:(k + 1) * 128, c0:c0 + cw], writes=[stb])
                kb.cast("pool", wdst[:, k, 0:cw], st[:, 0:cw], [stb], [wdstb])
        for ch in range(nch):
            st, stb = stg[si % 3]; si += 1
            P.dma(kb.dq(), st[:], wd_d[c0 + ch * 128:c0 + (ch + 1) * 128, :], writes=[stb])
            kb.cast("pool", wds[:, ch, :], st[:], [stb], [wdsb])

    def stage1(fg, ti):
        (wgs, wgsb), (wus, wusb), _ = wbuf[fg % 2]
        cw = min(512, DFF - fg * 512)
        tsl = slice(ti * 128, (ti + 1) * 128)
        b0 = 0 if ti % 2 == 0 else 4
        pg, pgb = kb.ps[b0], kb.psb[b0]
        pu, pub = kb.ps[b0 + 1], kb.psb[b0 + 1]
        sg, sgb = sgs[ti % 2]
        ab, abb = abs_[ti % 2]
        for k in range(8):
            mm(kb, pg[:, 0:cw], hnT[:, k, tsl], wgs[:, k, 0:cw], k == 0, [hnTb, wgsb], [pgb], sig=(k == 7))
        for k in range(8):
            mm(kb, pu[:, 0:cw], hnT[:, k, tsl], wus[:, k, 0:cw], k == 0, [hnTb, wusb], [pub], sig=(k == 7))
        P.op("act", lambda: nc.scalar.activation(out=sg[:, 0:cw], in_=pg[:, 0:cw], func=AF.Silu), reads=[pgb], writes=[sgb])
        P.op("dve", lambda: nc.vector.tensor_tensor(out=ab[:, 0:cw], in0=sg[:, 0:cw], in1=pu[:, 0:cw], op=ALU.mult), reads=[sgb, pub], writes=[abb])

    def stage2(fg, ti):
        _, _, (wds, wdsb) = wbuf[fg % 2]
        cw = min(512, DFF - fg * 512)
        nch = cw // 128
        ab, abb = abs_[ti % 2]
        aT, aTb = aTs[ti % 2]
        pt = kb.pt[kb.pti % 2]; ptb = kb.ptb[kb.pti % 2]; kb.pti += 1
        for j in range(nch):
            P.op("pe", lambda j=j: nc.tensor.transpose(pt[:, j * 128:(j + 1) * 128], ab[:, j * 128:(j + 1) * 128], kb.ident[:]),
                 reads=[abb, kb.identb], writes=[ptb], sig=(j == nch - 1))
        P.op("act", lambda: nc.scalar.copy(out=aT[:, 0:nch, :], in_=pt[:, 0:nch * 128].rearrange("p (a b) -> p a b", b=128)), reads=[ptb], writes=[aTb])
        for half in range(2):
            py, pyb = kb.ps[2 + half], kb.psb[2 + half]
            for ch in range(nch):
                mm(kb, py[:, :], aT[:, ch, :], wds[:, ch, half * 512:(half + 1) * 512], ch == 0, [aTb, wdsb], [pyb], sig=(ch == nch - 1))
            P.op("dve", lambda: nc.vector.tensor_tensor(out=H[:, ti, half * 512:(half + 1) * 512], in0=py[:, :], in1=H[:, ti, half * 512:(half + 1) * 512], op=ALU.add),
                 reads=[pyb, Hbs[ti]], writes=[Hbs[ti]])

    load_fg(0)
    for fg in range(6):
        if fg + 1 < 6:
            load_fg(fg + 1)
        stage1(fg, 0)
        for ti in range(NT):
            if ti + 1 < NT:
                stage1(fg, ti + 1)
            stage2(fg, ti)
    if final:
        o, ob = kb.sb("o", [128, D], F32)
        for ti in range(NT):
            kb.rmsnorm(H[:, ti, :], Hbs[ti], g2[:], g2b, o[:], ob, D, scr, scrb)
            P.dma(kb.dq(), h_d[ti * 128:(ti + 1) * 128, :], o[:], reads=[ob], final=True)
    else:
        for ti in range(NT):
            P.dma(kb.dq(), h_d[ti * 128:(ti + 1) * 128, :], H[:, ti, :], reads=[Hbs[ti]], final=True)
        norm_T(g2, g2b)
        win, winb = kb.sb("win", [128, 8, nproj], BF16)
        for k in range(8):
            st, stb = stg[si % 3]; si += 1
            P.dma(kb.dq(), st[:, 0:nproj], win_d[k * 128:(k + 1) * 128, :], writes=[stb])
            kb.cast("pool" if k % 2 else "act", win[:, k, :], st[:, 0:nproj], [stb], [winb])
        pro, prob = kb.sb("pro", [128, nproj], F32)
        for ti in range(NT):
            tsl = slice(ti * 128, (ti + 1) * 128)
            o0 = 0
            bi = 0
            while o0 < nproj:
                n = min(512, nproj - o0)
                ps, psb = kb.ps[bi % 2], kb.psb[bi % 2]
                for k in range(8):
                    mm(kb, ps[:, 0:n], hnT[:, k, tsl], win[:, k, o0:o0 + n], k == 0, [hnTb, winb], [psb], sig=(k == 7))
                P.op("act", lambda: nc.scalar.copy(out=pro[:, o0:o0 + n], in_=ps[:, 0:n]), reads=[psb], writes=[prob])
                o0 += n
                bi += 1
            P.dma(kb.dq(), pr_d[tsl, :], pro[:], reads=[prob], final=True)
    return kb


def run_F(inputs, hmids, layer, final):
    kb = build_F(final, 712)
    ident = np.eye(128, dtype=np.float32).astype(NPBF)
    maps = []
    for core in range(8):
        mp = {"hmid": np.asarray(hmids[core]), "identb": ident, "g_ffn": inputs["norm_ffn"][layer:layer + 1],
              "wg": inputs["ffn_gate"][layer], "wu": inputs["ffn_up"][layer], "wd": inputs["ffn_down"][layer]}
        if final:
            mp["g2"] = inputs["norm_final"].reshape(1, D)
        else:
            mp["g2"] = inputs["norm_mix"][layer + 1:layer + 2]
            mp["w_in2"] = inputs["dsa_w_in"][0]
        maps.append(mp)
    return kb.run(maps)


NIT = 16


def mem_setup(kb, mem_d, gmem_d, wkv_d):
    nc, P = kb.nc, kb.P
    kmT, kmTb = kb.sb("kmT", [64, 4, 256], BF16)
    vm1, vm1b = kb.sb("vm1", [128, 2, 4, 65], BF16)
    P.op("pool", lambda: nc.gpsimd.memset(vm1[:], 1.0), writes=[vm1b])
    with kb.scope():
        gm, gmb = kb.bcast_row(gmem_d, D, "gmem")
        wkv, wkvb = kb.sb("wkv", [128, 8, 512], BF16)
        mt, mtb = kb.sb("mt", [128, D], F32)
        mn, mnb = kb.sb("mn", [128, D], BF16)
        mnT, mnTb = kb.sb("mnT", [128, 8, 256], BF16)
        scr, scrb = kb.sb("mscr", [128, 4], F32)
        wst = [kb.sb("wst2", [128, 512], F32) for _ in range(2)]
        for k in range(8):
            st, stb = wst[k % 2]
            P.dma(kb.dq(), st[:], wkv_d[k * 128:(k + 1) * 128, :], writes=[stb])
            P.op("dve", lambda: nc.vector.tensor_copy(out=wkv[:, k, :], in_=st[:]), reads=[stb], writes=[wkvb])
        for tl in range(2):
            P.dma("sp", mt[:], mem_d[tl * 128:(tl + 1) * 128, :], writes=[mtb])
            kb.rmsnorm(mt[:], mtb, gm[:], gmb, mn[:], mnb, D, scr, scrb)
            pt = kb.pt[kb.pti % 2]; ptb = kb.ptb[kb.pti % 2]; kb.pti += 1
            for j in range(8):
                P.op("pe", lambda j=j: nc.tensor.transpose(pt[:, j * 128:(j + 1) * 128], mn[:, j * 128:(j + 1) * 128], kb.ident[:]),
                     reads=[mnb, kb.identb], writes=[ptb], sig=(j == 7))
            P.op("dve", lambda: nc.vector.tensor_copy(out=mnT[:, :, tl * 128:(tl + 1) * 128], in_=pt[:, :].rearrange("p (a b) -> p a b", b=128)), reads=[ptb], writes=[mnTb])
        for h in range(4):
            ps, psb = kb.ps[2], kb.psb[2]
            for k in range(8):
                mm(kb, ps[0:64, 0:256], wkv[:, k, h * 64:(h + 1) * 64], mnT[:, k, :], k == 0, [wkvb, mnTb], [psb], sig=(k == 7))
            P.op("dve", lambda: nc.vector.tensor_copy(out=kmT[:, h, :], in_=ps[0:64, 0:256]), reads=[psb], writes=[kmTb])
        for tl in range(2):
            ps, psb = kb.ps[3], kb.psb[3]
            for k in range(8):
                mm(kb, ps[:, 0:256], mnT[:, k, tl * 128:(tl + 1) * 128], wkv[:, k, 256:512], k == 0, [wkvb, mnTb], [psb], sig=(k == 7))
            P.op("dve", lambda: nc.vector.tensor_copy(out=vm1[:, tl, :, 0:64], in_=ps[:, 0:256].rearrange("p (h d) -> p h d", d=64)), reads=[psb], writes=[vm1b])
    return kmT, kmTb, vm1, vm1b


def load_wout(kb, wout_d):
    nc, P = kb.nc, kb.P
    wout, woutb = kb.sb("wout", [128, 8, D], BF16)
    with kb.scope():
        wst = [kb.sb("wst3", [128, 1024], F32) for _ in range(2)]
        for k in range(8):
            st, stb = wst[k % 2]
            P.dma(kb.dq(), st[:], wout_d[k * 128:(k + 1) * 128, :], writes=[stb])
            P.op("dve", lambda: nc.vector.tensor_copy(out=wout[:, k, :], in_=st[:]), reads=[stb], writes=[woutb])
    return wout, woutb


def rms_small(kb, x, xb, A, d, g, gb, out, outb, tmp, tmpb, ss, ssb):
    nc, P = kb.nc, kb.P
    P.op("dve", lambda: nc.vector.tensor_tensor(out=tmp, in0=x, in1=x, op=ALU.mult), reads=[xb], writes=[tmpb])
    P.op("dve", lambda: nc.vector.tensor_reduce(out=ss[:, 0:A], in_=tmp, axis=AX.X, op=ALU.add), reads=[tmpb], writes=[ssb])
    P.op("act", lambda: nc.scalar.activation(out=ss[:, A:2 * A], in_=ss[:, 0:A], func=AF.Ln, scale=1.0 / d, bias=kb.eps_t[:, 0:1]), reads=[ssb, kb.eps_b], writes=[ssb])
    P.op("act", lambda: nc.scalar.activation(out=ss[:, 0:A], in_=ss[:, A:2 * A], func=AF.Exp, scale=-0.5), reads=[ssb], writes=[ssb])
    r = ss[:, 0:A]
    rb_ = AP(tensor=r.tensor, offset=r.offset, ap=[list(r.ap[0]), list(r.ap[1]), [0, d]])
    g2 = g[:, 0:d]
    gb_ = AP(tensor=g2.tensor, offset=g2.offset, ap=[list(g2.ap[0]), [0, A], list(g2.ap[1])])
    P.op("dve", lambda: nc.vector.tensor_tensor(out=tmp, in0=x, in1=rb_, op=ALU.mult), reads=[xb, ssb], writes=[tmpb])
    P.op("dve", lambda: nc.vector.tensor_tensor(out=out, in0=tmp, in1=gb_, op=ALU.mult), reads=[tmpb, gb], writes=[outb])


def build_C(stage=99, nslots=NT, kb=None):
    kb = kb or K("C")
    nc, P = kb.nc, kb.P
    x_d = kb.din("x", [TOK, D])
    pr_d = kb.din("pr", [TOK, 712])
    ckv_d = kb.din("ckv_seq", [T, 128])
    kidx_d = kb.din("kidx_seq", [T, 64])
    id_d = kb.din("identb", [128, 128], BF16)
    an_d = kb.din("antib", [128, 128], BF16)
    relb_d = kb.din("rel_bias", [32, 12])
    oh_s_d = kb.din("oh_s", [33, Y_SLC])
    cm_d = kb.din("cm", [128, 512])
    pw_d = kb.din("pw", [128, NIT])
    qn_d = kb.din("q_norm", [1, 256]); kvn_d = kb.din("kv_norm", [1, 128]); kin_d = kb.din("kidx_norm", [1, 64])
    wqup_d = kb.din("w_q_up", [256, 768]); wuk_d = kb.din("w_uk", [128, 768]); wuv_d = kb.din("w_uv", [128, 768])
    wqi_d = kb.din("w_q_idx", [256, 512])
    mem_d = kb.din("mem", [256, D]); gmem_d = kb.din("g_mem", [1, D]); wkv_d = kb.din("w_mem_kv", [D, 512])
    wout_d = kb.din("w_out", [D, D])
    hmid_d = kb.dout("hmid", [TOK, D])

    P.dma("sp", kb.ident[:], id_d, writes=[kb.identb])
    P.dma("sp", kb.anti[:], an_d, writes=[kb.antib])
    kb.mk_eps()
    with kb.scope():
        tabs = build_tables(kb, relb_d, [oh_s_d], [("s", Y_SLC)])
    band_s, band_sb = load_band(kb, tabs[0][0], tabs[0][1], Y_SLC, 2048, 1, "band_s")
    ckvT, ckvTb = kb.sb("ckvT", [128, T], BF16)
    ckv1, ckv1b = kb.sb("ckv1", [128, 64, 129], BF16)
    kidxT, kidxTb = kb.sb("kidxT", [64, T], BF16)
    P.op("pool", lambda: nc.gpsimd.memset(ckv1[:], 1.0), writes=[ckv1b])
    gq, gqb = kb.bcast_row(qn_d, 256, "gq")
    with kb.scope():
        gkv, gkvb = kb.bcast_row(kvn_d, 128, "gkv")
        gki, gkib = kb.bcast_row(kin_d, 64, "gki")
        st, stb = kb.sb("kst", [128, 8, 128], F32)
        tmp, tmpb = kb.sb("ktmp", [128, 8, 128], F32)
        ss, ssb = kb.sb("kss", [128, 16], F32)
        kin, kinb = kb.sb("kin", [128, 8, 64], BF16)
        for i in range(8):
            P.dma(kb.dq(), st[:], ckv_d[i * 1024:(i + 1) * 1024, :].rearrange("(a p) c -> p a c", p=128), writes=[stb])
            rms_small(kb, st[:], stb, 8, 128, gkv, gkvb, ckv1[:, i * 8:(i + 1) * 8, 0:128], ckv1b, tmp[:], tmpb, ss, ssb)
            pt = kb.pt[kb.pti % 2]; ptb = kb.ptb[kb.pti % 2]; kb.pti += 1
            for j in range(8):
                P.op("pe", lambda j=j: nc.tensor.transpose(pt[:, j * 128:(j + 1) * 128], ckv1[:, i * 8 + j, 0:128], kb.ident[:]),
                     reads=[ckv1b, kb.identb], writes=[ptb], sig=(j == 7))
            P.op("act", lambda: nc.scalar.copy(out=ckvT[:, i * 1024:(i + 1) * 1024], in_=pt[:, :]), reads=[ptb], writes=[ckvTb])
        for i in range(8):
            P.dma(kb.dq(), st[:, :, 0:64], kidx_d[i * 1024:(i + 1) * 1024, :].rearrange("(a p) c -> p a c", p=128), writes=[stb])
            rms_small(kb, st[:, :, 0:64], stb, 8, 64, gki, gkib, kin[:], kinb, tmp[:, :, 0:64], tmpb, ss, ssb)
            pt = kb.pt[kb.pti % 2]; ptb = kb.ptb[kb.pti % 2]; kb.pti += 1
            for j in range(8):
                P.op("pe", lambda j=j: nc.tensor.transpose(pt[0:64, j * 128:(j + 1) * 128], kin[:, j, :], kb.ident[:]),
                     reads=[kinb, kb.identb], writes=[ptb], sig=(j == 7))
            P.op("act", lambda: nc.scalar.copy(out=kidxT[:, i * 1024:(i + 1) * 1024], in_=pt[0:64, :]), reads=[ptb], writes=[kidxTb])
    wqup, wqupb = kb.sb("wqup", [128, 2, 768], BF16)
    wqi, wqib = kb.sb("wqi", [128, 2, 512], BF16)
    wuv, wuvb = kb.sb("wuv", [128, 768], BF16)
    wukT, wukTb = kb.sb("wukT", [64, 12, 128], BF16)
    with kb.scope():
        wst = [kb.sb("wst4", [128, 768], F32) for _ in range(2)]
        wukb, wukbb = kb.sb("wukb", [128, 768], BF16)
        i = 0
        for (src, rows, ncol, dstf) in [(wqup_d, 0, 768, lambda: wqup[:, 0, :]), (wqup_d, 128, 768, lambda: wqup[:, 1, :]),
                                        (wqi_d, 0, 512, lambda: wqi[:, 0, :]), (wqi_d, 128, 512, lambda: wqi[:, 1, :]),
                                        (wuv_d, 0, 768, lambda: wuv[:]), (wuk_d, 0, 768, lambda: wukb[:])]:
            st, stb = wst[i % 2]; i += 1
            P.dma(kb.dq(), st[:, 0:ncol], src[rows:rows + 128, :], writes=[stb])
            dst = dstf()
            P.op("dve", lambda: nc.vector.tensor_copy(out=dst, in_=st[:, 0:ncol]), reads=[stb], writes=[wqupb, wqib, wuvb, wukbb])
        for h0 in (0, 8):
            nb = min(8, 12 - h0)
            pt = kb.pt[kb.pti % 2]; ptb = kb.ptb[kb.pti % 2]; kb.pti += 1
            for j in range(nb):
                P.op("pe", lambda j=j: nc.tensor.transpose(pt[0:64, j * 128:(j + 1) * 128], wukb[:, (h0 + j) * 64:(h0 + j + 1) * 64], kb.ident[:]),
                     reads=[wukbb, kb.identb], writes=[ptb], sig=(j == nb - 1))
            P.op("dve", lambda: nc.vector.tensor_copy(out=wukT[:, h0:h0 + nb, :], in_=pt[0:64, 0:nb * 128].rearrange("p (a b) -> p a b", b=128)), reads=[ptb], writes=[wukTb])
    kmT, kmTb, vm1, vm1b = mem_setup(kb, mem_d, gmem_d, wkv_d)
    wout, woutb = load_wout(kb, wout_d)
    cm, cmb = kb.sb("cm", [128, 512], F32)
    P.dma("sp", cm[:], cm_d, writes=[cmb])
    pw, pwb = kb.sb("pw", [128, NIT], F32)
    P.dma("sp", pw[:], pw_d, writes=[pwb])

    at = Attn(kb)
    score, scoreb = kb.sb("score", [128, T], F32)
    mb, mbb = kb.sb("mb", [128, T], BF16)
    mTs = [kb.sb("mT", [128, 8, 128], BF16) for _ in range(2)]
    big, bigb = kb.sb("big", [128, D], F32)
    cqn, cqnb = kb.sb("cqn", [128, 256], BF16)
    cqT, cqTb = kb.sb("cqT", [128, 2, 128], BF16)
    qhT, qhTb = kb.sb("qhT", [128, 12, 128], BF16)
    qabs, qabsb = kb.sb("qabs", [128, 12, 128], BF16)
    qiT, qiTb = kb.sb("qiT", [64, 8, 128], BF16)
    qmb16, qmb16b = kb.sb("qmb16", [128, 256], BF16)
    qmT, qmTb = kb.sb("qmT", [64, 4, 128], BF16)
    rts = [kb.sb("rt", [128, 512], F32) for _ in range(2)]
    catbf, catbfb = kb.sb("catbf", [128, D], BF16)
    catT, catTb = kb.sb("catT", [128, 8, 128], BF16)
    pm, pmb = kb.sb("pm", [128, 256], BF16)
    sm, smb = kb.sb("sm", [128, 16], F32)
    wv, wvb = kb.sb("wv", [128, 32], F32)
    bs, bsb = kb.sb("bs", [128, 8 + 2 * NIT], F32)
    accbank = 2

    def next_acc():
        nonlocal accbank
        b = accbank
        accbank = 2 + (accbank - 2 + 1) % 4
        return b

    for m in range(nslots):
        tsl = slice(m * 128, (m + 1) * 128)
        L = 128 * (4 * m + 4)
        nkt = 4 * m + 4
        P.dma("sp", big[:, 0:712], pr_d[tsl, :], writes=[bigb])
        if stage == 0:
            P.dma("sp", hmid_d[tsl, :], big[:], reads=[bigb], final=True)
            continue
        ssq = wv[:, 16:18]
        P.op("act", lambda: nc.scalar.activation(out=cqn[:], in_=big[:, 0:256], func=AF.Square, accum_out=wv[:, 16:17]), reads=[bigb], writes=[cqnb, wvb])
        P.op("act", lambda: nc.scalar.activation(out=wv[:, 17:18], in_=wv[:, 16:17], func=AF.Ln, scale=1.0 / 256, bias=kb.eps_t[:, 0:1]), reads=[wvb, kb.eps_b], writes=[wvb])
        P.op("act", lambda: nc.scalar.activation(out=wv[:, 18:19], in_=wv[:, 17:18], func=AF.Exp, scale=-0.5), reads=[wvb], writes=[wvb])
        P.op("dve", lambda: nc.vector.scalar_tensor_tensor(out=cqn[:], in0=big[:, 0:256], scalar=wv[:, 18:19], in1=gq[:], op0=ALU.mult, op1=ALU.mult), reads=[bigb, wvb, gqb], writes=[cqnb])
        pt = kb.pt[kb.pti % 2]; ptb = kb.ptb[kb.pti % 2]; kb.pti += 1
        for j in range(2):
            P.op("pe", lambda j=j: nc.tensor.transpose(pt[:, j * 128:(j + 1) * 128], cqn[:, j * 128:(j + 1) * 128], kb.ident[:]), reads=[cqnb, kb.identb], writes=[ptb], sig=(j == 1))
        P.op("dve", lambda: nc.vector.tensor_copy(out=cqT[:], in_=pt[:, 0:256].rearrange("p (a b) -> p a b", b=128)), reads=[ptb], writes=[cqTb])
        for b4 in range(3):
            ps, psb = kb.ps[b4 % 2], kb.psb[b4 % 2]
            for hh in range(4):
                h = 4 * b4 + hh
                for c in range(2):
                    mm(kb, ps[0:64, hh * 128:(hh + 1) * 128], wqup[:, c, h * 64:(h + 1) * 64], cqT[:, c, :], hh == 0 and c == 0, [wqupb, cqTb], [psb], sig=(hh == 3 and c == 1))
            P.op("act", lambda: nc.scalar.activation(out=qhT[0:64, 4 * b4:4 * b4 + 4, :], in_=ps[0:64, :].rearrange("p (a b) -> p a b", b=128), func=AF.Copy, scale=0.125), reads=[psb], writes=[qhTb])
        for b4 in range(2):
            ps, psb = kb.ps[b4 % 2], kb.psb[b4 % 2]
            for hh in range(4):
                h = 4 * b4 + hh
                for c in range(2):
                    mm(kb, ps[0:64, hh * 128:(hh + 1) * 128], wqi[:, c, h * 64:(h + 1) * 64], cqT[:, c, :], hh == 0 and c == 0, [wqib, cqTb], [psb], sig=(hh == 3 and c == 1))
            P.op("dve", lambda: nc.vector.tensor_copy(out=qiT[:, 4 * b4:4 * b4 + 4, :], in_=ps[0:64, :].rearrange("p (a b) -> p a b", b=128)), reads=[psb], writes=[qiTb])
        for b4 in range(3):
            ps, psb = kb.ps[b4 % 2], kb.psb[b4 % 2]
            for hh in range(4):
                h = 4 * b4 + hh
                mm(kb, ps[:, hh * 128:(hh + 1) * 128], wukT[:, h, :], qhT[0:64, h, :], hh == 0, [wukTb, qhTb], [psb], sig=(hh == 3))
            P.op("dve", lambda: nc.vector.tensor_copy(out=qabs[:, 4 * b4:4 * b4 + 4, :], in_=ps[:, :].rearrange("p (a b) -> p a b", b=128)), reads=[psb], writes=[qabsb])
        P.op("dve", lambda: nc.vector.tensor_scalar(out=wv[:, 0:8], in0=big[:, 448:456], scalar1=0.04419417382415922, scalar2=None, op0=ALU.mult), reads=[bigb], writes=[wvb])
        P.op("dve", lambda: nc.vector.tensor_scalar(out=wv[:, 8:16], in0=wv[:, 0:8], scalar1=0.0, scalar2=2.0, op0=ALU.is_ge, op1=ALU.mult), reads=[wvb], writes=[wvb])
        P.op("dve", lambda: nc.vector.tensor_scalar(out=wv[:, 8:16], in0=wv[:, 8:16], scalar1=-1.0, scalar2=None, op0=ALU.add), reads=[wvb], writes=[wvb])
        P.op("dve", lambda: nc.vector.tensor_tensor(out=wv[:, 0:8], in0=wv[:, 0:8], in1=wv[:, 8:16], op=ALU.mult), reads=[wvb], writes=[wvb])
        P.op("act", lambda: nc.scalar.activation(out=qmb16[:], in_=big[:, 456:712], func=AF.Copy, scale=0.125), reads=[bigb], writes=[qmb16b])
        pt = kb.pt[kb.pti % 2]; ptb = kb.ptb[kb.pti % 2]; kb.pti += 1
        for j in range(4):
            P.op("pe", lambda j=j: nc.tensor.transpose(pt[0:64, j * 128:(j + 1) * 128], qmb16[:, j * 64:(j + 1) * 64], kb.ident[:]), reads=[qmb16b, kb.identb], writes=[ptb], sig=(j == 3))
        P.op("dve", lambda: nc.vector.tensor_copy(out=qmT[:], in_=pt[0:64, 0:512].rearrange("p (a b) -> p a b", b=128)), reads=[ptb], writes=[qmTb])
        P.dma("pool", big[:], x_d[tsl, :], reads=[], writes=[bigb])
        if stage == 1:
            P.dma("sp", hmid_d[tsl, :], big[:], reads=[bigb], final=True)
            continue
        for kc in range(m + 1):
            csl = slice(kc * 512, (kc + 1) * 512)
            for h in range(8):
                mb_i = [0, 1, 6, 5][h % 4]
                rb_i = [2, 3, 4][(kc * 8 + h) % 3]
                ps, psb = kb.ps[mb_i], kb.psb[mb_i]
                rp, rpb = kb.ps[rb_i], kb.psb[rb_i]
                mm(kb, ps[:, :], qiT[:, h, :], kidxT[:, csl], True, [qiTb, kidxTb], [psb], sig=True)
                P.op("act", lambda: nc.scalar.activation(out=rp[:, :], in_=ps[:, :], func=AF.Relu, scale=wv[:, h:h + 1]), reads=[psb, wvb], writes=[rpb])
                if h == 0:
                    P.op("dve", lambda: nc.vector.tensor_scalar(out=score[:, csl], in0=rp[:, :], scalar1=wv[:, 8:9], scalar2=None, op0=ALU.mult), reads=[rpb, wvb], writes=[scoreb])
                else:
                    P.op("dve", lambda: nc.vector.scalar_tensor_tensor(out=score[:, csl], in0=rp[:, :], scalar=wv[:, 8 + h:9 + h], in1=score[:, csl], op0=ALU.mult, op1=ALU.add), reads=[rpb, wvb, scoreb], writes=[scoreb])
        if stage == 2:
            P.dma("sp", hmid_d[tsl, 0:512], score[:, 0:512], reads=[scoreb], final=True)
            continue
        P.op("dve", lambda: nc.vector.tensor_reduce(out=bs[:, 0:1], in_=score[:, 0:L], axis=AX.X, op=ALU.min), reads=[scoreb], writes=[bsb])
        P.op("dve", lambda: nc.vector.tensor_reduce(out=bs[:, 1:2], in_=score[:, 0:L], axis=AX.X, op=ALU.max), reads=[scoreb], writes=[bsb])
        P.op("dve", lambda: nc.vector.tensor_tensor(out=score[:, L - 512:L], in0=score[:, L - 512:L], in1=cm[:], op=ALU.add), reads=[scoreb, cmb], writes=[scoreb])
        P.op("dve", lambda: nc.vector.tensor_tensor(out=bs[:, 2:3], in0=bs[:, 1:2], in1=bs[:, 0:1], op=ALU.subtract), reads=[bsb], writes=[bsb])
        P.op("dve", lambda: nc.vector.tensor_scalar(out=bs[:, 8:8 + NIT], in0=pw[:], scalar1=bs[:, 2:3], scalar2=None, op0=ALU.mult), reads=[bsb, pwb], writes=[bsb])
        for it in range(NIT):
            hw = bs[:, 8 + it:9 + it]
            P.op("dve", lambda: nc.vector.tensor_tensor(out=bs[:, 3:4], in0=bs[:, 0:1], in1=hw, op=ALU.add), reads=[bsb], writes=[bsb])
            P.op("dve", lambda: nc.vector.tensor_scalar(out=mb[:, 0:L], in0=score[:, 0:L], scalar1=bs[:, 3:4], scalar2=None, op0=ALU.is_ge, op1=ALU.add, accum_out=bs[:, 4:5]),
                 reads=[scoreb, bsb], writes=[mbb, bsb])
            P.op("dve", lambda: nc.vector.tensor_scalar(out=bs[:, 5:6], in0=bs[:, 4:5], scalar1=255.5, scalar2=hw, op0=ALU.is_ge, op1=ALU.mult), reads=[bsb], writes=[bsb])
            P.op("dve", lambda: nc.vector.tensor_tensor(out=bs[:, 0:1], in0=bs[:, 0:1], in1=bs[:, 5:6], op=ALU.add), reads=[bsb], writes=[bsb])
        P.op("dve", lambda: nc.vector.tensor_scalar(out=mb[:, 0:L], in0=score[:, 0:L], scalar1=bs[:, 0:1], scalar2=None, op0=ALU.is_ge), reads=[scoreb, bsb], writes=[mbb])
        if stage == 3:
            P.dma("sp", hmid_d[tsl, 0:512], score[:, 0:512], reads=[scoreb], final=True)
            P.dma("sp", hmid_d[tsl, 512:512 + 8 + 2 * NIT], bs[:], reads=[bsb], final=True)
            continue
        banks = [next_acc() for _ in range(4)]
        for g8 in range(0, nkt, 8):
            nb = min(8, nkt - g8)
            mT, mTb = mTs[(g8 // 8) % 2]
            pt = kb.pt[kb.pti % 2]; ptb = kb.ptb[kb.pti % 2]; kb.pti += 1
            for j in range(nb):
                P.op("pe", lambda j=j: nc.tensor.transpose(pt[:, j * 128:(j + 1) * 128], mb[:, (g8 + j) * 128:(g8 + j + 1) * 128], kb.ident[:]), reads=[mbb, kb.identb], writes=[ptb], sig=(j == nb - 1))
            P.op("dve", lambda: nc.vector.tensor_copy(out=mT[:, 0:nb, :], in_=pt[:, 0:nb * 128].rearrange("p (a b) -> p a b", b=128)), reads=[ptb], writes=[mTb])
            for j in range(nb):
                kt = g8 + j
                u = kt - 4 * m
                for tr in range(4):
                    bA = banks[tr]
                    accs = [kb.ps[bA][:, 129 * hh:129 * hh + 129] for hh in range(3)]
                    accb = [kb.psb[bA]] * 3
                    s_terms = [(ckvT[:, kt * 128:(kt + 1) * 128], qabs[:, 3 * tr:3 * tr + 3, :], [ckvTb, qabsb])]
                    near = None
                    if u >= -12:
                        z0 = 128 * (3 - u)
                        near = (kb.anti[:], band_s[:, 3 * tr:3 * tr + 3, z0:z0 + 128], [kb.antib, band_sb])
                    at.unit(s_terms, near, accs, accb, ckv1[:, kt, :], [ckv1b], [kt == 0, False, False], mul_mask=(bc3(mT[:, j, :]), [mTb]))
        at.flush()
        for tr in range(4):
            bA = banks[tr]
            for hh in range(3):
                h = 3 * tr + hh
                O = kb.ps[bA][:, 129 * hh:129 * hh + 129]
                P.op("dve", lambda: nc.vector.tensor_scalar(out=sm[:, 0:1], in0=O[:, 128:129], scalar1=1e-30, scalar2=None, op0=ALU.max), reads=[kb.psb[bA]], writes=[smb])
                P.op("dve", lambda: nc.vector.reciprocal(out=sm[:, 1:2], in_=sm[:, 0:1]), reads=[smb], writes=[smb])
                P.op("dve", lambda: nc.vector.tensor_scalar(out=qabs[:, h, :], in0=O[:, 0:128], scalar1=sm[:, 1:2], scalar2=None, op0=ALU.mult), reads=[kb.psb[bA], smb], writes=[qabsb])
        for h0 in (0, 8):
            nb = min(8, 12 - h0)
            pt = kb.pt[kb.pti % 2]; ptb = kb.ptb[kb.pti % 2]; kb.pti += 1
            for j in range(nb):
                P.op("pe", lambda j=j: nc.tensor.transpose(pt[:, j * 128:(j + 1) * 128], qabs[:, h0 + j, :], kb.ident[:]), reads=[qabsb, kb.identb], writes=[ptb], sig=(j == nb - 1))
            P.op("dve", lambda: nc.vector.tensor_copy(out=qhT[:, h0:h0 + nb, :], in_=pt[:, 0:nb * 128].rearrange("p (a b) -> p a b", b=128)), reads=[ptb], writes=[qhTb])
        for h0 in (0, 8):
            nb = min(8, 12 - h0)
            bA = next_acc()
            ps, psb = kb.ps[bA], kb.psb[bA]
            for j in range(nb):
                h = h0 + j
                mm(kb, ps[:, j * 64:(j + 1) * 64], qhT[:, h, :], wuv[:, h * 64:(h + 1) * 64], j == 0, [qhTb, wuvb], [psb], sig=(j == nb - 1))
            P.op("act", lambda: nc.scalar.copy(out=catbf[:, h0 * 64:(h0 + nb) * 64], in_=ps[:, 0:nb * 64]), reads=[psb], writes=[catbfb])
        for h in range(4):
            sbk = at.next_sbank()
            ps, psb = kb.ps[sbk], kb.psb[sbk]
            for tl in range(2):
                mm(kb, ps[:, tl * 128:(tl + 1) * 128], kmT[:, h, tl * 128:(tl + 1) * 128], qmT[:, h, :], tl == 0, [kmTb, qmTb], [psb], sig=(tl == 1))
            P.op("act", lambda: nc.scalar.activation(out=pm[:], in_=ps[:, 0:256], func=AF.Exp), reads=[psb], writes=[pmb])
            bA = next_acc()
            O = kb.ps[bA][:, 0:65]
            for tl in range(2):
                mm(kb, O, pm[:, tl * 128:(tl + 1) * 128], vm1[:, tl, h, :], tl == 0, [pmb, vm1b], [kb.psb[bA]], sig=(tl == 1))
            P.op("dve", lambda: nc.vector.reciprocal(out=sm[:, 4:5], in_=O[:, 64:65]), reads=[kb.psb[bA]], writes=[smb])
            P.op("dve", lambda: nc.vector.tensor_scalar(out=catbf[:, 768 + h * 64:768 + (h + 1) * 64], in0=O[:, 0:64], scalar1=sm[:, 4:5], scalar2=None, op0=ALU.mult), reads=[kb.psb[bA], smb], writes=[catbfb])
        pt = kb.pt[kb.pti % 2]; ptb = kb.ptb[kb.pti % 2]; kb.pti += 1
        for j in range(8):
            P.op("pe", lambda j=j: nc.tensor.transpose(pt[:, j * 128:(j + 1) * 128], catbf[:, j * 128:(j + 1) * 128], kb.ident[:]),
                 reads=[catbfb, kb.identb], writes=[ptb], sig=(j == 7))
        P.op("dve", lambda: nc.vector.tensor_copy(out=catT[:], in_=pt[:, :].rearrange("p (a b) -> p a b", b=128)), reads=[ptb], writes=[catTb])
        for half in range(2):
            bA = next_acc()
            ps, psb = kb.ps[bA], kb.psb[bA]
            for k in range(8):
                mm(kb, ps[:, :], catT[:, k, :], wout[:, k, half * 512:(half + 1) * 512], k == 0, [catTb, woutb], [psb], sig=(k == 7))
            P.op("dve", lambda: nc.vector.tensor_tensor(out=big[:, half * 512:(half + 1) * 512], in0=ps[:, :], in1=big[:, half * 512:(half + 1) * 512], op=ALU.add), reads=[psb, bigb], writes=[bigb])
        P.dma("sp", hmid_d[tsl, :], big[:], reads=[bigb], final=True)
    return kb


def run_C(inputs, resF, stage=99, nslots=NT):
    kb = build_C(stage, nslots)
    ident = np.eye(128, dtype=np.float32)
    pw = np.tile((0.5 ** np.arange(1, NIT + 1)).astype(np.float32)[None, :], (128, 1))
    maps = []
    for core in range(8):
        b, c = core // 4, core % 4
        prs = [np.asarray(resF[4 * b + cc]["pr"]) for cc in range(4)]
        oh_s, _, _ = host_tables(c)
        z = np.arange(512)[None, :]
        q = np.arange(128)[:, None]
        cm = np.where(z <= 128 * c + q, 0.0, -1e30).astype(np.float32)
        mp = {"x": np.asarray(resF[core]["h"]), "pr": prs[c],
              "ckv_seq": gather_seq([np.ascontiguousarray(p[:, 256:384]) for p in prs], 0),
              "kidx_seq": gather_seq([np.ascontiguousarray(p[:, 384:448]) for p in prs], 0),
              "identb": ident.astype(NPBF), "antib": np.ascontiguousarray(ident[::-1]).astype(NPBF),
              "rel_bias": inputs["rel_bias"], "oh_s": oh_s, "cm": cm, "pw": pw,
              "q_norm": inputs["dsa_q_norm"][0:1], "kv_norm": inputs["dsa_kv_norm"][0:1], "kidx_norm": inputs["dsa_kidx_norm"][0:1],
              "w_q_up": inputs["dsa_w_q_up"][0], "w_uk": inputs["dsa_w_uk"][0].reshape(128, 768), "w_uv": inputs["dsa_w_uv"][0].reshape(128, 768),
              "w_q_idx": inputs["dsa_w_q_idx"][0],
              "mem": inputs["mem"][b], "g_mem": inputs["norm_mem"][1:2], "w_mem_kv": inputs["w_mem_kv"][1], "w_out": inputs["w_out"][1]}
        maps.append(mp)
    return kb.run(maps)


GROUPS = [[0, 1, 2, 3], [4, 5, 6, 7]]


def all_gather(kb, in_ap2d, out_ap2d):
    nc, P = kb.nc, kb.P
    P.barrier()
    kb.n += 1
    key = "cc%d" % kb.n
    sem = P.es.enter_context(nc.semaphore(key))
    P.sem[key] = sem
    P.cnt[key] = 0
    with nc.Block() as block:
        @block.gpsimd
        def _(g):
            g.collective_compute("AllGather", ALU.bypass, replica_groups=GROUPS, ins=[in_ap2d.opt()], outs=[out_ap2d.opt()]).then_inc(sem)
            g.wait_ge(sem, 1)
    P.cnt[key] = 1
    P.seen["pool"][key] = 1
    for e in P.eng:
        P._need(e, (key, 1))


def seq_rows(all_t, ncols_total, col0, ncol, m0, nm):
    return AP(tensor=all_t, offset=m0 * 128 * ncols_total + col0,
              ap=[[128 * ncols_total, nm], [2048 * ncols_total, 4], [ncols_total, 128], [1, ncol]])


def build_fused():
    kb = K("fused")
    nc, P = kb.nc, kb.P
    ext = {}

    def E(name, shape, dt=F32):
        ext[name] = nc.dram_tensor(name, list(shape), dt, kind="ExternalInput").ap()
        return ext[name]

    def S(name, shape, dt=F32):
        return nc.dram_tensor(name, list(shape), dt)

    x = E("x", [TOK, D])
    out = nc.dram_tensor("out", [TOK, D], F32, kind="ExternalOutput").ap()
    nm = {k: E(k, [1, D]) for k in ("norm_mix0", "norm_mix1", "norm_ffn0", "norm_ffn1", "norm_mem0", "norm_mem1", "norm_final")}
    nsa_w_in = E("nsa_w_in", [D, 1828]); gate_b = E("gate_b", [1, 36])
    ident = E("ident", [128, 128]); anti = E("anti", [128, 128])
    identb = E("identb", [128, 128], BF16); antib = E("antib", [128, 128], BF16)
    rel_bias = E("rel_bias", [32, 12])
    oh_s = E("oh_s", [33, Y_SLC]); oh_w = E("oh_w", [33, Y_WIN]); oh_c = E("oh_c", [33, Y_CMP])
    ew = E("ew", [128, T], BF16); ovl = E("ovl", [128, 4, 128], BF16); fb = E("fb", [NT, 128, 128])
    cmp_w = {k: E(k, shp) for k, shp in [("w1k", [2048, 256]), ("b1k", [256, 1]), ("w2k", [256, 64]), ("b2k", [64, 1]),
                                         ("w1v", [2048, 256]), ("b1v", [256, 1]), ("w2v", [256, 64]), ("b2v", [1, 64]),
                                         ("posk", [32, 64]), ("posv", [32, 64])]}
    mem = E("mem", [256, D])
    wkv = [E("w_mem_kv%d" % i, [D, 512]) for i in range(2)]
    wout = [E("w_out%d" % i, [D, D]) for i in range(2)]
    wg = [E("wg%d" % i, [D, DFF]) for i in range(2)]
    wu = [E("wu%d" % i, [D, DFF]) for i in range(2)]
    wd = [E("wd%d" % i, [DFF, D]) for i in range(2)]
    dsa_w_in = E("dsa_w_in", [D, 712])
    cm = E("cm", [128, 512]); pw = E("pw", [128, NIT])
    q_norm = E("q_norm", [1, 256]); kv_norm = E("kv_norm", [1, 128]); kidx_norm = E("kidx_norm", [1, 64])
    w_q_up = E("w_q_up", [256, 768]); w_uk = E("w_uk", [128, 768]); w_uv = E("w_uv", [128, 768]); w_q_idx = E("w_q_idx", [256, 512])

    fm_loc = S("fm_loc", [64, 24, TOK], BF16)
    tm_loc = S("tm_loc", [TOK, 292])
    kfm_loc = S("kfm_loc", [8, 64, TOK], BF16)
    kfm_all = S("kfm_all", [4, 4 * 128, TOK], BF16)
    vt_loc = S("vt_loc", [TOK, 256])
    vt_all = S("vt_all", [4, 4 * 512, 256])
    qT2_s = S("qT2_s", [128, 6, TOK], BF16)
    kseq = {k: S(k + "_s", [128, T], BF16) for k in ("kcrT2", "vcrT2", "ksT2", "kwT2")}
    vs_s = S("vs_s", [T, 128]); vw_s = S("vw_s", [T, 128])
    hmid0 = S("hmid0", [TOK, D]); h0 = S("h0", [TOK, D]); hmid1 = S("hmid1", [TOK, D])
    pr_loc = S("pr_loc", [TOK, 712])
    kv_loc = S("kv_loc", [TOK, 192]); kv_all = S("kv_all", [4, 4 * 512, 192])
    ckv_s = S("ckv_s", [T, 128]); kidx_s = S("kidx_s", [T, 64])

    kb.alias = {"x": x, "g": nm["norm_mix0"], "w_in": nsa_w_in, "gate_b": gate_b, "ident": ident, "anti": anti,
                "fmT": fm_loc.ap(), "tm": tm_loc.ap()}
    with kb.scope():
        build_A(kb)
    P.barrier()
    P.dma("sp", kfm_loc.ap(), AP(tensor=fm_loc, offset=12 * TOK, ap=[[TOK, 8], [24 * TOK, 64], [1, TOK]]))
    P.dma("pool", vt_loc.ap(), tm_loc.ap()[:, 0:256])
    for ch in range(4):
        all_gather(kb, kfm_loc.ap()[2 * ch:2 * ch + 2].rearrange("a d t -> (a d) t"), kfm_all.ap()[ch])
    for r4 in range(4):
        all_gather(kb, vt_loc.ap()[r4 * 512:(r4 + 1) * 512, :], vt_all.ap()[r4])
    for g in range(2):
        P.dma(kb.dq(), qT2_s.ap()[g * 64:(g + 1) * 64, :, :], fm_loc.ap()[:, 6 * g:6 * g + 6, :])
    for ch, k in enumerate(("kcrT2", "vcrT2", "ksT2", "kwT2")):
        for g in range(2):
            for cc in range(4):
                dst = AP(tensor=kseq[k], offset=(g * 64) * T + cc * 128, ap=[[T, 64], [512, 16], [1, 128]])
                src = AP(tensor=kfm_all, offset=((ch * 4 + cc) * 128 + g * 64) * TOK, ap=[[TOK, 64], [128, 16], [1, 128]])
                P.dma(kb.dq(), dst, src)
    for (dst_t, c0) in ((vs_s, 0), (vw_s, 128)):
        for cc in range(4):
            for r4 in range(4):
                dst = AP(tensor=dst_t, offset=(r4 * 16 * 128 + cc * 128) * 128, ap=[[512 * 128, 4], [128, 128], [1, 128]])
                src = AP(tensor=vt_all, offset=((r4 * 4 + cc) * 512) * 256 + c0, ap=[[128 * 256, 4], [256, 128], [1, 128]])
                P.dma(kb.dq(), dst, src)
    P.barrier()
    kb.alias = dict(cmp_w)
    kb.alias.update({"x": x, "identb": identb, "antib": antib, "rel_bias": rel_bias, "oh_s": oh_s, "oh_w": oh_w, "oh_c": oh_c,
                     "qT2": qT2_s.ap(), "qmT": fm_loc.ap()[:, 20:24, :], "gates": tm_loc.ap()[:, 256:292],
                     "ksT2": kseq["ksT2"].ap(), "kwT2": kseq["kwT2"].ap(), "kcrT2": kseq["kcrT2"].ap(), "vcrT2": kseq["vcrT2"].ap(),
                     "vs": vs_s.ap(), "vw": vw_s.ap(), "ew": ew, "ovl": ovl, "fb": fb,
                     "mem": mem, "g_mem": nm["norm_mem0"], "w_mem_kv": wkv[0], "w_out": wout[0], "hmid": hmid0.ap()})
    with kb.scope():
        build_B(0, kb)
    P.barrier()
    kb.alias = {"hmid": hmid0.ap(), "identb": identb, "g_ffn": nm["norm_ffn0"], "wg": wg[0], "wu": wu[0], "wd": wd[0],
                "g2": nm["norm_mix1"], "h": h0.ap(), "w_in2": dsa_w_in, "pr": pr_loc.ap()}
    with kb.scope():
        build_F(False, 712, kb)
    P.barrier()
    P.dma("sp", kv_loc.ap(), pr_loc.ap()[:, 256:448])
    for r4 in range(4):
        all_gather(kb, kv_loc.ap()[r4 * 512:(r4 + 1) * 512, :], kv_all.ap()[r4])
    for (dst_t, c0, nc_) in ((ckv_s, 0, 128), (kidx_s, 128, 64)):
        for cc in range(4):
            for r4 in range(4):
                dst = AP(tensor=dst_t, offset=(r4 * 16 * 128 + cc * 128) * nc_, ap=[[512 * nc_, 4], [nc_, 128], [1, nc_]])
                src = AP(tensor=kv_all, offset=((r4 * 4 + cc) * 512) * 192 + c0, ap=[[128 * 192, 4], [192, 128], [1, nc_]])
                P.dma(kb.dq(), dst, src)
    P.barrier()
    kb.alias = {"x": h0.ap(), "pr": pr_loc.ap(), "ckv_seq": ckv_s.ap(), "kidx_seq": kidx_s.ap(), "identb": identb, "antib": antib,
                "rel_bias": rel_bias, "oh_s": oh_s, "cm": cm, "pw": pw, "q_norm": q_norm, "kv_norm": kv_norm, "kidx_norm": kidx_norm,
                "w_q_up": w_q_up, "w_uk": w_uk, "w_uv": w_uv, "w_q_idx": w_q_idx, "mem": mem, "g_mem": nm["norm_mem1"],
                "w_mem_kv": wkv[1], "w_out": wout[1], "hmid": hmid1.ap()}
    with kb.scope():
        build_C(99, NT, kb)
    P.barrier()
    kb.alias = {"hmid": hmid1.ap(), "identb": identb, "g_ffn": nm["norm_ffn1"], "wg": wg[1], "wu": wu[1], "wd": wd[1],
                "g2": nm["norm_final"], "h": out}
    with kb.scope():
        build_F(True, 712, kb)
    kb.ext = ext
    return kb


def fused_maps(inputs):
    ident = np.eye(128, dtype=np.float32)
    anti = np.ascontiguousarray(ident[::-1])
    pw = np.tile((0.5 ** np.arange(1, NIT + 1)).astype(np.float32)[None, :], (128, 1))
    maps = []
    for core in range(8):
        b, c = core // 4, core % 4
        mp = consts_B(c)
        z = np.arange(512)[None, :]
        q = np.arange(128)[:, None]
        mp.update({
            "x": np.ascontiguousarray(inputs["x"][b][core_tokens(c)]),
            "norm_mix0": inputs["norm_mix"][0:1], "norm_mix1": inputs["norm_mix"][1:2],
            "norm_ffn0": inputs["norm_ffn"][0:1], "norm_ffn1": inputs["norm_ffn"][1:2],
            "norm_mem0": inputs["norm_mem"][0:1], "norm_mem1": inputs["norm_mem"][1:2],
            "norm_final": inputs["norm_final"].reshape(1, D),
            "nsa_w_in": inputs["nsa_w_in"][0], "gate_b": inputs["nsa_gate_b"][0:1],
            "ident": ident, "anti": anti, "rel_bias": inputs["rel_bias"],
            "w1k": inputs["nsa_cmp_k_w1"][0], "b1k": inputs["nsa_cmp_k_b1"][0].reshape(256, 1),
            "w2k": inputs["nsa_cmp_k_w2"][0], "b2k": inputs["nsa_cmp_k_b2"][0].reshape(64, 1),
            "w1v": inputs["nsa_cmp_v_w1"][0], "b1v": inputs["nsa_cmp_v_b1"][0].reshape(256, 1),
            "w2v": inputs["nsa_cmp_v_w2"][0], "b2v": inputs["nsa_cmp_v_b2"][0].reshape(1, 64),
            "posk": inputs["nsa_cmp_pos_k"][0], "posv": inputs["nsa_cmp_pos_v"][0],
            "mem": inputs["mem"][b],
            "w_mem_kv0": inputs["w_mem_kv"][0], "w_mem_kv1": inputs["w_mem_kv"][1],
            "w_out0": inputs["w_out"][0], "w_out1": inputs["w_out"][1],
            "wg0": inputs["ffn_gate"][0], "wg1": inputs["ffn_gate"][1],
            "wu0": inputs["ffn_up"][0], "wu1": inputs["ffn_up"][1],
            "wd0": inputs["ffn_down"][0], "wd1": inputs["ffn_down"][1],
            "dsa_w_in": inputs["dsa_w_in"][0],
            "cm": np.where(z <= 128 * c + q, 0.0, -1e30).astype(np.float32), "pw": pw,
            "q_norm": inputs["dsa_q_norm"][0:1], "kv_norm": inputs["dsa_kv_norm"][0:1], "kidx_norm": inputs["dsa_kidx_norm"][0:1],
            "w_q_up": inputs["dsa_w_q_up"][0], "w_uk": inputs["dsa_w_uk"][0].reshape(128, 768),
            "w_uv": inputs["dsa_w_uv"][0].reshape(128, 768), "w_q_idx": inputs["dsa_w_q_idx"][0],
        })
        maps.append(mp)
    return maps


def kernel(**inputs):
    inputs = {k: np.asarray(v) for k, v in inputs.items()}
    kb = build_fused()
    res = kb.run(fused_maps(inputs))
    return scatter_tokens(res, "out")
```
